# Optimizing a Trainium2 kernel written in Bass

```python
import jax
import jax.numpy as jnp
from jax import lax
import numpy as np

D_MODEL = 1024
BATCH = 4
SEQ = 4096
DEPTH = 4

HEAD_DIM = 64
N_GDN = 6
N_NSA = 6
N_NSA_KV = 2
NSA_REP = N_NSA // N_NSA_KV
N_SB = 4
D_GDN = N_GDN * HEAD_DIM
D_NSA = N_NSA * HEAD_DIM
D_NSA_KV = N_NSA_KV * HEAD_DIM
D_SB = N_SB * HEAD_DIM
D_MIX = D_GDN + D_NSA + D_SB
GDN_CONV = 4
GDN_CHUNK = 64
CMP_LEN = 32
CMP_STRIDE = 16
CMP_HIDDEN = 128
SEL_BLOCK = 64
SEL_TOPK = 16
WINDOW = 512
Q_BLOCK = 128
D_FF = 2816
FFN_CONV = 3
D_PLE = 256
ROPE_THETA = 10000.0
EPS = 1e-6
NEG = -1e30
FORCE_SCORE = 1e4
SPLIT_SIZES = (D_GDN, D_GDN, D_GDN, D_GDN, N_GDN, N_GDN,
               D_NSA, D_NSA_KV, D_NSA_KV, D_NSA_KV, D_NSA_KV, D_NSA_KV, D_NSA_KV, 3 * N_NSA,
               D_SB, D_SB, D_SB)
SPLIT_POINTS = tuple(int(c) for c in np.cumsum(SPLIT_SIZES)[:-1])
N_IN = int(sum(SPLIT_SIZES))

kernel_name = 'hybrid_gdn_nsa_stickbreaking_convffn_trunk'


def rmsnorm(x, w):
    xf = x.astype(jnp.float32)
    y = xf * lax.rsqrt(jnp.mean(xf * xf, axis=-1, keepdims=True) + EPS)
    return (y * w.astype(jnp.float32)).astype(x.dtype)


def head_rmsnorm(x, w):
    b, t, c = x.shape
    return rmsnorm(x.reshape(b, t, c // HEAD_DIM, HEAD_DIM), w).reshape(b, t, c)


def l2norm(x):
    return x * lax.rsqrt(jnp.sum(x * x, axis=-1, keepdims=True) + EPS)


def causal_dwconv(x, w):
    k, c = w.shape
    return lax.conv_general_dilated(x, w[:, None, :].astype(x.dtype), window_strides=(1,),
                                    padding=[(k - 1, 0)], dimension_numbers=('NWC', 'WIO', 'NWC'),
                                    feature_group_count=c)


def rope(x, pos):
    half = x.shape[-1] // 2
    inv = ROPE_THETA ** (-jnp.arange(half, dtype=jnp.float32) / half)
    ang = pos.astype(jnp.float32)[..., None] * inv
    cos = jnp.cos(ang)[:, :, None, :]
    sin = jnp.sin(ang)[:, :, None, :]
    xf = x.astype(jnp.float32)
    x1, x2 = xf[..., :half], xf[..., half:]
    return jnp.concatenate([x1 * cos - x2 * sin, x2 * cos + x1 * sin], axis=-1).astype(x.dtype)


def masked_softmax(s, mask):
    s = jnp.where(mask, s, NEG)
    e = jnp.where(mask, jnp.exp(s - jnp.max(s, axis=-1, keepdims=True)), 0.0)
    return e / jnp.maximum(jnp.sum(e, axis=-1, keepdims=True), 1e-30)


def gated_deltanet(q, k, v, z, a, b_logit, conv_w, a_log, dt_bias, norm_w):
    bsz, t, _ = q.shape
    H, D, C = N_GDN, HEAD_DIM, GDN_CHUNK
    n = t // C
    f32 = jnp.float32
    qkv = jax.nn.silu(causal_dwconv(jnp.concatenate([q, k, v], axis=-1), conv_w))
    q, k, v = jnp.split(qkv, 3, axis=-1)
    to_chunks = lambda u_: u_.astype(f32).reshape(bsz, n, C, H, D).transpose(0, 3, 1, 2, 4)
    q = l2norm(to_chunks(q)) * D ** -0.5
    k = l2norm(to_chunks(k))
    v = to_chunks(v)
    beta = jax.nn.sigmoid(b_logit.astype(f32)).reshape(bsz, n, C, H).transpose(0, 3, 1, 2)
    g = -jnp.exp(a_log.astype(f32)) * jax.nn.softplus(a.astype(f32) + dt_bias.astype(f32))
    g = jnp.cumsum(g.reshape(bsz, n, C, H).transpose(0, 3, 1, 2), axis=-1)
    idx = jnp.arange(C)
    incl = idx[:, None] >= idx[None, :]
    strict = idx[:, None] > idx[None, :]
    decay = jnp.exp(jnp.where(incl, g[..., :, None] - g[..., None, :], -jnp.inf))
    k_beta = k * beta[..., None]
    lower = jnp.where(strict, jnp.einsum('bhncd,bhnsd->bhncs', k_beta, k) * decay, 0.0)
    rhs = jnp.concatenate([v * beta[..., None], k_beta * jnp.exp(g)[..., None]], axis=-1)
    sol = lax.linalg.triangular_solve(lower, rhs, left_side=True, lower=True, unit_diagonal=True)
    u, w = jnp.split(sol, 2, axis=-1)
    attn = jnp.einsum('bhncd,bhnsd->bhncs', q, k) * decay

    def step(state, xs):
        q_i, k_i, u_i, w_i, g_i, a_i = xs
        v_new = u_i - jnp.einsum('bhcd,bhde->bhce', w_i, state)
        o_i = (jnp.einsum('bhcd,bhde->bhce', q_i * jnp.exp(g_i)[..., None], state)
               + jnp.einsum('bhcs,bhse->bhce', a_i, v_new))
        g_last = g_i[..., -1:]
        state = (state * jnp.exp(g_last)[..., None]
                 + jnp.einsum('bhcd,bhce->bhde', k_i * jnp.exp(g_last - g_i)[..., None], v_new))
        return state, o_i

    xs = tuple(jnp.moveaxis(t_, 2, 0) for t_ in (q, k, u, w, g, attn))
    _, o = lax.scan(step, jnp.zeros((bsz, H, D, D), f32), xs)
    o = o.transpose(1, 0, 3, 2, 4).reshape(bsz, t, H, D)
    o = rmsnorm(o, norm_w) * jax.nn.silu(z.astype(f32).reshape(bsz, t, H, D))
    return o.reshape(bsz, t, D_GDN).astype(z.dtype)


def compress(kv, pe, w1, w2):
    b, t, g, d = kv.shape
    n_cmp = (t - CMP_LEN) // CMP_STRIDE + 1
    idx = jnp.arange(n_cmp)[:, None] * CMP_STRIDE + jnp.arange(CMP_LEN)[None, :]
    blocks = kv[:, idx] + pe[None, None, :, None, :]
    blocks = blocks.transpose(0, 1, 3, 2, 4).reshape(b, n_cmp, g, CMP_LEN * d)
    return jax.nn.silu(blocks @ w1) @ w2


def native_sparse_attention(q, k_cmp, v_cmp, k_slc, v_slc, k_win, v_win, gates, positions,
                            pe_k, pe_v, ck_w1, ck_w2, cv_w1, cv_w2):
    b, t, _ = q.shape
    G, R, D = N_NSA_KV, NSA_REP, HEAD_DIM
    scale = D ** -0.5
    f32 = jnp.float32
    q = rope(q.reshape(b, t, N_NSA, D), positions).reshape(b, t, G, R, D)
    kv = lambda z_: z_.reshape(b, t, G, D)
    kc = compress(rope(kv(k_cmp), positions), pe_k, ck_w1, ck_w2)
    vc = compress(kv(v_cmp), pe_v, cv_w1, cv_w2)
    n_cmp = kc.shape[1]
    n_sel = t // SEL_BLOCK
    top_k = min(SEL_TOPK, n_sel)
    ks_blk = rope(kv(k_slc), positions).reshape(b, n_sel, SEL_BLOCK, G, D).transpose(0, 3, 1, 2, 4)
    vs_blk = kv(v_slc).reshape(b, n_sel, SEL_BLOCK, G, D).transpose(0, 3, 1, 2, 4)
    pad = ((0, 0), (WINDOW, 0), (0, 0), (0, 0))
    kw_pad = jnp.pad(rope(kv(k_win), positions), pad)
    vw_pad = jnp.pad(kv(v_win), pad)
    c0 = jnp.arange(n_cmp) * CMP_STRIDE
    s0 = jnp.arange(n_sel) * SEL_BLOCK
    overlap = jnp.clip(jnp.minimum(c0[:, None] + CMP_LEN, s0[None, :] + SEL_BLOCK)
                       - jnp.maximum(c0[:, None], s0[None, :]), 0).astype(f32) / CMP_LEN
    cmp_end = c0 + CMP_LEN - 1
    b_idx = jnp.arange(b)[:, None, None, None]
    g_idx = jnp.arange(G)[None, :, None, None]
    blk = jnp.arange(n_sel)

    def block(args):
        qb, gb, q0 = args
        tq = q0 + jnp.arange(Q_BLOCK)
        s_c = jnp.einsum('bqgrd,bigd->bgrqi', qb, kc).astype(f32) * scale
        p_c = masked_softmax(s_c, cmp_end[None, :] <= tq[:, None])
        o_c = jnp.einsum('bgrqi,bigd->bqgrd', p_c.astype(vc.dtype), vc)
        imp = jnp.einsum('bgrqi,ij->bgqj', p_c, overlap)
        cur = tq // SEL_BLOCK
        forced = (blk[None, :] == 0) | (blk[None, :] == cur[:, None]) | (blk[None, :] == cur[:, None] - 1)
        visible = blk[None, :] * SEL_BLOCK <= tq[:, None]
        score = jnp.where(forced, FORCE_SCORE, jnp.where(visible, imp, -1.0))
        _, sel = lax.top_k(score, top_k)
        k_sel = ks_blk[b_idx, g_idx, sel].reshape(b, G, Q_BLOCK, top_k * SEL_BLOCK, D)
        v_sel = vs_blk[b_idx, g_idx, sel].reshape(b, G, Q_BLOCK, top_k * SEL_BLOCK, D)
        key_pos = (sel[..., None] * SEL_BLOCK + jnp.arange(SEL_BLOCK)).reshape(b, G, 1, Q_BLOCK, top_k * SEL_BLOCK)
        s_s = jnp.einsum('bqgrd,bgqnd->bgrqn', qb, k_sel).astype(f32) * scale
        p_s = masked_softmax(s_s, key_pos <= tq[:, None])
        o_s = jnp.einsum('bgrqn,bgqnd->bqgrd', p_s.astype(v_sel.dtype), v_sel)
        kw = lax.dynamic_slice_in_dim(kw_pad, q0, WINDOW + Q_BLOCK, axis=1)
        vw = lax.dynamic_slice_in_dim(vw_pad, q0, WINDOW + Q_BLOCK, axis=1)
        pos_w = q0 - WINDOW + jnp.arange(WINDOW + Q_BLOCK)
        diff = tq[:, None] - pos_w[None, :]
        m_w = (diff >= 0) & (diff < WINDOW) & (pos_w >= 0)[None, :]
        s_w = jnp.einsum('bqgrd,bngd->bgrqn', qb, kw).astype(f32) * scale
        p_w = masked_softmax(s_w, m_w)
        o_w = jnp.einsum('bgrqn,bngd->bqgrd', p_w.astype(vw.dtype), vw)
        gt = jax.nn.sigmoid(gb.astype(f32))[..., None]
        return (gt[:, :, 0] * o_c + gt[:, :, 1] * o_s + gt[:, :, 2] * o_w).astype(qb.dtype)

    nb = t // Q_BLOCK
    q_blocks = q.reshape(b, nb, Q_BLOCK, G, R, D).transpose(1, 0, 2, 3, 4, 5)
    g_blocks = gates.reshape(b, nb, Q_BLOCK, 3, G, R).transpose(1, 0, 2, 3, 4, 5)
    out = lax.map(block, (q_blocks, g_blocks, jnp.arange(nb) * Q_BLOCK))
    return out.transpose(1, 0, 2, 3, 4, 5).reshape(b, t, D_NSA)


def stick_breaking_attention(q, k, v):
    b, t, _ = q.shape
    H, D = N_SB, HEAD_DIM
    scale = D ** -0.5
    k = k.reshape(b, t, H, D)
    v = v.reshape(b, t, H, D)
    nb = t // Q_BLOCK
    q_blocks = q.reshape(b, nb, Q_BLOCK, H, D).transpose(1, 0, 2, 3, 4)
    key_pos = jnp.arange(t)

    def block(args):
        qb, q0 = args
        tq = q0 + jnp.arange(Q_BLOCK)
        strict = key_pos[None, :] < tq[:, None]
        z = jnp.einsum('bqhd,bkhd->bhqk', qb, k).astype(jnp.float32) * scale
        log_1m = jnp.where(strict, jax.nn.log_sigmoid(-z), 0.0)
        log_stick = lax.cumsum(log_1m, axis=3, reverse=True) - log_1m
        a = jnp.where(strict, jnp.exp(jax.nn.log_sigmoid(z) + log_stick), 0.0)
        return jnp.einsum('bhqk,bkhd->bqhd', a.astype(v.dtype), v)

    out = lax.map(block, (q_blocks, jnp.arange(nb) * Q_BLOCK))
    return out.transpose(1, 0, 2, 3, 4).reshape(b, t, D_SB)


def conv_ffn(x, w_up, conv_w, w_down):
    u = causal_dwconv(x @ w_up, conv_w)
    gate, up = jnp.split(u, 2, axis=-1)
    return (jax.nn.silu(gate) * up) @ w_down


def setup_inputs(seed: int = 0) -> dict:
    key = jax.random.key(seed)
    keys = iter(jax.random.split(key, 40))
    f32 = jnp.float32

    def normal(shape, scale):
        return jax.random.normal(next(keys), shape, f32) * scale

    def gain(shape):
        return 1.0 + 0.01 * jax.random.normal(next(keys), shape, f32)

    x = normal((BATCH, SEQ, D_MODEL), 1.0)
    p = normal((DEPTH, BATCH, SEQ, D_PLE), 1.0)
    positions = (jax.random.randint(next(keys), (BATCH, 1), 0, 1024, jnp.int32)
                 + jnp.arange(SEQ, dtype=jnp.int32)[None, :])
    a_log = jnp.log(jax.random.uniform(next(keys), (DEPTH, N_GDN), f32, 1.0, 16.0))
    dt = jnp.exp(jax.random.uniform(next(keys), (DEPTH, N_GDN), f32, -6.907755, -2.302585))
    dt_bias = dt + jnp.log(-jnp.expm1(-dt))
    return {
        'x': x,
        'p': p,
        'positions': positions,
        'ln_mix': gain((DEPTH, D_MODEL)),
        'w_in': normal((DEPTH, D_MODEL, N_IN), D_MODEL ** -0.5),
        'gdn_conv': normal((DEPTH, GDN_CONV, 3 * D_GDN), GDN_CONV ** -0.5),
        'gdn_a_log': a_log,
        'gdn_dt_bias': dt_bias,
        'gdn_norm': gain((DEPTH, HEAD_DIM)),
        'nsa_pe_k': normal((DEPTH, CMP_LEN, HEAD_DIM), 0.1),
        'nsa_pe_v': normal((DEPTH, CMP_LEN, HEAD_DIM), 0.1),
        'nsa_cmp_k_w1': normal((DEPTH, CMP_LEN * HEAD_DIM, CMP_HIDDEN), (CMP_LEN * HEAD_DIM) ** -0.5),
        'nsa_cmp_k_w2': normal((DEPTH, CMP_HIDDEN, HEAD_DIM), CMP_HIDDEN ** -0.5),
        'nsa_cmp_v_w1': normal((DEPTH, CMP_LEN * HEAD_DIM, CMP_HIDDEN), (CMP_LEN * HEAD_DIM) ** -0.5),
        'nsa_cmp_v_w2': normal((DEPTH, CMP_HIDDEN, HEAD_DIM), CMP_HIDDEN ** -0.5),
        'nsa_norm': gain((DEPTH, HEAD_DIM)),
        'sb_norm': gain((DEPTH, HEAD_DIM)),
        'w_out': normal((DEPTH, D_MIX, D_MODEL), D_MIX ** -0.5),
        'ln_ffn': gain((DEPTH, D_MODEL)),
        'w_up': normal((DEPTH, D_MODEL, 2 * D_FF), D_MODEL ** -0.5),
        'ffn_conv': normal((DEPTH, FFN_CONV, 2 * D_FF), FFN_CONV ** -0.5),
        'w_down': normal((DEPTH, D_FF, D_MODEL), D_FF ** -0.5),
        'ln_ple': gain((DEPTH, D_MODEL)),
        'w_ple_gate': normal((DEPTH, D_MODEL, D_MODEL), D_MODEL ** -0.5),
        'w_ple': normal((DEPTH, D_PLE, D_MODEL), D_PLE ** -0.5),
        'ple_norm': gain((DEPTH, D_MODEL)),
        'ln_final': gain((D_MODEL,)),
    }


def reference(x, p, positions, ln_mix, w_in, gdn_conv, gdn_a_log, gdn_dt_bias, gdn_norm,
              nsa_pe_k, nsa_pe_v, nsa_cmp_k_w1, nsa_cmp_k_w2, nsa_cmp_v_w1, nsa_cmp_v_w2,
              nsa_norm, sb_norm, w_out, ln_ffn, w_up, ffn_conv, w_down,
              ln_ple, w_ple_gate, w_ple, ple_norm, ln_final):
    h = x
    for i in range(DEPTH):
        hn = rmsnorm(h, ln_mix[i])
        (g_q, g_k, g_v, g_z, g_a, g_b,
         n_q, n_kc, n_vc, n_ks, n_vs, n_kw, n_vw, n_gate,
         s_q, s_k, s_v) = jnp.split(hn @ w_in[i], SPLIT_POINTS, axis=-1)
        o_gdn = gated_deltanet(g_q, g_k, g_v, g_z, g_a, g_b, gdn_conv[i], gdn_a_log[i],
                               gdn_dt_bias[i], gdn_norm[i])
        o_nsa = native_sparse_attention(n_q, n_kc, n_vc, n_ks, n_vs, n_kw, n_vw, n_gate, positions,
                                        nsa_pe_k[i], nsa_pe_v[i], nsa_cmp_k_w1[i], nsa_cmp_k_w2[i],
                                        nsa_cmp_v_w1[i], nsa_cmp_v_w2[i])
        o_sb = stick_breaking_attention(s_q, s_k, s_v)
        mix = jnp.concatenate([o_gdn, head_rmsnorm(o_nsa, nsa_norm[i]), head_rmsnorm(o_sb, sb_norm[i])], axis=-1)
        h = h + mix @ w_out[i]
        h = h + conv_ffn(rmsnorm(h, ln_ffn[i]), w_up[i], ffn_conv[i], w_down[i])
        gate = jax.nn.sigmoid(rmsnorm(h, ln_ple[i]) @ w_ple_gate[i])
        h = h + gate * rmsnorm(p[i] @ w_ple[i], ple_norm[i])
    return rmsnorm(h, ln_final)
```

```python
import contextlib
import numpy as np
import concourse.bass as bass
import concourse.mybir as mybir
from concourse.bass_utils import run_bass_kernel_spmd

F32 = mybir.dt.float32
BF16 = mybir.dt.bfloat16
I32 = mybir.dt.int32
AF = mybir.ActivationFunctionType
ALU = mybir.AluOpType
AX = mybir.AxisListType

D_MODEL = 1024
HD = 64
N_IN_PAD = 3584
D_FF = 2816
EPS = 1e-6
NDS = 24
CPARTS = {'cmp', 'topk', 'sel', 'win'}


class Prog:
    def __init__(self, nc, stack):
        self.nc = nc
        self.stack = stack
        self.cengs = ['pe', 'act', 'dve', 'pool']
        self.engs = self.cengs + ['sp']
        self.sem = {e: stack.enter_context(nc.semaphore("s_" + e)) for e in self.cengs}
        self.dsem = [stack.enter_context(nc.semaphore("d%d" % i)) for i in range(NDS)]
        self.dcount = [0] * NDS
        self.dnext = 0
        self.nseq = {e: 0 for e in self.cengs}
        self.known = {e: {} for e in self.engs}
        self.lastw = {}
        self.readers = {}
        self.q = {e: [] for e in self.engs}
        self.pending_noinc = {e: False for e in self.cengs}

    def _semof(self, sk):
        if isinstance(sk, tuple):
            return self.dsem[sk[1]]
        return self.sem[sk]

    def _collect(self, eng, reads, writes):
        deps = {}
        def add(ev):
            if ev is None:
                return
            sk, val = ev
            if deps.get(sk, 0) < val:
                deps[sk] = val
        for k in reads:
            add(self.lastw.get(k))
        for k in writes:
            add(self.lastw.get(k))
            for ev in self.readers.get(k, ()):
                add(ev)
        waits = []
        for sk, val in deps.items():
            if eng == 'pe' and sk == 'pe':
                continue
            if self.known[eng].get(sk, 0) < val:
                self.known[eng][sk] = val
                waits.append((sk, val))
        return waits

    def _record(self, ev, reads, writes):
        for k in reads:
            self.readers.setdefault(k, []).append(ev)
        for k in writes:
            self.lastw[k] = ev
            self.readers[k] = []

    def op(self, eng, fn, reads=(), writes=(), inc=True):
        waits = self._collect(eng, reads, writes)
        if inc:
            self.nseq[eng] += 1
            ev = (eng, self.nseq[eng])
            self.pending_noinc[eng] = False
        else:
            ev = (eng, self.nseq[eng] + 1)
            self.pending_noinc[eng] = True
        self.q[eng].append((waits, fn, eng if inc else None))
        self._record(ev, reads, writes)

    def dma(self, out, in_, reads=(), writes=(), queue='sp', **kw):
        waits = self._collect(queue, reads, writes)
        j = self.dnext
        self.dnext = (j + 1) % NDS
        prev = self.dcount[j]
        sk = ('d', j)
        if prev > 0 and self.known[queue].get(sk, 0) < prev:
            self.known[queue][sk] = prev
            waits.append((sk, prev))
        self.dcount[j] += 16
        ev = (sk, self.dcount[j])
        self.q[queue].append((waits, lambda e: e.dma_start(out=out, in_=in_, **kw), sk))
        self._record(ev, reads, writes)

    def barrier(self):
        for e in self.engs:
            waits = []
            for f in self.cengs:
                if f == e:
                    continue
                v = self.nseq[f]
                if v > 0 and self.known[e].get(f, 0) < v:
                    self.known[e][f] = v
                    waits.append((f, v))
            for j in range(NDS):
                v = self.dcount[j]
                sk = ('d', j)
                if v > 0 and self.known[e].get(sk, 0) < v:
                    self.known[e][sk] = v
                    waits.append((sk, v))
            if waits:
                self.q[e].append((waits, None, None))
        self.lastw = {}
        self.readers = {}

    def flush(self):
        nc = self.nc
        for e in self.cengs:
            assert not self.pending_noinc[e], e
        q = self.q
        self.q = {e: [] for e in self.engs}

        def emit(name, e):
            for waits, fn, inc in q[name]:
                for sk, val in waits:
                    e.wait_ge(self._semof(sk), val)
                if fn is None:
                    continue
                ins = fn(e)
                if inc is not None:
                    if isinstance(inc, tuple):
                        ins.then_inc(self.dsem[inc[1]], 16)
                    else:
                        ins.then_inc(self.sem[inc], 1)

        with nc.Block() as block:
            @block.tensor
            def _(e):
                emit('pe', e)

            @block.scalar
            def _(e):
                emit('act', e)

            @block.vector
            def _(e):
                emit('dve', e)

            @block.gpsimd
            def _(e):
                emit('pool', e)

            @block.sync
            def _(e):
                emit('sp', e)


class Ring:
    def __init__(self, items):
        self.items = items
        self.i = 0

    def next(self):
        it = self.items[self.i % len(self.items)]
        self.i += 1
        return it


def build(T, depth, debug=(), stages="ABCDEFG", mix_input=False, ext=()):
    nc = bass.Bass("TRN2", target_bir_lowering=False)
    NT = T // 512
    din = lambda name, shape, dt=F32: nc.dram_tensor(name, shape, dt, kind="ExternalInput").ap()
    dscr = lambda name, shape, dt=F32: nc.dram_tensor(
        name, shape, dt, kind=("ExternalOutput" if name in debug else "ExternalInput" if name in ext else "Internal")).ap()

    xT = din("xT", [D_MODEL, T])
    pT = din("pT", [depth, 256, T])
    ln_mix = din("ln_mix", [depth, 128, 8])
    ln_ffn = din("ln_ffn", [depth, 128, 8])
    ln_ple = din("ln_ple", [depth, 128, 8])
    ple_norm = din("ple_norm", [depth, 128, 8])
    ln_final = din("ln_final", [128, 8])
    w_in = din("w_in", [depth, D_MODEL, N_IN_PAD])
    sb_norm = din("sb_norm", [depth, 128, 1])
    nsa_norm = din("nsa_norm", [depth, 128, 1])
    gdn_norm = din("gdn_norm", [depth, 128, 1])
    pos = din("pos", [1, T], I32)
    cmp_k_w1 = din("cmp_k_w1", [depth, 128, 32, 128])
    cmp_v_w1 = din("cmp_v_w1", [depth, 128, 32, 128])
    cmp_k_w2 = din("cmp_k_w2", [depth, 128, 64])
    cmp_v_w2 = din("cmp_v_w2", [depth, 128, 64])
    pe_kT = din("pe_kT", [depth, 128, 32])
    pe_vT = din("pe_vT", [depth, 128, 32])
    gdn_conv = din("gdn_conv", [depth, 128, 9, 4])
    gdn_alog = din("gdn_alog", [depth, 1, 6])
    gdn_dtb = din("gdn_dtb", [depth, 1, 6])
    gdn_normrow = din("gdn_normrow", [depth, 1, 64])
    gtok = dscr("gtok", [T, 13 * 128])
    w_out = din("w_out", [depth, D_MODEL, D_MODEL])
    w_up = din("w_up", [depth, D_MODEL, 2 * D_FF])
    ffn_conv = din("ffn_conv", [depth, 128, 44, 3])
    w_down = din("w_down", [depth, D_FF, D_MODEL])
    w_gate = din("w_gate", [depth, D_MODEL, D_MODEL])
    w_ple = din("w_ple", [depth, 256, D_MODEL])
    outT = nc.dram_tensor("outT", [D_MODEL, T], F32, kind="ExternalOutput").ap()
    projF = dscr("projF", [24 * 128, T])
    projT = dscr("projT", [T, 512])
    hT = dscr("hT", [D_MODEL, T])
    hT2 = dscr("hT2", [D_MODEL, T])
    if mix_input:
        mixT = din("mixT", [D_MODEL, T], BF16)
    else:
        mixT = dscr("mixT", [D_MODEL, T], BF16)
    pc = lambda ap: ap.rearrange("(c p) t -> p c t", p=128)

    with contextlib.ExitStack() as stack:
        P = Prog(nc, stack)
        sb = lambda name, shape, dt=F32: stack.enter_context(nc.sbuf_tensor(name, shape, dt))

        ones_bf = sb("ones_bf", [128, 128], BF16)
        P.op('pool', lambda e: e.memset(ones_bf[:], 1.0), writes=['ones_bf'])
        eps_t = sb("eps_t", [128, 1], F32)
        P.op('pool', lambda e: e.memset(eps_t[:], EPS), writes=['eps_t'])

        one_t = sb("one_t", [128, 1], F32)
        P.op('pool', lambda e: e.memset(one_t[:], 1.0), writes=['one_t'])
        ones512 = sb("ones512", [128, 512], BF16)
        P.op('pool', lambda e: e.memset(ones512[:], 1.0), writes=['ones512'])
        negOnes = sb("negOnes", [128, 128], BF16)
        P.op('pool', lambda e: e.memset(negOnes[:], -1.0), writes=['negOnes'])
        negU = sb("negU", [128, 128], BF16)
        P.op('pool', lambda e: e.affine_select(out=negU[:], in_=negOnes[:], pattern=[[-1, 128]],
                                               compare_op=ALU.is_ge, fill=0.0, base=0, channel_multiplier=1),
             reads=['negOnes'], writes=['negU'])
        dmask = sb("dmask", [128, 4, 512], BF16)
        for j in range(4):
            P.op('pool', lambda e, j=j: e.affine_select(out=dmask[:, j, :], in_=ones512[:], pattern=[[1, 512]],
                                                        compare_op=ALU.is_gt, fill=0.0, base=-128 * j,
                                                        channel_multiplier=-1),
                 reads=['ones512'], writes=['dmask'])
        cast_rr = [0]
        uidc = [0]

        def uid():
            uidc[0] += 1
            return '_%d' % uidc[0]

        def cast(dst, src, reads, writes, psum=False):
            eng = (['dve', 'act'][cast_rr[0] % 2]) if psum else (['pool', 'dve', 'act'][cast_rr[0] % 3])
            cast_rr[0] += 1
            if eng == 'act':
                P.op('act', lambda e: e.copy(out=dst, in_=src), reads=reads, writes=writes)
            else:
                P.op(eng, lambda e: e.tensor_copy(out=dst, in_=src), reads=reads, writes=writes)

        def warm(psW, n=10):
            for _ in range(n):
                P.op('pe', lambda e: e.matmul(psW[:], lhsT=ones_bf[:], rhs=ones512[:], start=True, stop=True),
                     reads=[], writes=[], inc=False)

        def emit_norm(h, gain, sq, ps_n, rstd, xn, nfeat=D_MODEL):
            nch = nfeat // 128
            P.op('act', lambda e: e.activation(out=sq[:], in_=h[:], func=AF.Square),
                 reads=[h.name], writes=[sq.name])
            for c in range(nch):
                P.op('pe', lambda e, c=c: e.matmul(ps_n[:], lhsT=ones_bf[:], rhs=sq[:, c, :],
                                                   start=(c == 0), stop=(c == nch - 1)),
                     reads=[sq.name, 'ones_bf'], writes=[ps_n.name], inc=(c == nch - 1))
            P.op('act', lambda e: e.activation(out=rstd[:], in_=ps_n[:], func=AF.Ln,
                                               bias=eps_t[:], scale=1.0 / nfeat),
                 reads=[ps_n.name, 'eps_t'], writes=[rstd.name])
            P.op('act', lambda e: e.activation(out=rstd[:], in_=rstd[:], func=AF.Exp, scale=-0.5),
                 reads=[rstd.name], writes=[rstd.name])
            for c in range(nch):
                P.op('dve', lambda e, c=c: e.scalar_tensor_tensor(
                    out=xn[:, c, :], in0=h[:, c, :], scalar=gain[:, c:c + 1], in1=rstd[:],
                    op0=ALU.mult, op1=ALU.mult),
                    reads=[h.name, gain.name, rstd.name], writes=[xn.name])

        NKB = T // 128
        NCMP = T // 16 - 1
        BIGM = 30000.0
        ident_bf = sb("ident_bf", [128, 128], BF16)
        P.op('pool', lambda e: e.affine_select(out=ident_bf[:], in_=ones_bf[:], pattern=[[-1, 128]],
                                               compare_op=ALU.is_equal, fill=0.0, base=0, channel_multiplier=1),
             reads=['ones_bf'], writes=['ident_bf'])
        ones_f = sb("ones_f", [128, 128], F32)
        P.op('pool', lambda e: e.memset(ones_f[:], 1.0), writes=['ones_f'])
        ident_f = sb("ident_f", [128, 128], F32)
        P.op('pool', lambda e: e.affine_select(out=ident_f[:], in_=ones_f[:], pattern=[[-1, 128]],
                                               compare_op=ALU.is_equal, fill=0.0, base=0, channel_multiplier=1),
             reads=['ones_f'], writes=['ident_f'])
        Uincl = sb("Uincl", [128, 128], F32)
        Lstrict = sb("Lstrict", [128, 128], F32)
        Lincl = sb("Lincl", [128, 128], F32)
        P.op('pool', lambda e: e.affine_select(out=Uincl[:], in_=ones_f[:], pattern=[[1, 128]], compare_op=ALU.is_ge,
                                               fill=0.0, base=0, channel_multiplier=-1), reads=['ones_f'], writes=['Uincl'])
        P.op('pool', lambda e: e.affine_select(out=Lstrict[:], in_=ones_f[:], pattern=[[-1, 128]], compare_op=ALU.is_gt,
                                               fill=0.0, base=0, channel_multiplier=1), reads=['ones_f'], writes=['Lstrict'])
        P.op('pool', lambda e: e.affine_select(out=Lincl[:], in_=ones_f[:], pattern=[[-1, 128]], compare_op=ALU.is_ge,
                                               fill=0.0, base=0, channel_multiplier=1), reads=['ones_f'], writes=['Lincl'])
        LBD = sb("LBD", [128, 128], F32)
        LOD = sb("LOD", [128, 128], F32)
        for cb in range(4):
            csl = slice(cb * 32, (cb + 1) * 32)
            P.op('pool', lambda e, csl=csl, cb=cb: e.affine_select(out=LBD[:, csl], in_=Lstrict[:, csl], pattern=[[0, 32]],
                                                                   compare_op=ALU.is_ge, fill=0.0, base=-32 * cb, channel_multiplier=1),
                 reads=['Lstrict'], writes=['LBD'])
            P.op('pool', lambda e, csl=csl, cb=cb: e.affine_select(out=LBD[:, csl], in_=LBD[:, csl], pattern=[[0, 32]],
                                                                   compare_op=ALU.is_ge, fill=0.0, base=32 * cb + 31, channel_multiplier=-1),
                 reads=['LBD'], writes=['LBD'])
        P.op('pool', lambda e: e.tensor_tensor(out=LOD[:], in0=Lstrict[:], in1=LBD[:], op=ALU.subtract),
             reads=['Lstrict', 'LBD'], writes=['LOD'])
        dmaskI = sb("dmaskI", [128, 4, 512], BF16)
        wmask = sb("wmask", [128, 4, 512], BF16)
        for j in range(4):
            P.op('pool', lambda e, j=j: e.affine_select(out=dmaskI[:, j, :], in_=ones512[:], pattern=[[1, 512]],
                                                        compare_op=ALU.is_ge, fill=0.0, base=-128 * j,
                                                        channel_multiplier=-1),
                 reads=['ones512'], writes=['dmaskI'])
            P.op('pool', lambda e, j=j: e.affine_select(out=wmask[:, j, :], in_=ones512[:], pattern=[[-1, 512]],
                                                        compare_op=ALU.is_ge, fill=0.0, base=128 * j - 1,
                                                        channel_multiplier=1),
                 reads=['ones512'], writes=['wmask'])
        Psw = sb("Psw", [128, 128], BF16)
        for mb, base in ((0, -32), (1, 0), (2, -96), (3, -64)):
            P.op('pool', lambda e, mb=mb, base=base: e.affine_select(
                out=Psw[:, mb * 32:(mb + 1) * 32], in_=ones_bf[:, 0:32], pattern=[[-1, 32]],
                compare_op=ALU.is_equal, fill=0.0, base=base, channel_multiplier=1),
                reads=['ones_bf'], writes=['Psw'])
        cmask = sb("cmask", [128, 2, T], BF16)
        Ebig = sb("Ebig", [128, T], BF16)
        oh = sb("oh", [128, 18, 64], BF16)
        ov = sb("ov", [128, 2, 64], BF16)
        keepB = sb("keepB", [128, 128], F32)
        addcB = sb("addcB", [128, 128], F32)
        cosT = sb("cosT", [128, T], BF16)
        sinS = sb("sinS", [128, T], BF16)
        _tmp_scope = contextlib.ExitStack()
        sbtmp = lambda name, shape, dt=F32: _tmp_scope.enter_context(nc.sbuf_tensor(name, shape, dt))
        onesT = _tmp_scope.enter_context(nc.sbuf_tensor("onesT", [128, T], BF16))
        P.op('pool', lambda e: e.memset(onesT[:], 1.0), writes=['onesT'])
        for ic in range(2):
            P.op('pool', lambda e, ic=ic: e.affine_select(out=cmask[:, ic, :], in_=onesT[:], pattern=[[1, T]],
                                                          compare_op=ALU.is_ge, fill=0.0, base=-31 - 2048 * ic,
                                                          channel_multiplier=-16),
                 reads=['onesT'], writes=['cmask'])
        P.op('pool', lambda e: e.memset(Ebig[:], BIGM), writes=['Ebig'])
        for ph in range(2):
            psl = slice(ph * 64, (ph + 1) * 64)
            P.op('pool', lambda e, psl=psl: e.affine_select(out=Ebig[psl, :], in_=Ebig[psl, :], pattern=[[1, T]], compare_op=ALU.is_ge,
                                                            fill=0.0, base=0, channel_multiplier=-64),
                 reads=['Ebig'], writes=['Ebig'])
            P.op('pool', lambda e, psl=psl: e.affine_select(out=Ebig[psl, :], in_=Ebig[psl, :], pattern=[[-1, T]], compare_op=ALU.is_ge,
                                                            fill=0.0, base=63, channel_multiplier=64),
                 reads=['Ebig'], writes=['Ebig'])
        ones18 = sbtmp("ones18", [128, 18, 64], BF16)
        P.op('pool', lambda e: e.memset(ones18[:], 1.0), writes=['ones18'])
        P.op('pool', lambda e: e.affine_select(out=oh[:], in_=ones18[:], pattern=[[-1, 18], [0, 64]],
                                               compare_op=ALU.is_equal, fill=0.0, base=-12, channel_multiplier=1),
             reads=['ones18'], writes=['oh'])
        ovA = sbtmp("ovA", [128, 2, 64], I32)
        P.op('pool', lambda e: e.iota(ovA[:], pattern=[[2048, 2], [-64, 64]], base=0, channel_multiplier=16),
             writes=['ovA'])
        ovF = sbtmp("ovF", [128, 2, 64], F32)
        ovG = sbtmp("ovG", [128, 2, 64], F32)
        P.op('dve', lambda e: e.tensor_copy(out=ovF[:], in_=ovA[:]), reads=['ovA'], writes=['ovF'])
        P.op('dve', lambda e: e.tensor_scalar(out=ovG[:], in0=ovF[:], scalar1=32.0, scalar2=64.0, op0=ALU.add, op1=ALU.min),
             reads=['ovF'], writes=['ovG'])
        P.op('dve', lambda e: e.tensor_scalar(out=ovF[:], in0=ovF[:], scalar1=0.0, scalar2=None, op0=ALU.max),
             reads=['ovF'], writes=['ovF'])
        P.op('dve', lambda e: e.tensor_tensor(out=ovG[:], in0=ovG[:], in1=ovF[:], op=ALU.subtract),
             reads=['ovF', 'ovG'], writes=['ovG'])
        P.op('dve', lambda e: e.tensor_scalar(out=ov[:], in0=ovG[:], scalar1=0.0, scalar2=1.0 / 32, op0=ALU.max, op1=ALU.mult),
             reads=['ovG'], writes=['ov'])
        negf = sbtmp("negf", [128, 128], F32)
        P.op('pool', lambda e: e.memset(negf[:], -1.0), writes=['negf'])
        for ph in range(2):
            psl = slice(ph * 64, (ph + 1) * 64)
            P.op('pool', lambda e, psl=psl, ph=ph: e.affine_select(
                out=keepB[psl, :], in_=ones_f[psl, :], pattern=[[-1, 128]], compare_op=ALU.is_ge, fill=0.0,
                base=62 + ph, channel_multiplier=0), reads=['ones_f'], writes=['keepB'])
            P.op('pool', lambda e, psl=psl, ph=ph: e.affine_select(
                out=addcB[psl, :], in_=negf[psl, :], pattern=[[1, 128]], compare_op=ALU.is_gt, fill=1e4,
                base=-64 - ph, channel_multiplier=0), reads=['negf'], writes=['addcB'])
            P.op('pool', lambda e, psl=psl, ph=ph: e.affine_select(
                out=addcB[psl, :], in_=addcB[psl, :], pattern=[[1, 128]], compare_op=ALU.is_ge, fill=0.0,
                base=-64 - ph + 1, channel_multiplier=0), reads=['addcB'], writes=['addcB'])
        with contextlib.ExitStack() as st:
            sbt = lambda name, shape, dt=F32, u=uid(): st.enter_context(nc.sbuf_tensor(name + u, shape, dt))
            pos_i = sbt("pos_i", [128, T], I32)
            ang = sbt("ang", [128, T], F32)
            tmpa = sbt("tmpa", [128, T], F32)
            ki = sbt("ki", [128, T], I32)
            tmpb = sbt("tmpb", [128, T], F32)
            pidx = sbt("pidx", [128, 1], I32)
            pf = sbt("pf", [128, 1], F32)
            inv = sbt("inv", [128, 1], F32)
            sgn = sbt("sgn", [128, 1], F32)
            P.dma(pos_i[:], pos.partition_broadcast(128), writes=['pos_i'])
            P.op('pool', lambda e: e.iota(pidx[:], pattern=[[0, 1]], base=0, channel_multiplier=1), writes=['pidx'])
            c123 = sbt("c123", [128, 3], F32)
            fl = sbt("fl", [128, 1], F32)
            P.op('dve', lambda e: e.tensor_copy(out=pf[:], in_=pidx[:]), reads=['pidx'], writes=['pf'])
            for i3, thr in enumerate((32.0, 64.0, 96.0)):
                P.op('dve', lambda e, i3=i3, thr=thr: e.tensor_scalar(out=c123[:, i3:i3 + 1], in0=pf[:], scalar1=thr,
                                                                       scalar2=None, op0=ALU.is_ge),
                     reads=['pf'], writes=['c123'])
            P.op('dve', lambda e: e.tensor_tensor(out=fl[:], in0=c123[:, 0:1], in1=c123[:, 1:2], op=ALU.add),
                 reads=['c123'], writes=['fl'])
            P.op('dve', lambda e: e.tensor_tensor(out=fl[:], in0=fl[:], in1=c123[:, 2:3], op=ALU.add),
                 reads=['c123', 'fl'], writes=['fl'])
            P.op('dve', lambda e: e.scalar_tensor_tensor(out=fl[:], in0=fl[:], scalar=-32.0, in1=pf[:], op0=ALU.mult, op1=ALU.add),
                 reads=['fl', 'pf'], writes=['fl'])
            P.op('act', lambda e: e.activation(out=inv[:], in_=fl[:], func=AF.Exp, scale=-float(np.log(10000.0)) / 32),
                 reads=['fl'], writes=['inv'])
            P.op('dve', lambda e: e.tensor_tensor(out=sgn[:], in0=c123[:, 0:1], in1=c123[:, 1:2], op=ALU.subtract),
                 reads=['c123'], writes=['sgn'])
            P.op('dve', lambda e: e.tensor_tensor(out=sgn[:], in0=sgn[:], in1=c123[:, 2:3], op=ALU.add),
                 reads=['c123', 'sgn'], writes=['sgn'])
            P.op('dve', lambda e: e.tensor_scalar(out=sgn[:], in0=sgn[:], scalar1=2.0, scalar2=-1.0, op0=ALU.mult, op1=ALU.add),
                 reads=['sgn'], writes=['sgn'])
            P.op('dve', lambda e: e.tensor_copy(out=ang[:], in_=pos_i[:]), reads=['pos_i'], writes=['ang'])
            P.op('dve', lambda e: e.tensor_scalar(out=ang[:], in0=ang[:], scalar1=inv[:, 0:1], scalar2=None, op0=ALU.mult),
                 reads=['ang', 'inv'], writes=['ang'])
            TWO_PI = 2.0 * float(np.pi)
            for which in range(2):
                src = ang
                if which == 1:
                    P.op('dve', lambda e: e.tensor_scalar(out=ang[:], in0=ang[:], scalar1=float(np.pi) / 2, scalar2=None, op0=ALU.add),
                         reads=['ang'], writes=['ang'])
                P.op('dve', lambda e: e.tensor_scalar(out=tmpa[:], in0=ang[:], scalar1=1.0 / TWO_PI, scalar2=None, op0=ALU.mult),
                     reads=['ang'], writes=['tmpa'])
                P.op('dve', lambda e: e.tensor_copy(out=ki[:], in_=tmpa[:]), reads=['tmpa'], writes=['ki'])
                P.op('dve', lambda e: e.tensor_copy(out=tmpa[:], in_=ki[:]), reads=['ki'], writes=['tmpa'])
                P.op('dve', lambda e: e.scalar_tensor_tensor(out=tmpa[:], in0=tmpa[:], scalar=-TWO_PI, in1=ang[:],
                                                             op0=ALU.mult, op1=ALU.add),
                     reads=['tmpa', 'ang'], writes=['tmpa'])
                for thr, opc, delta in ((float(np.pi), ALU.is_gt, -TWO_PI), (-float(np.pi), ALU.is_lt, TWO_PI)):
                    P.op('dve', lambda e, thr=thr, opc=opc, delta=delta: e.tensor_scalar(
                        out=tmpb[:], in0=tmpa[:], scalar1=thr, scalar2=delta, op0=opc, op1=ALU.mult),
                        reads=['tmpa'], writes=['tmpb'])
                    P.op('dve', lambda e: e.tensor_tensor(out=tmpa[:], in0=tmpa[:], in1=tmpb[:], op=ALU.add),
                         reads=['tmpa', 'tmpb'], writes=['tmpa'])
                P.op('dve', lambda e: e.tensor_scalar(out=tmpa[:], in0=tmpa[:], scalar1=3.14159, scalar2=-3.14159,
                                                      op0=ALU.min, op1=ALU.max), reads=['tmpa'], writes=['tmpa'])
                if which == 0:
                    P.op('act', lambda e: e.activation(out=sinS[:], in_=tmpa[:], func=AF.Sin, scale=sgn[:, 0:1]),
                         reads=['tmpa', 'sgn'], writes=['sinS'])
                else:
                    P.op('act', lambda e: e.activation(out=cosT[:], in_=tmpa[:], func=AF.Sin),
                         reads=['tmpa'], writes=['cosT'])
            P.barrier()
            P.flush()
        _tmp_scope.close()
        if not ('C' in stages and 'D' in stages) and not mix_input:
            zt = sb("zt", [128, 2048], BF16)
            P.op('pool', lambda e: e.memset(zt[:], 0.0), writes=['zt'])
            for r0 in range(0, 768, 128):
                for t0 in range(0, T, 2048):
                    n = min(2048, T - t0)
                    P.dma(mixT[r0:r0 + 128, t0:t0 + n], zt[:, 0:n], reads=['zt'])
            P.barrier()
            P.flush()
        for l in range(depth):
            hsrc = xT if l == 0 else hT
            if 'A' in stages:
              with contextlib.ExitStack() as st:
                sbt = lambda name, shape, dt=F32, u=uid(): st.enter_context(nc.sbuf_tensor(name + u, shape, dt))
                pst = lambda name, shape, dt=F32, u=uid(): st.enter_context(nc.psum_tensor(name + u, shape, dt))
                wbf = sbt("a_wbf", [128, 8, N_IN_PAD], BF16)
                wst = [sbt("a_wst%d" % i, [128, N_IN_PAD // 2], F32) for i in range(2)]
                gain = sbt("a_gain", [128, 8], F32)
                P.dma(gain[:], ln_mix[l], writes=[gain.name])
                for c in range(8):
                    for hf in range(2):
                        s = wst[hf]
                        csl = slice(hf * (N_IN_PAD // 2), (hf + 1) * (N_IN_PAD // 2))
                        P.dma(s[:], w_in[l, c * 128:(c + 1) * 128, csl], writes=[s.name])
                        cast(wbf[:, c, csl], s[:], [s.name], ['a_wbf'])
                h_sb = [sbt("a_h%d" % i, [128, 8, 512], F32) for i in range(2)]
                sq = sbt("a_sq", [128, 8, 512], BF16)
                xn = [sbt("a_xn%d" % i, [128, 8, 512], BF16) for i in range(2)]
                rstd = sbt("a_rstd", [128, 512], F32)
                ps_n = pst("a_psn", [128, 512])
                ps_o = [pst("a_pso%d" % i, [128, 512]) for i in range(4)]
                ost = [sbt("a_ost%d" % i, [128, 512], F32) for i in range(4)]
                for tt in range(NT):
                    tsl = slice(tt * 512, (tt + 1) * 512)
                    h = h_sb[tt % 2]
                    x_ = xn[tt % 2]
                    P.dma(h[:], pc(hsrc)[:, :, tsl], writes=[h.name])
                    emit_norm(h, gain, sq, ps_n, rstd, x_)
                    for m in range(24):
                        ps = ps_o[m % 4]
                        o = ost[m % 4]
                        for c in range(8):
                            P.op('pe', lambda e, c=c, m=m, ps=ps, x_=x_: e.matmul(
                                ps[:], lhsT=wbf[:, c, m * 128:(m + 1) * 128], rhs=x_[:, c, :],
                                start=(c == 0), stop=(c == 7)),
                                reads=[x_.name, 'a_wbf'], writes=[ps.name], inc=(c == 7))
                        cast(o[:], ps[:], [ps.name], [o.name], psum=True)
                        P.dma(projF[m * 128:(m + 1) * 128, tsl], o[:], reads=[o.name])
                    for ts in range(4):
                        ps = ps_o[ts % 4]
                        o = ost[ts % 4]
                        for c in range(8):
                            P.op('pe', lambda e, c=c, ts=ts, ps=ps, x_=x_: e.matmul(
                                ps[:], lhsT=x_[:, c, ts * 128:(ts + 1) * 128], rhs=wbf[:, c, 3072:3584],
                                start=(c == 0), stop=(c == 7)),
                                reads=[x_.name, 'a_wbf'], writes=[ps.name], inc=(c == 7))
                        cast(o[:], ps[:], [ps.name], [o.name], psum=True)
                        P.dma(projT[tt * 512 + ts * 128: tt * 512 + (ts + 1) * 128, :], o[:], reads=[o.name])
                P.barrier()
                P.flush()

            if 'B' in stages:
              with contextlib.ExitStack() as st:
                sbt = lambda name, shape, dt=F32, u=uid(): st.enter_context(nc.sbuf_tensor(name + u, shape, dt))
                pst = lambda name, shape, dt=F32, u=uid(): st.enter_context(nc.psum_tensor(name + u, shape, dt))
                NKB = T // 128
                qT = sbt("b_qT", [128, 2, T], BF16)
                kT = sbt("b_kT", [128, 2, T], BF16)
                vv = sbt("b_v", [128, NKB, 256], BF16)
                stg = [sbt("b_stg%d" % i, [128, 2048], F32) for i in range(2)]
                nw = sbt("b_nw", [128, 1], F32)
                P.dma(nw[:], sb_norm[l], writes=[nw.name])
                k = 0
                for c in range(2):
                    for t0 in range(0, T, 2048):
                        n = min(2048, T - t0)
                        s = stg[k % 2]; k += 1
                        P.dma(s[:, 0:n], projF[(19 + c) * 128:(20 + c) * 128, t0:t0 + n], writes=[s.name])
                        P.op('dve', lambda e, s=s, c=c, t0=t0, n=n: e.tensor_scalar(
                            out=qT[:, c, t0:t0 + n], in0=s[:, 0:n], scalar1=0.125, scalar2=None, op0=ALU.mult),
                            reads=[s.name], writes=['b_qT'])
                        s = stg[k % 2]; k += 1
                        P.dma(s[:, 0:n], projF[(21 + c) * 128:(22 + c) * 128, t0:t0 + n], writes=[s.name])
                        cast(kT[:, c, t0:t0 + n], s[:, 0:n], [s.name], ['b_kT'])
                pTv = projT.rearrange("(kb p) f -> p kb f", p=128)
                for kb0 in range(0, NKB, 8):
                    s = stg[k % 2]; k += 1
                    sv = s[:].rearrange("p (a f) -> p a f", f=256)
                    P.dma(sv, pTv[:, kb0:kb0 + 8, 256:512], writes=[s.name])
                    cast(vv[:, kb0:kb0 + 8, :], sv, [s.name], ['b_v'])
                e_sb = [sbt("b_e%d" % i, [128, 512], F32) for i in range(3)]
                sp_sb = [sbt("b_sp%d" % i, [128, 512], BF16) for i in range(3)]
                a_sb = [sbt("b_a%d" % i, [128, 512], BF16) for i in range(3)]
                racc = sbt("b_racc", [128, 512], BF16)
                sqo = sbt("b_sqo", [64, 512], BF16)
                rs = sbt("b_rs", [64, 512], F32)
                o_sb = [sbt("b_o%d" % i, [64, 512], BF16) for i in range(2)]
                psZ = [pst("b_psZ%d" % i, [128, 512]) for i in range(2)]
                psA = [pst("b_psA%d" % i, [128, 512]) for i in range(2)]
                psO = [pst("b_psO%d" % i, [64, 512]) for i in range(2)]
                psN = pst("b_psN", [64, 512])
                psW = pst("b_psW", [128, 512])
                it3 = [0]; it4 = [0]
                ho = 0
                for hh in range(4):
                    c = hh // 2
                    b0 = (hh % 2) * 64
                    for qt in range(NT):
                        qsl = slice(qt * 512, (qt + 1) * 512)
                        po = psO[ho % 2]
                        oo = o_sb[ho % 2]
                        ho += 1
                        nblk = 4 * (qt + 1)
                        warm(psW)
                        kbs = list(reversed(range(nblk)))
                        nb_ = len(kbs)
                        st_ = {}

                        def S1(t, kbs=kbs, qt=qt, b0=b0, c=c, qsl=qsl, st_=st_):
                            kb = kbs[t]
                            j = kb - 4 * qt
                            ksl = slice(kb * 128, (kb + 1) * 128)
                            pz = psZ[it3[0] % 2]; ee = e_sb[it3[0] % 3]; sp = sp_sb[it3[0] % 3]; it3[0] += 1
                            st_[t] = dict(kb=kb, j=j, ksl=ksl, sp=sp, ee=ee)
                            P.op('pe', lambda e: e.matmul(
                                pz[:], lhsT=kT[b0:b0 + 64, c, ksl], rhs=qT[b0:b0 + 64, c, qsl], start=True, stop=True),
                                reads=['b_kT', 'b_qT'], writes=[pz.name])
                            P.op('act', lambda e: e.activation(out=ee[:], in_=pz[:], func=AF.Exp),
                                 reads=[pz.name], writes=[ee.name])
                            P.op('act', lambda e: e.activation(out=sp[:], in_=ee[:], func=AF.Ln, bias=one_t[:]),
                                 reads=[ee.name, 'one_t'], writes=[sp.name])
                            if j >= 0:
                                P.op('dve', lambda e: e.tensor_tensor(out=sp[:], in0=sp[:], in1=dmask[:, j, :], op=ALU.mult),
                                     reads=[sp.name, 'dmask'], writes=[sp.name])

                        def S2(t, b0=b0, c=c, qsl=qsl, st_=st_):
                            d_ = st_[t]
                            kb, j, ksl, sp = d_['kb'], d_['j'], d_['ksl'], d_['sp']
                            first = (t == 0)
                            pa = psA[it4[0] % 2]; aa = a_sb[it4[0] % 3]; it4[0] += 1
                            d_['aa'] = aa
                            ee = d_['ee']
                            P.op('pe', lambda e: e.matmul(pa[:], lhsT=negU[:], rhs=sp[:], start=True, stop=first),
                                 reads=[sp.name, 'negU'], writes=[pa.name], inc=first)
                            if not first:
                                P.op('pe', lambda e: e.matmul(pa[:], lhsT=negOnes[:], rhs=racc[:], start=False, stop=True),
                                     reads=['b_racc', 'negOnes'], writes=[pa.name])
                            P.op('act', lambda e: e.activation(out=aa[:], in_=pa[:], func=AF.Exp),
                                 reads=[pa.name], writes=[aa.name])
                            P.op('dve', lambda e: e.tensor_tensor(out=aa[:], in0=aa[:], in1=ee[:], op=ALU.mult),
                                 reads=[aa.name, ee.name], writes=[aa.name])
                            if j >= 0:
                                P.op('dve', lambda e: e.tensor_tensor(out=aa[:], in0=aa[:], in1=dmask[:, j, :], op=ALU.mult),
                                     reads=[aa.name, 'dmask'], writes=[aa.name])
                            if kb > 0:
                                if first:
                                    P.op('pool', lambda e: e.tensor_copy(out=racc[:], in_=sp[:]),
                                         reads=[sp.name], writes=['b_racc'])
                                else:
                                    P.op('pool', lambda e: e.tensor_tensor(out=racc[:], in0=racc[:], in1=sp[:], op=ALU.add),
                                         reads=[sp.name, 'b_racc'], writes=['b_racc'])

                        def S3(t, po=po, hh=hh, st_=st_, nb_=nb_):
                            d_ = st_[t]
                            kb, aa = d_['kb'], d_['aa']
                            P.op('pe', lambda e: e.matmul(
                                po[:], lhsT=vv[:, kb, hh * 64:(hh + 1) * 64], rhs=aa[:], start=(t == 0), stop=(t == nb_ - 1)),
                                reads=[aa.name, 'b_v'], writes=[po.name], inc=True)

                        for t in range(nb_ + 2):
                            if t < nb_:
                                S1(t)
                            if 1 <= t <= nb_:
                                S2(t - 1)
                            if t >= 2:
                                S3(t - 2)
                        P.op('act', lambda e, po=po: e.activation(out=sqo[:], in_=po[:], func=AF.Square),
                             reads=[po.name], writes=['b_sqo'])
                        P.op('pe', lambda e: e.matmul(psN[:], lhsT=ones_bf[0:64, 0:64], rhs=sqo[:], start=True, stop=True),
                             reads=['b_sqo', 'ones_bf'], writes=['b_psN'])
                        P.op('act', lambda e: e.activation(out=rs[:], in_=psN[:], func=AF.Ln, bias=eps_t[0:64, :], scale=1.0 / 64),
                             reads=['b_psN', 'eps_t'], writes=['b_rs'])
                        P.op('act', lambda e: e.activation(out=rs[:], in_=rs[:], func=AF.Exp, scale=-0.5),
                             reads=['b_rs'], writes=['b_rs'])
                        P.op('dve', lambda e, po=po, oo=oo: e.scalar_tensor_tensor(
                            out=oo[:], in0=po[:], scalar=nw[0:64, :], in1=rs[:], op0=ALU.mult, op1=ALU.mult),
                            reads=[po.name, nw.name, 'b_rs'], writes=[oo.name])
                        P.dma(mixT[768 + hh * 64:768 + (hh + 1) * 64, qsl], oo[:], reads=[oo.name])
                P.barrier()
                P.flush()

            if 'C' in stages:
              with contextlib.ExitStack() as st:
                sbt = lambda name, shape, dt=F32, u=uid(): st.enter_context(nc.sbuf_tensor(name + u, shape, dt))
                qT = sbt("c_qT", [128, 3, T], BF16)
                ksT = sbt("c_ksT", [128, T], BF16)
                kwT = sbt("c_kwT", [128, T], BF16)
                vs = sbt("c_vs", [128, NKB, 128], BF16)
                vw = sbt("c_vw", [128, NKB, 128], BF16)
                sgT = sbt("c_sgT", [128, T], BF16)
                selT = sbt("c_selT", [128, T], BF16)
                kcT = sbt("c_kcT", [128, 256], BF16)
                vc = sbt("c_vc", [128, 2, 2, 64], BF16)
                nw = sbt("c_nw", [128, 1], F32)
                P.dma(nw[:], nsa_norm[l], writes=[nw.name])
                ICS = [(ic, min(128, NCMP - ic * 128)) for ic in range(2) if NCMP - ic * 128 > 0]
                with contextlib.ExitStack() as s1:
                    sb1 = lambda name, shape, dt=F32, u=uid(): s1.enter_context(nc.sbuf_tensor(name + u, shape, dt))
                    ps1 = lambda name, shape, dt=F32, u=uid(): s1.enter_context(nc.psum_tensor(name + u, shape, dt))
                    stg = [sb1("c_stg%d" % i, [128, 2048], F32) for i in range(2)]
                    kcr = sb1("c_kcr", [128, T], BF16)
                    vcb = sb1("c_vcb", [128, T], BF16)
                    xb_r = [sb1("c_xb%d" % i, [128, 512], BF16) for i in range(2)]
                    t1_r = [sb1("c_t1%d" % i, [128, 512], F32) for i in range(2)]
                    t2_r = [sb1("c_t2%d" % i, [128, 512], F32) for i in range(2)]
                    psR = [ps1("c_psR%d" % i, [128, 512]) for i in range(2)]
                    cnt = [0, 0]

                    def load_chunk(ch, fn):
                        for t0 in range(0, T, 2048):
                            n = min(2048, T - t0)
                            s = stg[cnt[0] % 2]; cnt[0] += 1
                            P.dma(s[:, 0:n], projF[ch * 128:(ch + 1) * 128, t0:t0 + n], writes=[s.name])
                            fn(s, t0, n)

                    def rope_to(dst, dkey, scale):
                        def fn(s, t0, n):
                            for sub in range(n // 512):
                                sl = slice(sub * 512, (sub + 1) * 512)
                                gsl = slice(t0 + sub * 512, t0 + (sub + 1) * 512)
                                i = cnt[1] % 2; cnt[1] += 1
                                xb, t1, t2, ps = xb_r[i], t1_r[i], t2_r[i], psR[i]
                                cast(xb[:], s[:, sl], [s.name], [xb.name])
                                P.op('pe', lambda e, ps=ps, xb=xb: e.matmul(ps[:], lhsT=Psw[:], rhs=xb[:], start=True, stop=True),
                                     reads=[xb.name, 'Psw'], writes=[ps.name])
                                P.op('dve', lambda e, t1=t1, s=s, sl=sl, gsl=gsl: e.scalar_tensor_tensor(
                                    out=t1[:], in0=s[:, sl], scalar=scale, in1=cosT[:, gsl], op0=ALU.mult, op1=ALU.mult),
                                    reads=[s.name, 'cosT'], writes=[t1.name])
                                P.op('dve', lambda e, t2=t2, ps=ps, gsl=gsl: e.scalar_tensor_tensor(
                                    out=t2[:], in0=ps[:], scalar=scale, in1=sinS[:, gsl], op0=ALU.mult, op1=ALU.mult),
                                    reads=[ps.name, 'sinS'], writes=[t2.name])
                                P.op('pool', lambda e, t1=t1, t2=t2, gsl=gsl: e.tensor_tensor(
                                    out=dst(gsl), in0=t1[:], in1=t2[:], op=ALU.add),
                                    reads=[t1.name, t2.name], writes=[dkey])
                        return fn

                    for r in range(3):
                        load_chunk(12 + r, rope_to(lambda gsl, r=r: qT[:, r, gsl], 'c_qT', 0.125))
                    load_chunk(15, rope_to(lambda gsl: kcr[:, gsl], 'c_kcr', 1.0))
                    load_chunk(17, rope_to(lambda gsl: ksT[:, gsl], 'c_ksT', 1.0))
                    load_chunk(18, rope_to(lambda gsl: kwT[:, gsl], 'c_kwT', 1.0))
                    load_chunk(16, lambda s, t0, n: cast(vcb[:, t0:t0 + n], s[:, 0:n], [s.name], ['c_vcb']))
                    load_chunk(23, lambda s, t0, n: P.op('act', lambda e: e.activation(
                        out=sgT[:, t0:t0 + n], in_=s[:, 0:n], func=AF.Sigmoid), reads=[s.name], writes=['c_sgT']))
                    pTv = projT.rearrange("(kb p) f -> p kb f", p=128)
                    for kb0 in range(0, NKB, 8):
                        for (dstv, c0) in ((vs, 0), (vw, 128)):
                            s = stg[cnt[0] % 2]; cnt[0] += 1
                            sv = s[:, 0:1024].rearrange("p (a f) -> p a f", f=128)
                            P.dma(sv, pTv[:, kb0:kb0 + 8, c0:c0 + 128], writes=[s.name])
                            cast(dstv[:, kb0:kb0 + 8, :], sv, [s.name], [dstv.name])
                    w1b = [sb1("c_w1%d" % i, [128, 32, 128], BF16) for i in range(2)]
                    w2kp = sb1("c_w2kp", [128, 2, 128], BF16)
                    w2v = sb1("c_w2v", [128, 64], BF16)
                    peb = [sb1("c_pe%d" % i, [128, 32], BF16) for i in range(2)]
                    bias = [sb1("c_bias%d" % i, [128, 1], F32) for i in range(2)]
                    hid = [[sb1("c_hid%d%d" % (i, g), [128, 256], BF16) for g in range(2)] for i in range(2)]
                    psH = [ps1("c_psH%d" % i, [128, 256]) for i in range(2)]
                    psB = ps1("c_psB", [128, 64])
                    P.op('pool', lambda e: e.memset(w2kp[:], 0.0), writes=['c_w2kp'])
                    for i, (w1d, w2d, ped) in enumerate(((cmp_k_w1, cmp_k_w2, pe_kT), (cmp_v_w1, cmp_v_w2, pe_vT))):
                        for hf in range(2):
                            s = stg[cnt[0] % 2]; cnt[0] += 1
                            sv = s[:].rearrange("p (a f) -> p a f", f=128)
                            P.dma(sv, w1d[l, :, hf * 16:(hf + 1) * 16, :], writes=[s.name])
                            cast(w1b[i][:, hf * 16:(hf + 1) * 16, :], sv, [s.name], [w1b[i].name])
                        s = stg[cnt[0] % 2]; cnt[0] += 1
                        P.dma(s[:, 0:64], w2d[l], writes=[s.name])
                        if i == 0:
                            for g in range(2):
                                cast(w2kp[:, g, g * 64:(g + 1) * 64], s[:, 0:64], [s.name], ['c_w2kp'])
                        else:
                            cast(w2v[:], s[:, 0:64], [s.name], ['c_w2v'])
                        s = stg[cnt[0] % 2]; cnt[0] += 1
                        P.dma(s[:, 0:32], ped[l], writes=[s.name])
                        cast(peb[i][:], s[:, 0:32], [s.name], [peb[i].name])
                        for ll in range(32):
                            P.op('pe', lambda e, i=i, ll=ll: e.matmul(
                                psB[:, 0:1], lhsT=w1b[i][0:64, ll, :], rhs=peb[i][0:64, ll:ll + 1],
                                start=(ll == 0), stop=(ll == 31)),
                                reads=[w1b[i].name, peb[i].name], writes=['c_psB'], inc=(ll == 31))
                        P.op('dve', lambda e, i=i: e.tensor_copy(out=bias[i][:], in_=psB[:, 0:1]),
                             reads=['c_psB'], writes=[bias[i].name])
                        src = kcr if i == 0 else vcb
                        srcv = src[:].rearrange("p (i s) -> p i s", s=16)
                        for g in range(2):
                            ph = psH[g]
                            for ll in range(32):
                                a, s0 = ll // 16, ll % 16
                                P.op('pe', lambda e, i=i, g=g, ll=ll, a=a, s0=s0, ph=ph, srcv=srcv: e.matmul(
                                    ph[:, 0:NCMP], lhsT=w1b[i][g * 64:(g + 1) * 64, ll, :],
                                    rhs=srcv[g * 64:(g + 1) * 64, a:a + NCMP, s0],
                                    start=(ll == 0), stop=(ll == 31)),
                                    reads=[w1b[i].name, 'c_kcr' if i == 0 else 'c_vcb'], writes=[ph.name],
                                    inc=(ll == 31))
                            P.op('act', lambda e, i=i, g=g, ph=ph: e.activation(
                                out=hid[i][g][:, 0:NCMP], in_=ph[:, 0:NCMP], func=AF.Silu, bias=bias[i][:]),
                                reads=[ph.name, bias[i].name], writes=[hid[i][g].name])
                    for g in range(2):
                        P.op('pe', lambda e, g=g: e.matmul(psH[0][:, 0:NCMP], lhsT=w2kp[:, g, :], rhs=hid[0][g][:, 0:NCMP],
                                                           start=(g == 0), stop=(g == 1)),
                             reads=['c_w2kp', hid[0][g].name], writes=[psH[0].name], inc=(g == 1))
                    P.op('act', lambda e: e.copy(out=kcT[:, 0:NCMP], in_=psH[0][:, 0:NCMP]),
                         reads=[psH[0].name], writes=['c_kcT'])
                    for g in range(2):
                        for (ic, n) in ICS:
                            P.op('pe', lambda e, g=g, ic=ic, n=n: e.matmul(
                                psB[0:n, 0:64], lhsT=hid[1][g][:, ic * 128:ic * 128 + n], rhs=w2v[:], start=True, stop=True),
                                reads=[hid[1][g].name, 'c_w2v'], writes=['c_psB'])
                            P.op('dve', lambda e, g=g, ic=ic, n=n: e.tensor_copy(out=vc[0:n, g, ic, :], in_=psB[0:n, 0:64]),
                                 reads=['c_psB'], writes=['c_vc'])
                    P.barrier()
                    P.flush()
                with contextlib.ExitStack() as s2:
                    sb2 = lambda name, shape, dt=F32, u=uid(): s2.enter_context(nc.sbuf_tensor(name + u, shape, dt))
                    ps2 = lambda name, shape, dt=F32, u=uid(): s2.enter_context(nc.psum_tensor(name + u, shape, dt))
                    psS = [ps2("c_psS%d" % i, [128, 512]) for i in range(2)]
                    psD = ps2("c_psD", [128, 512])
                    psW = ps2("c_psW", [128, 512])
                    psO = ps2("c_psO", [64, 512])
                    psDn = ps2("c_psDn", [64, 512])
                    psM = ps2("c_psM", [128, 512])
                    psG = ps2("c_psG", [64, 512])
                    pe_r = [sb2("c_pe_%d" % i, [128, 512], BF16) for i in range(2)]
                    a_r = [sb2("c_a%d" % i, [128, 512], BF16) for i in range(3)]
                    pn = [sb2("c_pn%d" % r, [128, 2, 512], BF16) for r in range(3)]
                    rden = sb2("c_rden", [128, 512], F32)
                    asum = sb2("c_asum", [128, 512], F32)
                    oc_sets = [[sb2("c_oc%d_%d" % (k, r), [64, 512], F32) for r in range(3)] for k in range(2)]
                    ob_sb = sb2("c_ob", [64, 512], F32)
                    acc = sb2("c_acc", [64, 512], F32)
                    acc2 = sb2("c_acc2", [64, 512], F32)
                    rd64 = sb2("c_rd64", [64, 512], F32)
                    sqo = sb2("c_sqo", [64, 512], BF16)
                    rs = sb2("c_rs", [64, 512], F32)
                    o_out = [sb2("c_oo%d" % i, [64, 512], BF16) for i in range(2)]
                    score = sb2("c_score", [128, 64], F32)
                    score2 = sb2("c_score2", [128, 64], F32)
                    m8a = sb2("c_m8a", [128, 8], F32)
                    m8b = sb2("c_m8b", [128, 8], F32)
                    selm = sb2("c_selm", [128, 128], F32)
                    it = [0]
                    itm = [0]
                    oi = [0]
                    P.op('pool', lambda e: e.memset(selm[:], 0.0), writes=['c_selm'])

                    def attn_core(g, r, qt, kT_src, ksrc_key, v_src, kbs, mask_of, with_sel, pump):
                        qsl = slice(qt * 512, (qt + 1) * 512)
                        gs = slice(g * 64, (g + 1) * 64)
                        warm(psW)
                        tl = {}

                        def score(bi):
                            kb = kbs[bi]
                            ksl = slice(kb * 128, (kb + 1) * 128)
                            ps = psS[itm[0] % 2]; aa = a_r[itm[0] % 3]; itm[0] += 1
                            tl[bi] = aa
                            P.op('pe', lambda e: e.matmul(
                                ps[:], lhsT=kT_src[gs, ksl], rhs=qT[gs, r, qsl], start=True, stop=not with_sel),
                                reads=[ksrc_key, 'c_qT'], writes=[ps.name], inc=not with_sel)
                            if with_sel:
                                P.op('pe', lambda e: e.matmul(
                                    ps[:], lhsT=Ebig[gs, ksl], rhs=selT[gs, qsl], start=False, stop=True),
                                    reads=['Ebig', ('c_selT', g, qt)], writes=[ps.name])
                            P.op('act', lambda e: e.activation(out=aa[:], in_=ps[:], func=AF.Exp),
                                 reads=[ps.name], writes=[aa.name])
                            m = mask_of(kb)
                            if m is not None:
                                mt, mkey = m
                                P.op('dve', lambda e: e.tensor_tensor(out=aa[:], in0=aa[:], in1=mt, op=ALU.mult),
                                     reads=[aa.name, mkey], writes=[aa.name])

                        def av(bi):
                            kb = kbs[bi]
                            aa = tl[bi]
                            first = (bi == 0)
                            last = (bi == len(kbs) - 1)
                            P.op('pe', lambda e: e.matmul(
                                psO[:], lhsT=v_src[:, kb, gs], rhs=aa[:], start=first, stop=last),
                                reads=[aa.name, v_src.name], writes=['c_psO'])
                            if first:
                                P.op('pool', lambda e: e.tensor_copy(out=asum[:], in_=aa[:]), reads=[aa.name], writes=['c_asum'])
                            else:
                                P.op('pool', lambda e: e.tensor_tensor(out=asum[:], in0=asum[:], in1=aa[:], op=ALU.add),
                                     reads=[aa.name, 'c_asum'], writes=['c_asum'])

                        for bi in range(len(kbs) + 1):
                            if bi < len(kbs):
                                score(bi)
                            if bi >= 1:
                                av(bi - 1)
                            pump()
                        P.op('pe', lambda e: e.matmul(psDn[:], lhsT=ones_f[:, 0:64], rhs=asum[:], start=True, stop=True),
                             reads=['c_asum', 'ones_f'], writes=['c_psDn'])
                        P.op('dve', lambda e: e.tensor_scalar(out=rd64[:], in0=psDn[:], scalar1=1e-30, scalar2=None, op0=ALU.max),
                             reads=['c_psDn'], writes=['c_rd64'])
                        P.op('dve', lambda e: e.reciprocal(out=rd64[:], in_=rd64[:]), reads=['c_rd64'], writes=['c_rd64'])
                        P.op('dve', lambda e: e.tensor_tensor(out=ob_sb[:], in0=psO[:], in1=rd64[:], op=ALU.mult),
                             reads=['c_psO', 'c_rd64'], writes=['c_ob'])

                    def pre_gen(g, qt, oc_sb):
                        gs = slice(g * 64, (g + 1) * 64)
                        selkey = ('c_selT', g, qt)
                        if True:
                            qsl = slice(qt * 512, (qt + 1) * 512)
                            ics = [(ic, n) for (ic, n) in ICS if 16 * ic * 128 + 31 <= qt * 512 + 511]
                            for r in (range(3) if 'cmp' in CPARTS else ()):
                                pes = []
                                for (ic, n) in ics:
                                    ps = psS[it[0] % 2]; pp = pe_r[it[0] % 2]; it[0] += 1
                                    P.op('pe', lambda e, ps=ps, ic=ic, n=n, r=r, gs=gs, qsl=qsl: e.matmul(
                                        ps[0:n, :], lhsT=kcT[gs, ic * 128:ic * 128 + n], rhs=qT[gs, r, qsl], start=True, stop=True),
                                        reads=['c_kcT', 'c_qT'], writes=[ps.name])
                                    P.op('act', lambda e, ps=ps, pp=pp, n=n: e.activation(out=pp[0:n, :], in_=ps[0:n, :], func=AF.Exp),
                                         reads=[ps.name], writes=[pp.name])
                                    P.op('dve', lambda e, pp=pp, n=n, ic=ic, qsl=qsl: e.tensor_tensor(
                                        out=pp[0:n, :], in0=pp[0:n, :], in1=cmask[0:n, ic, qsl], op=ALU.mult),
                                        reads=[pp.name, 'cmask'], writes=[pp.name])
                                    pes.append((ic, n, pp))
                                    yield
                                for bi, (ic, n, pp) in enumerate(pes):
                                    P.op('pe', lambda e, pp=pp, n=n, bi=bi, nb=len(pes): e.matmul(
                                        psD[:], lhsT=ones_bf[0:n, :], rhs=pp[0:n, :], start=(bi == 0), stop=(bi == nb - 1)),
                                        reads=[pp.name, 'ones_bf'], writes=['c_psD'], inc=(bi == len(pes) - 1))
                                P.op('dve', lambda e: e.tensor_scalar(out=rden[:], in0=psD[:], scalar1=1e-30, scalar2=None, op0=ALU.max),
                                     reads=['c_psD'], writes=['c_rden'])
                                P.op('dve', lambda e: e.reciprocal(out=rden[:], in_=rden[:]), reads=['c_rden'], writes=['c_rden'])
                                yield
                                for (ic, n, pp) in pes:
                                    P.op('dve', lambda e, pp=pp, n=n, ic=ic, r=r: e.tensor_tensor(
                                        out=pn[r][0:n, ic, :], in0=pp[0:n, :], in1=rden[0:n, :], op=ALU.mult),
                                        reads=[pp.name, 'c_rden'], writes=[pn[r].name])
                                for bi, (ic, n, pp) in enumerate(pes):
                                    P.op('pe', lambda e, n=n, ic=ic, r=r, g=g, bi=bi, nb=len(pes): e.matmul(
                                        psD[0:64, :], lhsT=vc[0:n, g, ic, :], rhs=pn[r][0:n, ic, :], start=(bi == 0), stop=(bi == nb - 1)),
                                        reads=['c_vc', pn[r].name], writes=['c_psD'], inc=(bi == len(pes) - 1))
                                P.op('act', lambda e, r=r, oc_sb=oc_sb: e.copy(out=oc_sb[r][:], in_=psD[0:64, :]),
                                     reads=['c_psD'], writes=[oc_sb[r].name])
                                yield
                            for ts in (range(4) if 'topk' in CPARTS else ()):
                                t0 = qt * 512 + ts * 128
                                tsl = slice(ts * 128, (ts + 1) * 128)
                                nmm = 3 * len(ics)
                                k = 0
                                for r in range(3):
                                    for (ic, n) in ics:
                                        P.op('pe', lambda e, r=r, ic=ic, n=n, tsl=tsl, k=k, nmm=nmm: e.matmul(
                                            psM[:, 0:64], lhsT=pn[r][0:n, ic, tsl], rhs=ov[0:n, ic, :],
                                            start=(k == 0), stop=(k == nmm - 1)),
                                            reads=[pn[r].name, 'ov'], writes=['c_psM'], inc=(k == nmm - 1))
                                        k += 1
                                yield
                                off = 64 - t0 // 64
                                P.op('dve', lambda e, off=off: e.tensor_tensor(
                                    out=score[:], in0=psM[:, 0:64], in1=keepB[:, off:off + 64], op=ALU.mult),
                                    reads=['c_psM', 'keepB'], writes=['c_score'])
                                P.op('dve', lambda e, off=off: e.tensor_tensor(
                                    out=score[:], in0=score[:], in1=addcB[:, off:off + 64], op=ALU.add),
                                    reads=['c_score', 'addcB'], writes=['c_score'])
                                P.op('dve', lambda e: e.memset(score[:, 0:1], 1e4), reads=[], writes=['c_score'])
                                yield
                                P.op('dve', lambda e: e.max(out=m8a[:], in_=score[:]), reads=['c_score'], writes=['c_m8a'])
                                P.op('dve', lambda e: e.match_replace(out=score2[:], in_to_replace=m8a[:], in_values=score[:],
                                                                      imm_value=-1e9),
                                     reads=['c_score', 'c_m8a'], writes=['c_score2'])
                                P.op('dve', lambda e: e.max(out=m8b[:], in_=score2[:]), reads=['c_score2'], writes=['c_m8b'])
                                P.op('dve', lambda e, gs=gs: e.tensor_scalar(out=selm[:, gs], in0=score[:], scalar1=m8b[:, 7:8], scalar2=-1.0,
                                                                      op0=ALU.is_ge, op1=ALU.add),
                                     reads=['c_score', 'c_m8b'], writes=['c_selm'])
                                yield
                                P.op('pe', lambda e: e.matmul(psM[:, 128:256], lhsT=selm[:], rhs=ident_f[:], start=True, stop=True),
                                     reads=['c_selm', 'ident_f'], writes=['c_psM'])
                                P.op('act', lambda e, gs=gs, t0=t0: e.copy(out=selT[gs, t0:t0 + 128], in_=psM[gs, 128:256]),
                                     reads=['c_psM'], writes=[selkey])
                            yield

                    def main_part(g, qt, oc_sb, pump):
                        gs = slice(g * 64, (g + 1) * 64)
                        if True:
                            qsl = slice(qt * 512, (qt + 1) * 512)
                            for r in range(3):
                                hh = 3 * g + r
                                def gate_mul(dst, src_ap, src_key, br, hh=hh, qsl=qsl):
                                    P.op('pe', lambda e: e.matmul(psG[:], lhsT=oh[:, br * 6 + hh, :], rhs=sgT[:, qsl], start=True, stop=True),
                                         reads=['oh', 'c_sgT'], writes=['c_psG'])
                                    P.op('dve', lambda e: e.tensor_tensor(out=dst[:], in0=src_ap, in1=psG[:], op=ALU.mult),
                                         reads=[src_key, 'c_psG'], writes=[dst.name])
                                gate_mul(acc, oc_sb[r][:], oc_sb[r].name, 0)
                                if 'sel' in CPARTS:
                                  attn_core(g, r, qt, ksT, 'c_ksT', vs, list(range(4 * (qt + 1))),
                                            lambda kb, qt=qt: ((dmaskI[:, kb - 4 * qt, :], 'dmaskI') if kb >= 4 * qt else None), True, pump)
                                gate_mul(acc2, ob_sb[:], 'c_ob', 1)
                                P.op('pool', lambda e: e.tensor_tensor(out=acc[:], in0=acc[:], in1=acc2[:], op=ALU.add),
                                     reads=[acc.name, acc2.name], writes=[acc.name])
                                if 'win' in CPARTS:
                                  attn_core(g, r, qt, kwT, 'c_kwT', vw, list(range(max(0, 4 * qt - 4), 4 * qt + 4)),
                                            lambda kb, qt=qt: ((dmaskI[:, kb - 4 * qt, :], 'dmaskI') if kb >= 4 * qt
                                                               else (wmask[:, kb - (4 * qt - 4), :], 'wmask')), False, pump)
                                gate_mul(acc2, ob_sb[:], 'c_ob', 2)
                                P.op('pool', lambda e: e.tensor_tensor(out=acc[:], in0=acc[:], in1=acc2[:], op=ALU.add),
                                     reads=[acc.name, acc2.name], writes=[acc.name])
                                oo = o_out[oi[0] % 2]; oi[0] += 1
                                P.op('act', lambda e: e.activation(out=sqo[:], in_=acc[:], func=AF.Square),
                                     reads=[acc.name], writes=['c_sqo'])
                                P.op('pe', lambda e: e.matmul(psG[:], lhsT=ones_bf[0:64, 0:64], rhs=sqo[:], start=True, stop=True),
                                     reads=['c_sqo', 'ones_bf'], writes=['c_psG'])
                                P.op('act', lambda e: e.activation(out=rs[:], in_=psG[:], func=AF.Ln, bias=eps_t[0:64, :], scale=1.0 / 64),
                                     reads=['c_psG', 'eps_t'], writes=['c_rs'])
                                P.op('act', lambda e: e.activation(out=rs[:], in_=rs[:], func=AF.Exp, scale=-0.5),
                                     reads=['c_rs'], writes=['c_rs'])
                                P.op('dve', lambda e, oo=oo: e.scalar_tensor_tensor(
                                    out=oo[:], in0=acc[:], scalar=nw[0:64, :], in1=rs[:], op0=ALU.mult, op1=ALU.mult),
                                    reads=[acc.name, nw.name, 'c_rs'], writes=[oo.name])
                                P.dma(mixT[384 + hh * 64:384 + (hh + 1) * 64, qsl], oo[:], reads=[oo.name])

                    order = [(g, qt) for g in range(2) for qt in range(NT)]

                    def drain(gen):
                        for _ in gen:
                            pass

                    drain(pre_gen(order[0][0], order[0][1], oc_sets[0]))
                    for idx, (g, qt) in enumerate(order):
                        nxt = (pre_gen(order[idx + 1][0], order[idx + 1][1], oc_sets[(idx + 1) % 2])
                               if idx + 1 < len(order) else None)

                        def pump(nxt=nxt):
                            if nxt is None:
                                return
                            for _ in range(3):
                                try:
                                    next(nxt)
                                except StopIteration:
                                    return
                        main_part(g, qt, oc_sets[idx % 2], pump)
                        if nxt is not None:
                            drain(nxt)
                    P.barrier()
                    P.flush()

            if 'D' in stages:
              NG = 13 * 128
              with contextlib.ExitStack() as st:
                sbt = lambda name, shape, dt=F32, u=uid(): st.enter_context(nc.sbuf_tensor(name + u, shape, dt))
                with contextlib.ExitStack() as s1:
                    sb1 = lambda name, shape, dt=F32, u=uid(): s1.enter_context(nc.sbuf_tensor(name + u, shape, dt))
                    ps1 = lambda name, shape, dt=F32, u=uid(): s1.enter_context(nc.psum_tensor(name + u, shape, dt))
                    xc = [sb1("d_xc%d" % i, [128, T + 3], F32) for i in range(2)]
                    cv = [sb1("d_cv%d" % i, [128, T], F32) for i in range(2)]
                    cw = sb1("d_cw", [128, 9, 4], F32)
                    tst = [sb1("d_tst%d" % i, [128, 512], F32) for i in range(3)]
                    psT = [ps1("d_psT%d" % i, [128, 512]) for i in range(3)]
                    P.dma(cw[:], gdn_conv[l], writes=['d_cw'])
                    for i in range(2):
                        P.op('pool', lambda e, i=i: e.memset(xc[i][:, 0:3], 0.0), writes=[xc[i].name])
                    gview = gtok.rearrange("(kb p) f -> p kb f", p=128)
                    k = 0
                    for ci, ch in enumerate(list(range(12)) + [23]):
                        x_ = xc[ci % 2]
                        c_ = cv[ci % 2]
                        P.dma(x_[:, 3:T + 3], projF[ch * 128:(ch + 1) * 128, :], writes=[x_.name])
                        if ch < 9:
                            P.op('dve', lambda e, x_=x_, c_=c_, ch=ch: e.tensor_scalar(
                                out=c_[:], in0=x_[:, 0:T], scalar1=cw[:, ch, 0:1], scalar2=None, op0=ALU.mult),
                                reads=[x_.name, 'd_cw'], writes=[c_.name])
                            for tap in (1, 2, 3):
                                P.op('dve', lambda e, x_=x_, c_=c_, ch=ch, tap=tap: e.scalar_tensor_tensor(
                                    out=c_[:], in0=x_[:, tap:tap + T], scalar=cw[:, ch, tap:tap + 1], in1=c_[:],
                                    op0=ALU.mult, op1=ALU.add),
                                    reads=[x_.name, 'd_cw', c_.name], writes=[c_.name])
                            P.op('act', lambda e, c_=c_: e.activation(out=c_[:], in_=c_[:], func=AF.Silu),
                                 reads=[c_.name], writes=[c_.name])
                            src, skey = (lambda sl, c_=c_: c_[:, sl]), c_.name
                        else:
                            src, skey = (lambda sl, x_=x_: x_[:, 3 + sl.start:3 + sl.stop]), x_.name
                        for kb0 in range(0, NKB, 4):
                            pt = psT[k % 3]; ts_ = tst[k % 3]; k += 1
                            for q4 in range(4):
                                kb = kb0 + q4
                                P.op('pe', lambda e, pt=pt, q4=q4, kb=kb, src=src: e.matmul(
                                    pt[:, q4 * 128:(q4 + 1) * 128], lhsT=src(slice(kb * 128, (kb + 1) * 128)), rhs=ident_f[:],
                                    start=True, stop=True),
                                    reads=[skey, 'ident_f'], writes=[pt.name], inc=(q4 == 3))
                            cast(ts_[:], pt[:], [pt.name], [ts_.name], psum=True)
                            P.dma(gview[:, kb0:kb0 + 4, ci * 128:(ci + 1) * 128],
                                  ts_[:].rearrange("p (a f) -> p a f", f=128), reads=[ts_.name])
                    P.barrier()
                    P.flush()
                with contextlib.ExitStack() as s2:
                    sb2 = lambda name, shape, dt=F32, u=uid(): s2.enter_context(nc.sbuf_tensor(name + u, shape, dt))
                    ps2 = lambda name, shape, dt=F32, u=uid(): s2.enter_context(nc.psum_tensor(name + u, shape, dt))
                    dtb = sb2("d_dtb", [128, 6], F32)
                    nA = sb2("d_nA", [128, 6], F32)
                    nwb = sb2("d_nwb", [128, 64], F32)
                    P.dma(dtb[:], gdn_dtb[l].partition_broadcast(128), writes=['d_dtb'])
                    P.dma(nA[:], gdn_alog[l].partition_broadcast(128), writes=['d_nA'])
                    P.dma(nwb[:], gdn_normrow[l].partition_broadcast(128), writes=['d_nwb'])
                    P.op('act', lambda e: e.activation(out=nA[:], in_=nA[:], func=AF.Exp), reads=['d_nA'], writes=['d_nA'])
                    P.op('dve', lambda e: e.tensor_scalar(out=nA[:], in0=nA[:], scalar1=-1.0, scalar2=None, op0=ALU.mult),
                         reads=['d_nA'], writes=['d_nA'])
                    X = [sb2("d_X%d" % i, [128, NG], F32) for i in range(2)]
                    S = [sb2("d_S%d" % h, [64, 64], F32) for h in range(6)]
                    for h in range(6):
                        P.op('pool', lambda e, h=h: e.memset(S[h][:], 0.0), writes=[S[h].name])
                    gt = sb2("d_gt", [128, 48], F32)
                    gsb = sb2("d_gsb", [128, 16], F32)
                    sqq = sb2("d_sqq", [128, 384], F32)
                    rn = sb2("d_rn", [128, 12], F32)
                    H = lambda name, shape, dt=F32: [sb2("%s%d" % (name, h), shape, dt) for h in range(6)]
                    qn = H("d_qn", [128, 64]); kn = H("d_kn", [128, 64]); qg = H("d_qg", [128, 64])
                    kd = H("d_kd", [128, 64])
                    qnT = H("d_qnT", [64, 128]); knT = H("d_knT", [64, 128]); qgT = H("d_qgT", [64, 128])
                    yf = H("d_yf", [128, 256])
                    MT = H("d_MT", [128, 128]); tA = H("d_tA", [128, 128]); tB = H("d_tB", [128, 128])
                    dgn = H("d_dgn", [128, 128])
                    dec = H("d_dec", [128, 128])
                    tmpk = H("d_tmpk", [128, 128])
                    Rm = [H("d_R%d_" % i, [128, 128]) for i in range(2)]
                    Qm = [H("d_Q%d_" % i, [128, 128]) for i in range(2)]
                    attn = H("d_attn", [128, 128])
                    attnT = H("d_attnT", [128, 128])
                    wT = H("d_wT", [64, 128])
                    vnew = H("d_vnew", [128, 64])
                    o_sb = sb2("d_o", [128, 384], F32)
                    zs = sb2("d_zs", [128, 384], F32)
                    res = sb2("d_res", [128, 384], F32)
                    ot = [sb2("d_ot%d" % i, [128, 3, 128], BF16) for i in range(2)]
                    pA = [ps2("d_pA%d" % i, [128, 128]) for i in range(3)]
                    psW = ps2("d_psW", [128, 512])
                    pB = [ps2("d_pB%d" % i, [128, 256]) for i in range(4)]
                    ia = [0]; ib = [0]

                    def nextA():
                        p_ = pA[ia[0] % 3]; ia[0] += 1
                        return p_

                    def nextB():
                        p_ = pB[ib[0] % 4]; ib[0] += 1
                        return p_

                    def evac(dst, dkey, src, skey):
                        cast(dst, src, [skey], [dkey], psum=True)

                    for n in range(NKB):
                        Xn = X[n % 2]
                        warm(psW)
                        P.dma(Xn[:], gtok[n * 128:(n + 1) * 128, :], writes=[Xn.name])
                        xk = Xn.name
                        A0 = 1536
                        P.op('act', lambda e, Xn=Xn: e.activation(out=gt[:, 0:6], in_=Xn[:, A0 + 6:A0 + 12], func=AF.Sigmoid),
                             reads=[xk], writes=['d_gt'])
                        P.op('dve', lambda e, Xn=Xn: e.tensor_tensor(out=gt[:, 6:12], in0=Xn[:, A0:A0 + 6], in1=dtb[:], op=ALU.add),
                             reads=[xk, 'd_dtb'], writes=['d_gt'])
                        P.op('act', lambda e: e.activation(out=gt[:, 6:12], in_=gt[:, 6:12], func=AF.Exp), reads=['d_gt'], writes=['d_gt'])
                        P.op('act', lambda e: e.activation(out=gt[:, 6:12], in_=gt[:, 6:12], func=AF.Ln, bias=one_t[:]),
                             reads=['d_gt', 'one_t'], writes=['d_gt'])
                        P.op('dve', lambda e: e.tensor_tensor(out=gt[:, 6:12], in0=gt[:, 6:12], in1=nA[:], op=ALU.mult),
                             reads=['d_gt', 'd_nA'], writes=['d_gt'])
                        pg = nextA()
                        P.op('pe', lambda e, pg=pg: e.matmul(pg[:, 0:6], lhsT=Uincl[:], rhs=gt[:, 6:12], start=True, stop=True),
                             reads=['d_gt', 'Uincl'], writes=[pg.name], inc=False)
                        P.op('pe', lambda e, pg=pg: e.matmul(pg[:, 6:12], lhsT=ones_f[:], rhs=gt[:, 6:12], start=True, stop=True),
                             reads=['d_gt', 'ones_f'], writes=[pg.name])
                        P.op('dve', lambda e, pg=pg: e.tensor_copy(out=gsb[:, 0:12], in_=pg[:, 0:12]), reads=[pg.name], writes=['d_gsb'])
                        P.op('act', lambda e: e.activation(out=gt[:, 12:18], in_=gsb[:, 0:6], func=AF.Exp), reads=['d_gsb'], writes=['d_gt'])
                        P.op('dve', lambda e: e.tensor_tensor(out=gt[:, 18:24], in0=gsb[:, 6:12], in1=gsb[:, 0:6], op=ALU.subtract),
                             reads=['d_gsb'], writes=['d_gt'])
                        P.op('act', lambda e: e.activation(out=gt[:, 18:24], in_=gt[:, 18:24], func=AF.Exp), reads=['d_gt'], writes=['d_gt'])
                        P.op('act', lambda e: e.activation(out=gt[:, 24:30], in_=gsb[:, 6:12], func=AF.Exp), reads=['d_gsb'], writes=['d_gt'])
                        P.op('dve', lambda e: e.tensor_tensor(out=gt[:, 30:36], in0=gt[:, 0:6], in1=gt[:, 12:18], op=ALU.mult),
                             reads=['d_gt'], writes=['d_gt'])
                        P.op('dve', lambda e: e.tensor_scalar(out=gt[:, 36:42], in0=gt[:, 0:6], scalar1=-1.0, scalar2=None, op0=ALU.mult),
                             reads=['d_gt'], writes=['d_gt'])
                        for qi, c0 in enumerate((0, 384)):
                            P.op('dve', lambda e, Xn=Xn, c0=c0: e.tensor_tensor(out=sqq[:], in0=Xn[:, c0:c0 + 384], in1=Xn[:, c0:c0 + 384], op=ALU.mult),
                                 reads=[xk], writes=['d_sqq'])
                            P.op('dve', lambda e, qi=qi: e.reduce_sum(out=rn[:, qi * 6:(qi + 1) * 6],
                                                                     in_=sqq[:].rearrange("p (h d) -> p h d", d=64), axis=AX.X),
                                 reads=['d_sqq'], writes=['d_rn'])
                        P.op('act', lambda e: e.activation(out=rn[:], in_=rn[:], func=AF.Ln, bias=eps_t[:]), reads=['d_rn', 'eps_t'], writes=['d_rn'])
                        P.op('act', lambda e: e.activation(out=rn[:], in_=rn[:], func=AF.Exp, scale=-0.5), reads=['d_rn'], writes=['d_rn'])
                        for h in range(6):
                            hs = slice(h * 64, (h + 1) * 64)
                            P.op('dve', lambda e, h=h, hs=hs, Xn=Xn: e.tensor_scalar(
                                out=qn[h][:], in0=Xn[:, hs], scalar1=rn[:, h:h + 1], scalar2=0.125, op0=ALU.mult, op1=ALU.mult),
                                reads=[xk, 'd_rn'], writes=[qn[h].name])
                            P.op('dve', lambda e, h=h, Xn=Xn: e.tensor_scalar(
                                out=kn[h][:], in0=Xn[:, 384 + h * 64:384 + (h + 1) * 64], scalar1=rn[:, 6 + h:7 + h], scalar2=None, op0=ALU.mult),
                                reads=[xk, 'd_rn'], writes=[kn[h].name])
                            P.op('pool', lambda e, h=h: e.tensor_scalar(
                                out=qg[h][:], in0=qn[h][:], scalar1=gt[:, 12 + h:13 + h], scalar2=None, op0=ALU.mult),
                                reads=[qn[h].name, 'd_gt'], writes=[qg[h].name])
                            P.op('pool', lambda e, h=h: e.tensor_scalar(
                                out=kd[h][:], in0=kn[h][:], scalar1=gt[:, 18 + h:19 + h], scalar2=None, op0=ALU.mult),
                                reads=[kn[h].name, 'd_gt'], writes=[kd[h].name])
                            P.op('dve', lambda e, h=h, Xn=Xn: e.tensor_scalar(
                                out=yf[h][:, 0:64], in0=Xn[:, 768 + h * 64:768 + (h + 1) * 64], scalar1=gt[:, h:h + 1], scalar2=None, op0=ALU.mult),
                                reads=[xk, 'd_gt'], writes=[yf[h].name])
                            P.op('dve', lambda e, h=h: e.tensor_scalar(
                                out=yf[h][:, 64:128], in0=kn[h][:], scalar1=gt[:, 30 + h:31 + h], scalar2=None, op0=ALU.mult),
                                reads=[kn[h].name, 'd_gt'], writes=[yf[h].name])
                            P.op('pool', lambda e, h=h: e.tensor_scalar(
                                out=dgn[h][:], in0=ident_f[:], scalar1=gsb[:, h:h + 1], scalar2=-1.0, op0=ALU.mult, op1=ALU.mult),
                                reads=['ident_f', 'd_gsb'], writes=[dgn[h].name])
                        for h in range(6):
                            for (srcT, dstT) in ((qn, qnT), (kn, knT), (qg, qgT)):
                                p_ = nextA()
                                P.op('pe', lambda e, p_=p_, srcT=srcT, h=h: e.matmul(p_[0:64, :], lhsT=srcT[h][:], rhs=ident_f[:], start=True, stop=True),
                                     reads=[srcT[h].name, 'ident_f'], writes=[p_.name])
                                evac(dstT[h][:], dstT[h].name, p_[0:64, :], p_.name)
                        for h in range(6):
                            p_ = nextB()
                            P.op('pe', lambda e, p_=p_, h=h: e.matmul(p_[:, 0:128], lhsT=ones_f[:], rhs=dgn[h][:], start=True, stop=True),
                                 reads=[dgn[h].name, 'ones_f'], writes=[p_.name])
                            P.op('dve', lambda e, p_=p_, h=h: e.tensor_scalar(
                                out=dec[h][:], in0=p_[:, 0:128], scalar1=gsb[:, h:h + 1], scalar2=0.0, op0=ALU.add, op1=ALU.min),
                                reads=[p_.name, 'd_gsb'], writes=[dec[h].name])
                            P.op('act', lambda e, h=h: e.activation(out=dec[h][:], in_=dec[h][:], func=AF.Exp),
                                 reads=[dec[h].name], writes=[dec[h].name])
                            p_ = nextB()
                            P.op('pe', lambda e, p_=p_, h=h: e.matmul(p_[:, 0:128], lhsT=knT[h][:], rhs=knT[h][:], start=True, stop=True),
                                 reads=[knT[h].name], writes=[p_.name])
                            P.op('dve', lambda e, p_=p_, h=h: e.tensor_tensor(out=tmpk[h][:], in0=p_[:, 0:128], in1=dec[h][:], op=ALU.mult),
                                 reads=[p_.name, dec[h].name], writes=[tmpk[h].name])
                            P.op('dve', lambda e, h=h: e.scalar_tensor_tensor(
                                out=Rm[0][h][:], in0=tmpk[h][:], scalar=gt[:, 36 + h:37 + h], in1=LBD[:], op0=ALU.mult, op1=ALU.mult),
                                reads=[tmpk[h].name, 'd_gt', 'LBD'], writes=[Rm[0][h].name])
                            P.op('dve', lambda e, h=h: e.scalar_tensor_tensor(
                                out=yf[h][:, 128:256], in0=tmpk[h][:], scalar=gt[:, 36 + h:37 + h], in1=LOD[:], op0=ALU.mult, op1=ALU.mult),
                                reads=[tmpk[h].name, 'd_gt', 'LOD'], writes=[yf[h].name])
                            p_ = nextB()
                            P.op('pe', lambda e, p_=p_, h=h: e.matmul(p_[:, 0:128], lhsT=qnT[h][:], rhs=knT[h][:], start=True, stop=True),
                                 reads=[qnT[h].name, knT[h].name], writes=[p_.name])
                            P.op('dve', lambda e, p_=p_, h=h: e.tensor_tensor(out=tmpk[h][:], in0=p_[:, 0:128], in1=dec[h][:], op=ALU.mult),
                                 reads=[p_.name, dec[h].name], writes=[tmpk[h].name])
                            P.op('pool', lambda e, h=h: e.tensor_tensor(out=attn[h][:], in0=tmpk[h][:], in1=Lincl[:], op=ALU.mult),
                                 reads=[tmpk[h].name, 'Lincl'], writes=[attn[h].name])
                            for (srcM, dstM) in ((Rm[0], Qm[0]), (attn, attnT)):
                                p_ = nextA()
                                P.op('pe', lambda e, p_=p_, srcM=srcM, h=h: e.matmul(p_[:, 0:128], lhsT=srcM[h][:], rhs=ident_f[:], start=True, stop=True),
                                     reads=[srcM[h].name, 'ident_f'], writes=[p_.name])
                                evac(dstM[h][:], dstM[h].name, p_[:, 0:128], p_.name)
                        for j in range(5):
                            cur, nxt = j % 2, (j + 1) % 2
                            for h in range(6):
                                p_ = nextB()
                                P.op('pe', lambda e, p_=p_, h=h, cur=cur: e.matmul(p_[:], lhsT=Qm[cur][h][:], rhs=yf[h][:], start=True, stop=True),
                                     reads=[Qm[cur][h].name, yf[h].name], writes=[p_.name])
                                P.op('dve', lambda e, p_=p_, h=h: e.tensor_tensor(out=yf[h][:], in0=yf[h][:], in1=p_[:], op=ALU.add),
                                     reads=[p_.name, yf[h].name], writes=[yf[h].name])
                                if j < 4:
                                    p_ = nextA()
                                    P.op('pe', lambda e, p_=p_, h=h, cur=cur: e.matmul(p_[:], lhsT=Qm[cur][h][:], rhs=Rm[cur][h][:], start=True, stop=True),
                                         reads=[Qm[cur][h].name, Rm[cur][h].name], writes=[p_.name])
                                    evac(Rm[nxt][h][:], Rm[nxt][h].name, p_[:], p_.name)
                                    p_ = nextA()
                                    P.op('pe', lambda e, p_=p_, h=h, cur=cur: e.matmul(p_[:], lhsT=Rm[cur][h][:], rhs=Qm[cur][h][:], start=True, stop=True),
                                         reads=[Qm[cur][h].name, Rm[cur][h].name], writes=[p_.name])
                                    evac(Qm[nxt][h][:], Qm[nxt][h].name, p_[:], p_.name)
                        for h in range(6):
                            p_ = nextA()
                            P.op('pe', lambda e, p_=p_, h=h: e.matmul(p_[:], lhsT=yf[h][:, 128:256], rhs=ident_f[:], start=True, stop=True),
                                 reads=[yf[h].name, 'ident_f'], writes=[p_.name])
                            evac(MT[h][:], MT[h].name, p_[:], p_.name)
                        for it3, (src3, dst3) in enumerate(((None, tA), (tA, tB), (tB, tA))):
                            for h in range(6):
                                p_ = nextA()
                                rhs_ap = yf[h][:, 0:128] if src3 is None else src3[h][:]
                                rkey = yf[h].name if src3 is None else src3[h].name
                                P.op('pe', lambda e, p_=p_, h=h, rhs_ap=rhs_ap: e.matmul(p_[:], lhsT=MT[h][:], rhs=rhs_ap, start=True, stop=True),
                                     reads=[MT[h].name, rkey], writes=[p_.name])
                                P.op('dve', lambda e, p_=p_, h=h, dst3=dst3: e.tensor_tensor(out=dst3[h][:], in0=yf[h][:, 0:128], in1=p_[:], op=ALU.add),
                                     reads=[p_.name, yf[h].name], writes=[dst3[h].name])
                        for h in range(6):
                            p_ = nextA()
                            P.op('pe', lambda e, p_=p_, h=h: e.matmul(p_[0:64, :], lhsT=tA[h][:, 64:128], rhs=ident_f[:], start=True, stop=True),
                                 reads=[tA[h].name, 'ident_f'], writes=[p_.name])
                            evac(wT[h][:], wT[h].name, p_[0:64, :], p_.name)
                        for h in range(6):
                            p1 = nextB()
                            P.op('pe', lambda e, p1=p1, h=h: e.matmul(p1[:, 0:64], lhsT=wT[h][:], rhs=S[h][:], start=True, stop=True),
                                 reads=[wT[h].name, S[h].name], writes=[p1.name])
                            P.op('dve', lambda e, p1=p1, h=h: e.tensor_tensor(out=vnew[h][:], in0=tA[h][:, 0:64], in1=p1[:, 0:64], op=ALU.subtract),
                                 reads=[p1.name, tA[h].name], writes=[vnew[h].name])
                            p2 = nextB()
                            P.op('pe', lambda e, p2=p2, h=h: e.matmul(p2[:, 0:64], lhsT=qgT[h][:], rhs=S[h][:], start=True, stop=False),
                                 reads=[qgT[h].name, S[h].name], writes=[p2.name], inc=False)
                            P.op('pe', lambda e, p2=p2, h=h: e.matmul(p2[:, 0:64], lhsT=attnT[h][:], rhs=vnew[h][:], start=False, stop=True),
                                 reads=[attnT[h].name, vnew[h].name], writes=[p2.name])
                            P.op('act', lambda e, p2=p2, h=h: e.copy(out=o_sb[:, h * 64:(h + 1) * 64], in_=p2[:, 0:64]),
                                 reads=[p2.name], writes=[('d_o', h)])
                            p3 = nextA()
                            P.op('pe', lambda e, p3=p3, h=h: e.matmul(p3[0:64, 0:64], lhsT=kd[h][:], rhs=vnew[h][:], start=True, stop=True),
                                 reads=[kd[h].name, vnew[h].name], writes=[p3.name])
                            P.op('dve', lambda e, p3=p3, h=h: e.scalar_tensor_tensor(
                                out=S[h][:], in0=S[h][:], scalar=gt[0:64, 24 + h:25 + h], in1=p3[0:64, 0:64], op0=ALU.mult, op1=ALU.add),
                                reads=[p3.name, S[h].name, 'd_gt'], writes=[S[h].name])
                        okeys = [('d_o', h) for h in range(6)]
                        P.op('dve', lambda e: e.tensor_tensor(out=sqq[:], in0=o_sb[:], in1=o_sb[:], op=ALU.mult), reads=okeys, writes=['d_sqq'])
                        P.op('dve', lambda e: e.reduce_sum(out=rn[:, 0:6], in_=sqq[:].rearrange("p (h d) -> p h d", d=64), axis=AX.X),
                             reads=['d_sqq'], writes=['d_rn'])
                        P.op('act', lambda e: e.activation(out=rn[:, 0:6], in_=rn[:, 0:6], func=AF.Ln, bias=eps_t[:], scale=1.0 / 64),
                             reads=['d_rn', 'eps_t'], writes=['d_rn'])
                        P.op('act', lambda e: e.activation(out=rn[:, 0:6], in_=rn[:, 0:6], func=AF.Exp, scale=-0.5), reads=['d_rn'], writes=['d_rn'])
                        P.op('act', lambda e, Xn=Xn: e.activation(out=zs[:], in_=Xn[:, 1152:1536], func=AF.Silu), reads=[xk], writes=['d_zs'])
                        for h in range(6):
                            hs = slice(h * 64, (h + 1) * 64)
                            P.op('dve', lambda e, h=h, hs=hs: e.scalar_tensor_tensor(
                                out=res[:, hs], in0=o_sb[:, hs], scalar=rn[:, h:h + 1], in1=nwb[:], op0=ALU.mult, op1=ALU.mult),
                                reads=okeys + ['d_rn', 'd_nwb'], writes=[('d_res', h)])
                            P.op('pool', lambda e, hs=hs: e.tensor_tensor(out=res[:, hs], in0=res[:, hs], in1=zs[:, hs], op=ALU.mult),
                                 reads=[('d_res', h), 'd_zs'], writes=[('d_res', h)])
                        rkeys = [('d_res', h) for h in range(6)]
                        oo = ot[n % 2]
                        for c3 in range(3):
                            p_ = nextA()
                            P.op('pe', lambda e, p_=p_, c3=c3: e.matmul(p_[:], lhsT=res[:, c3 * 128:(c3 + 1) * 128], rhs=ident_f[:], start=True, stop=True),
                                 reads=rkeys + ['ident_f'], writes=[p_.name])
                            evac(oo[:, c3, :], oo.name, p_[:], p_.name)
                        P.dma(mixT[0:384, n * 128:(n + 1) * 128].rearrange("(c p) t -> p c t", p=128), oo[:], reads=[oo.name])
                    P.barrier()
                    P.flush()


            if 'E' in stages:
              with contextlib.ExitStack() as st:
                sbt = lambda name, shape, dt=F32, u=uid(): st.enter_context(nc.sbuf_tensor(name + u, shape, dt))
                pst = lambda name, shape, dt=F32, u=uid(): st.enter_context(nc.psum_tensor(name + u, shape, dt))
                wob = sbt("e_w", [128, 8, D_MODEL], BF16)
                wst = [sbt("e_wst%d" % i, [128, D_MODEL], F32) for i in range(2)]
                for c in range(8):
                    s = wst[c % 2]
                    P.dma(s[:], w_out[l, c * 128:(c + 1) * 128, :], writes=[s.name])
                    cast(wob[:, c, :], s[:], [s.name], ['e_w'])
                h_sb = [sbt("e_h%d" % i, [128, 8, 512], F32) for i in range(2)]
                mx_sb = [sbt("e_mx%d" % i, [128, 8, 512], BF16) for i in range(2)]
                ps_o = [pst("e_ps%d" % i, [128, 512]) for i in range(4)]
                for tt in range(NT):
                    tsl = slice(tt * 512, (tt + 1) * 512)
                    h = h_sb[tt % 2]
                    mx = mx_sb[tt % 2]
                    P.dma(h[:], pc(hsrc)[:, :, tsl], writes=[h.name])
                    P.dma(mx[:], pc(mixT)[:, :, tsl], writes=[mx.name])
                    for m in range(8):
                        ps = ps_o[m % 4]
                        for c in range(8):
                            P.op('pe', lambda e, c=c, m=m, ps=ps, mx=mx: e.matmul(
                                ps[:], lhsT=wob[:, c, m * 128:(m + 1) * 128], rhs=mx[:, c, :],
                                start=(c == 0), stop=(c == 7)),
                                reads=[mx.name, 'e_w'], writes=[ps.name], inc=(c == 7))
                        P.op('dve', lambda e, m=m, ps=ps, h=h: e.tensor_tensor(
                            out=h[:, m, :], in0=h[:, m, :], in1=ps[:], op=ALU.add),
                            reads=[ps.name, h.name], writes=[h.name])
                    P.dma(pc(hT)[:, :, tsl], h[:], reads=[h.name])
                P.barrier()
                P.flush()

            if 'F' in stages:
              for half in range(2):
                with contextlib.ExitStack() as st:
                    sbt = lambda name, shape, dt=F32, u=uid(): st.enter_context(nc.sbuf_tensor(name + u, shape, dt))
                    pst = lambda name, shape, dt=F32, u=uid(): st.enter_context(nc.psum_tensor(name + u, shape, dt))
                    HF = D_FF // 2
                    wup = sbt("f_wup", [128, 8, 2 * HF], BF16)
                    wdn = sbt("f_wdn", [128, 11, D_MODEL], BF16)
                    wst = [sbt("f_wst%d" % i, [128, HF], F32) for i in range(2)]
                    gain = sbt("f_gain", [128, 8], F32)
                    cw = sbt("f_cw", [128, 44, 3], F32)
                    halo = sbt("f_halo", [128, 22, 2], F32)
                    P.dma(gain[:], ln_ffn[l], writes=[gain.name])
                    P.dma(cw[:], ffn_conv[l], writes=['f_cw'])
                    P.op('pool', lambda e: e.memset(halo[:], 0.0), writes=['f_halo'])
                    k = 0
                    for c in range(8):
                        for which in range(2):
                            s = wst[k % 2]
                            k += 1
                            c0 = which * D_FF + half * HF
                            P.dma(s[:], w_up[l, c * 128:(c + 1) * 128, c0:c0 + HF], writes=[s.name])
                            cast(wup[:, c, which * HF:(which + 1) * HF], s[:], [s.name], ['f_wup'])
                    for j in range(11):
                        s = wst[k % 2]
                        k += 1
                        r0 = (half * 11 + j) * 128
                        P.dma(s[:, 0:D_MODEL], w_down[l, r0:r0 + 128, :], writes=[s.name])
                        cast(wdn[:, j, :], s[:, 0:D_MODEL], [s.name], ['f_wdn'])
                    h_sb = sbt("f_h", [128, 8, 512], F32)
                    sq = sbt("f_sq", [128, 8, 512], BF16)
                    xn = sbt("f_xn", [128, 8, 512], BF16)
                    rstd = sbt("f_rstd", [128, 512], F32)
                    actv = sbt("f_act", [128, 11, 512], BF16)
                    u_sb = [sbt("f_u%d" % i, [128, 514], F32) for i in range(4)]
                    cv_sb = [sbt("f_cv%d" % i, [128, 512], F32) for i in range(4)]
                    ps_n = pst("f_psn", [128, 512])
                    ps_o = [pst("f_ps%d" % i, [128, 512]) for i in range(4)]
                    ucnt = 0
                    for tt in range(NT):
                        tsl = slice(tt * 512, (tt + 1) * 512)
                        h = h_sb
                        P.dma(h[:], pc(hT)[:, :, tsl], writes=[h.name])
                        emit_norm(h, gain, sq, ps_n, rstd, xn)
                        hacc = h
                        if half == 1:
                            P.dma(h[:], pc(hT2)[:, :, tsl], writes=[h.name])
                        for j in range(11):
                            cvs = []
                            for which in range(2):
                                ps = ps_o[ucnt % 4]
                                u = u_sb[ucnt % 4]
                                cv = cv_sb[ucnt % 4]
                                ucnt += 1
                                hidx = which * 11 + j
                                fidx = which * 22 + half * 11 + j
                                for c in range(8):
                                    P.op('pe', lambda e, c=c, ps=ps, which=which, j=j: e.matmul(
                                        ps[:], lhsT=wup[:, c, which * HF + j * 128: which * HF + (j + 1) * 128],
                                        rhs=xn[:, c, :], start=(c == 0), stop=(c == 7)),
                                        reads=[xn.name, 'f_wup'], writes=[ps.name], inc=(c == 7))
                                P.op('act', lambda e, ps=ps, u=u: e.copy(out=u[:, 2:514], in_=ps[:]),
                                     reads=[ps.name], writes=[u.name])
                                P.op('pool', lambda e, u=u, hidx=hidx: e.tensor_copy(out=u[:, 0:2], in_=halo[:, hidx, :]),
                                     reads=[('f_halo', hidx)], writes=[u.name])
                                P.op('dve', lambda e, u=u, cv=cv, fidx=fidx: e.tensor_scalar(
                                    out=cv[:], in0=u[:, 0:512], scalar1=cw[:, fidx, 0:1], scalar2=None, op0=ALU.mult),
                                    reads=[u.name, 'f_cw'], writes=[cv.name])
                                for tap in (1, 2):
                                    P.op('dve', lambda e, u=u, cv=cv, fidx=fidx, tap=tap: e.scalar_tensor_tensor(
                                        out=cv[:], in0=u[:, tap:tap + 512], scalar=cw[:, fidx, tap:tap + 1], in1=cv[:],
                                        op0=ALU.mult, op1=ALU.add),
                                        reads=[u.name, 'f_cw', cv.name], writes=[cv.name])
                                P.op('pool', lambda e, u=u, hidx=hidx: e.tensor_copy(out=halo[:, hidx, :], in_=u[:, 512:514]),
                                     reads=[u.name], writes=[('f_halo', hidx)])
                                cvs.append(cv)
                            cg, cu = cvs
                            P.op('act', lambda e, cg=cg: e.activation(out=cg[:], in_=cg[:], func=AF.Silu),
                                 reads=[cg.name], writes=[cg.name])
                            P.op('dve', lambda e, cg=cg, cu=cu, j=j: e.tensor_tensor(
                                out=actv[:, j, :], in0=cg[:], in1=cu[:], op=ALU.mult),
                                reads=[cg.name, cu.name], writes=[('f_act', j)])
                        akeys = [('f_act', j) for j in range(11)]
                        for m in range(8):
                            ps = ps_o[m % 4]
                            for j in range(11):
                                P.op('pe', lambda e, j=j, m=m, ps=ps: e.matmul(
                                    ps[:], lhsT=wdn[:, j, m * 128:(m + 1) * 128], rhs=actv[:, j, :],
                                    start=(j == 0), stop=(j == 10)),
                                    reads=akeys + ['f_wdn'], writes=[ps.name], inc=(j == 10))
                            P.op('dve', lambda e, m=m, ps=ps, hacc=hacc: e.tensor_tensor(
                                out=hacc[:, m, :], in0=hacc[:, m, :], in1=ps[:], op=ALU.add),
                                reads=[ps.name, hacc.name], writes=[hacc.name])
                        dst = hT2 if half == 0 else hT
                        P.dma(pc(dst)[:, :, tsl], hacc[:], reads=[hacc.name])
                    P.barrier()
                    P.flush()

            if 'G' in stages:
              with contextlib.ExitStack() as st:
                sbt = lambda name, shape, dt=F32, u=uid(): st.enter_context(nc.sbuf_tensor(name + u, shape, dt))
                pst = lambda name, shape, dt=F32, u=uid(): st.enter_context(nc.psum_tensor(name + u, shape, dt))
                last = (l == depth - 1)
                wg = sbt("g_wg", [128, 8, D_MODEL], BF16)
                wp = sbt("g_wp", [128, 2, D_MODEL], BF16)
                wst = [sbt("g_wst%d" % i, [128, D_MODEL], F32) for i in range(2)]
                gain = sbt("g_gain", [128, 8], F32)
                gpn = sbt("g_gpn", [128, 8], F32)
                gfin = sbt("g_gfin", [128, 8], F32)
                P.dma(gain[:], ln_ple[l], writes=[gain.name])
                P.dma(gpn[:], ple_norm[l], writes=[gpn.name])
                P.dma(gfin[:], ln_final, writes=[gfin.name])
                for c in range(8):
                    s = wst[c % 2]
                    P.dma(s[:], w_gate[l, c * 128:(c + 1) * 128, :], writes=[s.name])
                    cast(wg[:, c, :], s[:], [s.name], ['g_wg'])
                for c in range(2):
                    s = wst[c % 2]
                    P.dma(s[:], w_ple[l, c * 128:(c + 1) * 128, :], writes=[s.name])
                    cast(wp[:, c, :], s[:], [s.name], ['g_wp'])
                h_r = [sbt("g_h%d" % i, [128, 8, 512], F32) for i in range(2)]
                sq = sbt("g_sq", [128, 8, 512], BF16)
                xn_r = [sbt("g_xn%d" % i, [128, 8, 512], BF16) for i in range(2)]
                rstd = sbt("g_rstd", [128, 512], F32)
                p32_r = [sbt("g_p32", [128, 2, 512], F32)] * 2
                pbf_r = [sbt("g_pbf", [128, 2, 512], BF16)] * 2
                y_r = [sbt("g_y", [128, 8, 512], F32)] * 2
                gt_r = [sbt("g_gt%d" % i, [128, 8, 512], F32) for i in range(2)]
                ps_n = pst("g_psn", [128, 512])
                ps_o = [pst("g_ps%d" % i, [128, 512]) for i in range(4)]
                for tt in range(NT):
                    tsl = slice(tt * 512, (tt + 1) * 512)
                    h, xn, p32, pbf, y, gt = h_r[tt % 2], xn_r[tt % 2], p32_r[tt % 2], pbf_r[tt % 2], y_r[tt % 2], gt_r[tt % 2]
                    P.dma(h[:], pc(hT)[:, :, tsl], writes=[h.name])
                    P.dma(p32[:], pc(pT[l])[:, :, tsl], writes=[p32.name])
                    cast(pbf[:], p32[:], [p32.name], [pbf.name])
                    emit_norm(h, gain, sq, ps_n, rstd, xn)
                    for m in range(8):
                        ps = ps_o[m % 4]
                        for c in range(8):
                            P.op('pe', lambda e, c=c, m=m, ps=ps, h=h, xn=xn, y=y, gt=gt, pbf=pbf, p32=p32: e.matmul(
                                ps[:], lhsT=wg[:, c, m * 128:(m + 1) * 128], rhs=xn[:, c, :],
                                start=(c == 0), stop=(c == 7)),
                                reads=[xn.name, 'g_wg'], writes=[ps.name], inc=(c == 7))
                        P.op('act', lambda e, m=m, ps=ps, h=h, xn=xn, y=y, gt=gt, pbf=pbf, p32=p32: e.activation(out=gt[:, m, :], in_=ps[:], func=AF.Sigmoid),
                             reads=[ps.name], writes=[gt.name])
                    for m in range(8):
                        ps = ps_o[m % 4]
                        for c in range(2):
                            P.op('pe', lambda e, c=c, m=m, ps=ps, h=h, xn=xn, y=y, gt=gt, pbf=pbf, p32=p32: e.matmul(
                                ps[:], lhsT=wp[:, c, m * 128:(m + 1) * 128], rhs=pbf[:, c, :],
                                start=(c == 0), stop=(c == 1)),
                                reads=[pbf.name, 'g_wp'], writes=[ps.name], inc=(c == 1))
                        cast(y[:, m, :], ps[:], [ps.name], [y.name], psum=True)
                    emit_norm(y, gpn, sq, ps_n, rstd, y)
                    for m in range(8):
                        P.op('dve', lambda e, m=m, h=h, xn=xn, y=y, gt=gt, pbf=pbf, p32=p32: e.tensor_tensor(
                            out=gt[:, m, :], in0=gt[:, m, :], in1=y[:, m, :], op=ALU.mult),
                            reads=[gt.name, y.name], writes=[gt.name])
                        P.op('pool', lambda e, m=m, h=h, xn=xn, y=y, gt=gt, pbf=pbf, p32=p32: e.tensor_tensor(
                            out=h[:, m, :], in0=h[:, m, :], in1=gt[:, m, :], op=ALU.add),
                            reads=[gt.name, h.name], writes=[h.name])
                    if last:
                        emit_norm(h, gfin, sq, ps_n, rstd, y)
                        P.dma(pc(outT)[:, :, tsl], y[:], reads=[y.name])
                    else:
                        P.dma(pc(hT)[:, :, tsl], h[:], reads=[h.name])
                P.barrier()
                P.flush()

        P.barrier()
        P.flush()
    return nc


def _col_perm():
    r = lambda a, b: list(range(a, b))
    nq = [c for h in (0, 3, 1, 4, 2, 5) for c in r(1548 + h * 64, 1548 + (h + 1) * 64)]
    fm = r(0, 1536) + nq + r(1932, 2060) + r(2060, 2188) + r(2188, 2316) + r(2444, 2572) \
        + r(2718, 2974) + r(2974, 3230)
    small = r(1536, 1548) + r(2700, 2718)
    tm = r(2316, 2444) + r(2572, 2700) + r(3230, 3486)
    return fm, small, tm


def prep_w_in(w_in):
    fm, small, tm = _col_perm()
    d = w_in.shape[0]
    out = np.zeros((d, D_MODEL, N_IN_PAD), np.float32)
    out[:, :, 0:len(fm)] = w_in[:, :, fm]
    out[:, :, 2944:2944 + len(small)] = w_in[:, :, small]
    out[:, :, 3072:3584] = w_in[:, :, tm]
    return out


def vec_pc(v):
    sh = v.shape
    c = sh[-1] // 128
    return np.ascontiguousarray(np.swapaxes(v.reshape(sh[:-1] + (c, 128)), -1, -2))


def prep_conv(w):
    d, k, n = w.shape
    return np.ascontiguousarray(w.transpose(0, 2, 1).reshape(d, n // 128, 128, k).transpose(0, 2, 1, 3))


def dup128(v):
    return np.ascontiguousarray(np.concatenate([v, v], axis=-1)[..., None])


def prep_w1(w):
    d = w.shape[0]
    a = w.reshape(d, 32, 64, 128).transpose(0, 2, 1, 3)
    return np.ascontiguousarray(np.concatenate([a, a], axis=1))


def prep_pe(pe):
    a = pe.transpose(0, 2, 1)
    return np.ascontiguousarray(np.concatenate([a, a], axis=1))


STAGES = "ABCDEFG"


def kernel(**inputs):
    f32 = lambda a: np.ascontiguousarray(np.asarray(a, dtype=np.float32))
    x = f32(inputs['x'])
    B, T, _ = x.shape
    depth = int(np.asarray(inputs['w_in']).shape[0])
    p = f32(inputs['p'])
    shared = dict(
        ln_mix=vec_pc(f32(inputs['ln_mix'])), ln_ffn=vec_pc(f32(inputs['ln_ffn'])),
        ln_ple=vec_pc(f32(inputs['ln_ple'])), ple_norm=vec_pc(f32(inputs['ple_norm'])),
        ln_final=vec_pc(f32(inputs['ln_final'])),
        w_in=prep_w_in(f32(inputs['w_in'])), w_out=f32(inputs['w_out']), w_up=f32(inputs['w_up']),
        ffn_conv=prep_conv(f32(inputs['ffn_conv'])), w_down=f32(inputs['w_down']),
        w_gate=f32(inputs['w_ple_gate']), w_ple=f32(inputs['w_ple']),
        sb_norm=dup128(f32(inputs['sb_norm'])), nsa_norm=dup128(f32(inputs['nsa_norm'])),
        gdn_norm=dup128(f32(inputs['gdn_norm'])),
        gdn_conv=prep_conv(f32(inputs['gdn_conv'])), gdn_alog=f32(inputs['gdn_a_log'])[:, None, :],
        gdn_dtb=f32(inputs['gdn_dt_bias'])[:, None, :], gdn_normrow=f32(inputs['gdn_norm'])[:, None, :],
        cmp_k_w1=prep_w1(f32(inputs['nsa_cmp_k_w1'])), cmp_v_w1=prep_w1(f32(inputs['nsa_cmp_v_w1'])),
        cmp_k_w2=f32(inputs['nsa_cmp_k_w2']), cmp_v_w2=f32(inputs['nsa_cmp_v_w2']),
        pe_kT=prep_pe(f32(inputs['nsa_pe_k'])), pe_vT=prep_pe(f32(inputs['nsa_pe_v'])),
    )
    in_maps = []
    for core in range(8):
        b = core % B
        m = dict(shared)
        m['xT'] = np.ascontiguousarray(x[b].T)
        m['pT'] = np.ascontiguousarray(p[:, b].transpose(0, 2, 1))
        m['pos'] = np.ascontiguousarray(np.asarray(inputs['positions'])[b:b + 1].astype(np.int32))
        in_maps.append(m)
    nc = build(T, depth, stages=STAGES)
    res = run_bass_kernel_spmd(nc, in_maps, core_ids=list(range(8)))
    out = np.stack([np.ascontiguousarray(np.asarray(res.results[b]['outT']).T) for b in range(B)], axis=0)
    return out.astype(np.float32)
```

```python
import contextlib
import numpy as np
import concourse.bass as bass
import concourse.mybir as mybir
from concourse.bass_utils import run_bass_kernel_spmd

F32 = mybir.dt.float32
BF16 = mybir.dt.bfloat16
I32 = mybir.dt.int32
AF = mybir.ActivationFunctionType
ALU = mybir.AluOpType
AX = mybir.AxisListType

D_MODEL = 1024
HD = 64
N_IN_PAD = 3584
D_FF = 2816
EPS = 1e-6
NDS = 24
CPARTS = {'cmp', 'topk', 'sel', 'win'}


class Prog:
    def __init__(self, nc, stack):
        self.nc = nc
        self.stack = stack
        self.cengs = ['pe', 'act', 'dve', 'pool']
        self.engs = self.cengs + ['sp']
        self.sem = {e: stack.enter_context(nc.semaphore("s_" + e)) for e in self.cengs}
        self.dsem = [stack.enter_context(nc.semaphore("d%d" % i)) for i in range(NDS)]
        self.dcount = [0] * NDS
        self.dnext = 0
        self.nseq = {e: 0 for e in self.cengs}
        self.known = {e: {} for e in self.engs}
        self.lastw = {}
        self.readers = {}
        self.q = {e: [] for e in self.engs}
        self.pending_noinc = {e: False for e in self.cengs}

    def _semof(self, sk):
        if isinstance(sk, tuple):
            return self.dsem[sk[1]]
        return self.sem[sk]

    def _collect(self, eng, reads, writes):
        deps = {}
        def add(ev):
            if ev is None:
                return
            sk, val = ev
            if deps.get(sk, 0) < val:
                deps[sk] = val
        for k in reads:
            add(self.lastw.get(k))
        for k in writes:
            add(self.lastw.get(k))
            for ev in self.readers.get(k, ()):
                add(ev)
        waits = []
        for sk, val in deps.items():
            if eng == 'pe' and sk == 'pe':
                continue
            if self.known[eng].get(sk, 0) < val:
                self.known[eng][sk] = val
                waits.append((sk, val))
        return waits

    def _record(self, ev, reads, writes):
        for k in reads:
            self.readers.setdefault(k, []).append(ev)
        for k in writes:
            self.lastw[k] = ev
            self.readers[k] = []

    def op(self, eng, fn, reads=(), writes=(), inc=True):
        waits = self._collect(eng, reads, writes)
        if inc:
            self.nseq[eng] += 1
            ev = (eng, self.nseq[eng])
            self.pending_noinc[eng] = False
        else:
            ev = (eng, self.nseq[eng] + 1)
            self.pending_noinc[eng] = True
        self.q[eng].append((waits, fn, eng if inc else None))
        self._record(ev, reads, writes)

    def dma(self, out, in_, reads=(), writes=(), queue='sp', **kw):
        waits = self._collect(queue, reads, writes)
        j = self.dnext
        self.dnext = (j + 1) % NDS
        prev = self.dcount[j]
        sk = ('d', j)
        if prev > 0 and self.known[queue].get(sk, 0) < prev:
            self.known[queue][sk] = prev
            waits.append((sk, prev))
        self.dcount[j] += 16
        ev = (sk, self.dcount[j])
        self.q[queue].append((waits, lambda e: e.dma_start(out=out, in_=in_, **kw), sk))
        self._record(ev, reads, writes)

    def barrier(self):
        for e in self.engs:
            waits = []
            for f in self.cengs:
                if f == e:
                    continue
                v = self.nseq[f]
                if v > 0 and self.known[e].get(f, 0) < v:
                    self.known[e][f] = v
                    waits.append((f, v))
            for j in range(NDS):
                v = self.dcount[j]
                sk = ('d', j)
                if v > 0 and self.known[e].get(sk, 0) < v:
                    self.known[e][sk] = v
                    waits.append((sk, v))
            if waits:
                self.q[e].append((waits, None, None))
        self.lastw = {}
        self.readers = {}

    def flush(self):
        nc = self.nc
        for e in self.cengs:
            assert not self.pending_noinc[e], e
        q = self.q
        self.q = {e: [] for e in self.engs}

        def emit(name, e):
            for waits, fn, inc in q[name]:
                for sk, val in waits:
                    e.wait_ge(self._semof(sk), val)
                if fn is None:
                    continue
                ins = fn(e)
                if inc is not None:
                    if isinstance(inc, tuple):
                        ins.then_inc(self.dsem[inc[1]], 16)
                    else:
                        ins.then_inc(self.sem[inc], 1)

        with nc.Block() as block:
            @block.tensor
            def _(e):
                emit('pe', e)

            @block.scalar
            def _(e):
                emit('act', e)

            @block.vector
            def _(e):
                emit('dve', e)

            @block.gpsimd
            def _(e):
                emit('pool', e)

            @block.sync
            def _(e):
                emit('sp', e)


class Ring:
    def __init__(self, items):
        self.items = items
        self.i = 0

    def next(self):
        it = self.items[self.i % len(self.items)]
        self.i += 1
        return it


def build(T, depth, debug=(), stages="ABCDEFG", mix_input=False, ext=()):
    nc = bass.Bass("TRN2", target_bir_lowering=False)
    NT = T // 512
    din = lambda name, shape, dt=F32: nc.dram_tensor(name, shape, dt, kind="ExternalInput").ap()
    dscr = lambda name, shape, dt=F32: nc.dram_tensor(
        name, shape, dt, kind=("ExternalOutput" if name in debug else "ExternalInput" if name in ext else "Internal")).ap()

    xT = din("xT", [D_MODEL, T])
    pT = din("pT", [depth, 256, T])
    ln_mix = din("ln_mix", [depth, 128, 8])
    ln_ffn = din("ln_ffn", [depth, 128, 8])
    ln_ple = din("ln_ple", [depth, 128, 8])
    ple_norm = din("ple_norm", [depth, 128, 8])
    ln_final = din("ln_final", [128, 8])
    w_in = din("w_in", [depth, D_MODEL, N_IN_PAD])
    sb_norm = din("sb_norm", [depth, 128, 1])
    nsa_norm = din("nsa_norm", [depth, 128, 1])
    gdn_norm = din("gdn_norm", [depth, 128, 1])
    pos = din("pos", [1, T], I32)
    cmp_k_w1 = din("cmp_k_w1", [depth, 128, 32, 128])
    cmp_v_w1 = din("cmp_v_w1", [depth, 128, 32, 128])
    cmp_k_w2 = din("cmp_k_w2", [depth, 128, 64])
    cmp_v_w2 = din("cmp_v_w2", [depth, 128, 64])
    pe_kT = din("pe_kT", [depth, 128, 32])
    pe_vT = din("pe_vT", [depth, 128, 32])
    gdn_conv = din("gdn_conv", [depth, 128, 9, 4])
    gdn_alog = din("gdn_alog", [depth, 1, 6])
    gdn_dtb = din("gdn_dtb", [depth, 1, 6])
    gdn_normrow = din("gdn_normrow", [depth, 1, 64])
    gtok = dscr("gtok", [T, 13 * 128])
    w_out = din("w_out", [depth, D_MODEL, D_MODEL])
    w_up = din("w_up", [depth, D_MODEL, 2 * D_FF])
    ffn_conv = din("ffn_conv", [depth, 128, 44, 3])
    w_down = din("w_down", [depth, D_FF, D_MODEL])
    w_gate = din("w_gate", [depth, D_MODEL, D_MODEL])
    w_ple = din("w_ple", [depth, 256, D_MODEL])
    outT = nc.dram_tensor("outT", [D_MODEL, T], F32, kind="ExternalOutput").ap()
    projF = dscr("projF", [24 * 128, T])
    projT = dscr("projT", [T, 512])
    hT = dscr("hT", [D_MODEL, T])
    hT2 = dscr("hT2", [D_MODEL, T])
    if mix_input:
        mixT = din("mixT", [D_MODEL, T], BF16)
    else:
        mixT = dscr("mixT", [D_MODEL, T], BF16)
    pc = lambda ap: ap.rearrange("(c p) t -> p c t", p=128)

    with contextlib.ExitStack() as stack:
        P = Prog(nc, stack)
        sb = lambda name, shape, dt=F32: stack.enter_context(nc.sbuf_tensor(name, shape, dt))

        ones_bf = sb("ones_bf", [128, 128], BF16)
        P.op('pool', lambda e: e.memset(ones_bf[:], 1.0), writes=['ones_bf'])
        eps_t = sb("eps_t", [128, 1], F32)
        P.op('pool', lambda e: e.memset(eps_t[:], EPS), writes=['eps_t'])

        one_t = sb("one_t", [128, 1], F32)
        P.op('pool', lambda e: e.memset(one_t[:], 1.0), writes=['one_t'])
        ones512 = sb("ones512", [128, 512], BF16)
        P.op('pool', lambda e: e.memset(ones512[:], 1.0), writes=['ones512'])
        negOnes = sb("negOnes", [128, 128], BF16)
        P.op('pool', lambda e: e.memset(negOnes[:], -1.0), writes=['negOnes'])
        negU = sb("negU", [128, 128], BF16)
        P.op('pool', lambda e: e.affine_select(out=negU[:], in_=negOnes[:], pattern=[[-1, 128]],
                                               compare_op=ALU.is_ge, fill=0.0, base=0, channel_multiplier=1),
             reads=['negOnes'], writes=['negU'])
        dmask = sb("dmask", [128, 4, 512], BF16)
        for j in range(4):
            P.op('pool', lambda e, j=j: e.affine_select(out=dmask[:, j, :], in_=ones512[:], pattern=[[1, 512]],
                                                        compare_op=ALU.is_gt, fill=0.0, base=-128 * j,
                                                        channel_multiplier=-1),
                 reads=['ones512'], writes=['dmask'])
        cast_rr = [0]
        uidc = [0]

        def uid():
            uidc[0] += 1
            return '_%d' % uidc[0]

        def cast(dst, src, reads, writes, psum=False):
            eng = (['dve', 'act'][cast_rr[0] % 2]) if psum else (['pool', 'dve', 'act'][cast_rr[0] % 3])
            cast_rr[0] += 1
            if eng == 'act':
                P.op('act', lambda e: e.copy(out=dst, in_=src), reads=reads, writes=writes)
            else:
                P.op(eng, lambda e: e.tensor_copy(out=dst, in_=src), reads=reads, writes=writes)

        def warm(psW, n=10):
            for _ in range(n):
                P.op('pe', lambda e: e.matmul(psW[:], lhsT=ones_bf[:], rhs=ones512[:], start=True, stop=True),
                     reads=[], writes=[], inc=False)

        def emit_norm(h, gain, sq, ps_n, rstd, xn, nfeat=D_MODEL):
            nch = nfeat // 128
            P.op('act', lambda e: e.activation(out=sq[:], in_=h[:], func=AF.Square),
                 reads=[h.name], writes=[sq.name])
            for c in range(nch):
                P.op('pe', lambda e, c=c: e.matmul(ps_n[:], lhsT=ones_bf[:], rhs=sq[:, c, :],
                                                   start=(c == 0), stop=(c == nch - 1)),
                     reads=[sq.name, 'ones_bf'], writes=[ps_n.name], inc=(c == nch - 1))
            P.op('act', lambda e: e.activation(out=rstd[:], in_=ps_n[:], func=AF.Ln,
                                               bias=eps_t[:], scale=1.0 / nfeat),
                 reads=[ps_n.name, 'eps_t'], writes=[rstd.name])
            P.op('act', lambda e: e.activation(out=rstd[:], in_=rstd[:], func=AF.Exp, scale=-0.5),
                 reads=[rstd.name], writes=[rstd.name])
            for c in range(nch):
                P.op('dve', lambda e, c=c: e.scalar_tensor_tensor(
                    out=xn[:, c, :], in0=h[:, c, :], scalar=gain[:, c:c + 1], in1=rstd[:],
                    op0=ALU.mult, op1=ALU.mult),
                    reads=[h.name, gain.name, rstd.name], writes=[xn.name])

        NKB = T // 128
        NCMP = T // 16 - 1
        BIGM = 30000.0
        ident_bf = sb("ident_bf", [128, 128], BF16)
        P.op('pool', lambda e: e.affine_select(out=ident_bf[:], in_=ones_bf[:], pattern=[[-1, 128]],
                                               compare_op=ALU.is_equal, fill=0.0, base=0, channel_multiplier=1),
             reads=['ones_bf'], writes=['ident_bf'])
        ones_f = sb("ones_f", [128, 128], F32)
        P.op('pool', lambda e: e.memset(ones_f[:], 1.0), writes=['ones_f'])
        ident_f = sb("ident_f", [128, 128], F32)
        P.op('pool', lambda e: e.affine_select(out=ident_f[:], in_=ones_f[:], pattern=[[-1, 128]],
                                               compare_op=ALU.is_equal, fill=0.0, base=0, channel_multiplier=1),
             reads=['ones_f'], writes=['ident_f'])
        Uincl = sb("Uincl", [128, 128], F32)
        Lstrict = sb("Lstrict", [128, 128], F32)
        Lincl = sb("Lincl", [128, 128], F32)
        P.op('pool', lambda e: e.affine_select(out=Uincl[:], in_=ones_f[:], pattern=[[1, 128]], compare_op=ALU.is_ge,
                                               fill=0.0, base=0, channel_multiplier=-1), reads=['ones_f'], writes=['Uincl'])
        P.op('pool', lambda e: e.affine_select(out=Lstrict[:], in_=ones_f[:], pattern=[[-1, 128]], compare_op=ALU.is_gt,
                                               fill=0.0, base=0, channel_multiplier=1), reads=['ones_f'], writes=['Lstrict'])
        P.op('pool', lambda e: e.affine_select(out=Lincl[:], in_=ones_f[:], pattern=[[-1, 128]], compare_op=ALU.is_ge,
                                               fill=0.0, base=0, channel_multiplier=1), reads=['ones_f'], writes=['Lincl'])
        LBD = sb("LBD", [128, 128], F32)
        LOD = sb("LOD", [128, 128], F32)
        for cb in range(4):
            csl = slice(cb * 32, (cb + 1) * 32)
            P.op('pool', lambda e, csl=csl, cb=cb: e.affine_select(out=LBD[:, csl], in_=Lstrict[:, csl], pattern=[[0, 32]],
                                                                   compare_op=ALU.is_ge, fill=0.0, base=-32 * cb, channel_multiplier=1),
                 reads=['Lstrict'], writes=['LBD'])
            P.op('pool', lambda e, csl=csl, cb=cb: e.affine_select(out=LBD[:, csl], in_=LBD[:, csl], pattern=[[0, 32]],
                                                                   compare_op=ALU.is_ge, fill=0.0, base=32 * cb + 31, channel_multiplier=-1),
                 reads=['LBD'], writes=['LBD'])
        P.op('pool', lambda e: e.tensor_tensor(out=LOD[:], in0=Lstrict[:], in1=LBD[:], op=ALU.subtract),
             reads=['Lstrict', 'LBD'], writes=['LOD'])
        dmaskI = sb("dmaskI", [128, 4, 512], BF16)
        wmask = sb("wmask", [128, 4, 512], BF16)
        for j in range(4):
            P.op('pool', lambda e, j=j: e.affine_select(out=dmaskI[:, j, :], in_=ones512[:], pattern=[[1, 512]],
                                                        compare_op=ALU.is_ge, fill=0.0, base=-128 * j,
                                                        channel_multiplier=-1),
                 reads=['ones512'], writes=['dmaskI'])
            P.op('pool', lambda e, j=j: e.affine_select(out=wmask[:, j, :], in_=ones512[:], pattern=[[-1, 512]],
                                                        compare_op=ALU.is_ge, fill=0.0, base=128 * j - 1,
                                                        channel_multiplier=1),
                 reads=['ones512'], writes=['wmask'])
        Psw = sb("Psw", [128, 128], BF16)
        for mb, base in ((0, -32), (1, 0), (2, -96), (3, -64)):
            P.op('pool', lambda e, mb=mb, base=base: e.affine_select(
                out=Psw[:, mb * 32:(mb + 1) * 32], in_=ones_bf[:, 0:32], pattern=[[-1, 32]],
                compare_op=ALU.is_equal, fill=0.0, base=base, channel_multiplier=1),
                reads=['ones_bf'], writes=['Psw'])
        cmask = sb("cmask", [128, 2, T], BF16)
        Ebig = sb("Ebig", [128, T], BF16)
        oh = sb("oh", [128, 18, 64], BF16)
        ov = sb("ov", [128, 2, 64], BF16)
        keepB = sb("keepB", [128, 128], F32)
        addcB = sb("addcB", [128, 128], F32)
        cosT = sb("cosT", [128, T], BF16)
        sinS = sb("sinS", [128, T], BF16)
        _tmp_scope = contextlib.ExitStack()
        sbtmp = lambda name, shape, dt=F32: _tmp_scope.enter_context(nc.sbuf_tensor(name, shape, dt))
        onesT = _tmp_scope.enter_context(nc.sbuf_tensor("onesT", [128, T], BF16))
        P.op('pool', lambda e: e.memset(onesT[:], 1.0), writes=['onesT'])
        for ic in range(2):
            P.op('pool', lambda e, ic=ic: e.affine_select(out=cmask[:, ic, :], in_=onesT[:], pattern=[[1, T]],
                                                          compare_op=ALU.is_ge, fill=0.0, base=-31 - 2048 * ic,
                                                          channel_multiplier=-16),
                 reads=['onesT'], writes=['cmask'])
        P.op('pool', lambda e: e.memset(Ebig[:], BIGM), writes=['Ebig'])
        for ph in range(2):
            psl = slice(ph * 64, (ph + 1) * 64)
            P.op('pool', lambda e, psl=psl: e.affine_select(out=Ebig[psl, :], in_=Ebig[psl, :], pattern=[[1, T]], compare_op=ALU.is_ge,
                                                            fill=0.0, base=0, channel_multiplier=-64),
                 reads=['Ebig'], writes=['Ebig'])
            P.op('pool', lambda e, psl=psl: e.affine_select(out=Ebig[psl, :], in_=Ebig[psl, :], pattern=[[-1, T]], compare_op=ALU.is_ge,
                                                            fill=0.0, base=63, channel_multiplier=64),
                 reads=['Ebig'], writes=['Ebig'])
        ones18 = sbtmp("ones18", [128, 18, 64], BF16)
        P.op('pool', lambda e: e.memset(ones18[:], 1.0), writes=['ones18'])
        P.op('pool', lambda e: e.affine_select(out=oh[:], in_=ones18[:], pattern=[[-1, 18], [0, 64]],
                                               compare_op=ALU.is_equal, fill=0.0, base=-12, channel_multiplier=1),
             reads=['ones18'], writes=['oh'])
        ovA = sbtmp("ovA", [128, 2, 64], I32)
        P.op('pool', lambda e: e.iota(ovA[:], pattern=[[2048, 2], [-64, 64]], base=0, channel_multiplier=16),
             writes=['ovA'])
        ovF = sbtmp("ovF", [128, 2, 64], F32)
        ovG = sbtmp("ovG", [128, 2, 64], F32)
        P.op('dve', lambda e: e.tensor_copy(out=ovF[:], in_=ovA[:]), reads=['ovA'], writes=['ovF'])
        P.op('dve', lambda e: e.tensor_scalar(out=ovG[:], in0=ovF[:], scalar1=32.0, scalar2=64.0, op0=ALU.add, op1=ALU.min),
             reads=['ovF'], writes=['ovG'])
        P.op('dve', lambda e: e.tensor_scalar(out=ovF[:], in0=ovF[:], scalar1=0.0, scalar2=None, op0=ALU.max),
             reads=['ovF'], writes=['ovF'])
        P.op('dve', lambda e: e.tensor_tensor(out=ovG[:], in0=ovG[:], in1=ovF[:], op=ALU.subtract),
             reads=['ovF', 'ovG'], writes=['ovG'])
        P.op('dve', lambda e: e.tensor_scalar(out=ov[:], in0=ovG[:], scalar1=0.0, scalar2=1.0 / 32, op0=ALU.max, op1=ALU.mult),
             reads=['ovG'], writes=['ov'])
        negf = sbtmp("negf", [128, 128], F32)
        P.op('pool', lambda e: e.memset(negf[:], -1.0), writes=['negf'])
        for ph in range(2):
            psl = slice(ph * 64, (ph + 1) * 64)
            P.op('pool', lambda e, psl=psl, ph=ph: e.affine_select(
                out=keepB[psl, :], in_=ones_f[psl, :], pattern=[[-1, 128]], compare_op=ALU.is_ge, fill=0.0,
                base=62 + ph, channel_multiplier=0), reads=['ones_f'], writes=['keepB'])
            P.op('pool', lambda e, psl=psl, ph=ph: e.affine_select(
                out=addcB[psl, :], in_=negf[psl, :], pattern=[[1, 128]], compare_op=ALU.is_gt, fill=1e4,
                base=-64 - ph, channel_multiplier=0), reads=['negf'], writes=['addcB'])
            P.op('pool', lambda e, psl=psl, ph=ph: e.affine_select(
                out=addcB[psl, :], in_=addcB[psl, :], pattern=[[1, 128]], compare_op=ALU.is_ge, fill=0.0,
                base=-64 - ph + 1, channel_multiplier=0), reads=['addcB'], writes=['addcB'])
        with contextlib.ExitStack() as st:
            sbt = lambda name, shape, dt=F32, u=uid(): st.enter_context(nc.sbuf_tensor(name + u, shape, dt))
            pos_i = sbt("pos_i", [128, T], I32)
            ang = sbt("ang", [128, T], F32)
            tmpa = sbt("tmpa", [128, T], F32)
            ki = sbt("ki", [128, T], I32)
            tmpb = sbt("tmpb", [128, T], F32)
            pidx = sbt("pidx", [128, 1], I32)
            pf = sbt("pf", [128, 1], F32)
            inv = sbt("inv", [128, 1], F32)
            sgn = sbt("sgn", [128, 1], F32)
            P.dma(pos_i[:], pos.partition_broadcast(128), writes=['pos_i'])
            P.op('pool', lambda e: e.iota(pidx[:], pattern=[[0, 1]], base=0, channel_multiplier=1), writes=['pidx'])
            c123 = sbt("c123", [128, 3], F32)
            fl = sbt("fl", [128, 1], F32)
            P.op('dve', lambda e: e.tensor_copy(out=pf[:], in_=pidx[:]), reads=['pidx'], writes=['pf'])
            for i3, thr in enumerate((32.0, 64.0, 96.0)):
                P.op('dve', lambda e, i3=i3, thr=thr: e.tensor_scalar(out=c123[:, i3:i3 + 1], in0=pf[:], scalar1=thr,
                                                                       scalar2=None, op0=ALU.is_ge),
                     reads=['pf'], writes=['c123'])
            P.op('dve', lambda e: e.tensor_tensor(out=fl[:], in0=c123[:, 0:1], in1=c123[:, 1:2], op=ALU.add),
                 reads=['c123'], writes=['fl'])
            P.op('dve', lambda e: e.tensor_tensor(out=fl[:], in0=fl[:], in1=c123[:, 2:3], op=ALU.add),
                 reads=['c123', 'fl'], writes=['fl'])
            P.op('dve', lambda e: e.scalar_tensor_tensor(out=fl[:], in0=fl[:], scalar=-32.0, in1=pf[:], op0=ALU.mult, op1=ALU.add),
                 reads=['fl', 'pf'], writes=['fl'])
            P.op('act', lambda e: e.activation(out=inv[:], in_=fl[:], func=AF.Exp, scale=-float(np.log(10000.0)) / 32),
                 reads=['fl'], writes=['inv'])
            P.op('dve', lambda e: e.tensor_tensor(out=sgn[:], in0=c123[:, 0:1], in1=c123[:, 1:2], op=ALU.subtract),
                 reads=['c123'], writes=['sgn'])
            P.op('dve', lambda e: e.tensor_tensor(out=sgn[:], in0=sgn[:], in1=c123[:, 2:3], op=ALU.add),
                 reads=['c123', 'sgn'], writes=['sgn'])
            P.op('dve', lambda e: e.tensor_scalar(out=sgn[:], in0=sgn[:], scalar1=2.0, scalar2=-1.0, op0=ALU.mult, op1=ALU.add),
                 reads=['sgn'], writes=['sgn'])
            P.op('dve', lambda e: e.tensor_copy(out=ang[:], in_=pos_i[:]), reads=['pos_i'], writes=['ang'])
            P.op('dve', lambda e: e.tensor_scalar(out=ang[:], in0=ang[:], scalar1=inv[:, 0:1], scalar2=None, op0=ALU.mult),
                 reads=['ang', 'inv'], writes=['ang'])
            TWO_PI = 2.0 * float(np.pi)
            for which in range(2):
                src = ang
                if which == 1:
                    P.op('dve', lambda e: e.tensor_scalar(out=ang[:], in0=ang[:], scalar1=float(np.pi) / 2, scalar2=None, op0=ALU.add),
                         reads=['ang'], writes=['ang'])
                P.op('dve', lambda e: e.tensor_scalar(out=tmpa[:], in0=ang[:], scalar1=1.0 / TWO_PI, scalar2=None, op0=ALU.mult),
                     reads=['ang'], writes=['tmpa'])
                P.op('dve', lambda e: e.tensor_copy(out=ki[:], in_=tmpa[:]), reads=['tmpa'], writes=['ki'])
                P.op('dve', lambda e: e.tensor_copy(out=tmpa[:], in_=ki[:]), reads=['ki'], writes=['tmpa'])
                P.op('dve', lambda e: e.scalar_tensor_tensor(out=tmpa[:], in0=tmpa[:], scalar=-TWO_PI, in1=ang[:],
                                                             op0=ALU.mult, op1=ALU.add),
                     reads=['tmpa', 'ang'], writes=['tmpa'])
                for thr, opc, delta in ((float(np.pi), ALU.is_gt, -TWO_PI), (-float(np.pi), ALU.is_lt, TWO_PI)):
                    P.op('dve', lambda e, thr=thr, opc=opc, delta=delta: e.tensor_scalar(
                        out=tmpb[:], in0=tmpa[:], scalar1=thr, scalar2=delta, op0=opc, op1=ALU.mult),
                        reads=['tmpa'], writes=['tmpb'])
                    P.op('dve', lambda e: e.tensor_tensor(out=tmpa[:], in0=tmpa[:], in1=tmpb[:], op=ALU.add),
                         reads=['tmpa', 'tmpb'], writes=['tmpa'])
                P.op('dve', lambda e: e.tensor_scalar(out=tmpa[:], in0=tmpa[:], scalar1=3.14159, scalar2=-3.14159,
                                                      op0=ALU.min, op1=ALU.max), reads=['tmpa'], writes=['tmpa'])
                if which == 0:
                    P.op('act', lambda e: e.activation(out=sinS[:], in_=tmpa[:], func=AF.Sin, scale=sgn[:, 0:1]),
                         reads=['tmpa', 'sgn'], writes=['sinS'])
                else:
                    P.op('act', lambda e: e.activation(out=cosT[:], in_=tmpa[:], func=AF.Sin),
                         reads=['tmpa'], writes=['cosT'])
            P.barrier()
            P.flush()
        _tmp_scope.close()
        if not ('C' in stages and 'D' in stages) and not mix_input:
            zt = sb("zt", [128, 2048], BF16)
            P.op('pool', lambda e: e.memset(zt[:], 0.0), writes=['zt'])
            for r0 in range(0, 768, 128):
                for t0 in range(0, T, 2048):
                    n = min(2048, T - t0)
                    P.dma(mixT[r0:r0 + 128, t0:t0 + n], zt[:, 0:n], reads=['zt'])
            P.barrier()
            P.flush()
        for l in range(depth):
            hsrc = xT if l == 0 else hT
            if 'A' in stages:
              with contextlib.ExitStack() as st:
                sbt = lambda name, shape, dt=F32, u=uid(): st.enter_context(nc.sbuf_tensor(name + u, shape, dt))
                pst = lambda name, shape, dt=F32, u=uid(): st.enter_context(nc.psum_tensor(name + u, shape, dt))
                wbf = sbt("a_wbf", [128, 8, N_IN_PAD], BF16)
                wst = [sbt("a_wst%d" % i, [128, N_IN_PAD // 2], F32) for i in range(2)]
                gain = sbt("a_gain", [128, 8], F32)
                P.dma(gain[:], ln_mix[l], writes=[gain.name])
                for c in range(8):
                    for hf in range(2):
                        s = wst[hf]
                        csl = slice(hf * (N_IN_PAD // 2), (hf + 1) * (N_IN_PAD // 2))
                        P.dma(s[:], w_in[l, c * 128:(c + 1) * 128, csl], writes=[s.name])
                        cast(wbf[:, c, csl], s[:], [s.name], ['a_wbf'])
                h_sb = [sbt("a_h%d" % i, [128, 8, 512], F32) for i in range(2)]
                sq = sbt("a_sq", [128, 8, 512], BF16)
                xn = [sbt("a_xn%d" % i, [128, 8, 512], BF16) for i in range(2)]
                rstd = sbt("a_rstd", [128, 512], F32)
                ps_n = pst("a_psn", [128, 512])
                ps_o = [pst("a_pso%d" % i, [128, 512]) for i in range(4)]
                ost = [sbt("a_ost%d" % i, [128, 512], F32) for i in range(4)]
                for tt in range(NT):
                    tsl = slice(tt * 512, (tt + 1) * 512)
                    h = h_sb[tt % 2]
                    x_ = xn[tt % 2]
                    P.dma(h[:], pc(hsrc)[:, :, tsl], writes=[h.name])
                    emit_norm(h, gain, sq, ps_n, rstd, x_)
                    for m in range(24):
                        ps = ps_o[m % 4]
                        o = ost[m % 4]
                        for c in range(8):
                            P.op('pe', lambda e, c=c, m=m, ps=ps, x_=x_: e.matmul(
                                ps[:], lhsT=wbf[:, c, m * 128:(m + 1) * 128], rhs=x_[:, c, :],
                                start=(c == 0), stop=(c == 7)),
                                reads=[x_.name, 'a_wbf'], writes=[ps.name], inc=(c == 7))
                        cast(o[:], ps[:], [ps.name], [o.name], psum=True)
                        P.dma(projF[m * 128:(m + 1) * 128, tsl], o[:], reads=[o.name])
                    for ts in range(4):
                        ps = ps_o[ts % 4]
                        o = ost[ts % 4]
                        for c in range(8):
                            P.op('pe', lambda e, c=c, ts=ts, ps=ps, x_=x_: e.matmul(
                                ps[:], lhsT=x_[:, c, ts * 128:(ts + 1) * 128], rhs=wbf[:, c, 3072:3584],
                                start=(c == 0), stop=(c == 7)),
                                reads=[x_.name, 'a_wbf'], writes=[ps.name], inc=(c == 7))
                        cast(o[:], ps[:], [ps.name], [o.name], psum=True)
                        P.dma(projT[tt * 512 + ts * 128: tt * 512 + (ts + 1) * 128, :], o[:], reads=[o.name])
                P.barrier()
                P.flush()

            if 'B' in stages:
              with contextlib.ExitStack() as st:
                sbt = lambda name, shape, dt=F32, u=uid(): st.enter_context(nc.sbuf_tensor(name + u, shape, dt))
                pst = lambda name, shape, dt=F32, u=uid(): st.enter_context(nc.psum_tensor(name + u, shape, dt))
                NKB = T // 128
                qT = sbt("b_qT", [128, 2, T], BF16)
                kT = sbt("b_kT", [128, 2, T], BF16)
                vv = sbt("b_v", [128, NKB, 256], BF16)
                stg = [sbt("b_stg%d" % i, [128, 2048], F32) for i in range(2)]
                nw = sbt("b_nw", [128, 1], F32)
                P.dma(nw[:], sb_norm[l], writes=[nw.name])
                k = 0
                for c in range(2):
                    for t0 in range(0, T, 2048):
                        n = min(2048, T - t0)
                        s = stg[k % 2]; k += 1
                        P.dma(s[:, 0:n], projF[(19 + c) * 128:(20 + c) * 128, t0:t0 + n], writes=[s.name])
                        P.op('dve', lambda e, s=s, c=c, t0=t0, n=n: e.tensor_scalar(
                            out=qT[:, c, t0:t0 + n], in0=s[:, 0:n], scalar1=0.125, scalar2=None, op0=ALU.mult),
                            reads=[s.name], writes=['b_qT'])
                        s = stg[k % 2]; k += 1
                        P.dma(s[:, 0:n], projF[(21 + c) * 128:(22 + c) * 128, t0:t0 + n], writes=[s.name])
                        cast(kT[:, c, t0:t0 + n], s[:, 0:n], [s.name], ['b_kT'])
                pTv = projT.rearrange("(kb p) f -> p kb f", p=128)
                for kb0 in range(0, NKB, 8):
                    s = stg[k % 2]; k += 1
                    sv = s[:].rearrange("p (a f) -> p a f", f=256)
                    P.dma(sv, pTv[:, kb0:kb0 + 8, 256:512], writes=[s.name])
                    cast(vv[:, kb0:kb0 + 8, :], sv, [s.name], ['b_v'])
                e_sb = [sbt("b_e%d" % i, [128, 512], F32) for i in range(3)]
                sp_sb = [sbt("b_sp%d" % i, [128, 512], BF16) for i in range(3)]
                a_sb = [sbt("b_a%d" % i, [128, 512], BF16) for i in range(3)]
                racc = sbt("b_racc", [128, 512], BF16)
                sqo = sbt("b_sqo", [64, 512], BF16)
                rs = sbt("b_rs", [64, 512], F32)
                o_sb = [sbt("b_o%d" % i, [64, 512], BF16) for i in range(2)]
                psZ = [pst("b_psZ%d" % i, [128, 512]) for i in range(2)]
                psA = [pst("b_psA%d" % i, [128, 512]) for i in range(2)]
                psO = [pst("b_psO%d" % i, [64, 512]) for i in range(2)]
                psN = pst("b_psN", [64, 512])
                psW = pst("b_psW", [128, 512])
                it3 = [0]; it4 = [0]
                ho = 0
                for hh in range(4):
                    c = hh // 2
                    b0 = (hh % 2) * 64
                    for qt in range(NT):
                        qsl = slice(qt * 512, (qt + 1) * 512)
                        po = psO[ho % 2]
                        oo = o_sb[ho % 2]
                        ho += 1
                        nblk = 4 * (qt + 1)
                        warm(psW)
                        kbs = list(reversed(range(nblk)))
                        nb_ = len(kbs)
                        st_ = {}

                        def S1(t, kbs=kbs, qt=qt, b0=b0, c=c, qsl=qsl, st_=st_):
                            kb = kbs[t]
                            j = kb - 4 * qt
                            ksl = slice(kb * 128, (kb + 1) * 128)
                            pz = psZ[it3[0] % 2]; ee = e_sb[it3[0] % 3]; sp = sp_sb[it3[0] % 3]; it3[0] += 1
                            st_[t] = dict(kb=kb, j=j, ksl=ksl, sp=sp, ee=ee)
                            P.op('pe', lambda e: e.matmul(
                                pz[:], lhsT=kT[b0:b0 + 64, c, ksl], rhs=qT[b0:b0 + 64, c, qsl], start=True, stop=True),
                                reads=['b_kT', 'b_qT'], writes=[pz.name])
                            warm(psW, 1)
                            P.op('act', lambda e: e.activation(out=ee[:], in_=pz[:], func=AF.Exp),
                                 reads=[pz.name], writes=[ee.name])
                            P.op('act', lambda e: e.activation(out=sp[:], in_=ee[:], func=AF.Ln, bias=one_t[:]),
                                 reads=[ee.name, 'one_t'], writes=[sp.name])
                            if j >= 0:
                                P.op('dve', lambda e: e.tensor_tensor(out=sp[:], in0=sp[:], in1=dmask[:, j, :], op=ALU.mult),
                                     reads=[sp.name, 'dmask'], writes=[sp.name])

                        def S2(t, b0=b0, c=c, qsl=qsl, st_=st_):
                            d_ = st_[t]
                            kb, j, ksl, sp = d_['kb'], d_['j'], d_['ksl'], d_['sp']
                            first = (t == 0)
                            pa = psA[it4[0] % 2]; aa = a_sb[it4[0] % 3]; it4[0] += 1
                            d_['aa'] = aa
                            ee = d_['ee']
                            P.op('pe', lambda e: e.matmul(pa[:], lhsT=negU[:], rhs=sp[:], start=True, stop=first),
                                 reads=[sp.name, 'negU'], writes=[pa.name], inc=first)
                            if not first:
                                P.op('pe', lambda e: e.matmul(pa[:], lhsT=negOnes[:], rhs=racc[:], start=False, stop=True),
                                     reads=['b_racc', 'negOnes'], writes=[pa.name])
                            warm(psW, 2)
                            P.op('act', lambda e: e.activation(out=aa[:], in_=pa[:], func=AF.Exp),
                                 reads=[pa.name], writes=[aa.name])
                            P.op('dve', lambda e: e.tensor_tensor(out=aa[:], in0=aa[:], in1=ee[:], op=ALU.mult),
                                 reads=[aa.name, ee.name], writes=[aa.name])
                            if j >= 0:
                                P.op('dve', lambda e: e.tensor_tensor(out=aa[:], in0=aa[:], in1=dmask[:, j, :], op=ALU.mult),
                                     reads=[aa.name, 'dmask'], writes=[aa.name])
                            if kb > 0:
                                if first:
                                    P.op('pool', lambda e: e.tensor_copy(out=racc[:], in_=sp[:]),
                                         reads=[sp.name], writes=['b_racc'])
                                else:
                                    P.op('pool', lambda e: e.tensor_tensor(out=racc[:], in0=racc[:], in1=sp[:], op=ALU.add),
                                         reads=[sp.name, 'b_racc'], writes=['b_racc'])

                        def S3(t, po=po, hh=hh, st_=st_, nb_=nb_):
                            d_ = st_[t]
                            kb, aa = d_['kb'], d_['aa']
                            warm(psW, 1)
                            P.op('pe', lambda e: e.matmul(
                                po[:], lhsT=vv[:, kb, hh * 64:(hh + 1) * 64], rhs=aa[:], start=(t == 0), stop=(t == nb_ - 1)),
                                reads=[aa.name, 'b_v'], writes=[po.name], inc=True)

                        for t in range(nb_ + 2):
                            if t < nb_:
                                S1(t)
                            if 1 <= t <= nb_:
                                S2(t - 1)
                            if t >= 2:
                                S3(t - 2)
                        P.op('act', lambda e, po=po: e.activation(out=sqo[:], in_=po[:], func=AF.Square),
                             reads=[po.name], writes=['b_sqo'])
                        P.op('pe', lambda e: e.matmul(psN[:], lhsT=ones_bf[0:64, 0:64], rhs=sqo[:], start=True, stop=True),
                             reads=['b_sqo', 'ones_bf'], writes=['b_psN'])
                        P.op('act', lambda e: e.activation(out=rs[:], in_=psN[:], func=AF.Ln, bias=eps_t[0:64, :], scale=1.0 / 64),
                             reads=['b_psN', 'eps_t'], writes=['b_rs'])
                        P.op('act', lambda e: e.activation(out=rs[:], in_=rs[:], func=AF.Exp, scale=-0.5),
                             reads=['b_rs'], writes=['b_rs'])
                        P.op('dve', lambda e, po=po, oo=oo: e.scalar_tensor_tensor(
                            out=oo[:], in0=po[:], scalar=nw[0:64, :], in1=rs[:], op0=ALU.mult, op1=ALU.mult),
                            reads=[po.name, nw.name, 'b_rs'], writes=[oo.name])
                        P.dma(mixT[768 + hh * 64:768 + (hh + 1) * 64, qsl], oo[:], reads=[oo.name])
                P.barrier()
                P.flush()

            if 'C' in stages:
              with contextlib.ExitStack() as st:
                sbt = lambda name, shape, dt=F32, u=uid(): st.enter_context(nc.sbuf_tensor(name + u, shape, dt))
                qT = sbt("c_qT", [128, 3, T], BF16)
                ksT = sbt("c_ksT", [128, T], BF16)
                kwT = sbt("c_kwT", [128, T], BF16)
                vs = sbt("c_vs", [128, NKB, 128], BF16)
                vw = sbt("c_vw", [128, NKB, 128], BF16)
                sgT = sbt("c_sgT", [128, T], BF16)
                selT = sbt("c_selT", [128, T], BF16)
                kcT = sbt("c_kcT", [128, 256], BF16)
                vc = sbt("c_vc", [128, 2, 2, 64], BF16)
                nw = sbt("c_nw", [128, 1], F32)
                P.dma(nw[:], nsa_norm[l], writes=[nw.name])
                ICS = [(ic, min(128, NCMP - ic * 128)) for ic in range(2) if NCMP - ic * 128 > 0]
                with contextlib.ExitStack() as s1:
                    sb1 = lambda name, shape, dt=F32, u=uid(): s1.enter_context(nc.sbuf_tensor(name + u, shape, dt))
                    ps1 = lambda name, shape, dt=F32, u=uid(): s1.enter_context(nc.psum_tensor(name + u, shape, dt))
                    stg = [sb1("c_stg%d" % i, [128, 2048], F32) for i in range(2)]
                    kcr = sb1("c_kcr", [128, T], BF16)
                    vcb = sb1("c_vcb", [128, T], BF16)
                    xb_r = [sb1("c_xb%d" % i, [128, 512], BF16) for i in range(2)]
                    t1_r = [sb1("c_t1%d" % i, [128, 512], F32) for i in range(2)]
                    t2_r = [sb1("c_t2%d" % i, [128, 512], F32) for i in range(2)]
                    psR = [ps1("c_psR%d" % i, [128, 512]) for i in range(2)]
                    cnt = [0, 0]

                    def load_chunk(ch, fn):
                        for t0 in range(0, T, 2048):
                            n = min(2048, T - t0)
                            s = stg[cnt[0] % 2]; cnt[0] += 1
                            P.dma(s[:, 0:n], projF[ch * 128:(ch + 1) * 128, t0:t0 + n], writes=[s.name])
                            fn(s, t0, n)

                    def rope_to(dst, dkey, scale):
                        def fn(s, t0, n):
                            for sub in range(n // 512):
                                sl = slice(sub * 512, (sub + 1) * 512)
                                gsl = slice(t0 + sub * 512, t0 + (sub + 1) * 512)
                                i = cnt[1] % 2; cnt[1] += 1
                                xb, t1, t2, ps = xb_r[i], t1_r[i], t2_r[i], psR[i]
                                cast(xb[:], s[:, sl], [s.name], [xb.name])
                                P.op('pe', lambda e, ps=ps, xb=xb: e.matmul(ps[:], lhsT=Psw[:], rhs=xb[:], start=True, stop=True),
                                     reads=[xb.name, 'Psw'], writes=[ps.name])
                                P.op('dve', lambda e, t1=t1, s=s, sl=sl, gsl=gsl: e.scalar_tensor_tensor(
                                    out=t1[:], in0=s[:, sl], scalar=scale, in1=cosT[:, gsl], op0=ALU.mult, op1=ALU.mult),
                                    reads=[s.name, 'cosT'], writes=[t1.name])
                                P.op('dve', lambda e, t2=t2, ps=ps, gsl=gsl: e.scalar_tensor_tensor(
                                    out=t2[:], in0=ps[:], scalar=scale, in1=sinS[:, gsl], op0=ALU.mult, op1=ALU.mult),
                                    reads=[ps.name, 'sinS'], writes=[t2.name])
                                P.op('pool', lambda e, t1=t1, t2=t2, gsl=gsl: e.tensor_tensor(
                                    out=dst(gsl), in0=t1[:], in1=t2[:], op=ALU.add),
                                    reads=[t1.name, t2.name], writes=[dkey])
                        return fn

                    for r in range(3):
                        load_chunk(12 + r, rope_to(lambda gsl, r=r: qT[:, r, gsl], 'c_qT', 0.125))
                    load_chunk(15, rope_to(lambda gsl: kcr[:, gsl], 'c_kcr', 1.0))
                    load_chunk(17, rope_to(lambda gsl: ksT[:, gsl], 'c_ksT', 1.0))
                    load_chunk(18, rope_to(lambda gsl: kwT[:, gsl], 'c_kwT', 1.0))
                    load_chunk(16, lambda s, t0, n: cast(vcb[:, t0:t0 + n], s[:, 0:n], [s.name], ['c_vcb']))
                    load_chunk(23, lambda s, t0, n: P.op('act', lambda e: e.activation(
                        out=sgT[:, t0:t0 + n], in_=s[:, 0:n], func=AF.Sigmoid), reads=[s.name], writes=['c_sgT']))
                    pTv = projT.rearrange("(kb p) f -> p kb f", p=128)
                    for kb0 in range(0, NKB, 8):
                        for (dstv, c0) in ((vs, 0), (vw, 128)):
                            s = stg[cnt[0] % 2]; cnt[0] += 1
                            sv = s[:, 0:1024].rearrange("p (a f) -> p a f", f=128)
                            P.dma(sv, pTv[:, kb0:kb0 + 8, c0:c0 + 128], writes=[s.name])
                            cast(dstv[:, kb0:kb0 + 8, :], sv, [s.name], [dstv.name])
                    w1b = [sb1("c_w1%d" % i, [128, 32, 128], BF16) for i in range(2)]
                    w2kp = sb1("c_w2kp", [128, 2, 128], BF16)
                    w2v = sb1("c_w2v", [128, 64], BF16)
                    peb = [sb1("c_pe%d" % i, [128, 32], BF16) for i in range(2)]
                    bias = [sb1("c_bias%d" % i, [128, 1], F32) for i in range(2)]
                    hid = [[sb1("c_hid%d%d" % (i, g), [128, 256], BF16) for g in range(2)] for i in range(2)]
                    psH = [ps1("c_psH%d" % i, [128, 256]) for i in range(2)]
                    psB = ps1("c_psB", [128, 64])
                    P.op('pool', lambda e: e.memset(w2kp[:], 0.0), writes=['c_w2kp'])
                    for i, (w1d, w2d, ped) in enumerate(((cmp_k_w1, cmp_k_w2, pe_kT), (cmp_v_w1, cmp_v_w2, pe_vT))):
                        for hf in range(2):
                            s = stg[cnt[0] % 2]; cnt[0] += 1
                            sv = s[:].rearrange("p (a f) -> p a f", f=128)
                            P.dma(sv, w1d[l, :, hf * 16:(hf + 1) * 16, :], writes=[s.name])
                            cast(w1b[i][:, hf * 16:(hf + 1) * 16, :], sv, [s.name], [w1b[i].name])
                        s = stg[cnt[0] % 2]; cnt[0] += 1
                        P.dma(s[:, 0:64], w2d[l], writes=[s.name])
                        if i == 0:
                            for g in range(2):
                                cast(w2kp[:, g, g * 64:(g + 1) * 64], s[:, 0:64], [s.name], ['c_w2kp'])
                        else:
                            cast(w2v[:], s[:, 0:64], [s.name], ['c_w2v'])
                        s = stg[cnt[0] % 2]; cnt[0] += 1
                        P.dma(s[:, 0:32], ped[l], writes=[s.name])
                        cast(peb[i][:], s[:, 0:32], [s.name], [peb[i].name])
                        for ll in range(32):
                            P.op('pe', lambda e, i=i, ll=ll: e.matmul(
                                psB[:, 0:1], lhsT=w1b[i][0:64, ll, :], rhs=peb[i][0:64, ll:ll + 1],
                                start=(ll == 0), stop=(ll == 31)),
                                reads=[w1b[i].name, peb[i].name], writes=['c_psB'], inc=(ll == 31))
                        P.op('dve', lambda e, i=i: e.tensor_copy(out=bias[i][:], in_=psB[:, 0:1]),
                             reads=['c_psB'], writes=[bias[i].name])
                        src = kcr if i == 0 else vcb
                        srcv = src[:].rearrange("p (i s) -> p i s", s=16)
                        for g in range(2):
                            ph = psH[g]
                            for ll in range(32):
                                a, s0 = ll // 16, ll % 16
                                P.op('pe', lambda e, i=i, g=g, ll=ll, a=a, s0=s0, ph=ph, srcv=srcv: e.matmul(
                                    ph[:, 0:NCMP], lhsT=w1b[i][g * 64:(g + 1) * 64, ll, :],
                                    rhs=srcv[g * 64:(g + 1) * 64, a:a + NCMP, s0],
                                    start=(ll == 0), stop=(ll == 31)),
                                    reads=[w1b[i].name, 'c_kcr' if i == 0 else 'c_vcb'], writes=[ph.name],
                                    inc=(ll == 31))
                            P.op('act', lambda e, i=i, g=g, ph=ph: e.activation(
                                out=hid[i][g][:, 0:NCMP], in_=ph[:, 0:NCMP], func=AF.Silu, bias=bias[i][:]),
                                reads=[ph.name, bias[i].name], writes=[hid[i][g].name])
                    for g in range(2):
                        P.op('pe', lambda e, g=g: e.matmul(psH[0][:, 0:NCMP], lhsT=w2kp[:, g, :], rhs=hid[0][g][:, 0:NCMP],
                                                           start=(g == 0), stop=(g == 1)),
                             reads=['c_w2kp', hid[0][g].name], writes=[psH[0].name], inc=(g == 1))
                    P.op('act', lambda e: e.copy(out=kcT[:, 0:NCMP], in_=psH[0][:, 0:NCMP]),
                         reads=[psH[0].name], writes=['c_kcT'])
                    for g in range(2):
                        for (ic, n) in ICS:
                            P.op('pe', lambda e, g=g, ic=ic, n=n: e.matmul(
                                psB[0:n, 0:64], lhsT=hid[1][g][:, ic * 128:ic * 128 + n], rhs=w2v[:], start=True, stop=True),
                                reads=[hid[1][g].name, 'c_w2v'], writes=['c_psB'])
                            P.op('dve', lambda e, g=g, ic=ic, n=n: e.tensor_copy(out=vc[0:n, g, ic, :], in_=psB[0:n, 0:64]),
                                 reads=['c_psB'], writes=['c_vc'])
                    P.barrier()
                    P.flush()
                with contextlib.ExitStack() as s2:
                    sb2 = lambda name, shape, dt=F32, u=uid(): s2.enter_context(nc.sbuf_tensor(name + u, shape, dt))
                    ps2 = lambda name, shape, dt=F32, u=uid(): s2.enter_context(nc.psum_tensor(name + u, shape, dt))
                    psS = [ps2("c_psS%d" % i, [128, 512]) for i in range(2)]
                    psD = ps2("c_psD", [128, 512])
                    psW = ps2("c_psW", [128, 512])
                    psO = ps2("c_psO", [64, 512])
                    psDn = ps2("c_psDn", [64, 512])
                    psM = ps2("c_psM", [128, 512])
                    psG = ps2("c_psG", [64, 512])
                    pe_r = [sb2("c_pe_%d" % i, [128, 512], BF16) for i in range(2)]
                    a_r = [sb2("c_a%d" % i, [128, 512], BF16) for i in range(3)]
                    pn = [sb2("c_pn%d" % r, [128, 2, 512], BF16) for r in range(3)]
                    rden = sb2("c_rden", [128, 512], F32)
                    asum = sb2("c_asum", [128, 512], F32)
                    oc_sets = [[sb2("c_oc%d_%d" % (k, r), [64, 512], F32) for r in range(3)] for k in range(2)]
                    ob_sb = sb2("c_ob", [64, 512], F32)
                    acc = sb2("c_acc", [64, 512], F32)
                    acc2 = sb2("c_acc2", [64, 512], F32)
                    rd64 = sb2("c_rd64", [64, 512], F32)
                    sqo = sb2("c_sqo", [64, 512], BF16)
                    rs = sb2("c_rs", [64, 512], F32)
                    o_out = [sb2("c_oo%d" % i, [64, 512], BF16) for i in range(2)]
                    score = sb2("c_score", [128, 64], F32)
                    score2 = sb2("c_score2", [128, 64], F32)
                    m8a = sb2("c_m8a", [128, 8], F32)
                    m8b = sb2("c_m8b", [128, 8], F32)
                    selm = sb2("c_selm", [128, 128], F32)
                    it = [0]
                    itm = [0]
                    oi = [0]
                    P.op('pool', lambda e: e.memset(selm[:], 0.0), writes=['c_selm'])

                    def attn_core(g, r, qt, kT_src, ksrc_key, v_src, kbs, mask_of, with_sel, pump):
                        qsl = slice(qt * 512, (qt + 1) * 512)
                        gs = slice(g * 64, (g + 1) * 64)
                        tl = {}

                        def score(bi):
                            kb = kbs[bi]
                            ksl = slice(kb * 128, (kb + 1) * 128)
                            ps = psS[itm[0] % 2]; aa = a_r[itm[0] % 3]; itm[0] += 1
                            tl[bi] = aa
                            P.op('pe', lambda e: e.matmul(
                                ps[:], lhsT=kT_src[gs, ksl], rhs=qT[gs, r, qsl], start=True, stop=not with_sel),
                                reads=[ksrc_key, 'c_qT'], writes=[ps.name], inc=not with_sel)
                            if with_sel:
                                P.op('pe', lambda e: e.matmul(
                                    ps[:], lhsT=Ebig[gs, ksl], rhs=selT[gs, qsl], start=False, stop=True),
                                    reads=['Ebig', ('c_selT', g, qt)], writes=[ps.name])
                            P.op('act', lambda e: e.activation(out=aa[:], in_=ps[:], func=AF.Exp),
                                 reads=[ps.name], writes=[aa.name])
                            m = mask_of(kb)
                            if m is not None:
                                mt, mkey = m
                                P.op('dve', lambda e: e.tensor_tensor(out=aa[:], in0=aa[:], in1=mt, op=ALU.mult),
                                     reads=[aa.name, mkey], writes=[aa.name])

                        def av(bi):
                            kb = kbs[bi]
                            aa = tl[bi]
                            first = (bi == 0)
                            last = (bi == len(kbs) - 1)
                            P.op('pe', lambda e: e.matmul(
                                psO[:], lhsT=v_src[:, kb, gs], rhs=aa[:], start=first, stop=last),
                                reads=[aa.name, v_src.name], writes=['c_psO'])
                            if first:
                                P.op('pool', lambda e: e.tensor_copy(out=asum[:], in_=aa[:]), reads=[aa.name], writes=['c_asum'])
                            else:
                                P.op('pool', lambda e: e.tensor_tensor(out=asum[:], in0=asum[:], in1=aa[:], op=ALU.add),
                                     reads=[aa.name, 'c_asum'], writes=['c_asum'])

                        for bi in range(len(kbs) + 1):
                            if bi < len(kbs):
                                score(bi)
                            if bi >= 1:
                                av(bi - 1)
                            pump()
                        P.op('pe', lambda e: e.matmul(psDn[:], lhsT=ones_f[:, 0:64], rhs=asum[:], start=True, stop=True),
                             reads=['c_asum', 'ones_f'], writes=['c_psDn'])
                        P.op('dve', lambda e: e.tensor_scalar(out=rd64[:], in0=psDn[:], scalar1=1e-30, scalar2=None, op0=ALU.max),
                             reads=['c_psDn'], writes=['c_rd64'])
                        P.op('dve', lambda e: e.reciprocal(out=rd64[:], in_=rd64[:]), reads=['c_rd64'], writes=['c_rd64'])
                        P.op('dve', lambda e: e.tensor_tensor(out=ob_sb[:], in0=psO[:], in1=rd64[:], op=ALU.mult),
                             reads=['c_psO', 'c_rd64'], writes=['c_ob'])

                    def pre_gen(g, qt, oc_sb):
                        gs = slice(g * 64, (g + 1) * 64)
                        selkey = ('c_selT', g, qt)
                        if True:
                            qsl = slice(qt * 512, (qt + 1) * 512)
                            ics = [(ic, n) for (ic, n) in ICS if 16 * ic * 128 + 31 <= qt * 512 + 511]
                            for r in (range(3) if 'cmp' in CPARTS else ()):
                                pes = []
                                for (ic, n) in ics:
                                    ps = psS[it[0] % 2]; pp = pe_r[it[0] % 2]; it[0] += 1
                                    P.op('pe', lambda e, ps=ps, ic=ic, n=n, r=r, gs=gs, qsl=qsl: e.matmul(
                                        ps[0:n, :], lhsT=kcT[gs, ic * 128:ic * 128 + n], rhs=qT[gs, r, qsl], start=True, stop=True),
                                        reads=['c_kcT', 'c_qT'], writes=[ps.name])
                                    P.op('act', lambda e, ps=ps, pp=pp, n=n: e.activation(out=pp[0:n, :], in_=ps[0:n, :], func=AF.Exp),
                                         reads=[ps.name], writes=[pp.name])
                                    P.op('dve', lambda e, pp=pp, n=n, ic=ic, qsl=qsl: e.tensor_tensor(
                                        out=pp[0:n, :], in0=pp[0:n, :], in1=cmask[0:n, ic, qsl], op=ALU.mult),
                                        reads=[pp.name, 'cmask'], writes=[pp.name])
                                    pes.append((ic, n, pp))
                                    yield
                                for bi, (ic, n, pp) in enumerate(pes):
                                    P.op('pe', lambda e, pp=pp, n=n, bi=bi, nb=len(pes): e.matmul(
                                        psD[:], lhsT=ones_bf[0:n, :], rhs=pp[0:n, :], start=(bi == 0), stop=(bi == nb - 1)),
                                        reads=[pp.name, 'ones_bf'], writes=['c_psD'], inc=(bi == len(pes) - 1))
                                P.op('dve', lambda e: e.tensor_scalar(out=rden[:], in0=psD[:], scalar1=1e-30, scalar2=None, op0=ALU.max),
                                     reads=['c_psD'], writes=['c_rden'])
                                P.op('dve', lambda e: e.reciprocal(out=rden[:], in_=rden[:]), reads=['c_rden'], writes=['c_rden'])
                                yield
                                for (ic, n, pp) in pes:
                                    P.op('dve', lambda e, pp=pp, n=n, ic=ic, r=r: e.tensor_tensor(
                                        out=pn[r][0:n, ic, :], in0=pp[0:n, :], in1=rden[0:n, :], op=ALU.mult),
                                        reads=[pp.name, 'c_rden'], writes=[pn[r].name])
                                for bi, (ic, n, pp) in enumerate(pes):
                                    P.op('pe', lambda e, n=n, ic=ic, r=r, g=g, bi=bi, nb=len(pes): e.matmul(
                                        psD[0:64, :], lhsT=vc[0:n, g, ic, :], rhs=pn[r][0:n, ic, :], start=(bi == 0), stop=(bi == nb - 1)),
                                        reads=['c_vc', pn[r].name], writes=['c_psD'], inc=(bi == len(pes) - 1))
                                P.op('act', lambda e, r=r, oc_sb=oc_sb: e.copy(out=oc_sb[r][:], in_=psD[0:64, :]),
                                     reads=['c_psD'], writes=[oc_sb[r].name])
                                yield
                            for ts in (range(4) if 'topk' in CPARTS else ()):
                                t0 = qt * 512 + ts * 128
                                tsl = slice(ts * 128, (ts + 1) * 128)
                                nmm = 3 * len(ics)
                                k = 0
                                for r in range(3):
                                    for (ic, n) in ics:
                                        P.op('pe', lambda e, r=r, ic=ic, n=n, tsl=tsl, k=k, nmm=nmm: e.matmul(
                                            psM[:, 0:64], lhsT=pn[r][0:n, ic, tsl], rhs=ov[0:n, ic, :],
                                            start=(k == 0), stop=(k == nmm - 1)),
                                            reads=[pn[r].name, 'ov'], writes=['c_psM'], inc=(k == nmm - 1))
                                        k += 1
                                yield
                                off = 64 - t0 // 64
                                P.op('dve', lambda e, off=off: e.tensor_tensor(
                                    out=score[:], in0=psM[:, 0:64], in1=keepB[:, off:off + 64], op=ALU.mult),
                                    reads=['c_psM', 'keepB'], writes=['c_score'])
                                P.op('dve', lambda e, off=off: e.tensor_tensor(
                                    out=score[:], in0=score[:], in1=addcB[:, off:off + 64], op=ALU.add),
                                    reads=['c_score', 'addcB'], writes=['c_score'])
                                P.op('dve', lambda e: e.memset(score[:, 0:1], 1e4), reads=[], writes=['c_score'])
                                yield
                                P.op('dve', lambda e: e.max(out=m8a[:], in_=score[:]), reads=['c_score'], writes=['c_m8a'])
                                P.op('dve', lambda e: e.match_replace(out=score2[:], in_to_replace=m8a[:], in_values=score[:],
                                                                      imm_value=-1e9),
                                     reads=['c_score', 'c_m8a'], writes=['c_score2'])
                                P.op('dve', lambda e: e.max(out=m8b[:], in_=score2[:]), reads=['c_score2'], writes=['c_m8b'])
                                P.op('dve', lambda e, gs=gs: e.tensor_scalar(out=selm[:, gs], in0=score[:], scalar1=m8b[:, 7:8], scalar2=-1.0,
                                                                      op0=ALU.is_ge, op1=ALU.add),
                                     reads=['c_score', 'c_m8b'], writes=['c_selm'])
                                yield
                                P.op('pe', lambda e: e.matmul(psM[:, 128:256], lhsT=selm[:], rhs=ident_f[:], start=True, stop=True),
                                     reads=['c_selm', 'ident_f'], writes=['c_psM'])
                                P.op('act', lambda e, gs=gs, t0=t0: e.copy(out=selT[gs, t0:t0 + 128], in_=psM[gs, 128:256]),
                                     reads=['c_psM'], writes=[selkey])
                            yield

                    def main_part(g, qt, oc_sb, pump):
                        gs = slice(g * 64, (g + 1) * 64)
                        if True:
                            qsl = slice(qt * 512, (qt + 1) * 512)
                            for r in range(3):
                                hh = 3 * g + r
                                def gate_mul(dst, src_ap, src_key, br, hh=hh, qsl=qsl):
                                    P.op('pe', lambda e: e.matmul(psG[:], lhsT=oh[:, br * 6 + hh, :], rhs=sgT[:, qsl], start=True, stop=True),
                                         reads=['oh', 'c_sgT'], writes=['c_psG'])
                                    P.op('dve', lambda e: e.tensor_tensor(out=dst[:], in0=src_ap, in1=psG[:], op=ALU.mult),
                                         reads=[src_key, 'c_psG'], writes=[dst.name])
                                gate_mul(acc, oc_sb[r][:], oc_sb[r].name, 0)
                                if 'sel' in CPARTS:
                                  attn_core(g, r, qt, ksT, 'c_ksT', vs, list(range(4 * (qt + 1))),
                                            lambda kb, qt=qt: ((dmaskI[:, kb - 4 * qt, :], 'dmaskI') if kb >= 4 * qt else None), True, pump)
                                gate_mul(acc2, ob_sb[:], 'c_ob', 1)
                                P.op('pool', lambda e: e.tensor_tensor(out=acc[:], in0=acc[:], in1=acc2[:], op=ALU.add),
                                     reads=[acc.name, acc2.name], writes=[acc.name])
                                if 'win' in CPARTS:
                                  attn_core(g, r, qt, kwT, 'c_kwT', vw, list(range(max(0, 4 * qt - 4), 4 * qt + 4)),
                                            lambda kb, qt=qt: ((dmaskI[:, kb - 4 * qt, :], 'dmaskI') if kb >= 4 * qt
                                                               else (wmask[:, kb - (4 * qt - 4), :], 'wmask')), False, pump)
                                gate_mul(acc2, ob_sb[:], 'c_ob', 2)
                                P.op('pool', lambda e: e.tensor_tensor(out=acc[:], in0=acc[:], in1=acc2[:], op=ALU.add),
                                     reads=[acc.name, acc2.name], writes=[acc.name])
                                oo = o_out[oi[0] % 2]; oi[0] += 1
                                P.op('act', lambda e: e.activation(out=sqo[:], in_=acc[:], func=AF.Square),
                                     reads=[acc.name], writes=['c_sqo'])
                                P.op('pe', lambda e: e.matmul(psG[:], lhsT=ones_bf[0:64, 0:64], rhs=sqo[:], start=True, stop=True),
                                     reads=['c_sqo', 'ones_bf'], writes=['c_psG'])
                                P.op('act', lambda e: e.activation(out=rs[:], in_=psG[:], func=AF.Ln, bias=eps_t[0:64, :], scale=1.0 / 64),
                                     reads=['c_psG', 'eps_t'], writes=['c_rs'])
                                P.op('act', lambda e: e.activation(out=rs[:], in_=rs[:], func=AF.Exp, scale=-0.5),
                                     reads=['c_rs'], writes=['c_rs'])
                                P.op('dve', lambda e, oo=oo: e.scalar_tensor_tensor(
                                    out=oo[:], in0=acc[:], scalar=nw[0:64, :], in1=rs[:], op0=ALU.mult, op1=ALU.mult),
                                    reads=[acc.name, nw.name, 'c_rs'], writes=[oo.name])
                                P.dma(mixT[384 + hh * 64:384 + (hh + 1) * 64, qsl], oo[:], reads=[oo.name])

                    order = [(g, qt) for g in range(2) for qt in range(NT)]

                    def drain(gen):
                        for _ in gen:
                            pass

                    drain(pre_gen(order[0][0], order[0][1], oc_sets[0]))
                    for idx, (g, qt) in enumerate(order):
                        nxt = (pre_gen(order[idx + 1][0], order[idx + 1][1], oc_sets[(idx + 1) % 2])
                               if idx + 1 < len(order) else None)

                        def pump(nxt=nxt):
                            if nxt is None:
                                return
                            for _ in range(3):
                                try:
                                    next(nxt)
                                except StopIteration:
                                    return
                        main_part(g, qt, oc_sets[idx % 2], pump)
                        if nxt is not None:
                            drain(nxt)
                    P.barrier()
                    P.flush()

            if 'D' in stages:
              NG = 13 * 128
              with contextlib.ExitStack() as st:
                sbt = lambda name, shape, dt=F32, u=uid(): st.enter_context(nc.sbuf_tensor(name + u, shape, dt))
                with contextlib.ExitStack() as s1:
                    sb1 = lambda name, shape, dt=F32, u=uid(): s1.enter_context(nc.sbuf_tensor(name + u, shape, dt))
                    ps1 = lambda name, shape, dt=F32, u=uid(): s1.enter_context(nc.psum_tensor(name + u, shape, dt))
                    xc = [sb1("d_xc%d" % i, [128, T + 3], F32) for i in range(2)]
                    cv = [sb1("d_cv%d" % i, [128, T], F32) for i in range(2)]
                    cw = sb1("d_cw", [128, 9, 4], F32)
                    tst = [sb1("d_tst%d" % i, [128, 512], F32) for i in range(3)]
                    psT = [ps1("d_psT%d" % i, [128, 512]) for i in range(3)]
                    P.dma(cw[:], gdn_conv[l], writes=['d_cw'])
                    for i in range(2):
                        P.op('pool', lambda e, i=i: e.memset(xc[i][:, 0:3], 0.0), writes=[xc[i].name])
                    gview = gtok.rearrange("(kb p) f -> p kb f", p=128)
                    k = 0
                    for ci, ch in enumerate(list(range(12)) + [23]):
                        x_ = xc[ci % 2]
                        c_ = cv[ci % 2]
                        P.dma(x_[:, 3:T + 3], projF[ch * 128:(ch + 1) * 128, :], writes=[x_.name])
                        if ch < 9:
                            P.op('dve', lambda e, x_=x_, c_=c_, ch=ch: e.tensor_scalar(
                                out=c_[:], in0=x_[:, 0:T], scalar1=cw[:, ch, 0:1], scalar2=None, op0=ALU.mult),
                                reads=[x_.name, 'd_cw'], writes=[c_.name])
                            for tap in (1, 2, 3):
                                P.op('dve', lambda e, x_=x_, c_=c_, ch=ch, tap=tap: e.scalar_tensor_tensor(
                                    out=c_[:], in0=x_[:, tap:tap + T], scalar=cw[:, ch, tap:tap + 1], in1=c_[:],
                                    op0=ALU.mult, op1=ALU.add),
                                    reads=[x_.name, 'd_cw', c_.name], writes=[c_.name])
                            P.op('act', lambda e, c_=c_: e.activation(out=c_[:], in_=c_[:], func=AF.Silu),
                                 reads=[c_.name], writes=[c_.name])
                            src, skey = (lambda sl, c_=c_: c_[:, sl]), c_.name
                        else:
                            src, skey = (lambda sl, x_=x_: x_[:, 3 + sl.start:3 + sl.stop]), x_.name
                        for kb0 in range(0, NKB, 4):
                            pt = psT[k % 3]; ts_ = tst[k % 3]; k += 1
                            for q4 in range(4):
                                kb = kb0 + q4
                                P.op('pe', lambda e, pt=pt, q4=q4, kb=kb, src=src: e.matmul(
                                    pt[:, q4 * 128:(q4 + 1) * 128], lhsT=src(slice(kb * 128, (kb + 1) * 128)), rhs=ident_f[:],
                                    start=True, stop=True),
                                    reads=[skey, 'ident_f'], writes=[pt.name], inc=(q4 == 3))
                            cast(ts_[:], pt[:], [pt.name], [ts_.name], psum=True)
                            P.dma(gview[:, kb0:kb0 + 4, ci * 128:(ci + 1) * 128],
                                  ts_[:].rearrange("p (a f) -> p a f", f=128), reads=[ts_.name])
                    P.barrier()
                    P.flush()
                with contextlib.ExitStack() as s2:
                    sb2 = lambda name, shape, dt=F32, u=uid(): s2.enter_context(nc.sbuf_tensor(name + u, shape, dt))
                    ps2 = lambda name, shape, dt=F32, u=uid(): s2.enter_context(nc.psum_tensor(name + u, shape, dt))
                    dtb = sb2("d_dtb", [128, 6], F32)
                    nA = sb2("d_nA", [128, 6], F32)
                    nwb = sb2("d_nwb", [128, 64], F32)
                    P.dma(dtb[:], gdn_dtb[l].partition_broadcast(128), writes=['d_dtb'])
                    P.dma(nA[:], gdn_alog[l].partition_broadcast(128), writes=['d_nA'])
                    P.dma(nwb[:], gdn_normrow[l].partition_broadcast(128), writes=['d_nwb'])
                    P.op('act', lambda e: e.activation(out=nA[:], in_=nA[:], func=AF.Exp), reads=['d_nA'], writes=['d_nA'])
                    P.op('dve', lambda e: e.tensor_scalar(out=nA[:], in0=nA[:], scalar1=-1.0, scalar2=None, op0=ALU.mult),
                         reads=['d_nA'], writes=['d_nA'])
                    X = [sb2("d_X%d" % i, [128, NG], F32) for i in range(2)]
                    S = [sb2("d_S%d" % h, [64, 64], F32) for h in range(6)]
                    for h in range(6):
                        P.op('pool', lambda e, h=h: e.memset(S[h][:], 0.0), writes=[S[h].name])
                    gt = sb2("d_gt", [128, 48], F32)
                    gsb = sb2("d_gsb", [128, 16], F32)
                    sqq = sb2("d_sqq", [128, 384], F32)
                    rn = sb2("d_rn", [128, 12], F32)
                    H = lambda name, shape, dt=F32: [sb2("%s%d" % (name, h), shape, dt) for h in range(6)]
                    qn = H("d_qn", [128, 64]); kn = H("d_kn", [128, 64]); qg = H("d_qg", [128, 64])
                    kd = H("d_kd", [128, 64])
                    qnT = H("d_qnT", [64, 128]); knT = H("d_knT", [64, 128]); qgT = H("d_qgT", [64, 128])
                    yf = H("d_yf", [128, 256])
                    MT = H("d_MT", [128, 128]); tA = H("d_tA", [128, 128]); tB = H("d_tB", [128, 128])
                    dgn = H("d_dgn", [128, 128])
                    dec = H("d_dec", [128, 128])
                    tmpk = H("d_tmpk", [128, 128])
                    Rm = [H("d_R%d_" % i, [128, 128]) for i in range(2)]
                    Qm = [H("d_Q%d_" % i, [128, 128]) for i in range(2)]
                    attn = H("d_attn", [128, 128])
                    attnT = H("d_attnT", [128, 128])
                    wT = H("d_wT", [64, 128])
                    vnew = H("d_vnew", [128, 64])
                    o_sb = sb2("d_o", [128, 384], F32)
                    zs = sb2("d_zs", [128, 384], F32)
                    res = sb2("d_res", [128, 384], F32)
                    ot = [sb2("d_ot%d" % i, [128, 3, 128], BF16) for i in range(2)]
                    pA = [ps2("d_pA%d" % i, [128, 128]) for i in range(3)]
                    psW = ps2("d_psW", [128, 512])
                    pB = [ps2("d_pB%d" % i, [128, 256]) for i in range(4)]
                    ia = [0]; ib = [0]

                    def nextA():
                        p_ = pA[ia[0] % 3]; ia[0] += 1
                        return p_

                    def nextB():
                        p_ = pB[ib[0] % 4]; ib[0] += 1
                        return p_

                    def evac(dst, dkey, src, skey):
                        cast(dst, src, [skey], [dkey], psum=True)

                    for n in range(NKB):
                        Xn = X[n % 2]
                        P.dma(Xn[:], gtok[n * 128:(n + 1) * 128, :], writes=[Xn.name])
                        xk = Xn.name
                        A0 = 1536
                        P.op('act', lambda e, Xn=Xn: e.activation(out=gt[:, 0:6], in_=Xn[:, A0 + 6:A0 + 12], func=AF.Sigmoid),
                             reads=[xk], writes=['d_gt'])
                        P.op('dve', lambda e, Xn=Xn: e.tensor_tensor(out=gt[:, 6:12], in0=Xn[:, A0:A0 + 6], in1=dtb[:], op=ALU.add),
                             reads=[xk, 'd_dtb'], writes=['d_gt'])
                        P.op('act', lambda e: e.activation(out=gt[:, 6:12], in_=gt[:, 6:12], func=AF.Exp), reads=['d_gt'], writes=['d_gt'])
                        P.op('act', lambda e: e.activation(out=gt[:, 6:12], in_=gt[:, 6:12], func=AF.Ln, bias=one_t[:]),
                             reads=['d_gt', 'one_t'], writes=['d_gt'])
                        P.op('dve', lambda e: e.tensor_tensor(out=gt[:, 6:12], in0=gt[:, 6:12], in1=nA[:], op=ALU.mult),
                             reads=['d_gt', 'd_nA'], writes=['d_gt'])
                        pg = nextA()
                        P.op('pe', lambda e, pg=pg: e.matmul(pg[:, 0:6], lhsT=Uincl[:], rhs=gt[:, 6:12], start=True, stop=True),
                             reads=['d_gt', 'Uincl'], writes=[pg.name], inc=False)
                        P.op('pe', lambda e, pg=pg: e.matmul(pg[:, 6:12], lhsT=ones_f[:], rhs=gt[:, 6:12], start=True, stop=True),
                             reads=['d_gt', 'ones_f'], writes=[pg.name])
                        P.op('dve', lambda e, pg=pg: e.tensor_copy(out=gsb[:, 0:12], in_=pg[:, 0:12]), reads=[pg.name], writes=['d_gsb'])
                        P.op('act', lambda e: e.activation(out=gt[:, 12:18], in_=gsb[:, 0:6], func=AF.Exp), reads=['d_gsb'], writes=['d_gt'])
                        P.op('dve', lambda e: e.tensor_tensor(out=gt[:, 18:24], in0=gsb[:, 6:12], in1=gsb[:, 0:6], op=ALU.subtract),
                             reads=['d_gsb'], writes=['d_gt'])
                        P.op('act', lambda e: e.activation(out=gt[:, 18:24], in_=gt[:, 18:24], func=AF.Exp), reads=['d_gt'], writes=['d_gt'])
                        P.op('act', lambda e: e.activation(out=gt[:, 24:30], in_=gsb[:, 6:12], func=AF.Exp), reads=['d_gsb'], writes=['d_gt'])
                        P.op('dve', lambda e: e.tensor_tensor(out=gt[:, 30:36], in0=gt[:, 0:6], in1=gt[:, 12:18], op=ALU.mult),
                             reads=['d_gt'], writes=['d_gt'])
                        P.op('dve', lambda e: e.tensor_scalar(out=gt[:, 36:42], in0=gt[:, 0:6], scalar1=-1.0, scalar2=None, op0=ALU.mult),
                             reads=['d_gt'], writes=['d_gt'])
                        for qi, c0 in enumerate((0, 384)):
                            P.op('dve', lambda e, Xn=Xn, c0=c0: e.tensor_tensor(out=sqq[:], in0=Xn[:, c0:c0 + 384], in1=Xn[:, c0:c0 + 384], op=ALU.mult),
                                 reads=[xk], writes=['d_sqq'])
                            P.op('dve', lambda e, qi=qi: e.reduce_sum(out=rn[:, qi * 6:(qi + 1) * 6],
                                                                     in_=sqq[:].rearrange("p (h d) -> p h d", d=64), axis=AX.X),
                                 reads=['d_sqq'], writes=['d_rn'])
                        P.op('act', lambda e: e.activation(out=rn[:], in_=rn[:], func=AF.Ln, bias=eps_t[:]), reads=['d_rn', 'eps_t'], writes=['d_rn'])
                        P.op('act', lambda e: e.activation(out=rn[:], in_=rn[:], func=AF.Exp, scale=-0.5), reads=['d_rn'], writes=['d_rn'])
                        for h in range(6):
                            hs = slice(h * 64, (h + 1) * 64)
                            P.op('dve', lambda e, h=h, hs=hs, Xn=Xn: e.tensor_scalar(
                                out=qn[h][:], in0=Xn[:, hs], scalar1=rn[:, h:h + 1], scalar2=0.125, op0=ALU.mult, op1=ALU.mult),
                                reads=[xk, 'd_rn'], writes=[qn[h].name])
                            P.op('dve', lambda e, h=h, Xn=Xn: e.tensor_scalar(
                                out=kn[h][:], in0=Xn[:, 384 + h * 64:384 + (h + 1) * 64], scalar1=rn[:, 6 + h:7 + h], scalar2=None, op0=ALU.mult),
                                reads=[xk, 'd_rn'], writes=[kn[h].name])
                            P.op('pool', lambda e, h=h: e.tensor_scalar(
                                out=qg[h][:], in0=qn[h][:], scalar1=gt[:, 12 + h:13 + h], scalar2=None, op0=ALU.mult),
                                reads=[qn[h].name, 'd_gt'], writes=[qg[h].name])
                            P.op('pool', lambda e, h=h: e.tensor_scalar(
                                out=kd[h][:], in0=kn[h][:], scalar1=gt[:, 18 + h:19 + h], scalar2=None, op0=ALU.mult),
                                reads=[kn[h].name, 'd_gt'], writes=[kd[h].name])
                            P.op('dve', lambda e, h=h, Xn=Xn: e.tensor_scalar(
                                out=yf[h][:, 0:64], in0=Xn[:, 768 + h * 64:768 + (h + 1) * 64], scalar1=gt[:, h:h + 1], scalar2=None, op0=ALU.mult),
                                reads=[xk, 'd_gt'], writes=[yf[h].name])
                            P.op('dve', lambda e, h=h: e.tensor_scalar(
                                out=yf[h][:, 64:128], in0=kn[h][:], scalar1=gt[:, 30 + h:31 + h], scalar2=None, op0=ALU.mult),
                                reads=[kn[h].name, 'd_gt'], writes=[yf[h].name])
                            P.op('pool', lambda e, h=h: e.tensor_scalar(
                                out=dgn[h][:], in0=ident_f[:], scalar1=gsb[:, h:h + 1], scalar2=-1.0, op0=ALU.mult, op1=ALU.mult),
                                reads=['ident_f', 'd_gsb'], writes=[dgn[h].name])
                        for h in range(6):
                            for (srcT, dstT) in ((qn, qnT), (kn, knT), (qg, qgT)):
                                p_ = nextA()
                                P.op('pe', lambda e, p_=p_, srcT=srcT, h=h: e.matmul(p_[0:64, :], lhsT=srcT[h][:], rhs=ident_f[:], start=True, stop=True),
                                     reads=[srcT[h].name, 'ident_f'], writes=[p_.name])
                                evac(dstT[h][:], dstT[h].name, p_[0:64, :], p_.name)
                        for h in range(6):
                            p_ = nextB()
                            P.op('pe', lambda e, p_=p_, h=h: e.matmul(p_[:, 0:128], lhsT=ones_f[:], rhs=dgn[h][:], start=True, stop=True),
                                 reads=[dgn[h].name, 'ones_f'], writes=[p_.name])
                            P.op('dve', lambda e, p_=p_, h=h: e.tensor_scalar(
                                out=dec[h][:], in0=p_[:, 0:128], scalar1=gsb[:, h:h + 1], scalar2=0.0, op0=ALU.add, op1=ALU.min),
                                reads=[p_.name, 'd_gsb'], writes=[dec[h].name])
                            P.op('act', lambda e, h=h: e.activation(out=dec[h][:], in_=dec[h][:], func=AF.Exp),
                                 reads=[dec[h].name], writes=[dec[h].name])
                            p_ = nextB()
                            P.op('pe', lambda e, p_=p_, h=h: e.matmul(p_[:, 0:128], lhsT=knT[h][:], rhs=knT[h][:], start=True, stop=True),
                                 reads=[knT[h].name], writes=[p_.name])
                            P.op('dve', lambda e, p_=p_, h=h: e.tensor_tensor(out=tmpk[h][:], in0=p_[:, 0:128], in1=dec[h][:], op=ALU.mult),
                                 reads=[p_.name, dec[h].name], writes=[tmpk[h].name])
                            P.op('dve', lambda e, h=h: e.scalar_tensor_tensor(
                                out=Rm[0][h][:], in0=tmpk[h][:], scalar=gt[:, 36 + h:37 + h], in1=LBD[:], op0=ALU.mult, op1=ALU.mult),
                                reads=[tmpk[h].name, 'd_gt', 'LBD'], writes=[Rm[0][h].name])
                            P.op('dve', lambda e, h=h: e.scalar_tensor_tensor(
                                out=yf[h][:, 128:256], in0=tmpk[h][:], scalar=gt[:, 36 + h:37 + h], in1=LOD[:], op0=ALU.mult, op1=ALU.mult),
                                reads=[tmpk[h].name, 'd_gt', 'LOD'], writes=[yf[h].name])
                            p_ = nextB()
                            P.op('pe', lambda e, p_=p_, h=h: e.matmul(p_[:, 0:128], lhsT=qnT[h][:], rhs=knT[h][:], start=True, stop=True),
                                 reads=[qnT[h].name, knT[h].name], writes=[p_.name])
                            P.op('dve', lambda e, p_=p_, h=h: e.tensor_tensor(out=tmpk[h][:], in0=p_[:, 0:128], in1=dec[h][:], op=ALU.mult),
                                 reads=[p_.name, dec[h].name], writes=[tmpk[h].name])
                            P.op('pool', lambda e, h=h: e.tensor_tensor(out=attn[h][:], in0=tmpk[h][:], in1=Lincl[:], op=ALU.mult),
                                 reads=[tmpk[h].name, 'Lincl'], writes=[attn[h].name])
                            for (srcM, dstM) in ((Rm[0], Qm[0]), (attn, attnT)):
                                p_ = nextA()
                                P.op('pe', lambda e, p_=p_, srcM=srcM, h=h: e.matmul(p_[:, 0:128], lhsT=srcM[h][:], rhs=ident_f[:], start=True, stop=True),
                                     reads=[srcM[h].name, 'ident_f'], writes=[p_.name])
                                evac(dstM[h][:], dstM[h].name, p_[:, 0:128], p_.name)
                        for j in range(5):
                            cur, nxt = j % 2, (j + 1) % 2
                            for h in range(6):
                                p_ = nextB()
                                P.op('pe', lambda e, p_=p_, h=h, cur=cur: e.matmul(p_[:], lhsT=Qm[cur][h][:], rhs=yf[h][:], start=True, stop=True),
                                     reads=[Qm[cur][h].name, yf[h].name], writes=[p_.name])
                                P.op('dve', lambda e, p_=p_, h=h: e.tensor_tensor(out=yf[h][:], in0=yf[h][:], in1=p_[:], op=ALU.add),
                                     reads=[p_.name, yf[h].name], writes=[yf[h].name])
                                if j < 4:
                                    p_ = nextA()
                                    P.op('pe', lambda e, p_=p_, h=h, cur=cur: e.matmul(p_[:], lhsT=Qm[cur][h][:], rhs=Rm[cur][h][:], start=True, stop=True),
                                         reads=[Qm[cur][h].name, Rm[cur][h].name], writes=[p_.name])
                                    evac(Rm[nxt][h][:], Rm[nxt][h].name, p_[:], p_.name)
                                    p_ = nextA()
                                    P.op('pe', lambda e, p_=p_, h=h, cur=cur: e.matmul(p_[:], lhsT=Rm[cur][h][:], rhs=Qm[cur][h][:], start=True, stop=True),
                                         reads=[Qm[cur][h].name, Rm[cur][h].name], writes=[p_.name])
                                    evac(Qm[nxt][h][:], Qm[nxt][h].name, p_[:], p_.name)
                        for h in range(6):
                            p_ = nextA()
                            P.op('pe', lambda e, p_=p_, h=h: e.matmul(p_[:], lhsT=yf[h][:, 128:256], rhs=ident_f[:], start=True, stop=True),
                                 reads=[yf[h].name, 'ident_f'], writes=[p_.name])
                            evac(MT[h][:], MT[h].name, p_[:], p_.name)
                        for it3, (src3, dst3) in enumerate(((None, tA), (tA, tB), (tB, tA))):
                            for h in range(6):
                                p_ = nextA()
                                rhs_ap = yf[h][:, 0:128] if src3 is None else src3[h][:]
                                rkey = yf[h].name if src3 is None else src3[h].name
                                P.op('pe', lambda e, p_=p_, h=h, rhs_ap=rhs_ap: e.matmul(p_[:], lhsT=MT[h][:], rhs=rhs_ap, start=True, stop=True),
                                     reads=[MT[h].name, rkey], writes=[p_.name])
                                P.op('dve', lambda e, p_=p_, h=h, dst3=dst3: e.tensor_tensor(out=dst3[h][:], in0=yf[h][:, 0:128], in1=p_[:], op=ALU.add),
                                     reads=[p_.name, yf[h].name], writes=[dst3[h].name])
                        for h in range(6):
                            p_ = nextA()
                            P.op('pe', lambda e, p_=p_, h=h: e.matmul(p_[0:64, :], lhsT=tA[h][:, 64:128], rhs=ident_f[:], start=True, stop=True),
                                 reads=[tA[h].name, 'ident_f'], writes=[p_.name])
                            evac(wT[h][:], wT[h].name, p_[0:64, :], p_.name)
                        for h in range(6):
                            p1 = nextB()
                            P.op('pe', lambda e, p1=p1, h=h: e.matmul(p1[:, 0:64], lhsT=wT[h][:], rhs=S[h][:], start=True, stop=True),
                                 reads=[wT[h].name, S[h].name], writes=[p1.name])
                            P.op('dve', lambda e, p1=p1, h=h: e.tensor_tensor(out=vnew[h][:], in0=tA[h][:, 0:64], in1=p1[:, 0:64], op=ALU.subtract),
                                 reads=[p1.name, tA[h].name], writes=[vnew[h].name])
                            p2 = nextB()
                            P.op('pe', lambda e, p2=p2, h=h: e.matmul(p2[:, 0:64], lhsT=qgT[h][:], rhs=S[h][:], start=True, stop=False),
                                 reads=[qgT[h].name, S[h].name], writes=[p2.name], inc=False)
                            P.op('pe', lambda e, p2=p2, h=h: e.matmul(p2[:, 0:64], lhsT=attnT[h][:], rhs=vnew[h][:], start=False, stop=True),
                                 reads=[attnT[h].name, vnew[h].name], writes=[p2.name])
                            P.op('act', lambda e, p2=p2, h=h: e.copy(out=o_sb[:, h * 64:(h + 1) * 64], in_=p2[:, 0:64]),
                                 reads=[p2.name], writes=[('d_o', h)])
                            p3 = nextA()
                            P.op('pe', lambda e, p3=p3, h=h: e.matmul(p3[0:64, 0:64], lhsT=kd[h][:], rhs=vnew[h][:], start=True, stop=True),
                                 reads=[kd[h].name, vnew[h].name], writes=[p3.name])
                            P.op('dve', lambda e, p3=p3, h=h: e.scalar_tensor_tensor(
                                out=S[h][:], in0=S[h][:], scalar=gt[0:64, 24 + h:25 + h], in1=p3[0:64, 0:64], op0=ALU.mult, op1=ALU.add),
                                reads=[p3.name, S[h].name, 'd_gt'], writes=[S[h].name])
                        okeys = [('d_o', h) for h in range(6)]
                        P.op('dve', lambda e: e.tensor_tensor(out=sqq[:], in0=o_sb[:], in1=o_sb[:], op=ALU.mult), reads=okeys, writes=['d_sqq'])
                        P.op('dve', lambda e: e.reduce_sum(out=rn[:, 0:6], in_=sqq[:].rearrange("p (h d) -> p h d", d=64), axis=AX.X),
                             reads=['d_sqq'], writes=['d_rn'])
                        P.op('act', lambda e: e.activation(out=rn[:, 0:6], in_=rn[:, 0:6], func=AF.Ln, bias=eps_t[:], scale=1.0 / 64),
                             reads=['d_rn', 'eps_t'], writes=['d_rn'])
                        P.op('act', lambda e: e.activation(out=rn[:, 0:6], in_=rn[:, 0:6], func=AF.Exp, scale=-0.5), reads=['d_rn'], writes=['d_rn'])
                        P.op('act', lambda e, Xn=Xn: e.activation(out=zs[:], in_=Xn[:, 1152:1536], func=AF.Silu), reads=[xk], writes=['d_zs'])
                        for h in range(6):
                            hs = slice(h * 64, (h + 1) * 64)
                            P.op('dve', lambda e, h=h, hs=hs: e.scalar_tensor_tensor(
                                out=res[:, hs], in0=o_sb[:, hs], scalar=rn[:, h:h + 1], in1=nwb[:], op0=ALU.mult, op1=ALU.mult),
                                reads=okeys + ['d_rn', 'd_nwb'], writes=[('d_res', h)])
                            P.op('pool', lambda e, hs=hs: e.tensor_tensor(out=res[:, hs], in0=res[:, hs], in1=zs[:, hs], op=ALU.mult),
                                 reads=[('d_res', h), 'd_zs'], writes=[('d_res', h)])
                        rkeys = [('d_res', h) for h in range(6)]
                        oo = ot[n % 2]
                        for c3 in range(3):
                            p_ = nextA()
                            P.op('pe', lambda e, p_=p_, c3=c3: e.matmul(p_[:], lhsT=res[:, c3 * 128:(c3 + 1) * 128], rhs=ident_f[:], start=True, stop=True),
                                 reads=rkeys + ['ident_f'], writes=[p_.name])
                            evac(oo[:, c3, :], oo.name, p_[:], p_.name)
                        P.dma(mixT[0:384, n * 128:(n + 1) * 128].rearrange("(c p) t -> p c t", p=128), oo[:], reads=[oo.name])
                    P.barrier()
                    P.flush()


            if 'E' in stages:
              with contextlib.ExitStack() as st:
                sbt = lambda name, shape, dt=F32, u=uid(): st.enter_context(nc.sbuf_tensor(name + u, shape, dt))
                pst = lambda name, shape, dt=F32, u=uid(): st.enter_context(nc.psum_tensor(name + u, shape, dt))
                wob = sbt("e_w", [128, 8, D_MODEL], BF16)
                wst = [sbt("e_wst%d" % i, [128, D_MODEL], F32) for i in range(2)]
                for c in range(8):
                    s = wst[c % 2]
                    P.dma(s[:], w_out[l, c * 128:(c + 1) * 128, :], writes=[s.name])
                    cast(wob[:, c, :], s[:], [s.name], ['e_w'])
                h_sb = [sbt("e_h%d" % i, [128, 8, 512], F32) for i in range(2)]
                mx_sb = [sbt("e_mx%d" % i, [128, 8, 512], BF16) for i in range(2)]
                ps_o = [pst("e_ps%d" % i, [128, 512]) for i in range(4)]
                for tt in range(NT):
                    tsl = slice(tt * 512, (tt + 1) * 512)
                    h = h_sb[tt % 2]
                    mx = mx_sb[tt % 2]
                    P.dma(h[:], pc(hsrc)[:, :, tsl], writes=[h.name])
                    P.dma(mx[:], pc(mixT)[:, :, tsl], writes=[mx.name])
                    for m in range(8):
                        ps = ps_o[m % 4]
                        for c in range(8):
                            P.op('pe', lambda e, c=c, m=m, ps=ps, mx=mx: e.matmul(
                                ps[:], lhsT=wob[:, c, m * 128:(m + 1) * 128], rhs=mx[:, c, :],
                                start=(c == 0), stop=(c == 7)),
                                reads=[mx.name, 'e_w'], writes=[ps.name], inc=(c == 7))
                        P.op('dve', lambda e, m=m, ps=ps, h=h: e.tensor_tensor(
                            out=h[:, m, :], in0=h[:, m, :], in1=ps[:], op=ALU.add),
                            reads=[ps.name, h.name], writes=[h.name])
                    P.dma(pc(hT)[:, :, tsl], h[:], reads=[h.name])
                P.barrier()
                P.flush()

            if 'F' in stages:
              for half in range(2):
                with contextlib.ExitStack() as st:
                    sbt = lambda name, shape, dt=F32, u=uid(): st.enter_context(nc.sbuf_tensor(name + u, shape, dt))
                    pst = lambda name, shape, dt=F32, u=uid(): st.enter_context(nc.psum_tensor(name + u, shape, dt))
                    HF = D_FF // 2
                    wup = sbt("f_wup", [128, 8, 2 * HF], BF16)
                    wdn = sbt("f_wdn", [128, 11, D_MODEL], BF16)
                    wst = [sbt("f_wst%d" % i, [128, HF], F32) for i in range(2)]
                    gain = sbt("f_gain", [128, 8], F32)
                    cw = sbt("f_cw", [128, 44, 3], F32)
                    halo = sbt("f_halo", [128, 22, 2], F32)
                    P.dma(gain[:], ln_ffn[l], writes=[gain.name])
                    P.dma(cw[:], ffn_conv[l], writes=['f_cw'])
                    P.op('pool', lambda e: e.memset(halo[:], 0.0), writes=['f_halo'])
                    k = 0
                    for c in range(8):
                        for which in range(2):
                            s = wst[k % 2]
                            k += 1
                            c0 = which * D_FF + half * HF
                            P.dma(s[:], w_up[l, c * 128:(c + 1) * 128, c0:c0 + HF], writes=[s.name])
                            cast(wup[:, c, which * HF:(which + 1) * HF], s[:], [s.name], ['f_wup'])
                    for j in range(11):
                        s = wst[k % 2]
                        k += 1
                        r0 = (half * 11 + j) * 128
                        P.dma(s[:, 0:D_MODEL], w_down[l, r0:r0 + 128, :], writes=[s.name])
                        cast(wdn[:, j, :], s[:, 0:D_MODEL], [s.name], ['f_wdn'])
                    h_sb = sbt("f_h", [128, 8, 512], F32)
                    sq = sbt("f_sq", [128, 8, 512], BF16)
                    xn = sbt("f_xn", [128, 8, 512], BF16)
                    rstd = sbt("f_rstd", [128, 512], F32)
                    actv = sbt("f_act", [128, 11, 512], BF16)
                    u_sb = [sbt("f_u%d" % i, [128, 514], F32) for i in range(4)]
                    cv_sb = [sbt("f_cv%d" % i, [128, 512], F32) for i in range(4)]
                    ps_n = pst("f_psn", [128, 512])
                    ps_o = [pst("f_ps%d" % i, [128, 512]) for i in range(4)]
                    ucnt = 0
                    for tt in range(NT):
                        tsl = slice(tt * 512, (tt + 1) * 512)
                        h = h_sb
                        P.dma(h[:], pc(hT)[:, :, tsl], writes=[h.name])
                        emit_norm(h, gain, sq, ps_n, rstd, xn)
                        hacc = h
                        if half == 1:
                            P.dma(h[:], pc(hT2)[:, :, tsl], writes=[h.name])
                        for j in range(11):
                            cvs = []
                            for which in range(2):
                                ps = ps_o[ucnt % 4]
                                u = u_sb[ucnt % 4]
                                cv = cv_sb[ucnt % 4]
                                ucnt += 1
                                hidx = which * 11 + j
                                fidx = which * 22 + half * 11 + j
                                for c in range(8):
                                    P.op('pe', lambda e, c=c, ps=ps, which=which, j=j: e.matmul(
                                        ps[:], lhsT=wup[:, c, which * HF + j * 128: which * HF + (j + 1) * 128],
                                        rhs=xn[:, c, :], start=(c == 0), stop=(c == 7)),
                                        reads=[xn.name, 'f_wup'], writes=[ps.name], inc=(c == 7))
                                P.op('act', lambda e, ps=ps, u=u: e.copy(out=u[:, 2:514], in_=ps[:]),
                                     reads=[ps.name], writes=[u.name])
                                P.op('pool', lambda e, u=u, hidx=hidx: e.tensor_copy(out=u[:, 0:2], in_=halo[:, hidx, :]),
                                     reads=[('f_halo', hidx)], writes=[u.name])
                                P.op('dve', lambda e, u=u, cv=cv, fidx=fidx: e.tensor_scalar(
                                    out=cv[:], in0=u[:, 0:512], scalar1=cw[:, fidx, 0:1], scalar2=None, op0=ALU.mult),
                                    reads=[u.name, 'f_cw'], writes=[cv.name])
                                for tap in (1, 2):
                                    P.op('dve', lambda e, u=u, cv=cv, fidx=fidx, tap=tap: e.scalar_tensor_tensor(
                                        out=cv[:], in0=u[:, tap:tap + 512], scalar=cw[:, fidx, tap:tap + 1], in1=cv[:],
                                        op0=ALU.mult, op1=ALU.add),
                                        reads=[u.name, 'f_cw', cv.name], writes=[cv.name])
                                P.op('pool', lambda e, u=u, hidx=hidx: e.tensor_copy(out=halo[:, hidx, :], in_=u[:, 512:514]),
                                     reads=[u.name], writes=[('f_halo', hidx)])
                                cvs.append(cv)
                            cg, cu = cvs
                            P.op('act', lambda e, cg=cg: e.activation(out=cg[:], in_=cg[:], func=AF.Silu),
                                 reads=[cg.name], writes=[cg.name])
                            P.op('dve', lambda e, cg=cg, cu=cu, j=j: e.tensor_tensor(
                                out=actv[:, j, :], in0=cg[:], in1=cu[:], op=ALU.mult),
                                reads=[cg.name, cu.name], writes=[('f_act', j)])
                        akeys = [('f_act', j) for j in range(11)]
                        for m in range(8):
                            ps = ps_o[m % 4]
                            for j in range(11):
                                P.op('pe', lambda e, j=j, m=m, ps=ps: e.matmul(
                                    ps[:], lhsT=wdn[:, j, m * 128:(m + 1) * 128], rhs=actv[:, j, :],
                                    start=(j == 0), stop=(j == 10)),
                                    reads=akeys + ['f_wdn'], writes=[ps.name], inc=(j == 10))
                            P.op('dve', lambda e, m=m, ps=ps, hacc=hacc: e.tensor_tensor(
                                out=hacc[:, m, :], in0=hacc[:, m, :], in1=ps[:], op=ALU.add),
                                reads=[ps.name, hacc.name], writes=[hacc.name])
                        dst = hT2 if half == 0 else hT
                        P.dma(pc(dst)[:, :, tsl], hacc[:], reads=[hacc.name])
                    P.barrier()
                    P.flush()

            if 'G' in stages:
              with contextlib.ExitStack() as st:
                sbt = lambda name, shape, dt=F32, u=uid(): st.enter_context(nc.sbuf_tensor(name + u, shape, dt))
                pst = lambda name, shape, dt=F32, u=uid(): st.enter_context(nc.psum_tensor(name + u, shape, dt))
                last = (l == depth - 1)
                wg = sbt("g_wg", [128, 8, D_MODEL], BF16)
                wp = sbt("g_wp", [128, 2, D_MODEL], BF16)
                wst = [sbt("g_wst%d" % i, [128, D_MODEL], F32) for i in range(2)]
                gain = sbt("g_gain", [128, 8], F32)
                gpn = sbt("g_gpn", [128, 8], F32)
                gfin = sbt("g_gfin", [128, 8], F32)
                P.dma(gain[:], ln_ple[l], writes=[gain.name])
                P.dma(gpn[:], ple_norm[l], writes=[gpn.name])
                P.dma(gfin[:], ln_final, writes=[gfin.name])
                for c in range(8):
                    s = wst[c % 2]
                    P.dma(s[:], w_gate[l, c * 128:(c + 1) * 128, :], writes=[s.name])
                    cast(wg[:, c, :], s[:], [s.name], ['g_wg'])
                for c in range(2):
                    s = wst[c % 2]
                    P.dma(s[:], w_ple[l, c * 128:(c + 1) * 128, :], writes=[s.name])
                    cast(wp[:, c, :], s[:], [s.name], ['g_wp'])
                h_r = [sbt("g_h%d" % i, [128, 8, 512], F32) for i in range(2)]
                sq = sbt("g_sq", [128, 8, 512], BF16)
                xn_r = [sbt("g_xn%d" % i, [128, 8, 512], BF16) for i in range(2)]
                rstd = sbt("g_rstd", [128, 512], F32)
                p32_r = [sbt("g_p32", [128, 2, 512], F32)] * 2
                pbf_r = [sbt("g_pbf", [128, 2, 512], BF16)] * 2
                y_r = [sbt("g_y", [128, 8, 512], F32)] * 2
                gt_r = [sbt("g_gt%d" % i, [128, 8, 512], F32) for i in range(2)]
                ps_n = pst("g_psn", [128, 512])
                ps_o = [pst("g_ps%d" % i, [128, 512]) for i in range(4)]
                for tt in range(NT):
                    tsl = slice(tt * 512, (tt + 1) * 512)
                    h, xn, p32, pbf, y, gt = h_r[tt % 2], xn_r[tt % 2], p32_r[tt % 2], pbf_r[tt % 2], y_r[tt % 2], gt_r[tt % 2]
                    P.dma(h[:], pc(hT)[:, :, tsl], writes=[h.name])
                    P.dma(p32[:], pc(pT[l])[:, :, tsl], writes=[p32.name])
                    cast(pbf[:], p32[:], [p32.name], [pbf.name])
                    emit_norm(h, gain, sq, ps_n, rstd, xn)
                    for m in range(8):
                        ps = ps_o[m % 4]
                        for c in range(8):
                            P.op('pe', lambda e, c=c, m=m, ps=ps, h=h, xn=xn, y=y, gt=gt, pbf=pbf, p32=p32: e.matmul(
                                ps[:], lhsT=wg[:, c, m * 128:(m + 1) * 128], rhs=xn[:, c, :],
                                start=(c == 0), stop=(c == 7)),
                                reads=[xn.name, 'g_wg'], writes=[ps.name], inc=(c == 7))
                        P.op('act', lambda e, m=m, ps=ps, h=h, xn=xn, y=y, gt=gt, pbf=pbf, p32=p32: e.activation(out=gt[:, m, :], in_=ps[:], func=AF.Sigmoid),
                             reads=[ps.name], writes=[gt.name])
                    for m in range(8):
                        ps = ps_o[m % 4]
                        for c in range(2):
                            P.op('pe', lambda e, c=c, m=m, ps=ps, h=h, xn=xn, y=y, gt=gt, pbf=pbf, p32=p32: e.matmul(
                                ps[:], lhsT=wp[:, c, m * 128:(m + 1) * 128], rhs=pbf[:, c, :],
                                start=(c == 0), stop=(c == 1)),
                                reads=[pbf.name, 'g_wp'], writes=[ps.name], inc=(c == 1))
                        cast(y[:, m, :], ps[:], [ps.name], [y.name], psum=True)
                    emit_norm(y, gpn, sq, ps_n, rstd, y)
                    for m in range(8):
                        P.op('dve', lambda e, m=m, h=h, xn=xn, y=y, gt=gt, pbf=pbf, p32=p32: e.tensor_tensor(
                            out=gt[:, m, :], in0=gt[:, m, :], in1=y[:, m, :], op=ALU.mult),
                            reads=[gt.name, y.name], writes=[gt.name])
                        P.op('pool', lambda e, m=m, h=h, xn=xn, y=y, gt=gt, pbf=pbf, p32=p32: e.tensor_tensor(
                            out=h[:, m, :], in0=h[:, m, :], in1=gt[:, m, :], op=ALU.add),
                            reads=[gt.name, h.name], writes=[h.name])
                    if last:
                        emit_norm(h, gfin, sq, ps_n, rstd, y)
                        P.dma(pc(outT)[:, :, tsl], y[:], reads=[y.name])
                    else:
                        P.dma(pc(hT)[:, :, tsl], h[:], reads=[h.name])
                P.barrier()
                P.flush()

        P.barrier()
        P.flush()
    return nc


def _col_perm():
    r = lambda a, b: list(range(a, b))
    nq = [c for h in (0, 3, 1, 4, 2, 5) for c in r(1548 + h * 64, 1548 + (h + 1) * 64)]
    fm = r(0, 1536) + nq + r(1932, 2060) + r(2060, 2188) + r(2188, 2316) + r(2444, 2572) \
        + r(2718, 2974) + r(2974, 3230)
    small = r(1536, 1548) + r(2700, 2718)
    tm = r(2316, 2444) + r(2572, 2700) + r(3230, 3486)
    return fm, small, tm


def prep_w_in(w_in):
    fm, small, tm = _col_perm()
    d = w_in.shape[0]
    out = np.zeros((d, D_MODEL, N_IN_PAD), np.float32)
    out[:, :, 0:len(fm)] = w_in[:, :, fm]
    out[:, :, 2944:2944 + len(small)] = w_in[:, :, small]
    out[:, :, 3072:3584] = w_in[:, :, tm]
    return out


def vec_pc(v):
    sh = v.shape
    c = sh[-1] // 128
    return np.ascontiguousarray(np.swapaxes(v.reshape(sh[:-1] + (c, 128)), -1, -2))


def prep_conv(w):
    d, k, n = w.shape
    return np.ascontiguousarray(w.transpose(0, 2, 1).reshape(d, n // 128, 128, k).transpose(0, 2, 1, 3))


def dup128(v):
    return np.ascontiguousarray(np.concatenate([v, v], axis=-1)[..., None])


def prep_w1(w):
    d = w.shape[0]
    a = w.reshape(d, 32, 64, 128).transpose(0, 2, 1, 3)
    return np.ascontiguousarray(np.concatenate([a, a], axis=1))


def prep_pe(pe):
    a = pe.transpose(0, 2, 1)
    return np.ascontiguousarray(np.concatenate([a, a], axis=1))


STAGES = "ABCDEFG"


def kernel(**inputs):
    f32 = lambda a: np.ascontiguousarray(np.asarray(a, dtype=np.float32))
    x = f32(inputs['x'])
    B, T, _ = x.shape
    depth = int(np.asarray(inputs['w_in']).shape[0])
    p = f32(inputs['p'])
    shared = dict(
        ln_mix=vec_pc(f32(inputs['ln_mix'])), ln_ffn=vec_pc(f32(inputs['ln_ffn'])),
        ln_ple=vec_pc(f32(inputs['ln_ple'])), ple_norm=vec_pc(f32(inputs['ple_norm'])),
        ln_final=vec_pc(f32(inputs['ln_final'])),
        w_in=prep_w_in(f32(inputs['w_in'])), w_out=f32(inputs['w_out']), w_up=f32(inputs['w_up']),
        ffn_conv=prep_conv(f32(inputs['ffn_conv'])), w_down=f32(inputs['w_down']),
        w_gate=f32(inputs['w_ple_gate']), w_ple=f32(inputs['w_ple']),
        sb_norm=dup128(f32(inputs['sb_norm'])), nsa_norm=dup128(f32(inputs['nsa_norm'])),
        gdn_norm=dup128(f32(inputs['gdn_norm'])),
        gdn_conv=prep_conv(f32(inputs['gdn_conv'])), gdn_alog=f32(inputs['gdn_a_log'])[:, None, :],
        gdn_dtb=f32(inputs['gdn_dt_bias'])[:, None, :], gdn_normrow=f32(inputs['gdn_norm'])[:, None, :],
        cmp_k_w1=prep_w1(f32(inputs['nsa_cmp_k_w1'])), cmp_v_w1=prep_w1(f32(inputs['nsa_cmp_v_w1'])),
        cmp_k_w2=f32(inputs['nsa_cmp_k_w2']), cmp_v_w2=f32(inputs['nsa_cmp_v_w2']),
        pe_kT=prep_pe(f32(inputs['nsa_pe_k'])), pe_vT=prep_pe(f32(inputs['nsa_pe_v'])),
    )
    in_maps = []
    for core in range(8):
        b = core % B
        m = dict(shared)
        m['xT'] = np.ascontiguousarray(x[b].T)
        m['pT'] = np.ascontiguousarray(p[:, b].transpose(0, 2, 1))
        m['pos'] = np.ascontiguousarray(np.asarray(inputs['positions'])[b:b + 1].astype(np.int32))
        in_maps.append(m)
    nc = build(T, depth, stages=STAGES)
    res = run_bass_kernel_spmd(nc, in_maps, core_ids=list(range(8)))
    out = np.stack([np.ascontiguousarray(np.asarray(res.results[b]['outT']).T) for b in range(B)], axis=0)
    return out.astype(np.float32)
```

```python
import contextlib
import numpy as np
import concourse.bass as bass
import concourse.mybir as mybir
from concourse.bass_utils import run_bass_kernel_spmd

F32 = mybir.dt.float32
BF16 = mybir.dt.bfloat16
I32 = mybir.dt.int32
AF = mybir.ActivationFunctionType
ALU = mybir.AluOpType
AX = mybir.AxisListType

D_MODEL = 1024
HD = 64
N_IN_PAD = 3584
D_FF = 2816
EPS = 1e-6
NDS = 24
CPARTS = {'cmp', 'topk', 'sel', 'win'}


class Prog:
    def __init__(self, nc, stack):
        self.nc = nc
        self.stack = stack
        self.cengs = ['pe', 'act', 'dve', 'pool']
        self.engs = self.cengs + ['sp']
        self.sem = {e: stack.enter_context(nc.semaphore("s_" + e)) for e in self.cengs}
        self.dsem = [stack.enter_context(nc.semaphore("d%d" % i)) for i in range(NDS)]
        self.dcount = [0] * NDS
        self.dnext = 0
        self.nseq = {e: 0 for e in self.cengs}
        self.known = {e: {} for e in self.engs}
        self.lastw = {}
        self.readers = {}
        self.q = {e: [] for e in self.engs}
        self.pending_noinc = {e: False for e in self.cengs}

    def _semof(self, sk):
        if isinstance(sk, tuple):
            return self.dsem[sk[1]]
        return self.sem[sk]

    def _collect(self, eng, reads, writes):
        deps = {}
        def add(ev):
            if ev is None:
                return
            sk, val = ev
            if deps.get(sk, 0) < val:
                deps[sk] = val
        for k in reads:
            add(self.lastw.get(k))
        for k in writes:
            add(self.lastw.get(k))
            for ev in self.readers.get(k, ()):
                add(ev)
        waits = []
        for sk, val in deps.items():
            if eng == 'pe' and sk == 'pe':
                continue
            if self.known[eng].get(sk, 0) < val:
                self.known[eng][sk] = val
                waits.append((sk, val))
        return waits

    def _record(self, ev, reads, writes):
        for k in reads:
            self.readers.setdefault(k, []).append(ev)
        for k in writes:
            self.lastw[k] = ev
            self.readers[k] = []

    def op(self, eng, fn, reads=(), writes=(), inc=True):
        waits = self._collect(eng, reads, writes)
        if inc:
            self.nseq[eng] += 1
            ev = (eng, self.nseq[eng])
            self.pending_noinc[eng] = False
        else:
            ev = (eng, self.nseq[eng] + 1)
            self.pending_noinc[eng] = True
        self.q[eng].append((waits, fn, eng if inc else None))
        self._record(ev, reads, writes)

    def dma(self, out, in_, reads=(), writes=(), queue='sp', **kw):
        waits = self._collect(queue, reads, writes)
        j = self.dnext
        self.dnext = (j + 1) % NDS
        prev = self.dcount[j]
        sk = ('d', j)
        if prev > 0 and self.known[queue].get(sk, 0) < prev:
            self.known[queue][sk] = prev
            waits.append((sk, prev))
        self.dcount[j] += 16
        ev = (sk, self.dcount[j])
        self.q[queue].append((waits, lambda e: e.dma_start(out=out, in_=in_, **kw), sk))
        self._record(ev, reads, writes)

    def barrier(self):
        for e in self.engs:
            waits = []
            for f in self.cengs:
                if f == e:
                    continue
                v = self.nseq[f]
                if v > 0 and self.known[e].get(f, 0) < v:
                    self.known[e][f] = v
                    waits.append((f, v))
            for j in range(NDS):
                v = self.dcount[j]
                sk = ('d', j)
                if v > 0 and self.known[e].get(sk, 0) < v:
                    self.known[e][sk] = v
                    waits.append((sk, v))
            if waits:
                self.q[e].append((waits, None, None))
        self.lastw = {}
        self.readers = {}

    def flush(self):
        nc = self.nc
        for e in self.cengs:
            assert not self.pending_noinc[e], e
        q = self.q
        self.q = {e: [] for e in self.engs}

        def emit(name, e):
            for waits, fn, inc in q[name]:
                for sk, val in waits:
                    e.wait_ge(self._semof(sk), val)
                if fn is None:
                    continue
                ins = fn(e)
                if inc is not None:
                    if isinstance(inc, tuple):
                        ins.then_inc(self.dsem[inc[1]], 16)
                    else:
                        ins.then_inc(self.sem[inc], 1)

        with nc.Block() as block:
            @block.tensor
            def _(e):
                emit('pe', e)

            @block.scalar
            def _(e):
                emit('act', e)

            @block.vector
            def _(e):
                emit('dve', e)

            @block.gpsimd
            def _(e):
                emit('pool', e)

            @block.sync
            def _(e):
                emit('sp', e)


class Ring:
    def __init__(self, items):
        self.items = items
        self.i = 0

    def next(self):
        it = self.items[self.i % len(self.items)]
        self.i += 1
        return it


def build(T, depth, debug=(), stages="ABCDEFG", mix_input=False, ext=()):
    nc = bass.Bass("TRN2", target_bir_lowering=False)
    NT = T // 512
    din = lambda name, shape, dt=F32: nc.dram_tensor(name, shape, dt, kind="ExternalInput").ap()
    dscr = lambda name, shape, dt=F32: nc.dram_tensor(
        name, shape, dt, kind=("ExternalOutput" if name in debug else "ExternalInput" if name in ext else "Internal")).ap()

    xT = din("xT", [D_MODEL, T])
    pT = din("pT", [depth, 256, T])
    ln_mix = din("ln_mix", [depth, 128, 8])
    ln_ffn = din("ln_ffn", [depth, 128, 8])
    ln_ple = din("ln_ple", [depth, 128, 8])
    ple_norm = din("ple_norm", [depth, 128, 8])
    ln_final = din("ln_final", [128, 8])
    w_in = din("w_in", [depth, D_MODEL, N_IN_PAD])
    sb_norm = din("sb_norm", [depth, 128, 1])
    nsa_norm = din("nsa_norm", [depth, 128, 1])
    gdn_norm = din("gdn_norm", [depth, 128, 1])
    pos = din("pos", [1, T], I32)
    cmp_k_w1 = din("cmp_k_w1", [depth, 128, 32, 128])
    cmp_v_w1 = din("cmp_v_w1", [depth, 128, 32, 128])
    cmp_k_w2 = din("cmp_k_w2", [depth, 128, 64])
    cmp_v_w2 = din("cmp_v_w2", [depth, 128, 64])
    pe_kT = din("pe_kT", [depth, 128, 32])
    pe_vT = din("pe_vT", [depth, 128, 32])
    gdn_conv = din("gdn_conv", [depth, 128, 9, 4])
    gdn_alog = din("gdn_alog", [depth, 1, 6])
    gdn_dtb = din("gdn_dtb", [depth, 1, 6])
    gdn_normrow = din("gdn_normrow", [depth, 1, 64])
    gtok = dscr("gtok", [T, 13 * 128])
    w_out = din("w_out", [depth, D_MODEL, D_MODEL])
    w_up = din("w_up", [depth, D_MODEL, 2 * D_FF])
    ffn_conv = din("ffn_conv", [depth, 128, 44, 3])
    w_down = din("w_down", [depth, D_FF, D_MODEL])
    w_gate = din("w_gate", [depth, D_MODEL, D_MODEL])
    w_ple = din("w_ple", [depth, 256, D_MODEL])
    outT = nc.dram_tensor("outT", [D_MODEL, T], F32, kind="ExternalOutput").ap()
    projF = dscr("projF", [24 * 128, T])
    projT = dscr("projT", [T, 512])
    hT = dscr("hT", [D_MODEL, T])
    hT2 = dscr("hT2", [D_MODEL, T])
    if mix_input:
        mixT = din("mixT", [D_MODEL, T], BF16)
    else:
        mixT = dscr("mixT", [D_MODEL, T], BF16)
    pc = lambda ap: ap.rearrange("(c p) t -> p c t", p=128)

    with contextlib.ExitStack() as stack:
        P = Prog(nc, stack)
        sb = lambda name, shape, dt=F32: stack.enter_context(nc.sbuf_tensor(name, shape, dt))

        ones_bf = sb("ones_bf", [128, 128], BF16)
        P.op('pool', lambda e: e.memset(ones_bf[:], 1.0), writes=['ones_bf'])
        eps_t = sb("eps_t", [128, 1], F32)
        P.op('pool', lambda e: e.memset(eps_t[:], EPS), writes=['eps_t'])

        one_t = sb("one_t", [128, 1], F32)
        P.op('pool', lambda e: e.memset(one_t[:], 1.0), writes=['one_t'])
        ones512 = sb("ones512", [128, 512], BF16)
        P.op('pool', lambda e: e.memset(ones512[:], 1.0), writes=['ones512'])
        negOnes = sb("negOnes", [128, 128], BF16)
        P.op('pool', lambda e: e.memset(negOnes[:], -1.0), writes=['negOnes'])
        negU = sb("negU", [128, 128], BF16)
        P.op('pool', lambda e: e.affine_select(out=negU[:], in_=negOnes[:], pattern=[[-1, 128]],
                                               compare_op=ALU.is_ge, fill=0.0, base=0, channel_multiplier=1),
             reads=['negOnes'], writes=['negU'])
        dmask = sb("dmask", [128, 4, 512], BF16)
        for j in range(4):
            P.op('pool', lambda e, j=j: e.affine_select(out=dmask[:, j, :], in_=ones512[:], pattern=[[1, 512]],
                                                        compare_op=ALU.is_gt, fill=0.0, base=-128 * j,
                                                        channel_multiplier=-1),
                 reads=['ones512'], writes=['dmask'])
        cast_rr = [0]
        uidc = [0]

        def uid():
            uidc[0] += 1
            return '_%d' % uidc[0]

        def cast(dst, src, reads, writes, psum=False):
            eng = (['dve', 'act'][cast_rr[0] % 2]) if psum else (['pool', 'dve', 'act'][cast_rr[0] % 3])
            cast_rr[0] += 1
            if eng == 'act':
                P.op('act', lambda e: e.copy(out=dst, in_=src), reads=reads, writes=writes)
            else:
                P.op(eng, lambda e: e.tensor_copy(out=dst, in_=src), reads=reads, writes=writes)

        def emit_norm(h, gain, sq, ps_n, rstd, xn, nfeat=D_MODEL):
            nch = nfeat // 128
            P.op('act', lambda e: e.activation(out=sq[:], in_=h[:], func=AF.Square),
                 reads=[h.name], writes=[sq.name])
            for c in range(nch):
                P.op('pe', lambda e, c=c: e.matmul(ps_n[:], lhsT=ones_bf[:], rhs=sq[:, c, :],
                                                   start=(c == 0), stop=(c == nch - 1)),
                     reads=[sq.name, 'ones_bf'], writes=[ps_n.name], inc=(c == nch - 1))
            P.op('act', lambda e: e.activation(out=rstd[:], in_=ps_n[:], func=AF.Ln,
                                               bias=eps_t[:], scale=1.0 / nfeat),
                 reads=[ps_n.name, 'eps_t'], writes=[rstd.name])
            P.op('act', lambda e: e.activation(out=rstd[:], in_=rstd[:], func=AF.Exp, scale=-0.5),
                 reads=[rstd.name], writes=[rstd.name])
            for c in range(nch):
                P.op('dve', lambda e, c=c: e.scalar_tensor_tensor(
                    out=xn[:, c, :], in0=h[:, c, :], scalar=gain[:, c:c + 1], in1=rstd[:],
                    op0=ALU.mult, op1=ALU.mult),
                    reads=[h.name, gain.name, rstd.name], writes=[xn.name])

        NKB = T // 128
        NCMP = T // 16 - 1
        BIGM = 30000.0
        ident_bf = sb("ident_bf", [128, 128], BF16)
        P.op('pool', lambda e: e.affine_select(out=ident_bf[:], in_=ones_bf[:], pattern=[[-1, 128]],
                                               compare_op=ALU.is_equal, fill=0.0, base=0, channel_multiplier=1),
             reads=['ones_bf'], writes=['ident_bf'])
        ones_f = sb("ones_f", [128, 128], F32)
        P.op('pool', lambda e: e.memset(ones_f[:], 1.0), writes=['ones_f'])
        ident_f = sb("ident_f", [128, 128], F32)
        P.op('pool', lambda e: e.affine_select(out=ident_f[:], in_=ones_f[:], pattern=[[-1, 128]],
                                               compare_op=ALU.is_equal, fill=0.0, base=0, channel_multiplier=1),
             reads=['ones_f'], writes=['ident_f'])
        Uincl = sb("Uincl", [128, 128], F32)
        Lstrict = sb("Lstrict", [128, 128], F32)
        Lincl = sb("Lincl", [128, 128], F32)
        P.op('pool', lambda e: e.affine_select(out=Uincl[:], in_=ones_f[:], pattern=[[1, 128]], compare_op=ALU.is_ge,
                                               fill=0.0, base=0, channel_multiplier=-1), reads=['ones_f'], writes=['Uincl'])
        P.op('pool', lambda e: e.affine_select(out=Lstrict[:], in_=ones_f[:], pattern=[[-1, 128]], compare_op=ALU.is_gt,
                                               fill=0.0, base=0, channel_multiplier=1), reads=['ones_f'], writes=['Lstrict'])
        P.op('pool', lambda e: e.affine_select(out=Lincl[:], in_=ones_f[:], pattern=[[-1, 128]], compare_op=ALU.is_ge,
                                               fill=0.0, base=0, channel_multiplier=1), reads=['ones_f'], writes=['Lincl'])
        LBD = sb("LBD", [128, 128], F32)
        LOD = sb("LOD", [128, 128], F32)
        for cb in range(4):
            csl = slice(cb * 32, (cb + 1) * 32)
            P.op('pool', lambda e, csl=csl, cb=cb: e.affine_select(out=LBD[:, csl], in_=Lstrict[:, csl], pattern=[[0, 32]],
                                                                   compare_op=ALU.is_ge, fill=0.0, base=-32 * cb, channel_multiplier=1),
                 reads=['Lstrict'], writes=['LBD'])
            P.op('pool', lambda e, csl=csl, cb=cb: e.affine_select(out=LBD[:, csl], in_=LBD[:, csl], pattern=[[0, 32]],
                                                                   compare_op=ALU.is_ge, fill=0.0, base=32 * cb + 31, channel_multiplier=-1),
                 reads=['LBD'], writes=['LBD'])
        P.op('pool', lambda e: e.tensor_tensor(out=LOD[:], in0=Lstrict[:], in1=LBD[:], op=ALU.subtract),
             reads=['Lstrict', 'LBD'], writes=['LOD'])
        dmaskI = sb("dmaskI", [128, 4, 512], BF16)
        wmask = sb("wmask", [128, 4, 512], BF16)
        for j in range(4):
            P.op('pool', lambda e, j=j: e.affine_select(out=dmaskI[:, j, :], in_=ones512[:], pattern=[[1, 512]],
                                                        compare_op=ALU.is_ge, fill=0.0, base=-128 * j,
                                                        channel_multiplier=-1),
                 reads=['ones512'], writes=['dmaskI'])
            P.op('pool', lambda e, j=j: e.affine_select(out=wmask[:, j, :], in_=ones512[:], pattern=[[-1, 512]],
                                                        compare_op=ALU.is_ge, fill=0.0, base=128 * j - 1,
                                                        channel_multiplier=1),
                 reads=['ones512'], writes=['wmask'])
        Psw = sb("Psw", [128, 128], BF16)
        for mb, base in ((0, -32), (1, 0), (2, -96), (3, -64)):
            P.op('pool', lambda e, mb=mb, base=base: e.affine_select(
                out=Psw[:, mb * 32:(mb + 1) * 32], in_=ones_bf[:, 0:32], pattern=[[-1, 32]],
                compare_op=ALU.is_equal, fill=0.0, base=base, channel_multiplier=1),
                reads=['ones_bf'], writes=['Psw'])
        cmask = sb("cmask", [128, 2, T], BF16)
        Ebig = sb("Ebig", [128, T], BF16)
        oh = sb("oh", [128, 18, 64], BF16)
        ov = sb("ov", [128, 2, 64], BF16)
        keepB = sb("keepB", [128, 128], F32)
        addcB = sb("addcB", [128, 128], F32)
        cosT = sb("cosT", [128, T], BF16)
        sinS = sb("sinS", [128, T], BF16)
        _tmp_scope = contextlib.ExitStack()
        sbtmp = lambda name, shape, dt=F32: _tmp_scope.enter_context(nc.sbuf_tensor(name, shape, dt))
        onesT = _tmp_scope.enter_context(nc.sbuf_tensor("onesT", [128, T], BF16))
        P.op('pool', lambda e: e.memset(onesT[:], 1.0), writes=['onesT'])
        for ic in range(2):
            P.op('pool', lambda e, ic=ic: e.affine_select(out=cmask[:, ic, :], in_=onesT[:], pattern=[[1, T]],
                                                          compare_op=ALU.is_ge, fill=0.0, base=-31 - 2048 * ic,
                                                          channel_multiplier=-16),
                 reads=['onesT'], writes=['cmask'])
        P.op('pool', lambda e: e.memset(Ebig[:], BIGM), writes=['Ebig'])
        for ph in range(2):
            psl = slice(ph * 64, (ph + 1) * 64)
            P.op('pool', lambda e, psl=psl: e.affine_select(out=Ebig[psl, :], in_=Ebig[psl, :], pattern=[[1, T]], compare_op=ALU.is_ge,
                                                            fill=0.0, base=0, channel_multiplier=-64),
                 reads=['Ebig'], writes=['Ebig'])
            P.op('pool', lambda e, psl=psl: e.affine_select(out=Ebig[psl, :], in_=Ebig[psl, :], pattern=[[-1, T]], compare_op=ALU.is_ge,
                                                            fill=0.0, base=63, channel_multiplier=64),
                 reads=['Ebig'], writes=['Ebig'])
        ones18 = sbtmp("ones18", [128, 18, 64], BF16)
        P.op('pool', lambda e: e.memset(ones18[:], 1.0), writes=['ones18'])
        P.op('pool', lambda e: e.affine_select(out=oh[:], in_=ones18[:], pattern=[[-1, 18], [0, 64]],
                                               compare_op=ALU.is_equal, fill=0.0, base=-12, channel_multiplier=1),
             reads=['ones18'], writes=['oh'])
        ovA = sbtmp("ovA", [128, 2, 64], I32)
        P.op('pool', lambda e: e.iota(ovA[:], pattern=[[2048, 2], [-64, 64]], base=0, channel_multiplier=16),
             writes=['ovA'])
        ovF = sbtmp("ovF", [128, 2, 64], F32)
        ovG = sbtmp("ovG", [128, 2, 64], F32)
        P.op('dve', lambda e: e.tensor_copy(out=ovF[:], in_=ovA[:]), reads=['ovA'], writes=['ovF'])
        P.op('dve', lambda e: e.tensor_scalar(out=ovG[:], in0=ovF[:], scalar1=32.0, scalar2=64.0, op0=ALU.add, op1=ALU.min),
             reads=['ovF'], writes=['ovG'])
        P.op('dve', lambda e: e.tensor_scalar(out=ovF[:], in0=ovF[:], scalar1=0.0, scalar2=None, op0=ALU.max),
             reads=['ovF'], writes=['ovF'])
        P.op('dve', lambda e: e.tensor_tensor(out=ovG[:], in0=ovG[:], in1=ovF[:], op=ALU.subtract),
             reads=['ovF', 'ovG'], writes=['ovG'])
        P.op('dve', lambda e: e.tensor_scalar(out=ov[:], in0=ovG[:], scalar1=0.0, scalar2=1.0 / 32, op0=ALU.max, op1=ALU.mult),
             reads=['ovG'], writes=['ov'])
        negf = sbtmp("negf", [128, 128], F32)
        P.op('pool', lambda e: e.memset(negf[:], -1.0), writes=['negf'])
        for ph in range(2):
            psl = slice(ph * 64, (ph + 1) * 64)
            P.op('pool', lambda e, psl=psl, ph=ph: e.affine_select(
                out=keepB[psl, :], in_=ones_f[psl, :], pattern=[[-1, 128]], compare_op=ALU.is_ge, fill=0.0,
                base=62 + ph, channel_multiplier=0), reads=['ones_f'], writes=['keepB'])
            P.op('pool', lambda e, psl=psl, ph=ph: e.affine_select(
                out=addcB[psl, :], in_=negf[psl, :], pattern=[[1, 128]], compare_op=ALU.is_gt, fill=1e4,
                base=-64 - ph, channel_multiplier=0), reads=['negf'], writes=['addcB'])
            P.op('pool', lambda e, psl=psl, ph=ph: e.affine_select(
                out=addcB[psl, :], in_=addcB[psl, :], pattern=[[1, 128]], compare_op=ALU.is_ge, fill=0.0,
                base=-64 - ph + 1, channel_multiplier=0), reads=['addcB'], writes=['addcB'])
        with contextlib.ExitStack() as st:
            sbt = lambda name, shape, dt=F32, u=uid(): st.enter_context(nc.sbuf_tensor(name + u, shape, dt))
            pos_i = sbt("pos_i", [128, T], I32)
            ang = sbt("ang", [128, T], F32)
            tmpa = sbt("tmpa", [128, T], F32)
            ki = sbt("ki", [128, T], I32)
            tmpb = sbt("tmpb", [128, T], F32)
            pidx = sbt("pidx", [128, 1], I32)
            pf = sbt("pf", [128, 1], F32)
            inv = sbt("inv", [128, 1], F32)
            sgn = sbt("sgn", [128, 1], F32)
            P.dma(pos_i[:], pos.partition_broadcast(128), writes=['pos_i'])
            P.op('pool', lambda e: e.iota(pidx[:], pattern=[[0, 1]], base=0, channel_multiplier=1), writes=['pidx'])
            c123 = sbt("c123", [128, 3], F32)
            fl = sbt("fl", [128, 1], F32)
            P.op('dve', lambda e: e.tensor_copy(out=pf[:], in_=pidx[:]), reads=['pidx'], writes=['pf'])
            for i3, thr in enumerate((32.0, 64.0, 96.0)):
                P.op('dve', lambda e, i3=i3, thr=thr: e.tensor_scalar(out=c123[:, i3:i3 + 1], in0=pf[:], scalar1=thr,
                                                                       scalar2=None, op0=ALU.is_ge),
                     reads=['pf'], writes=['c123'])
            P.op('dve', lambda e: e.tensor_tensor(out=fl[:], in0=c123[:, 0:1], in1=c123[:, 1:2], op=ALU.add),
                 reads=['c123'], writes=['fl'])
            P.op('dve', lambda e: e.tensor_tensor(out=fl[:], in0=fl[:], in1=c123[:, 2:3], op=ALU.add),
                 reads=['c123', 'fl'], writes=['fl'])
            P.op('dve', lambda e: e.scalar_tensor_tensor(out=fl[:], in0=fl[:], scalar=-32.0, in1=pf[:], op0=ALU.mult, op1=ALU.add),
                 reads=['fl', 'pf'], writes=['fl'])
            P.op('act', lambda e: e.activation(out=inv[:], in_=fl[:], func=AF.Exp, scale=-float(np.log(10000.0)) / 32),
                 reads=['fl'], writes=['inv'])
            P.op('dve', lambda e: e.tensor_tensor(out=sgn[:], in0=c123[:, 0:1], in1=c123[:, 1:2], op=ALU.subtract),
                 reads=['c123'], writes=['sgn'])
            P.op('dve', lambda e: e.tensor_tensor(out=sgn[:], in0=sgn[:], in1=c123[:, 2:3], op=ALU.add),
                 reads=['c123', 'sgn'], writes=['sgn'])
            P.op('dve', lambda e: e.tensor_scalar(out=sgn[:], in0=sgn[:], scalar1=2.0, scalar2=-1.0, op0=ALU.mult, op1=ALU.add),
                 reads=['sgn'], writes=['sgn'])
            P.op('dve', lambda e: e.tensor_copy(out=ang[:], in_=pos_i[:]), reads=['pos_i'], writes=['ang'])
            P.op('dve', lambda e: e.tensor_scalar(out=ang[:], in0=ang[:], scalar1=inv[:, 0:1], scalar2=None, op0=ALU.mult),
                 reads=['ang', 'inv'], writes=['ang'])
            TWO_PI = 2.0 * float(np.pi)
            for which in range(2):
                src = ang
                if which == 1:
                    P.op('dve', lambda e: e.tensor_scalar(out=ang[:], in0=ang[:], scalar1=float(np.pi) / 2, scalar2=None, op0=ALU.add),
                         reads=['ang'], writes=['ang'])
                P.op('dve', lambda e: e.tensor_scalar(out=tmpa[:], in0=ang[:], scalar1=1.0 / TWO_PI, scalar2=None, op0=ALU.mult),
                     reads=['ang'], writes=['tmpa'])
                P.op('dve', lambda e: e.tensor_copy(out=ki[:], in_=tmpa[:]), reads=['tmpa'], writes=['ki'])
                P.op('dve', lambda e: e.tensor_copy(out=tmpa[:], in_=ki[:]), reads=['ki'], writes=['tmpa'])
                P.op('dve', lambda e: e.scalar_tensor_tensor(out=tmpa[:], in0=tmpa[:], scalar=-TWO_PI, in1=ang[:],
                                                             op0=ALU.mult, op1=ALU.add),
                     reads=['tmpa', 'ang'], writes=['tmpa'])
                for thr, opc, delta in ((float(np.pi), ALU.is_gt, -TWO_PI), (-float(np.pi), ALU.is_lt, TWO_PI)):
                    P.op('dve', lambda e, thr=thr, opc=opc, delta=delta: e.tensor_scalar(
                        out=tmpb[:], in0=tmpa[:], scalar1=thr, scalar2=delta, op0=opc, op1=ALU.mult),
                        reads=['tmpa'], writes=['tmpb'])
                    P.op('dve', lambda e: e.tensor_tensor(out=tmpa[:], in0=tmpa[:], in1=tmpb[:], op=ALU.add),
                         reads=['tmpa', 'tmpb'], writes=['tmpa'])
                P.op('dve', lambda e: e.tensor_scalar(out=tmpa[:], in0=tmpa[:], scalar1=3.14159, scalar2=-3.14159,
                                                      op0=ALU.min, op1=ALU.max), reads=['tmpa'], writes=['tmpa'])
                if which == 0:
                    P.op('act', lambda e: e.activation(out=sinS[:], in_=tmpa[:], func=AF.Sin, scale=sgn[:, 0:1]),
                         reads=['tmpa', 'sgn'], writes=['sinS'])
                else:
                    P.op('act', lambda e: e.activation(out=cosT[:], in_=tmpa[:], func=AF.Sin),
                         reads=['tmpa'], writes=['cosT'])
            P.barrier()
            P.flush()
        _tmp_scope.close()
        if not ('C' in stages and 'D' in stages) and not mix_input:
            zt = sb("zt", [128, 2048], BF16)
            P.op('pool', lambda e: e.memset(zt[:], 0.0), writes=['zt'])
            for r0 in range(0, 768, 128):
                for t0 in range(0, T, 2048):
                    n = min(2048, T - t0)
                    P.dma(mixT[r0:r0 + 128, t0:t0 + n], zt[:, 0:n], reads=['zt'])
            P.barrier()
            P.flush()
        for l in range(depth):
            hsrc = xT if l == 0 else hT
            if 'A' in stages:
              with contextlib.ExitStack() as st:
                sbt = lambda name, shape, dt=F32, u=uid(): st.enter_context(nc.sbuf_tensor(name + u, shape, dt))
                pst = lambda name, shape, dt=F32, u=uid(): st.enter_context(nc.psum_tensor(name + u, shape, dt))
                wbf = sbt("a_wbf", [128, 8, N_IN_PAD], BF16)
                wst = [sbt("a_wst%d" % i, [128, N_IN_PAD // 2], F32) for i in range(2)]
                gain = sbt("a_gain", [128, 8], F32)
                P.dma(gain[:], ln_mix[l], writes=[gain.name])
                for c in range(8):
                    for hf in range(2):
                        s = wst[hf]
                        csl = slice(hf * (N_IN_PAD // 2), (hf + 1) * (N_IN_PAD // 2))
                        P.dma(s[:], w_in[l, c * 128:(c + 1) * 128, csl], writes=[s.name])
                        cast(wbf[:, c, csl], s[:], [s.name], ['a_wbf'])
                h_sb = [sbt("a_h%d" % i, [128, 8, 512], F32) for i in range(2)]
                sq = sbt("a_sq", [128, 8, 512], BF16)
                xn = [sbt("a_xn%d" % i, [128, 8, 512], BF16) for i in range(2)]
                rstd = sbt("a_rstd", [128, 512], F32)
                ps_n = pst("a_psn", [128, 512])
                ps_o = [pst("a_pso%d" % i, [128, 512]) for i in range(4)]
                ost = [sbt("a_ost%d" % i, [128, 512], F32) for i in range(4)]
                for tt in range(NT):
                    tsl = slice(tt * 512, (tt + 1) * 512)
                    h = h_sb[tt % 2]
                    x_ = xn[tt % 2]
                    P.dma(h[:], pc(hsrc)[:, :, tsl], writes=[h.name])
                    emit_norm(h, gain, sq, ps_n, rstd, x_)
                    for m in range(24):
                        ps = ps_o[m % 4]
                        o = ost[m % 4]
                        for c in range(8):
                            P.op('pe', lambda e, c=c, m=m, ps=ps, x_=x_: e.matmul(
                                ps[:], lhsT=wbf[:, c, m * 128:(m + 1) * 128], rhs=x_[:, c, :],
                                start=(c == 0), stop=(c == 7)),
                                reads=[x_.name, 'a_wbf'], writes=[ps.name], inc=(c == 7))
                        cast(o[:], ps[:], [ps.name], [o.name], psum=True)
                        P.dma(projF[m * 128:(m + 1) * 128, tsl], o[:], reads=[o.name])
                    for ts in range(4):
                        ps = ps_o[ts % 4]
                        o = ost[ts % 4]
                        for c in range(8):
                            P.op('pe', lambda e, c=c, ts=ts, ps=ps, x_=x_: e.matmul(
                                ps[:], lhsT=x_[:, c, ts * 128:(ts + 1) * 128], rhs=wbf[:, c, 3072:3584],
                                start=(c == 0), stop=(c == 7)),
                                reads=[x_.name, 'a_wbf'], writes=[ps.name], inc=(c == 7))
                        cast(o[:], ps[:], [ps.name], [o.name], psum=True)
                        P.dma(projT[tt * 512 + ts * 128: tt * 512 + (ts + 1) * 128, :], o[:], reads=[o.name])
                P.barrier()
                P.flush()

            if 'B' in stages:
              with contextlib.ExitStack() as st:
                sbt = lambda name, shape, dt=F32, u=uid(): st.enter_context(nc.sbuf_tensor(name + u, shape, dt))
                pst = lambda name, shape, dt=F32, u=uid(): st.enter_context(nc.psum_tensor(name + u, shape, dt))
                NKB = T // 128
                qT = sbt("b_qT", [128, 2, T], BF16)
                kT = sbt("b_kT", [128, 2, T], BF16)
                vv = sbt("b_v", [128, NKB, 256], BF16)
                stg = [sbt("b_stg%d" % i, [128, 2048], F32) for i in range(2)]
                nw = sbt("b_nw", [128, 1], F32)
                P.dma(nw[:], sb_norm[l], writes=[nw.name])
                k = 0
                for c in range(2):
                    for t0 in range(0, T, 2048):
                        n = min(2048, T - t0)
                        s = stg[k % 2]; k += 1
                        P.dma(s[:, 0:n], projF[(19 + c) * 128:(20 + c) * 128, t0:t0 + n], writes=[s.name])
                        P.op('dve', lambda e, s=s, c=c, t0=t0, n=n: e.tensor_scalar(
                            out=qT[:, c, t0:t0 + n], in0=s[:, 0:n], scalar1=0.125, scalar2=None, op0=ALU.mult),
                            reads=[s.name], writes=['b_qT'])
                        s = stg[k % 2]; k += 1
                        P.dma(s[:, 0:n], projF[(21 + c) * 128:(22 + c) * 128, t0:t0 + n], writes=[s.name])
                        cast(kT[:, c, t0:t0 + n], s[:, 0:n], [s.name], ['b_kT'])
                pTv = projT.rearrange("(kb p) f -> p kb f", p=128)
                for kb0 in range(0, NKB, 8):
                    s = stg[k % 2]; k += 1
                    sv = s[:].rearrange("p (a f) -> p a f", f=256)
                    P.dma(sv, pTv[:, kb0:kb0 + 8, 256:512], writes=[s.name])
                    cast(vv[:, kb0:kb0 + 8, :], sv, [s.name], ['b_v'])
                e_sb = [sbt("b_e%d" % i, [128, 512], F32) for i in range(3)]
                sp_sb = [sbt("b_sp%d" % i, [128, 512], BF16) for i in range(3)]
                a_sb = [sbt("b_a%d" % i, [128, 512], BF16) for i in range(3)]
                racc = sbt("b_racc", [128, 512], BF16)
                sqo = sbt("b_sqo", [64, 512], BF16)
                rs = sbt("b_rs", [64, 512], F32)
                o_sb = [sbt("b_o%d" % i, [64, 512], BF16) for i in range(2)]
                psZ = [pst("b_psZ%d" % i, [128, 512]) for i in range(2)]
                psA = [pst("b_psA%d" % i, [128, 512]) for i in range(2)]
                psO = [pst("b_psO%d" % i, [64, 512]) for i in range(2)]
                psN = pst("b_psN", [64, 512])
                it3 = [0]; it4 = [0]
                ho = 0
                for hh in range(4):
                    c = hh // 2
                    b0 = (hh % 2) * 64
                    for qt in range(NT):
                        qsl = slice(qt * 512, (qt + 1) * 512)
                        po = psO[ho % 2]
                        oo = o_sb[ho % 2]
                        ho += 1
                        nblk = 4 * (qt + 1)
                        kbs = list(reversed(range(nblk)))
                        nb_ = len(kbs)
                        st_ = {}

                        def S1(t, kbs=kbs, qt=qt, b0=b0, c=c, qsl=qsl, st_=st_):
                            kb = kbs[t]
                            j = kb - 4 * qt
                            ksl = slice(kb * 128, (kb + 1) * 128)
                            pz = psZ[it3[0] % 2]; ee = e_sb[it3[0] % 3]; sp = sp_sb[it3[0] % 3]; it3[0] += 1
                            st_[t] = dict(kb=kb, j=j, ksl=ksl, sp=sp, ee=ee)
                            P.op('pe', lambda e: e.matmul(
                                pz[:], lhsT=kT[b0:b0 + 64, c, ksl], rhs=qT[b0:b0 + 64, c, qsl], start=True, stop=True),
                                reads=['b_kT', 'b_qT'], writes=[pz.name])
                            P.op('act', lambda e: e.activation(out=ee[:], in_=pz[:], func=AF.Exp),
                                 reads=[pz.name], writes=[ee.name])
                            P.op('act', lambda e: e.activation(out=sp[:], in_=ee[:], func=AF.Ln, bias=one_t[:]),
                                 reads=[ee.name, 'one_t'], writes=[sp.name])
                            if j >= 0:
                                P.op('dve', lambda e: e.tensor_tensor(out=sp[:], in0=sp[:], in1=dmask[:, j, :], op=ALU.mult),
                                     reads=[sp.name, 'dmask'], writes=[sp.name])

                        def S2(t, b0=b0, c=c, qsl=qsl, st_=st_):
                            d_ = st_[t]
                            kb, j, ksl, sp = d_['kb'], d_['j'], d_['ksl'], d_['sp']
                            first = (t == 0)
                            pa = psA[it4[0] % 2]; aa = a_sb[it4[0] % 3]; it4[0] += 1
                            d_['aa'] = aa
                            ee = d_['ee']
                            P.op('pe', lambda e: e.matmul(pa[:], lhsT=negU[:], rhs=sp[:], start=True, stop=first),
                                 reads=[sp.name, 'negU'], writes=[pa.name], inc=first)
                            if not first:
                                P.op('pe', lambda e: e.matmul(pa[:], lhsT=negOnes[:], rhs=racc[:], start=False, stop=True),
                                     reads=['b_racc', 'negOnes'], writes=[pa.name])
                            P.op('act', lambda e: e.activation(out=aa[:], in_=pa[:], func=AF.Exp),
                                 reads=[pa.name], writes=[aa.name])
                            P.op('dve', lambda e: e.tensor_tensor(out=aa[:], in0=aa[:], in1=ee[:], op=ALU.mult),
                                 reads=[aa.name, ee.name], writes=[aa.name])
                            if j >= 0:
                                P.op('dve', lambda e: e.tensor_tensor(out=aa[:], in0=aa[:], in1=dmask[:, j, :], op=ALU.mult),
                                     reads=[aa.name, 'dmask'], writes=[aa.name])
                            if kb > 0:
                                if first:
                                    P.op('pool', lambda e: e.tensor_copy(out=racc[:], in_=sp[:]),
                                         reads=[sp.name], writes=['b_racc'])
                                else:
                                    P.op('pool', lambda e: e.tensor_tensor(out=racc[:], in0=racc[:], in1=sp[:], op=ALU.add),
                                         reads=[sp.name, 'b_racc'], writes=['b_racc'])

                        def S3(t, po=po, hh=hh, st_=st_, nb_=nb_):
                            d_ = st_[t]
                            kb, aa = d_['kb'], d_['aa']
                            P.op('pe', lambda e: e.matmul(
                                po[:], lhsT=vv[:, kb, hh * 64:(hh + 1) * 64], rhs=aa[:], start=(t == 0), stop=(t == nb_ - 1)),
                                reads=[aa.name, 'b_v'], writes=[po.name], inc=True)

                        for t in range(nb_ + 2):
                            if t < nb_:
                                S1(t)
                            if 1 <= t <= nb_:
                                S2(t - 1)
                            if t >= 2:
                                S3(t - 2)
                        P.op('act', lambda e, po=po: e.activation(out=sqo[:], in_=po[:], func=AF.Square),
                             reads=[po.name], writes=['b_sqo'])
                        P.op('pe', lambda e: e.matmul(psN[:], lhsT=ones_bf[0:64, 0:64], rhs=sqo[:], start=True, stop=True),
                             reads=['b_sqo', 'ones_bf'], writes=['b_psN'])
                        P.op('act', lambda e: e.activation(out=rs[:], in_=psN[:], func=AF.Ln, bias=eps_t[0:64, :], scale=1.0 / 64),
                             reads=['b_psN', 'eps_t'], writes=['b_rs'])
                        P.op('act', lambda e: e.activation(out=rs[:], in_=rs[:], func=AF.Exp, scale=-0.5),
                             reads=['b_rs'], writes=['b_rs'])
                        P.op('dve', lambda e, po=po, oo=oo: e.scalar_tensor_tensor(
                            out=oo[:], in0=po[:], scalar=nw[0:64, :], in1=rs[:], op0=ALU.mult, op1=ALU.mult),
                            reads=[po.name, nw.name, 'b_rs'], writes=[oo.name])
                        P.dma(mixT[768 + hh * 64:768 + (hh + 1) * 64, qsl], oo[:], reads=[oo.name])
                P.barrier()
                P.flush()

            if 'C' in stages:
              with contextlib.ExitStack() as st:
                sbt = lambda name, shape, dt=F32, u=uid(): st.enter_context(nc.sbuf_tensor(name + u, shape, dt))
                qT = sbt("c_qT", [128, 3, T], BF16)
                ksT = sbt("c_ksT", [128, T], BF16)
                kwT = sbt("c_kwT", [128, T], BF16)
                vs = sbt("c_vs", [128, NKB, 128], BF16)
                vw = sbt("c_vw", [128, NKB, 128], BF16)
                sgT = sbt("c_sgT", [128, T], BF16)
                selT = sbt("c_selT", [128, T], BF16)
                kcT = sbt("c_kcT", [128, 256], BF16)
                vc = sbt("c_vc", [128, 2, 2, 64], BF16)
                nw = sbt("c_nw", [128, 1], F32)
                P.dma(nw[:], nsa_norm[l], writes=[nw.name])
                ICS = [(ic, min(128, NCMP - ic * 128)) for ic in range(2) if NCMP - ic * 128 > 0]
                with contextlib.ExitStack() as s1:
                    sb1 = lambda name, shape, dt=F32, u=uid(): s1.enter_context(nc.sbuf_tensor(name + u, shape, dt))
                    ps1 = lambda name, shape, dt=F32, u=uid(): s1.enter_context(nc.psum_tensor(name + u, shape, dt))
                    stg = [sb1("c_stg%d" % i, [128, 2048], F32) for i in range(2)]
                    kcr = sb1("c_kcr", [128, T], BF16)
                    vcb = sb1("c_vcb", [128, T], BF16)
                    xb_r = [sb1("c_xb%d" % i, [128, 512], BF16) for i in range(2)]
                    t1_r = [sb1("c_t1%d" % i, [128, 512], F32) for i in range(2)]
                    t2_r = [sb1("c_t2%d" % i, [128, 512], F32) for i in range(2)]
                    psR = [ps1("c_psR%d" % i, [128, 512]) for i in range(2)]
                    cnt = [0, 0]

                    def load_chunk(ch, fn):
                        for t0 in range(0, T, 2048):
                            n = min(2048, T - t0)
                            s = stg[cnt[0] % 2]; cnt[0] += 1
                            P.dma(s[:, 0:n], projF[ch * 128:(ch + 1) * 128, t0:t0 + n], writes=[s.name])
                            fn(s, t0, n)

                    def rope_to(dst, dkey, scale):
                        def fn(s, t0, n):
                            for sub in range(n // 512):
                                sl = slice(sub * 512, (sub + 1) * 512)
                                gsl = slice(t0 + sub * 512, t0 + (sub + 1) * 512)
                                i = cnt[1] % 2; cnt[1] += 1
                                xb, t1, t2, ps = xb_r[i], t1_r[i], t2_r[i], psR[i]
                                cast(xb[:], s[:, sl], [s.name], [xb.name])
                                P.op('pe', lambda e, ps=ps, xb=xb: e.matmul(ps[:], lhsT=Psw[:], rhs=xb[:], start=True, stop=True),
                                     reads=[xb.name, 'Psw'], writes=[ps.name])
                                P.op('dve', lambda e, t1=t1, s=s, sl=sl, gsl=gsl: e.scalar_tensor_tensor(
                                    out=t1[:], in0=s[:, sl], scalar=scale, in1=cosT[:, gsl], op0=ALU.mult, op1=ALU.mult),
                                    reads=[s.name, 'cosT'], writes=[t1.name])
                                P.op('dve', lambda e, t2=t2, ps=ps, gsl=gsl: e.scalar_tensor_tensor(
                                    out=t2[:], in0=ps[:], scalar=scale, in1=sinS[:, gsl], op0=ALU.mult, op1=ALU.mult),
                                    reads=[ps.name, 'sinS'], writes=[t2.name])
                                P.op('pool', lambda e, t1=t1, t2=t2, gsl=gsl: e.tensor_tensor(
                                    out=dst(gsl), in0=t1[:], in1=t2[:], op=ALU.add),
                                    reads=[t1.name, t2.name], writes=[dkey])
                        return fn

                    for r in range(3):
                        load_chunk(12 + r, rope_to(lambda gsl, r=r: qT[:, r, gsl], 'c_qT', 0.125))
                    load_chunk(15, rope_to(lambda gsl: kcr[:, gsl], 'c_kcr', 1.0))
                    load_chunk(17, rope_to(lambda gsl: ksT[:, gsl], 'c_ksT', 1.0))
                    load_chunk(18, rope_to(lambda gsl: kwT[:, gsl], 'c_kwT', 1.0))
                    load_chunk(16, lambda s, t0, n: cast(vcb[:, t0:t0 + n], s[:, 0:n], [s.name], ['c_vcb']))
                    load_chunk(23, lambda s, t0, n: P.op('act', lambda e: e.activation(
                        out=sgT[:, t0:t0 + n], in_=s[:, 0:n], func=AF.Sigmoid), reads=[s.name], writes=['c_sgT']))
                    pTv = projT.rearrange("(kb p) f -> p kb f", p=128)
                    for kb0 in range(0, NKB, 8):
                        for (dstv, c0) in ((vs, 0), (vw, 128)):
                            s = stg[cnt[0] % 2]; cnt[0] += 1
                            sv = s[:, 0:1024].rearrange("p (a f) -> p a f", f=128)
                            P.dma(sv, pTv[:, kb0:kb0 + 8, c0:c0 + 128], writes=[s.name])
                            cast(dstv[:, kb0:kb0 + 8, :], sv, [s.name], [dstv.name])
                    w1b = [sb1("c_w1%d" % i, [128, 32, 128], BF16) for i in range(2)]
                    w2kp = sb1("c_w2kp", [128, 2, 128], BF16)
                    w2v = sb1("c_w2v", [128, 64], BF16)
                    peb = [sb1("c_pe%d" % i, [128, 32], BF16) for i in range(2)]
                    bias = [sb1("c_bias%d" % i, [128, 1], F32) for i in range(2)]
                    hid = [[sb1("c_hid%d%d" % (i, g), [128, 256], BF16) for g in range(2)] for i in range(2)]
                    psH = [ps1("c_psH%d" % i, [128, 256]) for i in range(2)]
                    psB = ps1("c_psB", [128, 64])
                    P.op('pool', lambda e: e.memset(w2kp[:], 0.0), writes=['c_w2kp'])
                    for i, (w1d, w2d, ped) in enumerate(((cmp_k_w1, cmp_k_w2, pe_kT), (cmp_v_w1, cmp_v_w2, pe_vT))):
                        for hf in range(2):
                            s = stg[cnt[0] % 2]; cnt[0] += 1
                            sv = s[:].rearrange("p (a f) -> p a f", f=128)
                            P.dma(sv, w1d[l, :, hf * 16:(hf + 1) * 16, :], writes=[s.name])
                            cast(w1b[i][:, hf * 16:(hf + 1) * 16, :], sv, [s.name], [w1b[i].name])
                        s = stg[cnt[0] % 2]; cnt[0] += 1
                        P.dma(s[:, 0:64], w2d[l], writes=[s.name])
                        if i == 0:
                            for g in range(2):
                                cast(w2kp[:, g, g * 64:(g + 1) * 64], s[:, 0:64], [s.name], ['c_w2kp'])
                        else:
                            cast(w2v[:], s[:, 0:64], [s.name], ['c_w2v'])
                        s = stg[cnt[0] % 2]; cnt[0] += 1
                        P.dma(s[:, 0:32], ped[l], writes=[s.name])
                        cast(peb[i][:], s[:, 0:32], [s.name], [peb[i].name])
                        for ll in range(32):
                            P.op('pe', lambda e, i=i, ll=ll: e.matmul(
                                psB[:, 0:1], lhsT=w1b[i][0:64, ll, :], rhs=peb[i][0:64, ll:ll + 1],
                                start=(ll == 0), stop=(ll == 31)),
                                reads=[w1b[i].name, peb[i].name], writes=['c_psB'], inc=(ll == 31))
                        P.op('dve', lambda e, i=i: e.tensor_copy(out=bias[i][:], in_=psB[:, 0:1]),
                             reads=['c_psB'], writes=[bias[i].name])
                        src = kcr if i == 0 else vcb
                        srcv = src[:].rearrange("p (i s) -> p i s", s=16)
                        for g in range(2):
                            ph = psH[g]
                            for ll in range(32):
                                a, s0 = ll // 16, ll % 16
                                P.op('pe', lambda e, i=i, g=g, ll=ll, a=a, s0=s0, ph=ph, srcv=srcv: e.matmul(
                                    ph[:, 0:NCMP], lhsT=w1b[i][g * 64:(g + 1) * 64, ll, :],
                                    rhs=srcv[g * 64:(g + 1) * 64, a:a + NCMP, s0],
                                    start=(ll == 0), stop=(ll == 31)),
                                    reads=[w1b[i].name, 'c_kcr' if i == 0 else 'c_vcb'], writes=[ph.name],
                                    inc=(ll == 31))
                            P.op('act', lambda e, i=i, g=g, ph=ph: e.activation(
                                out=hid[i][g][:, 0:NCMP], in_=ph[:, 0:NCMP], func=AF.Silu, bias=bias[i][:]),
                                reads=[ph.name, bias[i].name], writes=[hid[i][g].name])
                    for g in range(2):
                        P.op('pe', lambda e, g=g: e.matmul(psH[0][:, 0:NCMP], lhsT=w2kp[:, g, :], rhs=hid[0][g][:, 0:NCMP],
                                                           start=(g == 0), stop=(g == 1)),
                             reads=['c_w2kp', hid[0][g].name], writes=[psH[0].name], inc=(g == 1))
                    P.op('act', lambda e: e.copy(out=kcT[:, 0:NCMP], in_=psH[0][:, 0:NCMP]),
                         reads=[psH[0].name], writes=['c_kcT'])
                    for g in range(2):
                        for (ic, n) in ICS:
                            P.op('pe', lambda e, g=g, ic=ic, n=n: e.matmul(
                                psB[0:n, 0:64], lhsT=hid[1][g][:, ic * 128:ic * 128 + n], rhs=w2v[:], start=True, stop=True),
                                reads=[hid[1][g].name, 'c_w2v'], writes=['c_psB'])
                            P.op('dve', lambda e, g=g, ic=ic, n=n: e.tensor_copy(out=vc[0:n, g, ic, :], in_=psB[0:n, 0:64]),
                                 reads=['c_psB'], writes=['c_vc'])
                    P.barrier()
                    P.flush()
                with contextlib.ExitStack() as s2:
                    sb2 = lambda name, shape, dt=F32, u=uid(): s2.enter_context(nc.sbuf_tensor(name + u, shape, dt))
                    ps2 = lambda name, shape, dt=F32, u=uid(): s2.enter_context(nc.psum_tensor(name + u, shape, dt))
                    psS = [ps2("c_psS%d" % i, [128, 512]) for i in range(2)]
                    psD = ps2("c_psD", [128, 512])
                    psOc = ps2("c_psOc", [64, 512])
                    psO = ps2("c_psO", [64, 512])
                    psDn = ps2("c_psDn", [64, 512])
                    psM = ps2("c_psM", [128, 512])
                    psG = ps2("c_psG", [64, 512])
                    pe_r = [sb2("c_pe_%d" % i, [128, 512], BF16) for i in range(2)]
                    a_r = [sb2("c_a%d" % i, [128, 512], BF16) for i in range(3)]
                    pn = [sb2("c_pn%d" % r, [128, 2, 512], BF16) for r in range(3)]
                    rden = sb2("c_rden", [128, 512], F32)
                    asum = sb2("c_asum", [128, 512], F32)
                    oc_sets = [[sb2("c_oc%d_%d" % (k, r), [64, 512], F32) for r in range(3)] for k in range(2)]
                    ob_sb = sb2("c_ob", [64, 512], F32)
                    acc = sb2("c_acc", [64, 512], F32)
                    acc2 = sb2("c_acc2", [64, 512], F32)
                    rd64 = sb2("c_rd64", [64, 512], F32)
                    sqo = sb2("c_sqo", [64, 512], BF16)
                    rs = sb2("c_rs", [64, 512], F32)
                    o_out = [sb2("c_oo%d" % i, [64, 512], BF16) for i in range(2)]
                    score = sb2("c_score", [128, 64], F32)
                    score2 = sb2("c_score2", [128, 64], F32)
                    m8a = sb2("c_m8a", [128, 8], F32)
                    m8b = sb2("c_m8b", [128, 8], F32)
                    selm = sb2("c_selm", [128, 128], F32)
                    it = [0]
                    itm = [0]
                    oi = [0]
                    P.op('pool', lambda e: e.memset(selm[:], 0.0), writes=['c_selm'])

                    def attn_core(g, r, qt, kT_src, ksrc_key, v_src, kbs, mask_of, with_sel, pump):
                        qsl = slice(qt * 512, (qt + 1) * 512)
                        gs = slice(g * 64, (g + 1) * 64)
                        tl = {}

                        def score(bi):
                            kb = kbs[bi]
                            ksl = slice(kb * 128, (kb + 1) * 128)
                            ps = psS[itm[0] % 2]; aa = a_r[itm[0] % 3]; itm[0] += 1
                            tl[bi] = aa
                            P.op('pe', lambda e: e.matmul(
                                ps[:], lhsT=kT_src[gs, ksl], rhs=qT[gs, r, qsl], start=True, stop=not with_sel),
                                reads=[ksrc_key, 'c_qT'], writes=[ps.name], inc=not with_sel)
                            if with_sel:
                                P.op('pe', lambda e: e.matmul(
                                    ps[:], lhsT=Ebig[gs, ksl], rhs=selT[gs, qsl], start=False, stop=True),
                                    reads=['Ebig', ('c_selT', g, qt)], writes=[ps.name])
                            P.op('act', lambda e: e.activation(out=aa[:], in_=ps[:], func=AF.Exp),
                                 reads=[ps.name], writes=[aa.name])
                            m = mask_of(kb)
                            if m is not None:
                                mt, mkey = m
                                P.op('dve', lambda e: e.tensor_tensor(out=aa[:], in0=aa[:], in1=mt, op=ALU.mult),
                                     reads=[aa.name, mkey], writes=[aa.name])

                        def av(bi):
                            kb = kbs[bi]
                            aa = tl[bi]
                            first = (bi == 0)
                            last = (bi == len(kbs) - 1)
                            P.op('pe', lambda e: e.matmul(
                                psO[:], lhsT=v_src[:, kb, gs], rhs=aa[:], start=first, stop=last),
                                reads=[aa.name, v_src.name], writes=['c_psO'])
                            if first:
                                P.op('pool', lambda e: e.tensor_copy(out=asum[:], in_=aa[:]), reads=[aa.name], writes=['c_asum'])
                            else:
                                P.op('pool', lambda e: e.tensor_tensor(out=asum[:], in0=asum[:], in1=aa[:], op=ALU.add),
                                     reads=[aa.name, 'c_asum'], writes=['c_asum'])

                        for bi in range(len(kbs) + 1):
                            if bi < len(kbs):
                                score(bi)
                            if bi >= 1:
                                av(bi - 1)
                            pump()
                        P.op('pe', lambda e: e.matmul(psDn[:], lhsT=ones_f[:, 0:64], rhs=asum[:], start=True, stop=True),
                             reads=['c_asum', 'ones_f'], writes=['c_psDn'])
                        P.op('dve', lambda e: e.tensor_scalar(out=rd64[:], in0=psDn[:], scalar1=1e-30, scalar2=None, op0=ALU.max),
                             reads=['c_psDn'], writes=['c_rd64'])
                        P.op('dve', lambda e: e.reciprocal(out=rd64[:], in_=rd64[:]), reads=['c_rd64'], writes=['c_rd64'])
                        P.op('dve', lambda e: e.tensor_tensor(out=ob_sb[:], in0=psO[:], in1=rd64[:], op=ALU.mult),
                             reads=['c_psO', 'c_rd64'], writes=['c_ob'])

                    def pre_gen(g, qt, oc_sb):
                        gs = slice(g * 64, (g + 1) * 64)
                        selkey = ('c_selT', g, qt)
                        if True:
                            qsl = slice(qt * 512, (qt + 1) * 512)
                            ics = [(ic, n) for (ic, n) in ICS if 16 * ic * 128 + 31 <= qt * 512 + 511]
                            for r in (range(3) if 'cmp' in CPARTS else ()):
                                pes = []
                                for (ic, n) in ics:
                                    ps = psS[it[0] % 2]; pp = pe_r[it[0] % 2]; it[0] += 1
                                    P.op('pe', lambda e, ps=ps, ic=ic, n=n, r=r, gs=gs, qsl=qsl: e.matmul(
                                        ps[0:n, :], lhsT=kcT[gs, ic * 128:ic * 128 + n], rhs=qT[gs, r, qsl], start=True, stop=True),
                                        reads=['c_kcT', 'c_qT'], writes=[ps.name])
                                    P.op('act', lambda e, ps=ps, pp=pp, n=n: e.activation(out=pp[0:n, :], in_=ps[0:n, :], func=AF.Exp),
                                         reads=[ps.name], writes=[pp.name])
                                    P.op('dve', lambda e, pp=pp, n=n, ic=ic, qsl=qsl: e.tensor_tensor(
                                        out=pp[0:n, :], in0=pp[0:n, :], in1=cmask[0:n, ic, qsl], op=ALU.mult),
                                        reads=[pp.name, 'cmask'], writes=[pp.name])
                                    pes.append((ic, n, pp))
                                    yield
                                for bi, (ic, n, pp) in enumerate(pes):
                                    P.op('pe', lambda e, pp=pp, n=n, bi=bi, nb=len(pes): e.matmul(
                                        psD[:], lhsT=ones_bf[0:n, :], rhs=pp[0:n, :], start=(bi == 0), stop=(bi == nb - 1)),
                                        reads=[pp.name, 'ones_bf'], writes=['c_psD'], inc=(bi == len(pes) - 1))
                                P.op('dve', lambda e: e.tensor_scalar(out=rden[:], in0=psD[:], scalar1=1e-30, scalar2=None, op0=ALU.max),
                                     reads=['c_psD'], writes=['c_rden'])
                                P.op('dve', lambda e: e.reciprocal(out=rden[:], in_=rden[:]), reads=['c_rden'], writes=['c_rden'])
                                yield
                                for (ic, n, pp) in pes:
                                    P.op('dve', lambda e, pp=pp, n=n, ic=ic, r=r: e.tensor_tensor(
                                        out=pn[r][0:n, ic, :], in0=pp[0:n, :], in1=rden[0:n, :], op=ALU.mult),
                                        reads=[pp.name, 'c_rden'], writes=[pn[r].name])
                                for bi, (ic, n, pp) in enumerate(pes):
                                    P.op('pe', lambda e, n=n, ic=ic, r=r, g=g, bi=bi, nb=len(pes): e.matmul(
                                        psOc[:], lhsT=vc[0:n, g, ic, :], rhs=pn[r][0:n, ic, :], start=(bi == 0), stop=(bi == nb - 1)),
                                        reads=['c_vc', pn[r].name], writes=['c_psOc'], inc=(bi == len(pes) - 1))
                                P.op('act', lambda e, r=r, oc_sb=oc_sb: e.copy(out=oc_sb[r][:], in_=psOc[:]),
                                     reads=['c_psOc'], writes=[oc_sb[r].name])
                                yield
                            for ts in (range(4) if 'topk' in CPARTS else ()):
                                t0 = qt * 512 + ts * 128
                                tsl = slice(ts * 128, (ts + 1) * 128)
                                nmm = 3 * len(ics)
                                k = 0
                                for r in range(3):
                                    for (ic, n) in ics:
                                        P.op('pe', lambda e, r=r, ic=ic, n=n, tsl=tsl, k=k, nmm=nmm: e.matmul(
                                            psM[:, 0:64], lhsT=pn[r][0:n, ic, tsl], rhs=ov[0:n, ic, :],
                                            start=(k == 0), stop=(k == nmm - 1)),
                                            reads=[pn[r].name, 'ov'], writes=['c_psM'], inc=(k == nmm - 1))
                                        k += 1
                                yield
                                off = 64 - t0 // 64
                                P.op('dve', lambda e, off=off: e.tensor_tensor(
                                    out=score[:], in0=psM[:, 0:64], in1=keepB[:, off:off + 64], op=ALU.mult),
                                    reads=['c_psM', 'keepB'], writes=['c_score'])
                                P.op('dve', lambda e, off=off: e.tensor_tensor(
                                    out=score[:], in0=score[:], in1=addcB[:, off:off + 64], op=ALU.add),
                                    reads=['c_score', 'addcB'], writes=['c_score'])
                                P.op('dve', lambda e: e.memset(score[:, 0:1], 1e4), reads=[], writes=['c_score'])
                                yield
                                P.op('dve', lambda e: e.max(out=m8a[:], in_=score[:]), reads=['c_score'], writes=['c_m8a'])
                                P.op('dve', lambda e: e.match_replace(out=score2[:], in_to_replace=m8a[:], in_values=score[:],
                                                                      imm_value=-1e9),
                                     reads=['c_score', 'c_m8a'], writes=['c_score2'])
                                P.op('dve', lambda e: e.max(out=m8b[:], in_=score2[:]), reads=['c_score2'], writes=['c_m8b'])
                                P.op('dve', lambda e, gs=gs: e.tensor_scalar(out=selm[:, gs], in0=score[:], scalar1=m8b[:, 7:8], scalar2=-1.0,
                                                                      op0=ALU.is_ge, op1=ALU.add),
                                     reads=['c_score', 'c_m8b'], writes=['c_selm'])
                                yield
                                P.op('pe', lambda e: e.matmul(psM[:, 128:256], lhsT=selm[:], rhs=ident_f[:], start=True, stop=True),
                                     reads=['c_selm', 'ident_f'], writes=['c_psM'])
                                P.op('act', lambda e, gs=gs, t0=t0: e.copy(out=selT[gs, t0:t0 + 128], in_=psM[gs, 128:256]),
                                     reads=['c_psM'], writes=[selkey])
                            yield

                    def main_part(g, qt, oc_sb, pump):
                        gs = slice(g * 64, (g + 1) * 64)
                        if True:
                            qsl = slice(qt * 512, (qt + 1) * 512)
                            for r in range(3):
                                hh = 3 * g + r
                                def gate_mul(dst, src_ap, src_key, br, hh=hh, qsl=qsl):
                                    P.op('pe', lambda e: e.matmul(psG[:], lhsT=oh[:, br * 6 + hh, :], rhs=sgT[:, qsl], start=True, stop=True),
                                         reads=['oh', 'c_sgT'], writes=['c_psG'])
                                    P.op('dve', lambda e: e.tensor_tensor(out=dst[:], in0=src_ap, in1=psG[:], op=ALU.mult),
                                         reads=[src_key, 'c_psG'], writes=[dst.name])
                                gate_mul(acc, oc_sb[r][:], oc_sb[r].name, 0)
                                if 'sel' in CPARTS:
                                  attn_core(g, r, qt, ksT, 'c_ksT', vs, list(range(4 * (qt + 1))),
                                            lambda kb, qt=qt: ((dmaskI[:, kb - 4 * qt, :], 'dmaskI') if kb >= 4 * qt else None), True, pump)
                                gate_mul(acc2, ob_sb[:], 'c_ob', 1)
                                P.op('pool', lambda e: e.tensor_tensor(out=acc[:], in0=acc[:], in1=acc2[:], op=ALU.add),
                                     reads=[acc.name, acc2.name], writes=[acc.name])
                                if 'win' in CPARTS:
                                  attn_core(g, r, qt, kwT, 'c_kwT', vw, list(range(max(0, 4 * qt - 4), 4 * qt + 4)),
                                            lambda kb, qt=qt: ((dmaskI[:, kb - 4 * qt, :], 'dmaskI') if kb >= 4 * qt
                                                               else (wmask[:, kb - (4 * qt - 4), :], 'wmask')), False, pump)
                                gate_mul(acc2, ob_sb[:], 'c_ob', 2)
                                P.op('pool', lambda e: e.tensor_tensor(out=acc[:], in0=acc[:], in1=acc2[:], op=ALU.add),
                                     reads=[acc.name, acc2.name], writes=[acc.name])
                                oo = o_out[oi[0] % 2]; oi[0] += 1
                                P.op('act', lambda e: e.activation(out=sqo[:], in_=acc[:], func=AF.Square),
                                     reads=[acc.name], writes=['c_sqo'])
                                P.op('pe', lambda e: e.matmul(psG[:], lhsT=ones_bf[0:64, 0:64], rhs=sqo[:], start=True, stop=True),
                                     reads=['c_sqo', 'ones_bf'], writes=['c_psG'])
                                P.op('act', lambda e: e.activation(out=rs[:], in_=psG[:], func=AF.Ln, bias=eps_t[0:64, :], scale=1.0 / 64),
                                     reads=['c_psG', 'eps_t'], writes=['c_rs'])
                                P.op('act', lambda e: e.activation(out=rs[:], in_=rs[:], func=AF.Exp, scale=-0.5),
                                     reads=['c_rs'], writes=['c_rs'])
                                P.op('dve', lambda e, oo=oo: e.scalar_tensor_tensor(
                                    out=oo[:], in0=acc[:], scalar=nw[0:64, :], in1=rs[:], op0=ALU.mult, op1=ALU.mult),
                                    reads=[acc.name, nw.name, 'c_rs'], writes=[oo.name])
                                P.dma(mixT[384 + hh * 64:384 + (hh + 1) * 64, qsl], oo[:], reads=[oo.name])

                    order = [(g, qt) for g in range(2) for qt in range(NT)]

                    def drain(gen):
                        for _ in gen:
                            pass

                    drain(pre_gen(order[0][0], order[0][1], oc_sets[0]))
                    for idx, (g, qt) in enumerate(order):
                        nxt = (pre_gen(order[idx + 1][0], order[idx + 1][1], oc_sets[(idx + 1) % 2])
                               if idx + 1 < len(order) else None)

                        def pump(nxt=nxt):
                            if nxt is None:
                                return
                            for _ in range(3):
                                try:
                                    next(nxt)
                                except StopIteration:
                                    return
                        main_part(g, qt, oc_sets[idx % 2], pump)
                        if nxt is not None:
                            drain(nxt)
                    P.barrier()
                    P.flush()

            if 'D' in stages:
              NG = 13 * 128
              with contextlib.ExitStack() as st:
                sbt = lambda name, shape, dt=F32, u=uid(): st.enter_context(nc.sbuf_tensor(name + u, shape, dt))
                with contextlib.ExitStack() as s1:
                    sb1 = lambda name, shape, dt=F32, u=uid(): s1.enter_context(nc.sbuf_tensor(name + u, shape, dt))
                    ps1 = lambda name, shape, dt=F32, u=uid(): s1.enter_context(nc.psum_tensor(name + u, shape, dt))
                    xc = [sb1("d_xc%d" % i, [128, T + 3], F32) for i in range(2)]
                    cv = [sb1("d_cv%d" % i, [128, T], F32) for i in range(2)]
                    cw = sb1("d_cw", [128, 9, 4], F32)
                    tst = [sb1("d_tst%d" % i, [128, 512], F32) for i in range(3)]
                    psT = [ps1("d_psT%d" % i, [128, 512]) for i in range(3)]
                    P.dma(cw[:], gdn_conv[l], writes=['d_cw'])
                    for i in range(2):
                        P.op('pool', lambda e, i=i: e.memset(xc[i][:, 0:3], 0.0), writes=[xc[i].name])
                    gview = gtok.rearrange("(kb p) f -> p kb f", p=128)
                    k = 0
                    for ci, ch in enumerate(list(range(12)) + [23]):
                        x_ = xc[ci % 2]
                        c_ = cv[ci % 2]
                        P.dma(x_[:, 3:T + 3], projF[ch * 128:(ch + 1) * 128, :], writes=[x_.name])
                        if ch < 9:
                            P.op('dve', lambda e, x_=x_, c_=c_, ch=ch: e.tensor_scalar(
                                out=c_[:], in0=x_[:, 0:T], scalar1=cw[:, ch, 0:1], scalar2=None, op0=ALU.mult),
                                reads=[x_.name, 'd_cw'], writes=[c_.name])
                            for tap in (1, 2, 3):
                                P.op('dve', lambda e, x_=x_, c_=c_, ch=ch, tap=tap: e.scalar_tensor_tensor(
                                    out=c_[:], in0=x_[:, tap:tap + T], scalar=cw[:, ch, tap:tap + 1], in1=c_[:],
                                    op0=ALU.mult, op1=ALU.add),
                                    reads=[x_.name, 'd_cw', c_.name], writes=[c_.name])
                            P.op('act', lambda e, c_=c_: e.activation(out=c_[:], in_=c_[:], func=AF.Silu),
                                 reads=[c_.name], writes=[c_.name])
                            src, skey = (lambda sl, c_=c_: c_[:, sl]), c_.name
                        else:
                            src, skey = (lambda sl, x_=x_: x_[:, 3 + sl.start:3 + sl.stop]), x_.name
                        for kb0 in range(0, NKB, 4):
                            pt = psT[k % 3]; ts_ = tst[k % 3]; k += 1
                            for q4 in range(4):
                                kb = kb0 + q4
                                P.op('pe', lambda e, pt=pt, q4=q4, kb=kb, src=src: e.matmul(
                                    pt[:, q4 * 128:(q4 + 1) * 128], lhsT=src(slice(kb * 128, (kb + 1) * 128)), rhs=ident_f[:],
                                    start=True, stop=True),
                                    reads=[skey, 'ident_f'], writes=[pt.name], inc=(q4 == 3))
                            cast(ts_[:], pt[:], [pt.name], [ts_.name], psum=True)
                            P.dma(gview[:, kb0:kb0 + 4, ci * 128:(ci + 1) * 128],
                                  ts_[:].rearrange("p (a f) -> p a f", f=128), reads=[ts_.name])
                    P.barrier()
                    P.flush()
                with contextlib.ExitStack() as s2:
                    sb2 = lambda name, shape, dt=F32, u=uid(): s2.enter_context(nc.sbuf_tensor(name + u, shape, dt))
                    ps2 = lambda name, shape, dt=F32, u=uid(): s2.enter_context(nc.psum_tensor(name + u, shape, dt))
                    dtb = sb2("d_dtb", [128, 6], F32)
                    nA = sb2("d_nA", [128, 6], F32)
                    nwb = sb2("d_nwb", [128, 64], F32)
                    P.dma(dtb[:], gdn_dtb[l].partition_broadcast(128), writes=['d_dtb'])
                    P.dma(nA[:], gdn_alog[l].partition_broadcast(128), writes=['d_nA'])
                    P.dma(nwb[:], gdn_normrow[l].partition_broadcast(128), writes=['d_nwb'])
                    P.op('act', lambda e: e.activation(out=nA[:], in_=nA[:], func=AF.Exp), reads=['d_nA'], writes=['d_nA'])
                    P.op('dve', lambda e: e.tensor_scalar(out=nA[:], in0=nA[:], scalar1=-1.0, scalar2=None, op0=ALU.mult),
                         reads=['d_nA'], writes=['d_nA'])
                    X = [sb2("d_X%d" % i, [128, NG], F32) for i in range(2)]
                    S = [sb2("d_S%d" % h, [64, 64], F32) for h in range(6)]
                    for h in range(6):
                        P.op('pool', lambda e, h=h: e.memset(S[h][:], 0.0), writes=[S[h].name])
                    gt = sb2("d_gt", [128, 48], F32)
                    gsb = sb2("d_gsb", [128, 16], F32)
                    sqq = sb2("d_sqq", [128, 384], F32)
                    rn = sb2("d_rn", [128, 12], F32)
                    H = lambda name, shape, dt=F32: [sb2("%s%d" % (name, h), shape, dt) for h in range(6)]
                    qn = H("d_qn", [128, 64]); kn = H("d_kn", [128, 64]); qg = H("d_qg", [128, 64])
                    kd = H("d_kd", [128, 64])
                    qnT = H("d_qnT", [64, 128]); knT = H("d_knT", [64, 128]); qgT = H("d_qgT", [64, 128])
                    yf = H("d_yf", [128, 256])
                    MT = H("d_MT", [128, 128]); tA = H("d_tA", [128, 128]); tB = H("d_tB", [128, 128])
                    dgn = H("d_dgn", [128, 128])
                    dec = H("d_dec", [128, 128])
                    tmpk = H("d_tmpk", [128, 128])
                    Rm = [H("d_R%d_" % i, [128, 128]) for i in range(2)]
                    Qm = [H("d_Q%d_" % i, [128, 128]) for i in range(2)]
                    attn = H("d_attn", [128, 128])
                    attnT = H("d_attnT", [128, 128])
                    wT = H("d_wT", [64, 128])
                    vnew = H("d_vnew", [128, 64])
                    o_sb = sb2("d_o", [128, 384], F32)
                    zs = sb2("d_zs", [128, 384], F32)
                    res = sb2("d_res", [128, 384], F32)
                    ot = [sb2("d_ot%d" % i, [128, 3, 128], BF16) for i in range(2)]
                    pA = [ps2("d_pA%d" % i, [128, 128]) for i in range(4)]
                    pB = [ps2("d_pB%d" % i, [128, 256]) for i in range(4)]
                    ia = [0]; ib = [0]

                    def nextA():
                        p_ = pA[ia[0] % 4]; ia[0] += 1
                        return p_

                    def nextB():
                        p_ = pB[ib[0] % 4]; ib[0] += 1
                        return p_

                    def evac(dst, dkey, src, skey):
                        cast(dst, src, [skey], [dkey], psum=True)

                    for n in range(NKB):
                        Xn = X[n % 2]
                        P.dma(Xn[:], gtok[n * 128:(n + 1) * 128, :], writes=[Xn.name])
                        xk = Xn.name
                        A0 = 1536
                        P.op('act', lambda e, Xn=Xn: e.activation(out=gt[:, 0:6], in_=Xn[:, A0 + 6:A0 + 12], func=AF.Sigmoid),
                             reads=[xk], writes=['d_gt'])
                        P.op('dve', lambda e, Xn=Xn: e.tensor_tensor(out=gt[:, 6:12], in0=Xn[:, A0:A0 + 6], in1=dtb[:], op=ALU.add),
                             reads=[xk, 'd_dtb'], writes=['d_gt'])
                        P.op('act', lambda e: e.activation(out=gt[:, 6:12], in_=gt[:, 6:12], func=AF.Exp), reads=['d_gt'], writes=['d_gt'])
                        P.op('act', lambda e: e.activation(out=gt[:, 6:12], in_=gt[:, 6:12], func=AF.Ln, bias=one_t[:]),
                             reads=['d_gt', 'one_t'], writes=['d_gt'])
                        P.op('dve', lambda e: e.tensor_tensor(out=gt[:, 6:12], in0=gt[:, 6:12], in1=nA[:], op=ALU.mult),
                             reads=['d_gt', 'd_nA'], writes=['d_gt'])
                        pg = nextA()
                        P.op('pe', lambda e, pg=pg: e.matmul(pg[:, 0:6], lhsT=Uincl[:], rhs=gt[:, 6:12], start=True, stop=True),
                             reads=['d_gt', 'Uincl'], writes=[pg.name], inc=False)
                        P.op('pe', lambda e, pg=pg: e.matmul(pg[:, 6:12], lhsT=ones_f[:], rhs=gt[:, 6:12], start=True, stop=True),
                             reads=['d_gt', 'ones_f'], writes=[pg.name])
                        P.op('dve', lambda e, pg=pg: e.tensor_copy(out=gsb[:, 0:12], in_=pg[:, 0:12]), reads=[pg.name], writes=['d_gsb'])
                        P.op('act', lambda e: e.activation(out=gt[:, 12:18], in_=gsb[:, 0:6], func=AF.Exp), reads=['d_gsb'], writes=['d_gt'])
                        P.op('dve', lambda e: e.tensor_tensor(out=gt[:, 18:24], in0=gsb[:, 6:12], in1=gsb[:, 0:6], op=ALU.subtract),
                             reads=['d_gsb'], writes=['d_gt'])
                        P.op('act', lambda e: e.activation(out=gt[:, 18:24], in_=gt[:, 18:24], func=AF.Exp), reads=['d_gt'], writes=['d_gt'])
                        P.op('act', lambda e: e.activation(out=gt[:, 24:30], in_=gsb[:, 6:12], func=AF.Exp), reads=['d_gsb'], writes=['d_gt'])
                        P.op('dve', lambda e: e.tensor_tensor(out=gt[:, 30:36], in0=gt[:, 0:6], in1=gt[:, 12:18], op=ALU.mult),
                             reads=['d_gt'], writes=['d_gt'])
                        P.op('dve', lambda e: e.tensor_scalar(out=gt[:, 36:42], in0=gt[:, 0:6], scalar1=-1.0, scalar2=None, op0=ALU.mult),
                             reads=['d_gt'], writes=['d_gt'])
                        for qi, c0 in enumerate((0, 384)):
                            P.op('dve', lambda e, Xn=Xn, c0=c0: e.tensor_tensor(out=sqq[:], in0=Xn[:, c0:c0 + 384], in1=Xn[:, c0:c0 + 384], op=ALU.mult),
                                 reads=[xk], writes=['d_sqq'])
                            P.op('dve', lambda e, qi=qi: e.reduce_sum(out=rn[:, qi * 6:(qi + 1) * 6],
                                                                     in_=sqq[:].rearrange("p (h d) -> p h d", d=64), axis=AX.X),
                                 reads=['d_sqq'], writes=['d_rn'])
                        P.op('act', lambda e: e.activation(out=rn[:], in_=rn[:], func=AF.Ln, bias=eps_t[:]), reads=['d_rn', 'eps_t'], writes=['d_rn'])
                        P.op('act', lambda e: e.activation(out=rn[:], in_=rn[:], func=AF.Exp, scale=-0.5), reads=['d_rn'], writes=['d_rn'])
                        for h in range(6):
                            hs = slice(h * 64, (h + 1) * 64)
                            P.op('dve', lambda e, h=h, hs=hs, Xn=Xn: e.tensor_scalar(
                                out=qn[h][:], in0=Xn[:, hs], scalar1=rn[:, h:h + 1], scalar2=0.125, op0=ALU.mult, op1=ALU.mult),
                                reads=[xk, 'd_rn'], writes=[qn[h].name])
                            P.op('dve', lambda e, h=h, Xn=Xn: e.tensor_scalar(
                                out=kn[h][:], in0=Xn[:, 384 + h * 64:384 + (h + 1) * 64], scalar1=rn[:, 6 + h:7 + h], scalar2=None, op0=ALU.mult),
                                reads=[xk, 'd_rn'], writes=[kn[h].name])
                            P.op('pool', lambda e, h=h: e.tensor_scalar(
                                out=qg[h][:], in0=qn[h][:], scalar1=gt[:, 12 + h:13 + h], scalar2=None, op0=ALU.mult),
                                reads=[qn[h].name, 'd_gt'], writes=[qg[h].name])
                            P.op('pool', lambda e, h=h: e.tensor_scalar(
                                out=kd[h][:], in0=kn[h][:], scalar1=gt[:, 18 + h:19 + h], scalar2=None, op0=ALU.mult),
                                reads=[kn[h].name, 'd_gt'], writes=[kd[h].name])
                            P.op('dve', lambda e, h=h, Xn=Xn: e.tensor_scalar(
                                out=yf[h][:, 0:64], in0=Xn[:, 768 + h * 64:768 + (h + 1) * 64], scalar1=gt[:, h:h + 1], scalar2=None, op0=ALU.mult),
                                reads=[xk, 'd_gt'], writes=[yf[h].name])
                            P.op('dve', lambda e, h=h: e.tensor_scalar(
                                out=yf[h][:, 64:128], in0=kn[h][:], scalar1=gt[:, 30 + h:31 + h], scalar2=None, op0=ALU.mult),
                                reads=[kn[h].name, 'd_gt'], writes=[yf[h].name])
                            P.op('pool', lambda e, h=h: e.tensor_scalar(
                                out=dgn[h][:], in0=ident_f[:], scalar1=gsb[:, h:h + 1], scalar2=-1.0, op0=ALU.mult, op1=ALU.mult),
                                reads=['ident_f', 'd_gsb'], writes=[dgn[h].name])
                        for h in range(6):
                            for (srcT, dstT) in ((qn, qnT), (kn, knT), (qg, qgT)):
                                p_ = nextA()
                                P.op('pe', lambda e, p_=p_, srcT=srcT, h=h: e.matmul(p_[0:64, :], lhsT=srcT[h][:], rhs=ident_f[:], start=True, stop=True),
                                     reads=[srcT[h].name, 'ident_f'], writes=[p_.name])
                                evac(dstT[h][:], dstT[h].name, p_[0:64, :], p_.name)
                        for h in range(6):
                            p_ = nextB()
                            P.op('pe', lambda e, p_=p_, h=h: e.matmul(p_[:, 0:128], lhsT=ones_f[:], rhs=dgn[h][:], start=True, stop=True),
                                 reads=[dgn[h].name, 'ones_f'], writes=[p_.name])
                            P.op('dve', lambda e, p_=p_, h=h: e.tensor_scalar(
                                out=dec[h][:], in0=p_[:, 0:128], scalar1=gsb[:, h:h + 1], scalar2=0.0, op0=ALU.add, op1=ALU.min),
                                reads=[p_.name, 'd_gsb'], writes=[dec[h].name])
                            P.op('act', lambda e, h=h: e.activation(out=dec[h][:], in_=dec[h][:], func=AF.Exp),
                                 reads=[dec[h].name], writes=[dec[h].name])
                            p_ = nextB()
                            P.op('pe', lambda e, p_=p_, h=h: e.matmul(p_[:, 0:128], lhsT=knT[h][:], rhs=knT[h][:], start=True, stop=True),
                                 reads=[knT[h].name], writes=[p_.name])
                            P.op('dve', lambda e, p_=p_, h=h: e.tensor_tensor(out=tmpk[h][:], in0=p_[:, 0:128], in1=dec[h][:], op=ALU.mult),
                                 reads=[p_.name, dec[h].name], writes=[tmpk[h].name])
                            P.op('dve', lambda e, h=h: e.scalar_tensor_tensor(
                                out=Rm[0][h][:], in0=tmpk[h][:], scalar=gt[:, 36 + h:37 + h], in1=LBD[:], op0=ALU.mult, op1=ALU.mult),
                                reads=[tmpk[h].name, 'd_gt', 'LBD'], writes=[Rm[0][h].name])
                            P.op('dve', lambda e, h=h: e.scalar_tensor_tensor(
                                out=yf[h][:, 128:256], in0=tmpk[h][:], scalar=gt[:, 36 + h:37 + h], in1=LOD[:], op0=ALU.mult, op1=ALU.mult),
                                reads=[tmpk[h].name, 'd_gt', 'LOD'], writes=[yf[h].name])
                            p_ = nextB()
                            P.op('pe', lambda e, p_=p_, h=h: e.matmul(p_[:, 0:128], lhsT=qnT[h][:], rhs=knT[h][:], start=True, stop=True),
                                 reads=[qnT[h].name, knT[h].name], writes=[p_.name])
                            P.op('dve', lambda e, p_=p_, h=h: e.tensor_tensor(out=tmpk[h][:], in0=p_[:, 0:128], in1=dec[h][:], op=ALU.mult),
                                 reads=[p_.name, dec[h].name], writes=[tmpk[h].name])
                            P.op('pool', lambda e, h=h: e.tensor_tensor(out=attn[h][:], in0=tmpk[h][:], in1=Lincl[:], op=ALU.mult),
                                 reads=[tmpk[h].name, 'Lincl'], writes=[attn[h].name])
                            for (srcM, dstM) in ((Rm[0], Qm[0]), (attn, attnT)):
                                p_ = nextA()
                                P.op('pe', lambda e, p_=p_, srcM=srcM, h=h: e.matmul(p_[:, 0:128], lhsT=srcM[h][:], rhs=ident_f[:], start=True, stop=True),
                                     reads=[srcM[h].name, 'ident_f'], writes=[p_.name])
                                evac(dstM[h][:], dstM[h].name, p_[:, 0:128], p_.name)
                        for j in range(5):
                            cur, nxt = j % 2, (j + 1) % 2
                            for h in range(6):
                                p_ = nextB()
                                P.op('pe', lambda e, p_=p_, h=h, cur=cur: e.matmul(p_[:], lhsT=Qm[cur][h][:], rhs=yf[h][:], start=True, stop=True),
                                     reads=[Qm[cur][h].name, yf[h].name], writes=[p_.name])
                                P.op('dve', lambda e, p_=p_, h=h: e.tensor_tensor(out=yf[h][:], in0=yf[h][:], in1=p_[:], op=ALU.add),
                                     reads=[p_.name, yf[h].name], writes=[yf[h].name])
                                if j < 3:
                                    p_ = nextA()
                                    P.op('pe', lambda e, p_=p_, h=h, cur=cur: e.matmul(p_[:], lhsT=Qm[cur][h][:], rhs=Rm[cur][h][:], start=True, stop=True),
                                         reads=[Qm[cur][h].name, Rm[cur][h].name], writes=[p_.name])
                                    evac(Rm[nxt][h][:], Rm[nxt][h].name, p_[:], p_.name)
                                if j < 4:
                                    p_ = nextA()
                                    P.op('pe', lambda e, p_=p_, h=h, cur=cur: e.matmul(p_[:], lhsT=Rm[cur][h][:], rhs=Qm[cur][h][:], start=True, stop=True),
                                         reads=[Qm[cur][h].name, Rm[cur][h].name], writes=[p_.name])
                                    evac(Qm[nxt][h][:], Qm[nxt][h].name, p_[:], p_.name)
                        for h in range(6):
                            p_ = nextA()
                            P.op('pe', lambda e, p_=p_, h=h: e.matmul(p_[:], lhsT=yf[h][:, 128:256], rhs=ident_f[:], start=True, stop=True),
                                 reads=[yf[h].name, 'ident_f'], writes=[p_.name])
                            evac(MT[h][:], MT[h].name, p_[:], p_.name)
                        for it3, (src3, dst3) in enumerate(((None, tA), (tA, tB), (tB, tA))):
                            for h in range(6):
                                p_ = nextA()
                                rhs_ap = yf[h][:, 0:128] if src3 is None else src3[h][:]
                                rkey = yf[h].name if src3 is None else src3[h].name
                                P.op('pe', lambda e, p_=p_, h=h, rhs_ap=rhs_ap: e.matmul(p_[:], lhsT=MT[h][:], rhs=rhs_ap, start=True, stop=True),
                                     reads=[MT[h].name, rkey], writes=[p_.name])
                                P.op('dve', lambda e, p_=p_, h=h, dst3=dst3: e.tensor_tensor(out=dst3[h][:], in0=yf[h][:, 0:128], in1=p_[:], op=ALU.add),
                                     reads=[p_.name, yf[h].name], writes=[dst3[h].name])
                        for h in range(6):
                            p_ = nextA()
                            P.op('pe', lambda e, p_=p_, h=h: e.matmul(p_[0:64, :], lhsT=tA[h][:, 64:128], rhs=ident_f[:], start=True, stop=True),
                                 reads=[tA[h].name, 'ident_f'], writes=[p_.name])
                            evac(wT[h][:], wT[h].name, p_[0:64, :], p_.name)
                        for h in range(6):
                            p1 = nextB()
                            P.op('pe', lambda e, p1=p1, h=h: e.matmul(p1[:, 0:64], lhsT=wT[h][:], rhs=S[h][:], start=True, stop=True),
                                 reads=[wT[h].name, S[h].name], writes=[p1.name])
                            P.op('dve', lambda e, p1=p1, h=h: e.tensor_tensor(out=vnew[h][:], in0=tA[h][:, 0:64], in1=p1[:, 0:64], op=ALU.subtract),
                                 reads=[p1.name, tA[h].name], writes=[vnew[h].name])
                            p2 = nextB()
                            P.op('pe', lambda e, p2=p2, h=h: e.matmul(p2[:, 0:64], lhsT=qgT[h][:], rhs=S[h][:], start=True, stop=False),
                                 reads=[qgT[h].name, S[h].name], writes=[p2.name], inc=False)
                            P.op('pe', lambda e, p2=p2, h=h: e.matmul(p2[:, 0:64], lhsT=attnT[h][:], rhs=vnew[h][:], start=False, stop=True),
                                 reads=[attnT[h].name, vnew[h].name], writes=[p2.name])
                            P.op('act', lambda e, p2=p2, h=h: e.copy(out=o_sb[:, h * 64:(h + 1) * 64], in_=p2[:, 0:64]),
                                 reads=[p2.name], writes=[('d_o', h)])
                            p3 = nextA()
                            P.op('pe', lambda e, p3=p3, h=h: e.matmul(p3[0:64, 0:64], lhsT=kd[h][:], rhs=vnew[h][:], start=True, stop=True),
                                 reads=[kd[h].name, vnew[h].name], writes=[p3.name])
                            P.op('dve', lambda e, p3=p3, h=h: e.scalar_tensor_tensor(
                                out=S[h][:], in0=S[h][:], scalar=gt[0:64, 24 + h:25 + h], in1=p3[0:64, 0:64], op0=ALU.mult, op1=ALU.add),
                                reads=[p3.name, S[h].name, 'd_gt'], writes=[S[h].name])
                        okeys = [('d_o', h) for h in range(6)]
                        P.op('dve', lambda e: e.tensor_tensor(out=sqq[:], in0=o_sb[:], in1=o_sb[:], op=ALU.mult), reads=okeys, writes=['d_sqq'])
                        P.op('dve', lambda e: e.reduce_sum(out=rn[:, 0:6], in_=sqq[:].rearrange("p (h d) -> p h d", d=64), axis=AX.X),
                             reads=['d_sqq'], writes=['d_rn'])
                        P.op('act', lambda e: e.activation(out=rn[:, 0:6], in_=rn[:, 0:6], func=AF.Ln, bias=eps_t[:], scale=1.0 / 64),
                             reads=['d_rn', 'eps_t'], writes=['d_rn'])
                        P.op('act', lambda e: e.activation(out=rn[:, 0:6], in_=rn[:, 0:6], func=AF.Exp, scale=-0.5), reads=['d_rn'], writes=['d_rn'])
                        P.op('act', lambda e, Xn=Xn: e.activation(out=zs[:], in_=Xn[:, 1152:1536], func=AF.Silu), reads=[xk], writes=['d_zs'])
                        for h in range(6):
                            hs = slice(h * 64, (h + 1) * 64)
                            P.op('dve', lambda e, h=h, hs=hs: e.scalar_tensor_tensor(
                                out=res[:, hs], in0=o_sb[:, hs], scalar=rn[:, h:h + 1], in1=nwb[:], op0=ALU.mult, op1=ALU.mult),
                                reads=okeys + ['d_rn', 'd_nwb'], writes=[('d_res', h)])
                            P.op('pool', lambda e, hs=hs: e.tensor_tensor(out=res[:, hs], in0=res[:, hs], in1=zs[:, hs], op=ALU.mult),
                                 reads=[('d_res', h), 'd_zs'], writes=[('d_res', h)])
                        rkeys = [('d_res', h) for h in range(6)]
                        oo = ot[n % 2]
                        for c3 in range(3):
                            p_ = nextA()
                            P.op('pe', lambda e, p_=p_, c3=c3: e.matmul(p_[:], lhsT=res[:, c3 * 128:(c3 + 1) * 128], rhs=ident_f[:], start=True, stop=True),
                                 reads=rkeys + ['ident_f'], writes=[p_.name])
                            evac(oo[:, c3, :], oo.name, p_[:], p_.name)
                        P.dma(mixT[0:384, n * 128:(n + 1) * 128].rearrange("(c p) t -> p c t", p=128), oo[:], reads=[oo.name])
                    P.barrier()
                    P.flush()


            if 'E' in stages:
              with contextlib.ExitStack() as st:
                sbt = lambda name, shape, dt=F32, u=uid(): st.enter_context(nc.sbuf_tensor(name + u, shape, dt))
                pst = lambda name, shape, dt=F32, u=uid(): st.enter_context(nc.psum_tensor(name + u, shape, dt))
                wob = sbt("e_w", [128, 8, D_MODEL], BF16)
                wst = [sbt("e_wst%d" % i, [128, D_MODEL], F32) for i in range(2)]
                for c in range(8):
                    s = wst[c % 2]
                    P.dma(s[:], w_out[l, c * 128:(c + 1) * 128, :], writes=[s.name])
                    cast(wob[:, c, :], s[:], [s.name], ['e_w'])
                h_sb = [sbt("e_h%d" % i, [128, 8, 512], F32) for i in range(2)]
                mx_sb = [sbt("e_mx%d" % i, [128, 8, 512], BF16) for i in range(2)]
                ps_o = [pst("e_ps%d" % i, [128, 512]) for i in range(4)]
                for tt in range(NT):
                    tsl = slice(tt * 512, (tt + 1) * 512)
                    h = h_sb[tt % 2]
                    mx = mx_sb[tt % 2]
                    P.dma(h[:], pc(hsrc)[:, :, tsl], writes=[h.name])
                    P.dma(mx[:], pc(mixT)[:, :, tsl], writes=[mx.name])
                    for m in range(8):
                        ps = ps_o[m % 4]
                        for c in range(8):
                            P.op('pe', lambda e, c=c, m=m, ps=ps, mx=mx: e.matmul(
                                ps[:], lhsT=wob[:, c, m * 128:(m + 1) * 128], rhs=mx[:, c, :],
                                start=(c == 0), stop=(c == 7)),
                                reads=[mx.name, 'e_w'], writes=[ps.name], inc=(c == 7))
                        P.op('dve', lambda e, m=m, ps=ps, h=h: e.tensor_tensor(
                            out=h[:, m, :], in0=h[:, m, :], in1=ps[:], op=ALU.add),
                            reads=[ps.name, h.name], writes=[h.name])
                    P.dma(pc(hT)[:, :, tsl], h[:], reads=[h.name])
                P.barrier()
                P.flush()

            if 'F' in stages:
              for half in range(2):
                with contextlib.ExitStack() as st:
                    sbt = lambda name, shape, dt=F32, u=uid(): st.enter_context(nc.sbuf_tensor(name + u, shape, dt))
                    pst = lambda name, shape, dt=F32, u=uid(): st.enter_context(nc.psum_tensor(name + u, shape, dt))
                    HF = D_FF // 2
                    wup = sbt("f_wup", [128, 8, 2 * HF], BF16)
                    wdn = sbt("f_wdn", [128, 11, D_MODEL], BF16)
                    wst = [sbt("f_wst%d" % i, [128, HF], F32) for i in range(2)]
                    gain = sbt("f_gain", [128, 8], F32)
                    cw = sbt("f_cw", [128, 44, 3], F32)
                    halo = sbt("f_halo", [128, 22, 2], F32)
                    P.dma(gain[:], ln_ffn[l], writes=[gain.name])
                    P.dma(cw[:], ffn_conv[l], writes=['f_cw'])
                    P.op('pool', lambda e: e.memset(halo[:], 0.0), writes=['f_halo'])
                    k = 0
                    for c in range(8):
                        for which in range(2):
                            s = wst[k % 2]
                            k += 1
                            c0 = which * D_FF + half * HF
                            P.dma(s[:], w_up[l, c * 128:(c + 1) * 128, c0:c0 + HF], writes=[s.name])
                            cast(wup[:, c, which * HF:(which + 1) * HF], s[:], [s.name], ['f_wup'])
                    for j in range(11):
                        s = wst[k % 2]
                        k += 1
                        r0 = (half * 11 + j) * 128
                        P.dma(s[:, 0:D_MODEL], w_down[l, r0:r0 + 128, :], writes=[s.name])
                        cast(wdn[:, j, :], s[:, 0:D_MODEL], [s.name], ['f_wdn'])
                    h_sb = sbt("f_h", [128, 8, 512], F32)
                    sq = sbt("f_sq", [128, 8, 512], BF16)
                    xn = sbt("f_xn", [128, 8, 512], BF16)
                    rstd = sbt("f_rstd", [128, 512], F32)
                    actv = sbt("f_act", [128, 11, 512], BF16)
                    u_sb = [sbt("f_u%d" % i, [128, 514], F32) for i in range(4)]
                    cv_sb = [sbt("f_cv%d" % i, [128, 512], F32) for i in range(4)]
                    ps_n = pst("f_psn", [128, 512])
                    ps_o = [pst("f_ps%d" % i, [128, 512]) for i in range(4)]
                    ucnt = 0
                    for tt in range(NT):
                        tsl = slice(tt * 512, (tt + 1) * 512)
                        h = h_sb
                        P.dma(h[:], pc(hT)[:, :, tsl], writes=[h.name])
                        emit_norm(h, gain, sq, ps_n, rstd, xn)
                        hacc = h
                        if half == 1:
                            P.dma(h[:], pc(hT2)[:, :, tsl], writes=[h.name])
                        for j in range(11):
                            cvs = []
                            for which in range(2):
                                ps = ps_o[ucnt % 4]
                                u = u_sb[ucnt % 4]
                                cv = cv_sb[ucnt % 4]
                                ucnt += 1
                                hidx = which * 11 + j
                                fidx = which * 22 + half * 11 + j
                                for c in range(8):
                                    P.op('pe', lambda e, c=c, ps=ps, which=which, j=j: e.matmul(
                                        ps[:], lhsT=wup[:, c, which * HF + j * 128: which * HF + (j + 1) * 128],
                                        rhs=xn[:, c, :], start=(c == 0), stop=(c == 7)),
                                        reads=[xn.name, 'f_wup'], writes=[ps.name], inc=(c == 7))
                                P.op('act', lambda e, ps=ps, u=u: e.copy(out=u[:, 2:514], in_=ps[:]),
                                     reads=[ps.name], writes=[u.name])
                                P.op('pool', lambda e, u=u, hidx=hidx: e.tensor_copy(out=u[:, 0:2], in_=halo[:, hidx, :]),
                                     reads=[('f_halo', hidx)], writes=[u.name])
                                P.op('dve', lambda e, u=u, cv=cv, fidx=fidx: e.tensor_scalar(
                                    out=cv[:], in0=u[:, 0:512], scalar1=cw[:, fidx, 0:1], scalar2=None, op0=ALU.mult),
                                    reads=[u.name, 'f_cw'], writes=[cv.name])
                                for tap in (1, 2):
                                    P.op('dve', lambda e, u=u, cv=cv, fidx=fidx, tap=tap: e.scalar_tensor_tensor(
                                        out=cv[:], in0=u[:, tap:tap + 512], scalar=cw[:, fidx, tap:tap + 1], in1=cv[:],
                                        op0=ALU.mult, op1=ALU.add),
                                        reads=[u.name, 'f_cw', cv.name], writes=[cv.name])
                                P.op('pool', lambda e, u=u, hidx=hidx: e.tensor_copy(out=halo[:, hidx, :], in_=u[:, 512:514]),
                                     reads=[u.name], writes=[('f_halo', hidx)])
                                cvs.append(cv)
                            cg, cu = cvs
                            P.op('act', lambda e, cg=cg: e.activation(out=cg[:], in_=cg[:], func=AF.Silu),
                                 reads=[cg.name], writes=[cg.name])
                            P.op('dve', lambda e, cg=cg, cu=cu, j=j: e.tensor_tensor(
                                out=actv[:, j, :], in0=cg[:], in1=cu[:], op=ALU.mult),
                                reads=[cg.name, cu.name], writes=[('f_act', j)])
                        akeys = [('f_act', j) for j in range(11)]
                        for m in range(8):
                            ps = ps_o[m % 4]
                            for j in range(11):
                                P.op('pe', lambda e, j=j, m=m, ps=ps: e.matmul(
                                    ps[:], lhsT=wdn[:, j, m * 128:(m + 1) * 128], rhs=actv[:, j, :],
                                    start=(j == 0), stop=(j == 10)),
                                    reads=akeys + ['f_wdn'], writes=[ps.name], inc=(j == 10))
                            P.op('dve', lambda e, m=m, ps=ps, hacc=hacc: e.tensor_tensor(
                                out=hacc[:, m, :], in0=hacc[:, m, :], in1=ps[:], op=ALU.add),
                                reads=[ps.name, hacc.name], writes=[hacc.name])
                        dst = hT2 if half == 0 else hT
                        P.dma(pc(dst)[:, :, tsl], hacc[:], reads=[hacc.name])
                    P.barrier()
                    P.flush()

            if 'G' in stages:
              with contextlib.ExitStack() as st:
                sbt = lambda name, shape, dt=F32, u=uid(): st.enter_context(nc.sbuf_tensor(name + u, shape, dt))
                pst = lambda name, shape, dt=F32, u=uid(): st.enter_context(nc.psum_tensor(name + u, shape, dt))
                last = (l == depth - 1)
                wg = sbt("g_wg", [128, 8, D_MODEL], BF16)
                wp = sbt("g_wp", [128, 2, D_MODEL], BF16)
                wst = [sbt("g_wst%d" % i, [128, D_MODEL], F32) for i in range(2)]
                gain = sbt("g_gain", [128, 8], F32)
                gpn = sbt("g_gpn", [128, 8], F32)
                gfin = sbt("g_gfin", [128, 8], F32)
                P.dma(gain[:], ln_ple[l], writes=[gain.name])
                P.dma(gpn[:], ple_norm[l], writes=[gpn.name])
                P.dma(gfin[:], ln_final, writes=[gfin.name])
                for c in range(8):
                    s = wst[c % 2]
                    P.dma(s[:], w_gate[l, c * 128:(c + 1) * 128, :], writes=[s.name])
                    cast(wg[:, c, :], s[:], [s.name], ['g_wg'])
                for c in range(2):
                    s = wst[c % 2]
                    P.dma(s[:], w_ple[l, c * 128:(c + 1) * 128, :], writes=[s.name])
                    cast(wp[:, c, :], s[:], [s.name], ['g_wp'])
                h_r = [sbt("g_h%d" % i, [128, 8, 512], F32) for i in range(2)]
                sq = sbt("g_sq", [128, 8, 512], BF16)
                xn_r = [sbt("g_xn%d" % i, [128, 8, 512], BF16) for i in range(2)]
                rstd = sbt("g_rstd", [128, 512], F32)
                p32_r = [sbt("g_p32", [128, 2, 512], F32)] * 2
                pbf_r = [sbt("g_pbf", [128, 2, 512], BF16)] * 2
                y_r = [sbt("g_y", [128, 8, 512], F32)] * 2
                gt_r = [sbt("g_gt%d" % i, [128, 8, 512], F32) for i in range(2)]
                ps_n = pst("g_psn", [128, 512])
                ps_o = [pst("g_ps%d" % i, [128, 512]) for i in range(4)]
                for tt in range(NT):
                    tsl = slice(tt * 512, (tt + 1) * 512)
                    h, xn, p32, pbf, y, gt = h_r[tt % 2], xn_r[tt % 2], p32_r[tt % 2], pbf_r[tt % 2], y_r[tt % 2], gt_r[tt % 2]
                    P.dma(h[:], pc(hT)[:, :, tsl], writes=[h.name])
                    P.dma(p32[:], pc(pT[l])[:, :, tsl], writes=[p32.name])
                    cast(pbf[:], p32[:], [p32.name], [pbf.name])
                    emit_norm(h, gain, sq, ps_n, rstd, xn)
                    for m in range(8):
                        ps = ps_o[m % 4]
                        for c in range(8):
                            P.op('pe', lambda e, c=c, m=m, ps=ps, h=h, xn=xn, y=y, gt=gt, pbf=pbf, p32=p32: e.matmul(
                                ps[:], lhsT=wg[:, c, m * 128:(m + 1) * 128], rhs=xn[:, c, :],
                                start=(c == 0), stop=(c == 7)),
                                reads=[xn.name, 'g_wg'], writes=[ps.name], inc=(c == 7))
                        P.op('act', lambda e, m=m, ps=ps, h=h, xn=xn, y=y, gt=gt, pbf=pbf, p32=p32: e.activation(out=gt[:, m, :], in_=ps[:], func=AF.Sigmoid),
                             reads=[ps.name], writes=[gt.name])
                    for m in range(8):
                        ps = ps_o[m % 4]
                        for c in range(2):
                            P.op('pe', lambda e, c=c, m=m, ps=ps, h=h, xn=xn, y=y, gt=gt, pbf=pbf, p32=p32: e.matmul(
                                ps[:], lhsT=wp[:, c, m * 128:(m + 1) * 128], rhs=pbf[:, c, :],
                                start=(c == 0), stop=(c == 1)),
                                reads=[pbf.name, 'g_wp'], writes=[ps.name], inc=(c == 1))
                        cast(y[:, m, :], ps[:], [ps.name], [y.name], psum=True)
                    emit_norm(y, gpn, sq, ps_n, rstd, y)
                    for m in range(8):
                        P.op('dve', lambda e, m=m, h=h, xn=xn, y=y, gt=gt, pbf=pbf, p32=p32: e.tensor_tensor(
                            out=gt[:, m, :], in0=gt[:, m, :], in1=y[:, m, :], op=ALU.mult),
                            reads=[gt.name, y.name], writes=[gt.name])
                        P.op('pool', lambda e, m=m, h=h, xn=xn, y=y, gt=gt, pbf=pbf, p32=p32: e.tensor_tensor(
                            out=h[:, m, :], in0=h[:, m, :], in1=gt[:, m, :], op=ALU.add),
                            reads=[gt.name, h.name], writes=[h.name])
                    if last:
                        emit_norm(h, gfin, sq, ps_n, rstd, y)
                        P.dma(pc(outT)[:, :, tsl], y[:], reads=[y.name])
                    else:
                        P.dma(pc(hT)[:, :, tsl], h[:], reads=[h.name])
                P.barrier()
                P.flush()

        P.barrier()
        P.flush()
    return nc


def _col_perm():
    r = lambda a, b: list(range(a, b))
    nq = [c for h in (0, 3, 1, 4, 2, 5) for c in r(1548 + h * 64, 1548 + (h + 1) * 64)]
    fm = r(0, 1536) + nq + r(1932, 2060) + r(2060, 2188) + r(2188, 2316) + r(2444, 2572) \
        + r(2718, 2974) + r(2974, 3230)
    small = r(1536, 1548) + r(2700, 2718)
    tm = r(2316, 2444) + r(2572, 2700) + r(3230, 3486)
    return fm, small, tm


def prep_w_in(w_in):
    fm, small, tm = _col_perm()
    d = w_in.shape[0]
    out = np.zeros((d, D_MODEL, N_IN_PAD), np.float32)
    out[:, :, 0:len(fm)] = w_in[:, :, fm]
    out[:, :, 2944:2944 + len(small)] = w_in[:, :, small]
    out[:, :, 3072:3584] = w_in[:, :, tm]
    return out


def vec_pc(v):
    sh = v.shape
    c = sh[-1] // 128
    return np.ascontiguousarray(np.swapaxes(v.reshape(sh[:-1] + (c, 128)), -1, -2))


def prep_conv(w):
    d, k, n = w.shape
    return np.ascontiguousarray(w.transpose(0, 2, 1).reshape(d, n // 128, 128, k).transpose(0, 2, 1, 3))


def dup128(v):
    return np.ascontiguousarray(np.concatenate([v, v], axis=-1)[..., None])


def prep_w1(w):
    d = w.shape[0]
    a = w.reshape(d, 32, 64, 128).transpose(0, 2, 1, 3)
    return np.ascontiguousarray(np.concatenate([a, a], axis=1))


def prep_pe(pe):
    a = pe.transpose(0, 2, 1)
    return np.ascontiguousarray(np.concatenate([a, a], axis=1))


STAGES = "ABCDEFG"


def kernel(**inputs):
    f32 = lambda a: np.ascontiguousarray(np.asarray(a, dtype=np.float32))
    x = f32(inputs['x'])
    B, T, _ = x.shape
    depth = int(np.asarray(inputs['w_in']).shape[0])
    p = f32(inputs['p'])
    shared = dict(
        ln_mix=vec_pc(f32(inputs['ln_mix'])), ln_ffn=vec_pc(f32(inputs['ln_ffn'])),
        ln_ple=vec_pc(f32(inputs['ln_ple'])), ple_norm=vec_pc(f32(inputs['ple_norm'])),
        ln_final=vec_pc(f32(inputs['ln_final'])),
        w_in=prep_w_in(f32(inputs['w_in'])), w_out=f32(inputs['w_out']), w_up=f32(inputs['w_up']),
        ffn_conv=prep_conv(f32(inputs['ffn_conv'])), w_down=f32(inputs['w_down']),
        w_gate=f32(inputs['w_ple_gate']), w_ple=f32(inputs['w_ple']),
        sb_norm=dup128(f32(inputs['sb_norm'])), nsa_norm=dup128(f32(inputs['nsa_norm'])),
        gdn_norm=dup128(f32(inputs['gdn_norm'])),
        gdn_conv=prep_conv(f32(inputs['gdn_conv'])), gdn_alog=f32(inputs['gdn_a_log'])[:, None, :],
        gdn_dtb=f32(inputs['gdn_dt_bias'])[:, None, :], gdn_normrow=f32(inputs['gdn_norm'])[:, None, :],
        cmp_k_w1=prep_w1(f32(inputs['nsa_cmp_k_w1'])), cmp_v_w1=prep_w1(f32(inputs['nsa_cmp_v_w1'])),
        cmp_k_w2=f32(inputs['nsa_cmp_k_w2']), cmp_v_w2=f32(inputs['nsa_cmp_v_w2']),
        pe_kT=prep_pe(f32(inputs['nsa_pe_k'])), pe_vT=prep_pe(f32(inputs['nsa_pe_v'])),
    )
    in_maps = []
    for core in range(8):
        b = core % B
        m = dict(shared)
        m['xT'] = np.ascontiguousarray(x[b].T)
        m['pT'] = np.ascontiguousarray(p[:, b].transpose(0, 2, 1))
        m['pos'] = np.ascontiguousarray(np.asarray(inputs['positions'])[b:b + 1].astype(np.int32))
        in_maps.append(m)
    nc = build(T, depth, stages=STAGES)
    res = run_bass_kernel_spmd(nc, in_maps, core_ids=list(range(8)))
    out = np.stack([np.ascontiguousarray(np.asarray(res.results[b]['outT']).T) for b in range(B)], axis=0)
    return out.astype(np.float32)
```

```python
import contextlib
import numpy as np
import concourse.bass as bass
import concourse.mybir as mybir
from concourse.bass_utils import run_bass_kernel_spmd

F32 = mybir.dt.float32
BF16 = mybir.dt.bfloat16
I32 = mybir.dt.int32
AF = mybir.ActivationFunctionType
ALU = mybir.AluOpType
AX = mybir.AxisListType

D_MODEL = 1024
HD = 64
N_IN_PAD = 3584
D_FF = 2816
EPS = 1e-6
NDS = 24
CPARTS = {'cmp', 'topk', 'sel', 'win'}


class Prog:
    def __init__(self, nc, stack):
        self.nc = nc
        self.stack = stack
        self.cengs = ['pe', 'act', 'dve', 'pool']
        self.engs = self.cengs + ['sp']
        self.sem = {e: stack.enter_context(nc.semaphore("s_" + e)) for e in self.cengs}
        self.dsem = [stack.enter_context(nc.semaphore("d%d" % i)) for i in range(NDS)]
        self.dcount = [0] * NDS
        self.dnext = 0
        self.nseq = {e: 0 for e in self.cengs}
        self.known = {e: {} for e in self.engs}
        self.lastw = {}
        self.readers = {}
        self.q = {e: [] for e in self.engs}
        self.pending_noinc = {e: False for e in self.cengs}

    def _semof(self, sk):
        if isinstance(sk, tuple):
            return self.dsem[sk[1]]
        return self.sem[sk]

    def _collect(self, eng, reads, writes):
        deps = {}
        def add(ev):
            if ev is None:
                return
            sk, val = ev
            if deps.get(sk, 0) < val:
                deps[sk] = val
        for k in reads:
            add(self.lastw.get(k))
        for k in writes:
            add(self.lastw.get(k))
            for ev in self.readers.get(k, ()):
                add(ev)
        waits = []
        for sk, val in deps.items():
            if eng == 'pe' and sk == 'pe':
                continue
            if self.known[eng].get(sk, 0) < val:
                self.known[eng][sk] = val
                waits.append((sk, val))
        return waits

    def _record(self, ev, reads, writes):
        for k in reads:
            self.readers.setdefault(k, []).append(ev)
        for k in writes:
            self.lastw[k] = ev
            self.readers[k] = []

    def op(self, eng, fn, reads=(), writes=(), inc=True):
        waits = self._collect(eng, reads, writes)
        if inc:
            self.nseq[eng] += 1
            ev = (eng, self.nseq[eng])
            self.pending_noinc[eng] = False
        else:
            ev = (eng, self.nseq[eng] + 1)
            self.pending_noinc[eng] = True
        self.q[eng].append((waits, fn, eng if inc else None))
        self._record(ev, reads, writes)

    def dma(self, out, in_, reads=(), writes=(), queue='sp', **kw):
        waits = self._collect(queue, reads, writes)
        j = self.dnext
        self.dnext = (j + 1) % NDS
        prev = self.dcount[j]
        sk = ('d', j)
        if prev > 0 and self.known[queue].get(sk, 0) < prev:
            self.known[queue][sk] = prev
            waits.append((sk, prev))
        self.dcount[j] += 16
        ev = (sk, self.dcount[j])
        self.q[queue].append((waits, lambda e: e.dma_start(out=out, in_=in_, **kw), sk))
        self._record(ev, reads, writes)

    def barrier(self):
        for e in self.engs:
            waits = []
            for f in self.cengs:
                if f == e:
                    continue
                v = self.nseq[f]
                if v > 0 and self.known[e].get(f, 0) < v:
                    self.known[e][f] = v
                    waits.append((f, v))
            for j in range(NDS):
                v = self.dcount[j]
                sk = ('d', j)
                if v > 0 and self.known[e].get(sk, 0) < v:
                    self.known[e][sk] = v
                    waits.append((sk, v))
            if waits:
                self.q[e].append((waits, None, None))
        self.lastw = {}
        self.readers = {}

    def flush(self):
        nc = self.nc
        for e in self.cengs:
            assert not self.pending_noinc[e], e
        q = self.q
        self.q = {e: [] for e in self.engs}

        def emit(name, e):
            for waits, fn, inc in q[name]:
                for sk, val in waits:
                    e.wait_ge(self._semof(sk), val)
                if fn is None:
                    continue
                ins = fn(e)
                if inc is not None:
                    if isinstance(inc, tuple):
                        ins.then_inc(self.dsem[inc[1]], 16)
                    else:
                        ins.then_inc(self.sem[inc], 1)

        with nc.Block() as block:
            @block.tensor
            def _(e):
                emit('pe', e)

            @block.scalar
            def _(e):
                emit('act', e)

            @block.vector
            def _(e):
                emit('dve', e)

            @block.gpsimd
            def _(e):
                emit('pool', e)

            @block.sync
            def _(e):
                emit('sp', e)


class Ring:
    def __init__(self, items):
        self.items = items
        self.i = 0

    def next(self):
        it = self.items[self.i % len(self.items)]
        self.i += 1
        return it


def build(T, depth, debug=(), stages="ABCDEFG", mix_input=False, ext=()):
    nc = bass.Bass("TRN2", target_bir_lowering=False)
    NT = T // 512
    din = lambda name, shape, dt=F32: nc.dram_tensor(name, shape, dt, kind="ExternalInput").ap()
    dscr = lambda name, shape, dt=F32: nc.dram_tensor(
        name, shape, dt, kind=("ExternalOutput" if name in debug else "ExternalInput" if name in ext else "Internal")).ap()

    xT = din("xT", [D_MODEL, T])
    pT = din("pT", [depth, 256, T])
    ln_mix = din("ln_mix", [depth, 128, 8])
    ln_ffn = din("ln_ffn", [depth, 128, 8])
    ln_ple = din("ln_ple", [depth, 128, 8])
    ple_norm = din("ple_norm", [depth, 128, 8])
    ln_final = din("ln_final", [128, 8])
    w_in = din("w_in", [depth, D_MODEL, N_IN_PAD])
    sb_norm = din("sb_norm", [depth, 128, 1])
    nsa_norm = din("nsa_norm", [depth, 128, 1])
    gdn_norm = din("gdn_norm", [depth, 128, 1])
    pos = din("pos", [1, T], I32)
    cmp_k_w1 = din("cmp_k_w1", [depth, 128, 32, 128])
    cmp_v_w1 = din("cmp_v_w1", [depth, 128, 32, 128])
    cmp_k_w2 = din("cmp_k_w2", [depth, 128, 64])
    cmp_v_w2 = din("cmp_v_w2", [depth, 128, 64])
    pe_kT = din("pe_kT", [depth, 128, 32])
    pe_vT = din("pe_vT", [depth, 128, 32])
    gdn_conv = din("gdn_conv", [depth, 128, 9, 4])
    gdn_alog = din("gdn_alog", [depth, 1, 6])
    gdn_dtb = din("gdn_dtb", [depth, 1, 6])
    gdn_normrow = din("gdn_normrow", [depth, 1, 64])
    gtok = dscr("gtok", [T, 13 * 128])
    w_out = din("w_out", [depth, D_MODEL, D_MODEL])
    w_up = din("w_up", [depth, D_MODEL, 2 * D_FF])
    ffn_conv = din("ffn_conv", [depth, 128, 44, 3])
    w_down = din("w_down", [depth, D_FF, D_MODEL])
    w_gate = din("w_gate", [depth, D_MODEL, D_MODEL])
    w_ple = din("w_ple", [depth, 256, D_MODEL])
    outT = nc.dram_tensor("outT", [D_MODEL, T], F32, kind="ExternalOutput").ap()
    projF = dscr("projF", [24 * 128, T])
    projT = dscr("projT", [T, 512])
    hT = dscr("hT", [D_MODEL, T])
    hT2 = dscr("hT2", [D_MODEL, T])
    if mix_input:
        mixT = din("mixT", [D_MODEL, T], BF16)
    else:
        mixT = dscr("mixT", [D_MODEL, T], BF16)
    pc = lambda ap: ap.rearrange("(c p) t -> p c t", p=128)

    with contextlib.ExitStack() as stack:
        P = Prog(nc, stack)
        sb = lambda name, shape, dt=F32: stack.enter_context(nc.sbuf_tensor(name, shape, dt))

        ones_bf = sb("ones_bf", [128, 128], BF16)
        P.op('pool', lambda e: e.memset(ones_bf[:], 1.0), writes=['ones_bf'])
        eps_t = sb("eps_t", [128, 1], F32)
        P.op('pool', lambda e: e.memset(eps_t[:], EPS), writes=['eps_t'])

        one_t = sb("one_t", [128, 1], F32)
        P.op('pool', lambda e: e.memset(one_t[:], 1.0), writes=['one_t'])
        ones512 = sb("ones512", [128, 512], BF16)
        P.op('pool', lambda e: e.memset(ones512[:], 1.0), writes=['ones512'])
        negOnes = sb("negOnes", [128, 128], BF16)
        P.op('pool', lambda e: e.memset(negOnes[:], -1.0), writes=['negOnes'])
        negU = sb("negU", [128, 128], BF16)
        P.op('pool', lambda e: e.affine_select(out=negU[:], in_=negOnes[:], pattern=[[-1, 128]],
                                               compare_op=ALU.is_ge, fill=0.0, base=0, channel_multiplier=1),
             reads=['negOnes'], writes=['negU'])
        dmask = sb("dmask", [128, 4, 512], BF16)
        for j in range(4):
            P.op('pool', lambda e, j=j: e.affine_select(out=dmask[:, j, :], in_=ones512[:], pattern=[[1, 512]],
                                                        compare_op=ALU.is_gt, fill=0.0, base=-128 * j,
                                                        channel_multiplier=-1),
                 reads=['ones512'], writes=['dmask'])
        cast_rr = [0]
        uidc = [0]

        def uid():
            uidc[0] += 1
            return '_%d' % uidc[0]

        def cast(dst, src, reads, writes, psum=False):
            eng = (['dve', 'act'][cast_rr[0] % 2]) if psum else (['pool', 'dve', 'act'][cast_rr[0] % 3])
            cast_rr[0] += 1
            if eng == 'act':
                P.op('act', lambda e: e.copy(out=dst, in_=src), reads=reads, writes=writes)
            else:
                P.op(eng, lambda e: e.tensor_copy(out=dst, in_=src), reads=reads, writes=writes)

        def emit_norm(h, gain, sq, ps_n, rstd, xn, nfeat=D_MODEL):
            nch = nfeat // 128
            P.op('act', lambda e: e.activation(out=sq[:], in_=h[:], func=AF.Square),
                 reads=[h.name], writes=[sq.name])
            for c in range(nch):
                P.op('pe', lambda e, c=c: e.matmul(ps_n[:], lhsT=ones_bf[:], rhs=sq[:, c, :],
                                                   start=(c == 0), stop=(c == nch - 1)),
                     reads=[sq.name, 'ones_bf'], writes=[ps_n.name], inc=(c == nch - 1))
            P.op('act', lambda e: e.activation(out=rstd[:], in_=ps_n[:], func=AF.Ln,
                                               bias=eps_t[:], scale=1.0 / nfeat),
                 reads=[ps_n.name, 'eps_t'], writes=[rstd.name])
            P.op('act', lambda e: e.activation(out=rstd[:], in_=rstd[:], func=AF.Exp, scale=-0.5),
                 reads=[rstd.name], writes=[rstd.name])
            for c in range(nch):
                P.op('dve', lambda e, c=c: e.scalar_tensor_tensor(
                    out=xn[:, c, :], in0=h[:, c, :], scalar=gain[:, c:c + 1], in1=rstd[:],
                    op0=ALU.mult, op1=ALU.mult),
                    reads=[h.name, gain.name, rstd.name], writes=[xn.name])

        NKB = T // 128
        NCMP = T // 16 - 1
        BIGM = 30000.0
        ident_bf = sb("ident_bf", [128, 128], BF16)
        P.op('pool', lambda e: e.affine_select(out=ident_bf[:], in_=ones_bf[:], pattern=[[-1, 128]],
                                               compare_op=ALU.is_equal, fill=0.0, base=0, channel_multiplier=1),
             reads=['ones_bf'], writes=['ident_bf'])
        ones_f = sb("ones_f", [128, 128], F32)
        P.op('pool', lambda e: e.memset(ones_f[:], 1.0), writes=['ones_f'])
        ident_f = sb("ident_f", [128, 128], F32)
        P.op('pool', lambda e: e.affine_select(out=ident_f[:], in_=ones_f[:], pattern=[[-1, 128]],
                                               compare_op=ALU.is_equal, fill=0.0, base=0, channel_multiplier=1),
             reads=['ones_f'], writes=['ident_f'])
        Uincl = sb("Uincl", [128, 128], F32)
        Lstrict = sb("Lstrict", [128, 128], F32)
        Lincl = sb("Lincl", [128, 128], F32)
        P.op('pool', lambda e: e.affine_select(out=Uincl[:], in_=ones_f[:], pattern=[[1, 128]], compare_op=ALU.is_ge,
                                               fill=0.0, base=0, channel_multiplier=-1), reads=['ones_f'], writes=['Uincl'])
        P.op('pool', lambda e: e.affine_select(out=Lstrict[:], in_=ones_f[:], pattern=[[-1, 128]], compare_op=ALU.is_gt,
                                               fill=0.0, base=0, channel_multiplier=1), reads=['ones_f'], writes=['Lstrict'])
        P.op('pool', lambda e: e.affine_select(out=Lincl[:], in_=ones_f[:], pattern=[[-1, 128]], compare_op=ALU.is_ge,
                                               fill=0.0, base=0, channel_multiplier=1), reads=['ones_f'], writes=['Lincl'])
        LBD = sb("LBD", [128, 128], F32)
        LOD = sb("LOD", [128, 128], F32)
        for cb in range(4):
            csl = slice(cb * 32, (cb + 1) * 32)
            P.op('pool', lambda e, csl=csl, cb=cb: e.affine_select(out=LBD[:, csl], in_=Lstrict[:, csl], pattern=[[0, 32]],
                                                                   compare_op=ALU.is_ge, fill=0.0, base=-32 * cb, channel_multiplier=1),
                 reads=['Lstrict'], writes=['LBD'])
            P.op('pool', lambda e, csl=csl, cb=cb: e.affine_select(out=LBD[:, csl], in_=LBD[:, csl], pattern=[[0, 32]],
                                                                   compare_op=ALU.is_ge, fill=0.0, base=32 * cb + 31, channel_multiplier=-1),
                 reads=['LBD'], writes=['LBD'])
        P.op('pool', lambda e: e.tensor_tensor(out=LOD[:], in0=Lstrict[:], in1=LBD[:], op=ALU.subtract),
             reads=['Lstrict', 'LBD'], writes=['LOD'])
        dmaskI = sb("dmaskI", [128, 4, 512], BF16)
        wmask = sb("wmask", [128, 4, 512], BF16)
        for j in range(4):
            P.op('pool', lambda e, j=j: e.affine_select(out=dmaskI[:, j, :], in_=ones512[:], pattern=[[1, 512]],
                                                        compare_op=ALU.is_ge, fill=0.0, base=-128 * j,
                                                        channel_multiplier=-1),
                 reads=['ones512'], writes=['dmaskI'])
            P.op('pool', lambda e, j=j: e.affine_select(out=wmask[:, j, :], in_=ones512[:], pattern=[[-1, 512]],
                                                        compare_op=ALU.is_ge, fill=0.0, base=128 * j - 1,
                                                        channel_multiplier=1),
                 reads=['ones512'], writes=['wmask'])
        Psw = sb("Psw", [128, 128], BF16)
        for mb, base in ((0, -32), (1, 0), (2, -96), (3, -64)):
            P.op('pool', lambda e, mb=mb, base=base: e.affine_select(
                out=Psw[:, mb * 32:(mb + 1) * 32], in_=ones_bf[:, 0:32], pattern=[[-1, 32]],
                compare_op=ALU.is_equal, fill=0.0, base=base, channel_multiplier=1),
                reads=['ones_bf'], writes=['Psw'])
        cmask = sb("cmask", [128, 2, T], BF16)
        Ebig = sb("Ebig", [128, T], BF16)
        oh = sb("oh", [128, 18, 64], BF16)
        ov = sb("ov", [128, 2, 64], BF16)
        keepB = sb("keepB", [128, 128], F32)
        addcB = sb("addcB", [128, 128], F32)
        cosT = sb("cosT", [128, T], BF16)
        sinS = sb("sinS", [128, T], BF16)
        _tmp_scope = contextlib.ExitStack()
        sbtmp = lambda name, shape, dt=F32: _tmp_scope.enter_context(nc.sbuf_tensor(name, shape, dt))
        onesT = _tmp_scope.enter_context(nc.sbuf_tensor("onesT", [128, T], BF16))
        P.op('pool', lambda e: e.memset(onesT[:], 1.0), writes=['onesT'])
        for ic in range(2):
            P.op('pool', lambda e, ic=ic: e.affine_select(out=cmask[:, ic, :], in_=onesT[:], pattern=[[1, T]],
                                                          compare_op=ALU.is_ge, fill=0.0, base=-31 - 2048 * ic,
                                                          channel_multiplier=-16),
                 reads=['onesT'], writes=['cmask'])
        P.op('pool', lambda e: e.memset(Ebig[:], BIGM), writes=['Ebig'])
        for ph in range(2):
            psl = slice(ph * 64, (ph + 1) * 64)
            P.op('pool', lambda e, psl=psl: e.affine_select(out=Ebig[psl, :], in_=Ebig[psl, :], pattern=[[1, T]], compare_op=ALU.is_ge,
                                                            fill=0.0, base=0, channel_multiplier=-64),
                 reads=['Ebig'], writes=['Ebig'])
            P.op('pool', lambda e, psl=psl: e.affine_select(out=Ebig[psl, :], in_=Ebig[psl, :], pattern=[[-1, T]], compare_op=ALU.is_ge,
                                                            fill=0.0, base=63, channel_multiplier=64),
                 reads=['Ebig'], writes=['Ebig'])
        ones18 = sbtmp("ones18", [128, 18, 64], BF16)
        P.op('pool', lambda e: e.memset(ones18[:], 1.0), writes=['ones18'])
        P.op('pool', lambda e: e.affine_select(out=oh[:], in_=ones18[:], pattern=[[-1, 18], [0, 64]],
                                               compare_op=ALU.is_equal, fill=0.0, base=-12, channel_multiplier=1),
             reads=['ones18'], writes=['oh'])
        ovA = sbtmp("ovA", [128, 2, 64], I32)
        P.op('pool', lambda e: e.iota(ovA[:], pattern=[[2048, 2], [-64, 64]], base=0, channel_multiplier=16),
             writes=['ovA'])
        ovF = sbtmp("ovF", [128, 2, 64], F32)
        ovG = sbtmp("ovG", [128, 2, 64], F32)
        P.op('dve', lambda e: e.tensor_copy(out=ovF[:], in_=ovA[:]), reads=['ovA'], writes=['ovF'])
        P.op('dve', lambda e: e.tensor_scalar(out=ovG[:], in0=ovF[:], scalar1=32.0, scalar2=64.0, op0=ALU.add, op1=ALU.min),
             reads=['ovF'], writes=['ovG'])
        P.op('dve', lambda e: e.tensor_scalar(out=ovF[:], in0=ovF[:], scalar1=0.0, scalar2=None, op0=ALU.max),
             reads=['ovF'], writes=['ovF'])
        P.op('dve', lambda e: e.tensor_tensor(out=ovG[:], in0=ovG[:], in1=ovF[:], op=ALU.subtract),
             reads=['ovF', 'ovG'], writes=['ovG'])
        P.op('dve', lambda e: e.tensor_scalar(out=ov[:], in0=ovG[:], scalar1=0.0, scalar2=1.0 / 32, op0=ALU.max, op1=ALU.mult),
             reads=['ovG'], writes=['ov'])
        negf = sbtmp("negf", [128, 128], F32)
        P.op('pool', lambda e: e.memset(negf[:], -1.0), writes=['negf'])
        for ph in range(2):
            psl = slice(ph * 64, (ph + 1) * 64)
            P.op('pool', lambda e, psl=psl, ph=ph: e.affine_select(
                out=keepB[psl, :], in_=ones_f[psl, :], pattern=[[-1, 128]], compare_op=ALU.is_ge, fill=0.0,
                base=62 + ph, channel_multiplier=0), reads=['ones_f'], writes=['keepB'])
            P.op('pool', lambda e, psl=psl, ph=ph: e.affine_select(
                out=addcB[psl, :], in_=negf[psl, :], pattern=[[1, 128]], compare_op=ALU.is_gt, fill=1e4,
                base=-64 - ph, channel_multiplier=0), reads=['negf'], writes=['addcB'])
            P.op('pool', lambda e, psl=psl, ph=ph: e.affine_select(
                out=addcB[psl, :], in_=addcB[psl, :], pattern=[[1, 128]], compare_op=ALU.is_ge, fill=0.0,
                base=-64 - ph + 1, channel_multiplier=0), reads=['addcB'], writes=['addcB'])
        with contextlib.ExitStack() as st:
            sbt = lambda name, shape, dt=F32, u=uid(): st.enter_context(nc.sbuf_tensor(name + u, shape, dt))
            pos_i = sbt("pos_i", [128, T], I32)
            ang = sbt("ang", [128, T], F32)
            tmpa = sbt("tmpa", [128, T], F32)
            ki = sbt("ki", [128, T], I32)
            tmpb = sbt("tmpb", [128, T], F32)
            pidx = sbt("pidx", [128, 1], I32)
            pf = sbt("pf", [128, 1], F32)
            inv = sbt("inv", [128, 1], F32)
            sgn = sbt("sgn", [128, 1], F32)
            P.dma(pos_i[:], pos.partition_broadcast(128), writes=['pos_i'])
            P.op('pool', lambda e: e.iota(pidx[:], pattern=[[0, 1]], base=0, channel_multiplier=1), writes=['pidx'])
            c123 = sbt("c123", [128, 3], F32)
            fl = sbt("fl", [128, 1], F32)
            P.op('dve', lambda e: e.tensor_copy(out=pf[:], in_=pidx[:]), reads=['pidx'], writes=['pf'])
            for i3, thr in enumerate((32.0, 64.0, 96.0)):
                P.op('dve', lambda e, i3=i3, thr=thr: e.tensor_scalar(out=c123[:, i3:i3 + 1], in0=pf[:], scalar1=thr,
                                                                       scalar2=None, op0=ALU.is_ge),
                     reads=['pf'], writes=['c123'])
            P.op('dve', lambda e: e.tensor_tensor(out=fl[:], in0=c123[:, 0:1], in1=c123[:, 1:2], op=ALU.add),
                 reads=['c123'], writes=['fl'])
            P.op('dve', lambda e: e.tensor_tensor(out=fl[:], in0=fl[:], in1=c123[:, 2:3], op=ALU.add),
                 reads=['c123', 'fl'], writes=['fl'])
            P.op('dve', lambda e: e.scalar_tensor_tensor(out=fl[:], in0=fl[:], scalar=-32.0, in1=pf[:], op0=ALU.mult, op1=ALU.add),
                 reads=['fl', 'pf'], writes=['fl'])
            P.op('act', lambda e: e.activation(out=inv[:], in_=fl[:], func=AF.Exp, scale=-float(np.log(10000.0)) / 32),
                 reads=['fl'], writes=['inv'])
            P.op('dve', lambda e: e.tensor_tensor(out=sgn[:], in0=c123[:, 0:1], in1=c123[:, 1:2], op=ALU.subtract),
                 reads=['c123'], writes=['sgn'])
            P.op('dve', lambda e: e.tensor_tensor(out=sgn[:], in0=sgn[:], in1=c123[:, 2:3], op=ALU.add),
                 reads=['c123', 'sgn'], writes=['sgn'])
            P.op('dve', lambda e: e.tensor_scalar(out=sgn[:], in0=sgn[:], scalar1=2.0, scalar2=-1.0, op0=ALU.mult, op1=ALU.add),
                 reads=['sgn'], writes=['sgn'])
            P.op('dve', lambda e: e.tensor_copy(out=ang[:], in_=pos_i[:]), reads=['pos_i'], writes=['ang'])
            P.op('dve', lambda e: e.tensor_scalar(out=ang[:], in0=ang[:], scalar1=inv[:, 0:1], scalar2=None, op0=ALU.mult),
                 reads=['ang', 'inv'], writes=['ang'])
            TWO_PI = 2.0 * float(np.pi)
            for which in range(2):
                src = ang
                if which == 1:
                    P.op('dve', lambda e: e.tensor_scalar(out=ang[:], in0=ang[:], scalar1=float(np.pi) / 2, scalar2=None, op0=ALU.add),
                         reads=['ang'], writes=['ang'])
                P.op('dve', lambda e: e.tensor_scalar(out=tmpa[:], in0=ang[:], scalar1=1.0 / TWO_PI, scalar2=None, op0=ALU.mult),
                     reads=['ang'], writes=['tmpa'])
                P.op('dve', lambda e: e.tensor_copy(out=ki[:], in_=tmpa[:]), reads=['tmpa'], writes=['ki'])
                P.op('dve', lambda e: e.tensor_copy(out=tmpa[:], in_=ki[:]), reads=['ki'], writes=['tmpa'])
                P.op('dve', lambda e: e.scalar_tensor_tensor(out=tmpa[:], in0=tmpa[:], scalar=-TWO_PI, in1=ang[:],
                                                             op0=ALU.mult, op1=ALU.add),
                     reads=['tmpa', 'ang'], writes=['tmpa'])
                for thr, opc, delta in ((float(np.pi), ALU.is_gt, -TWO_PI), (-float(np.pi), ALU.is_lt, TWO_PI)):
                    P.op('dve', lambda e, thr=thr, opc=opc, delta=delta: e.tensor_scalar(
                        out=tmpb[:], in0=tmpa[:], scalar1=thr, scalar2=delta, op0=opc, op1=ALU.mult),
                        reads=['tmpa'], writes=['tmpb'])
                    P.op('dve', lambda e: e.tensor_tensor(out=tmpa[:], in0=tmpa[:], in1=tmpb[:], op=ALU.add),
                         reads=['tmpa', 'tmpb'], writes=['tmpa'])
                P.op('dve', lambda e: e.tensor_scalar(out=tmpa[:], in0=tmpa[:], scalar1=3.14159, scalar2=-3.14159,
                                                      op0=ALU.min, op1=ALU.max), reads=['tmpa'], writes=['tmpa'])
                if which == 0:
                    P.op('act', lambda e: e.activation(out=sinS[:], in_=tmpa[:], func=AF.Sin, scale=sgn[:, 0:1]),
                         reads=['tmpa', 'sgn'], writes=['sinS'])
                else:
                    P.op('act', lambda e: e.activation(out=cosT[:], in_=tmpa[:], func=AF.Sin),
                         reads=['tmpa'], writes=['cosT'])
            P.barrier()
            P.flush()
        _tmp_scope.close()
        if not ('C' in stages and 'D' in stages) and not mix_input:
            zt = sb("zt", [128, 2048], BF16)
            P.op('pool', lambda e: e.memset(zt[:], 0.0), writes=['zt'])
            for r0 in range(0, 768, 128):
                for t0 in range(0, T, 2048):
                    n = min(2048, T - t0)
                    P.dma(mixT[r0:r0 + 128, t0:t0 + n], zt[:, 0:n], reads=['zt'])
            P.barrier()
            P.flush()
        for l in range(depth):
            hsrc = xT if l == 0 else hT
            if 'A' in stages:
              with contextlib.ExitStack() as st:
                sbt = lambda name, shape, dt=F32, u=uid(): st.enter_context(nc.sbuf_tensor(name + u, shape, dt))
                pst = lambda name, shape, dt=F32, u=uid(): st.enter_context(nc.psum_tensor(name + u, shape, dt))
                wbf = sbt("a_wbf", [128, 8, N_IN_PAD], BF16)
                wst = [sbt("a_wst%d" % i, [128, N_IN_PAD // 2], F32) for i in range(2)]
                gain = sbt("a_gain", [128, 8], F32)
                P.dma(gain[:], ln_mix[l], writes=[gain.name])
                for c in range(8):
                    for hf in range(2):
                        s = wst[hf]
                        csl = slice(hf * (N_IN_PAD // 2), (hf + 1) * (N_IN_PAD // 2))
                        P.dma(s[:], w_in[l, c * 128:(c + 1) * 128, csl], writes=[s.name])
                        cast(wbf[:, c, csl], s[:], [s.name], ['a_wbf'])
                h_sb = [sbt("a_h%d" % i, [128, 8, 512], F32) for i in range(2)]
                sq = sbt("a_sq", [128, 8, 512], BF16)
                xn = [sbt("a_xn%d" % i, [128, 8, 512], BF16) for i in range(2)]
                rstd = sbt("a_rstd", [128, 512], F32)
                ps_n = pst("a_psn", [128, 512])
                ps_o = [pst("a_pso%d" % i, [128, 512]) for i in range(4)]
                ost = [sbt("a_ost%d" % i, [128, 512], F32) for i in range(4)]
                for tt in range(NT):
                    tsl = slice(tt * 512, (tt + 1) * 512)
                    h = h_sb[tt % 2]
                    x_ = xn[tt % 2]
                    P.dma(h[:], pc(hsrc)[:, :, tsl], writes=[h.name])
                    emit_norm(h, gain, sq, ps_n, rstd, x_)
                    for m in range(24):
                        ps = ps_o[m % 4]
                        o = ost[m % 4]
                        for c in range(8):
                            P.op('pe', lambda e, c=c, m=m, ps=ps, x_=x_: e.matmul(
                                ps[:], lhsT=wbf[:, c, m * 128:(m + 1) * 128], rhs=x_[:, c, :],
                                start=(c == 0), stop=(c == 7)),
                                reads=[x_.name, 'a_wbf'], writes=[ps.name], inc=(c == 7))
                        cast(o[:], ps[:], [ps.name], [o.name], psum=True)
                        P.dma(projF[m * 128:(m + 1) * 128, tsl], o[:], reads=[o.name], queue='act')
                    for ts in range(4):
                        ps = ps_o[ts % 4]
                        o = ost[ts % 4]
                        for c in range(8):
                            P.op('pe', lambda e, c=c, ts=ts, ps=ps, x_=x_: e.matmul(
                                ps[:], lhsT=x_[:, c, ts * 128:(ts + 1) * 128], rhs=wbf[:, c, 3072:3584],
                                start=(c == 0), stop=(c == 7)),
                                reads=[x_.name, 'a_wbf'], writes=[ps.name], inc=(c == 7))
                        cast(o[:], ps[:], [ps.name], [o.name], psum=True)
                        P.dma(projT[tt * 512 + ts * 128: tt * 512 + (ts + 1) * 128, :], o[:], reads=[o.name], queue='act')
                P.barrier()
                P.flush()

            if 'B' in stages:
              with contextlib.ExitStack() as st:
                sbt = lambda name, shape, dt=F32, u=uid(): st.enter_context(nc.sbuf_tensor(name + u, shape, dt))
                pst = lambda name, shape, dt=F32, u=uid(): st.enter_context(nc.psum_tensor(name + u, shape, dt))
                NKB = T // 128
                qT = sbt("b_qT", [128, 2, T], BF16)
                kT = sbt("b_kT", [128, 2, T], BF16)
                vv = sbt("b_v", [128, NKB, 256], BF16)
                stg = [sbt("b_stg%d" % i, [128, 2048], F32) for i in range(2)]
                nw = sbt("b_nw", [128, 1], F32)
                P.dma(nw[:], sb_norm[l], writes=[nw.name])
                k = 0
                for c in range(2):
                    for t0 in range(0, T, 2048):
                        n = min(2048, T - t0)
                        s = stg[k % 2]; k += 1
                        P.dma(s[:, 0:n], projF[(19 + c) * 128:(20 + c) * 128, t0:t0 + n], writes=[s.name])
                        P.op('dve', lambda e, s=s, c=c, t0=t0, n=n: e.tensor_scalar(
                            out=qT[:, c, t0:t0 + n], in0=s[:, 0:n], scalar1=0.125, scalar2=None, op0=ALU.mult),
                            reads=[s.name], writes=['b_qT'])
                        s = stg[k % 2]; k += 1
                        P.dma(s[:, 0:n], projF[(21 + c) * 128:(22 + c) * 128, t0:t0 + n], writes=[s.name])
                        cast(kT[:, c, t0:t0 + n], s[:, 0:n], [s.name], ['b_kT'])
                pTv = projT.rearrange("(kb p) f -> p kb f", p=128)
                for kb0 in range(0, NKB, 8):
                    s = stg[k % 2]; k += 1
                    sv = s[:].rearrange("p (a f) -> p a f", f=256)
                    P.dma(sv, pTv[:, kb0:kb0 + 8, 256:512], writes=[s.name])
                    cast(vv[:, kb0:kb0 + 8, :], sv, [s.name], ['b_v'])
                e_sb = [sbt("b_e%d" % i, [128, 512], F32) for i in range(3)]
                sp_sb = [sbt("b_sp%d" % i, [128, 512], BF16) for i in range(3)]
                a_sb = [sbt("b_a%d" % i, [128, 512], BF16) for i in range(3)]
                racc = sbt("b_racc", [128, 512], BF16)
                sqo = sbt("b_sqo", [64, 512], BF16)
                rs = sbt("b_rs", [64, 512], F32)
                o_sb = [sbt("b_o%d" % i, [64, 512], BF16) for i in range(2)]
                psZ = [pst("b_psZ%d" % i, [128, 512]) for i in range(2)]
                psA = [pst("b_psA%d" % i, [128, 512]) for i in range(2)]
                psO = [pst("b_psO%d" % i, [64, 512]) for i in range(2)]
                psN = pst("b_psN", [64, 512])
                it3 = [0]; it4 = [0]
                ho = 0
                for hh in range(4):
                    c = hh // 2
                    b0 = (hh % 2) * 64
                    for qt in range(NT):
                        qsl = slice(qt * 512, (qt + 1) * 512)
                        po = psO[ho % 2]
                        oo = o_sb[ho % 2]
                        ho += 1
                        nblk = 4 * (qt + 1)
                        kbs = list(reversed(range(nblk)))
                        nb_ = len(kbs)
                        st_ = {}

                        def S1(t, kbs=kbs, qt=qt, b0=b0, c=c, qsl=qsl, st_=st_):
                            kb = kbs[t]
                            j = kb - 4 * qt
                            ksl = slice(kb * 128, (kb + 1) * 128)
                            pz = psZ[it3[0] % 2]; ee = e_sb[it3[0] % 3]; sp = sp_sb[it3[0] % 3]; it3[0] += 1
                            st_[t] = dict(kb=kb, j=j, ksl=ksl, sp=sp, ee=ee)
                            P.op('pe', lambda e: e.matmul(
                                pz[:], lhsT=kT[b0:b0 + 64, c, ksl], rhs=qT[b0:b0 + 64, c, qsl], start=True, stop=True),
                                reads=['b_kT', 'b_qT'], writes=[pz.name])
                            P.op('act', lambda e: e.activation(out=ee[:], in_=pz[:], func=AF.Exp),
                                 reads=[pz.name], writes=[ee.name])
                            P.op('act', lambda e: e.activation(out=sp[:], in_=ee[:], func=AF.Ln, bias=one_t[:]),
                                 reads=[ee.name, 'one_t'], writes=[sp.name])
                            if j >= 0:
                                P.op('dve', lambda e: e.tensor_tensor(out=sp[:], in0=sp[:], in1=dmask[:, j, :], op=ALU.mult),
                                     reads=[sp.name, 'dmask'], writes=[sp.name])

                        def S2(t, b0=b0, c=c, qsl=qsl, st_=st_):
                            d_ = st_[t]
                            kb, j, ksl, sp = d_['kb'], d_['j'], d_['ksl'], d_['sp']
                            first = (t == 0)
                            pa = psA[it4[0] % 2]; aa = a_sb[it4[0] % 3]; it4[0] += 1
                            d_['aa'] = aa
                            ee = d_['ee']
                            P.op('pe', lambda e: e.matmul(pa[:], lhsT=negU[:], rhs=sp[:], start=True, stop=first),
                                 reads=[sp.name, 'negU'], writes=[pa.name], inc=first)
                            if not first:
                                P.op('pe', lambda e: e.matmul(pa[:], lhsT=negOnes[:], rhs=racc[:], start=False, stop=True),
                                     reads=['b_racc', 'negOnes'], writes=[pa.name])
                            P.op('act', lambda e: e.activation(out=aa[:], in_=pa[:], func=AF.Exp),
                                 reads=[pa.name], writes=[aa.name])
                            P.op('dve', lambda e: e.tensor_tensor(out=aa[:], in0=aa[:], in1=ee[:], op=ALU.mult),
                                 reads=[aa.name, ee.name], writes=[aa.name])
                            if j >= 0:
                                P.op('dve', lambda e: e.tensor_tensor(out=aa[:], in0=aa[:], in1=dmask[:, j, :], op=ALU.mult),
                                     reads=[aa.name, 'dmask'], writes=[aa.name])
                            if kb > 0:
                                if first:
                                    P.op('pool', lambda e: e.tensor_copy(out=racc[:], in_=sp[:]),
                                         reads=[sp.name], writes=['b_racc'])
                                else:
                                    P.op('pool', lambda e: e.tensor_tensor(out=racc[:], in0=racc[:], in1=sp[:], op=ALU.add),
                                         reads=[sp.name, 'b_racc'], writes=['b_racc'])

                        def S3(t, po=po, hh=hh, st_=st_, nb_=nb_):
                            d_ = st_[t]
                            kb, aa = d_['kb'], d_['aa']
                            P.op('pe', lambda e: e.matmul(
                                po[:], lhsT=vv[:, kb, hh * 64:(hh + 1) * 64], rhs=aa[:], start=(t == 0), stop=(t == nb_ - 1)),
                                reads=[aa.name, 'b_v'], writes=[po.name], inc=True)

                        for t in range(nb_ + 2):
                            if t < nb_:
                                S1(t)
                            if 1 <= t <= nb_:
                                S2(t - 1)
                            if t >= 2:
                                S3(t - 2)
                        P.op('act', lambda e, po=po: e.activation(out=sqo[:], in_=po[:], func=AF.Square),
                             reads=[po.name], writes=['b_sqo'])
                        P.op('pe', lambda e: e.matmul(psN[:], lhsT=ones_bf[0:64, 0:64], rhs=sqo[:], start=True, stop=True),
                             reads=['b_sqo', 'ones_bf'], writes=['b_psN'])
                        P.op('act', lambda e: e.activation(out=rs[:], in_=psN[:], func=AF.Ln, bias=eps_t[0:64, :], scale=1.0 / 64),
                             reads=['b_psN', 'eps_t'], writes=['b_rs'])
                        P.op('act', lambda e: e.activation(out=rs[:], in_=rs[:], func=AF.Exp, scale=-0.5),
                             reads=['b_rs'], writes=['b_rs'])
                        P.op('dve', lambda e, po=po, oo=oo: e.scalar_tensor_tensor(
                            out=oo[:], in0=po[:], scalar=nw[0:64, :], in1=rs[:], op0=ALU.mult, op1=ALU.mult),
                            reads=[po.name, nw.name, 'b_rs'], writes=[oo.name])
                        P.dma(mixT[768 + hh * 64:768 + (hh + 1) * 64, qsl], oo[:], reads=[oo.name])
                P.barrier()
                P.flush()

            if 'C' in stages:
              with contextlib.ExitStack() as st:
                sbt = lambda name, shape, dt=F32, u=uid(): st.enter_context(nc.sbuf_tensor(name + u, shape, dt))
                qT = sbt("c_qT", [128, 3, T], BF16)
                ksT = sbt("c_ksT", [128, T], BF16)
                kwT = sbt("c_kwT", [128, T], BF16)
                vs = sbt("c_vs", [128, NKB, 128], BF16)
                vw = sbt("c_vw", [128, NKB, 128], BF16)
                sgT = sbt("c_sgT", [128, T], BF16)
                selT = sbt("c_selT", [128, T], BF16)
                kcT = sbt("c_kcT", [128, 256], BF16)
                vc = sbt("c_vc", [128, 2, 2, 64], BF16)
                nw = sbt("c_nw", [128, 1], F32)
                P.dma(nw[:], nsa_norm[l], writes=[nw.name])
                ICS = [(ic, min(128, NCMP - ic * 128)) for ic in range(2) if NCMP - ic * 128 > 0]
                with contextlib.ExitStack() as s1:
                    sb1 = lambda name, shape, dt=F32, u=uid(): s1.enter_context(nc.sbuf_tensor(name + u, shape, dt))
                    ps1 = lambda name, shape, dt=F32, u=uid(): s1.enter_context(nc.psum_tensor(name + u, shape, dt))
                    stg = [sb1("c_stg%d" % i, [128, 2048], F32) for i in range(2)]
                    kcr = sb1("c_kcr", [128, T], BF16)
                    vcb = sb1("c_vcb", [128, T], BF16)
                    xb_r = [sb1("c_xb%d" % i, [128, 512], BF16) for i in range(2)]
                    t1_r = [sb1("c_t1%d" % i, [128, 512], F32) for i in range(2)]
                    t2_r = [sb1("c_t2%d" % i, [128, 512], F32) for i in range(2)]
                    psR = [ps1("c_psR%d" % i, [128, 512]) for i in range(2)]
                    cnt = [0, 0]

                    def load_chunk(ch, fn):
                        for t0 in range(0, T, 2048):
                            n = min(2048, T - t0)
                            s = stg[cnt[0] % 2]; cnt[0] += 1
                            P.dma(s[:, 0:n], projF[ch * 128:(ch + 1) * 128, t0:t0 + n], writes=[s.name])
                            fn(s, t0, n)

                    def rope_to(dst, dkey, scale):
                        def fn(s, t0, n):
                            for sub in range(n // 512):
                                sl = slice(sub * 512, (sub + 1) * 512)
                                gsl = slice(t0 + sub * 512, t0 + (sub + 1) * 512)
                                i = cnt[1] % 2; cnt[1] += 1
                                xb, t1, t2, ps = xb_r[i], t1_r[i], t2_r[i], psR[i]
                                cast(xb[:], s[:, sl], [s.name], [xb.name])
                                P.op('pe', lambda e, ps=ps, xb=xb: e.matmul(ps[:], lhsT=Psw[:], rhs=xb[:], start=True, stop=True),
                                     reads=[xb.name, 'Psw'], writes=[ps.name])
                                P.op('dve', lambda e, t1=t1, s=s, sl=sl, gsl=gsl: e.scalar_tensor_tensor(
                                    out=t1[:], in0=s[:, sl], scalar=scale, in1=cosT[:, gsl], op0=ALU.mult, op1=ALU.mult),
                                    reads=[s.name, 'cosT'], writes=[t1.name])
                                P.op('dve', lambda e, t2=t2, ps=ps, gsl=gsl: e.scalar_tensor_tensor(
                                    out=t2[:], in0=ps[:], scalar=scale, in1=sinS[:, gsl], op0=ALU.mult, op1=ALU.mult),
                                    reads=[ps.name, 'sinS'], writes=[t2.name])
                                P.op('pool', lambda e, t1=t1, t2=t2, gsl=gsl: e.tensor_tensor(
                                    out=dst(gsl), in0=t1[:], in1=t2[:], op=ALU.add),
                                    reads=[t1.name, t2.name], writes=[dkey])
                        return fn

                    for r in range(3):
                        load_chunk(12 + r, rope_to(lambda gsl, r=r: qT[:, r, gsl], 'c_qT', 0.125))
                    load_chunk(15, rope_to(lambda gsl: kcr[:, gsl], 'c_kcr', 1.0))
                    load_chunk(17, rope_to(lambda gsl: ksT[:, gsl], 'c_ksT', 1.0))
                    load_chunk(18, rope_to(lambda gsl: kwT[:, gsl], 'c_kwT', 1.0))
                    load_chunk(16, lambda s, t0, n: cast(vcb[:, t0:t0 + n], s[:, 0:n], [s.name], ['c_vcb']))
                    load_chunk(23, lambda s, t0, n: P.op('act', lambda e: e.activation(
                        out=sgT[:, t0:t0 + n], in_=s[:, 0:n], func=AF.Sigmoid), reads=[s.name], writes=['c_sgT']))
                    pTv = projT.rearrange("(kb p) f -> p kb f", p=128)
                    for kb0 in range(0, NKB, 8):
                        for (dstv, c0) in ((vs, 0), (vw, 128)):
                            s = stg[cnt[0] % 2]; cnt[0] += 1
                            sv = s[:, 0:1024].rearrange("p (a f) -> p a f", f=128)
                            P.dma(sv, pTv[:, kb0:kb0 + 8, c0:c0 + 128], writes=[s.name])
                            cast(dstv[:, kb0:kb0 + 8, :], sv, [s.name], [dstv.name])
                    w1b = [sb1("c_w1%d" % i, [128, 32, 128], BF16) for i in range(2)]
                    w2kp = sb1("c_w2kp", [128, 2, 128], BF16)
                    w2v = sb1("c_w2v", [128, 64], BF16)
                    peb = [sb1("c_pe%d" % i, [128, 32], BF16) for i in range(2)]
                    bias = [sb1("c_bias%d" % i, [128, 1], F32) for i in range(2)]
                    hid = [[sb1("c_hid%d%d" % (i, g), [128, 256], BF16) for g in range(2)] for i in range(2)]
                    psH = [ps1("c_psH%d" % i, [128, 256]) for i in range(2)]
                    psB = ps1("c_psB", [128, 64])
                    P.op('pool', lambda e: e.memset(w2kp[:], 0.0), writes=['c_w2kp'])
                    for i, (w1d, w2d, ped) in enumerate(((cmp_k_w1, cmp_k_w2, pe_kT), (cmp_v_w1, cmp_v_w2, pe_vT))):
                        for hf in range(2):
                            s = stg[cnt[0] % 2]; cnt[0] += 1
                            sv = s[:].rearrange("p (a f) -> p a f", f=128)
                            P.dma(sv, w1d[l, :, hf * 16:(hf + 1) * 16, :], writes=[s.name])
                            cast(w1b[i][:, hf * 16:(hf + 1) * 16, :], sv, [s.name], [w1b[i].name])
                        s = stg[cnt[0] % 2]; cnt[0] += 1
                        P.dma(s[:, 0:64], w2d[l], writes=[s.name])
                        if i == 0:
                            for g in range(2):
                                cast(w2kp[:, g, g * 64:(g + 1) * 64], s[:, 0:64], [s.name], ['c_w2kp'])
                        else:
                            cast(w2v[:], s[:, 0:64], [s.name], ['c_w2v'])
                        s = stg[cnt[0] % 2]; cnt[0] += 1
                        P.dma(s[:, 0:32], ped[l], writes=[s.name])
                        cast(peb[i][:], s[:, 0:32], [s.name], [peb[i].name])
                        for ll in range(32):
                            P.op('pe', lambda e, i=i, ll=ll: e.matmul(
                                psB[:, 0:1], lhsT=w1b[i][0:64, ll, :], rhs=peb[i][0:64, ll:ll + 1],
                                start=(ll == 0), stop=(ll == 31)),
                                reads=[w1b[i].name, peb[i].name], writes=['c_psB'], inc=(ll == 31))
                        P.op('dve', lambda e, i=i: e.tensor_copy(out=bias[i][:], in_=psB[:, 0:1]),
                             reads=['c_psB'], writes=[bias[i].name])
                        src = kcr if i == 0 else vcb
                        srcv = src[:].rearrange("p (i s) -> p i s", s=16)
                        for g in range(2):
                            ph = psH[g]
                            for ll in range(32):
                                a, s0 = ll // 16, ll % 16
                                P.op('pe', lambda e, i=i, g=g, ll=ll, a=a, s0=s0, ph=ph, srcv=srcv: e.matmul(
                                    ph[:, 0:NCMP], lhsT=w1b[i][g * 64:(g + 1) * 64, ll, :],
                                    rhs=srcv[g * 64:(g + 1) * 64, a:a + NCMP, s0],
                                    start=(ll == 0), stop=(ll == 31)),
                                    reads=[w1b[i].name, 'c_kcr' if i == 0 else 'c_vcb'], writes=[ph.name],
                                    inc=(ll == 31))
                            P.op('act', lambda e, i=i, g=g, ph=ph: e.activation(
                                out=hid[i][g][:, 0:NCMP], in_=ph[:, 0:NCMP], func=AF.Silu, bias=bias[i][:]),
                                reads=[ph.name, bias[i].name], writes=[hid[i][g].name])
                    for g in range(2):
                        P.op('pe', lambda e, g=g: e.matmul(psH[0][:, 0:NCMP], lhsT=w2kp[:, g, :], rhs=hid[0][g][:, 0:NCMP],
                                                           start=(g == 0), stop=(g == 1)),
                             reads=['c_w2kp', hid[0][g].name], writes=[psH[0].name], inc=(g == 1))
                    P.op('act', lambda e: e.copy(out=kcT[:, 0:NCMP], in_=psH[0][:, 0:NCMP]),
                         reads=[psH[0].name], writes=['c_kcT'])
                    for g in range(2):
                        for (ic, n) in ICS:
                            P.op('pe', lambda e, g=g, ic=ic, n=n: e.matmul(
                                psB[0:n, 0:64], lhsT=hid[1][g][:, ic * 128:ic * 128 + n], rhs=w2v[:], start=True, stop=True),
                                reads=[hid[1][g].name, 'c_w2v'], writes=['c_psB'])
                            P.op('dve', lambda e, g=g, ic=ic, n=n: e.tensor_copy(out=vc[0:n, g, ic, :], in_=psB[0:n, 0:64]),
                                 reads=['c_psB'], writes=['c_vc'])
                    P.barrier()
                    P.flush()
                with contextlib.ExitStack() as s2:
                    sb2 = lambda name, shape, dt=F32, u=uid(): s2.enter_context(nc.sbuf_tensor(name + u, shape, dt))
                    ps2 = lambda name, shape, dt=F32, u=uid(): s2.enter_context(nc.psum_tensor(name + u, shape, dt))
                    psS = [ps2("c_psS%d" % i, [128, 512]) for i in range(2)]
                    psD = ps2("c_psD", [128, 512])
                    psOc = ps2("c_psOc", [64, 512])
                    psO = ps2("c_psO", [64, 512])
                    psDn = ps2("c_psDn", [64, 512])
                    psM = ps2("c_psM", [128, 512])
                    psG = ps2("c_psG", [64, 512])
                    pe_r = [sb2("c_pe_%d" % i, [128, 512], BF16) for i in range(2)]
                    a_r = [sb2("c_a%d" % i, [128, 512], BF16) for i in range(3)]
                    pn = [sb2("c_pn%d" % r, [128, 2, 512], BF16) for r in range(3)]
                    rden = sb2("c_rden", [128, 512], F32)
                    asum = sb2("c_asum", [128, 512], F32)
                    oc_sets = [[sb2("c_oc%d_%d" % (k, r), [64, 512], F32) for r in range(3)] for k in range(2)]
                    ob_sb = sb2("c_ob", [64, 512], F32)
                    acc = sb2("c_acc", [64, 512], F32)
                    acc2 = sb2("c_acc2", [64, 512], F32)
                    rd64 = sb2("c_rd64", [64, 512], F32)
                    sqo = sb2("c_sqo", [64, 512], BF16)
                    rs = sb2("c_rs", [64, 512], F32)
                    o_out = [sb2("c_oo%d" % i, [64, 512], BF16) for i in range(2)]
                    score = sb2("c_score", [128, 64], F32)
                    score2 = sb2("c_score2", [128, 64], F32)
                    m8a = sb2("c_m8a", [128, 8], F32)
                    m8b = sb2("c_m8b", [128, 8], F32)
                    selm = sb2("c_selm", [128, 128], F32)
                    it = [0]
                    itm = [0]
                    oi = [0]
                    P.op('pool', lambda e: e.memset(selm[:], 0.0), writes=['c_selm'])

                    def attn_core(g, r, qt, kT_src, ksrc_key, v_src, kbs, mask_of, with_sel, pump):
                        qsl = slice(qt * 512, (qt + 1) * 512)
                        gs = slice(g * 64, (g + 1) * 64)
                        tl = {}

                        def score(bi):
                            kb = kbs[bi]
                            ksl = slice(kb * 128, (kb + 1) * 128)
                            ps = psS[itm[0] % 2]; aa = a_r[itm[0] % 3]; itm[0] += 1
                            tl[bi] = aa
                            P.op('pe', lambda e: e.matmul(
                                ps[:], lhsT=kT_src[gs, ksl], rhs=qT[gs, r, qsl], start=True, stop=not with_sel),
                                reads=[ksrc_key, 'c_qT'], writes=[ps.name], inc=not with_sel)
                            if with_sel:
                                P.op('pe', lambda e: e.matmul(
                                    ps[:], lhsT=Ebig[gs, ksl], rhs=selT[gs, qsl], start=False, stop=True),
                                    reads=['Ebig', ('c_selT', g, qt)], writes=[ps.name])
                            P.op('act', lambda e: e.activation(out=aa[:], in_=ps[:], func=AF.Exp),
                                 reads=[ps.name], writes=[aa.name])
                            m = mask_of(kb)
                            if m is not None:
                                mt, mkey = m
                                P.op('dve', lambda e: e.tensor_tensor(out=aa[:], in0=aa[:], in1=mt, op=ALU.mult),
                                     reads=[aa.name, mkey], writes=[aa.name])

                        def av(bi):
                            kb = kbs[bi]
                            aa = tl[bi]
                            first = (bi == 0)
                            last = (bi == len(kbs) - 1)
                            P.op('pe', lambda e: e.matmul(
                                psO[:], lhsT=v_src[:, kb, gs], rhs=aa[:], start=first, stop=last),
                                reads=[aa.name, v_src.name], writes=['c_psO'])
                            if first:
                                P.op('pool', lambda e: e.tensor_copy(out=asum[:], in_=aa[:]), reads=[aa.name], writes=['c_asum'])
                            else:
                                P.op('pool', lambda e: e.tensor_tensor(out=asum[:], in0=asum[:], in1=aa[:], op=ALU.add),
                                     reads=[aa.name, 'c_asum'], writes=['c_asum'])

                        for bi in range(len(kbs) + 1):
                            if bi < len(kbs):
                                score(bi)
                            if bi >= 1:
                                av(bi - 1)
                            pump()
                        P.op('pe', lambda e: e.matmul(psDn[:], lhsT=ones_f[:, 0:64], rhs=asum[:], start=True, stop=True),
                             reads=['c_asum', 'ones_f'], writes=['c_psDn'])
                        P.op('dve', lambda e: e.tensor_scalar(out=rd64[:], in0=psDn[:], scalar1=1e-30, scalar2=None, op0=ALU.max),
                             reads=['c_psDn'], writes=['c_rd64'])
                        P.op('dve', lambda e: e.reciprocal(out=rd64[:], in_=rd64[:]), reads=['c_rd64'], writes=['c_rd64'])
                        P.op('dve', lambda e: e.tensor_tensor(out=ob_sb[:], in0=psO[:], in1=rd64[:], op=ALU.mult),
                             reads=['c_psO', 'c_rd64'], writes=['c_ob'])

                    def pre_gen(g, qt, oc_sb):
                        gs = slice(g * 64, (g + 1) * 64)
                        selkey = ('c_selT', g, qt)
                        if True:
                            qsl = slice(qt * 512, (qt + 1) * 512)
                            ics = [(ic, n) for (ic, n) in ICS if 16 * ic * 128 + 31 <= qt * 512 + 511]
                            for r in (range(3) if 'cmp' in CPARTS else ()):
                                pes = []
                                for (ic, n) in ics:
                                    ps = psS[it[0] % 2]; pp = pe_r[it[0] % 2]; it[0] += 1
                                    P.op('pe', lambda e, ps=ps, ic=ic, n=n, r=r, gs=gs, qsl=qsl: e.matmul(
                                        ps[0:n, :], lhsT=kcT[gs, ic * 128:ic * 128 + n], rhs=qT[gs, r, qsl], start=True, stop=True),
                                        reads=['c_kcT', 'c_qT'], writes=[ps.name])
                                    P.op('act', lambda e, ps=ps, pp=pp, n=n: e.activation(out=pp[0:n, :], in_=ps[0:n, :], func=AF.Exp),
                                         reads=[ps.name], writes=[pp.name])
                                    P.op('dve', lambda e, pp=pp, n=n, ic=ic, qsl=qsl: e.tensor_tensor(
                                        out=pp[0:n, :], in0=pp[0:n, :], in1=cmask[0:n, ic, qsl], op=ALU.mult),
                                        reads=[pp.name, 'cmask'], writes=[pp.name])
                                    pes.append((ic, n, pp))
                                    yield
                                for bi, (ic, n, pp) in enumerate(pes):
                                    P.op('pe', lambda e, pp=pp, n=n, bi=bi, nb=len(pes): e.matmul(
                                        psD[:], lhsT=ones_bf[0:n, :], rhs=pp[0:n, :], start=(bi == 0), stop=(bi == nb - 1)),
                                        reads=[pp.name, 'ones_bf'], writes=['c_psD'], inc=(bi == len(pes) - 1))
                                P.op('dve', lambda e: e.tensor_scalar(out=rden[:], in0=psD[:], scalar1=1e-30, scalar2=None, op0=ALU.max),
                                     reads=['c_psD'], writes=['c_rden'])
                                P.op('dve', lambda e: e.reciprocal(out=rden[:], in_=rden[:]), reads=['c_rden'], writes=['c_rden'])
                                yield
                                for (ic, n, pp) in pes:
                                    P.op('dve', lambda e, pp=pp, n=n, ic=ic, r=r: e.tensor_tensor(
                                        out=pn[r][0:n, ic, :], in0=pp[0:n, :], in1=rden[0:n, :], op=ALU.mult),
                                        reads=[pp.name, 'c_rden'], writes=[pn[r].name])
                                for bi, (ic, n, pp) in enumerate(pes):
                                    P.op('pe', lambda e, n=n, ic=ic, r=r, g=g, bi=bi, nb=len(pes): e.matmul(
                                        psOc[:], lhsT=vc[0:n, g, ic, :], rhs=pn[r][0:n, ic, :], start=(bi == 0), stop=(bi == nb - 1)),
                                        reads=['c_vc', pn[r].name], writes=['c_psOc'], inc=(bi == len(pes) - 1))
                                P.op('act', lambda e, r=r, oc_sb=oc_sb: e.copy(out=oc_sb[r][:], in_=psOc[:]),
                                     reads=['c_psOc'], writes=[oc_sb[r].name])
                                yield
                            for ts in (range(4) if 'topk' in CPARTS else ()):
                                t0 = qt * 512 + ts * 128
                                tsl = slice(ts * 128, (ts + 1) * 128)
                                nmm = 3 * len(ics)
                                k = 0
                                for r in range(3):
                                    for (ic, n) in ics:
                                        P.op('pe', lambda e, r=r, ic=ic, n=n, tsl=tsl, k=k, nmm=nmm: e.matmul(
                                            psM[:, 0:64], lhsT=pn[r][0:n, ic, tsl], rhs=ov[0:n, ic, :],
                                            start=(k == 0), stop=(k == nmm - 1)),
                                            reads=[pn[r].name, 'ov'], writes=['c_psM'], inc=(k == nmm - 1))
                                        k += 1
                                yield
                                off = 64 - t0 // 64
                                P.op('dve', lambda e, off=off: e.tensor_tensor(
                                    out=score[:], in0=psM[:, 0:64], in1=keepB[:, off:off + 64], op=ALU.mult),
                                    reads=['c_psM', 'keepB'], writes=['c_score'])
                                P.op('dve', lambda e, off=off: e.tensor_tensor(
                                    out=score[:], in0=score[:], in1=addcB[:, off:off + 64], op=ALU.add),
                                    reads=['c_score', 'addcB'], writes=['c_score'])
                                P.op('dve', lambda e: e.memset(score[:, 0:1], 1e4), reads=[], writes=['c_score'])
                                yield
                                P.op('dve', lambda e: e.max(out=m8a[:], in_=score[:]), reads=['c_score'], writes=['c_m8a'])
                                P.op('dve', lambda e: e.match_replace(out=score2[:], in_to_replace=m8a[:], in_values=score[:],
                                                                      imm_value=-1e9),
                                     reads=['c_score', 'c_m8a'], writes=['c_score2'])
                                P.op('dve', lambda e: e.max(out=m8b[:], in_=score2[:]), reads=['c_score2'], writes=['c_m8b'])
                                P.op('dve', lambda e, gs=gs: e.tensor_scalar(out=selm[:, gs], in0=score[:], scalar1=m8b[:, 7:8], scalar2=-1.0,
                                                                      op0=ALU.is_ge, op1=ALU.add),
                                     reads=['c_score', 'c_m8b'], writes=['c_selm'])
                                yield
                                P.op('pe', lambda e: e.matmul(psM[:, 128:256], lhsT=selm[:], rhs=ident_f[:], start=True, stop=True),
                                     reads=['c_selm', 'ident_f'], writes=['c_psM'])
                                P.op('act', lambda e, gs=gs, t0=t0: e.copy(out=selT[gs, t0:t0 + 128], in_=psM[gs, 128:256]),
                                     reads=['c_psM'], writes=[selkey])
                            yield

                    def main_part(g, qt, oc_sb, pump):
                        gs = slice(g * 64, (g + 1) * 64)
                        if True:
                            qsl = slice(qt * 512, (qt + 1) * 512)
                            for r in range(3):
                                hh = 3 * g + r
                                def gate_mul(dst, src_ap, src_key, br, hh=hh, qsl=qsl):
                                    P.op('pe', lambda e: e.matmul(psG[:], lhsT=oh[:, br * 6 + hh, :], rhs=sgT[:, qsl], start=True, stop=True),
                                         reads=['oh', 'c_sgT'], writes=['c_psG'])
                                    P.op('dve', lambda e: e.tensor_tensor(out=dst[:], in0=src_ap, in1=psG[:], op=ALU.mult),
                                         reads=[src_key, 'c_psG'], writes=[dst.name])
                                gate_mul(acc, oc_sb[r][:], oc_sb[r].name, 0)
                                if 'sel' in CPARTS:
                                  attn_core(g, r, qt, ksT, 'c_ksT', vs, list(range(4 * (qt + 1))),
                                            lambda kb, qt=qt: ((dmaskI[:, kb - 4 * qt, :], 'dmaskI') if kb >= 4 * qt else None), True, pump)
                                gate_mul(acc2, ob_sb[:], 'c_ob', 1)
                                P.op('pool', lambda e: e.tensor_tensor(out=acc[:], in0=acc[:], in1=acc2[:], op=ALU.add),
                                     reads=[acc.name, acc2.name], writes=[acc.name])
                                if 'win' in CPARTS:
                                  attn_core(g, r, qt, kwT, 'c_kwT', vw, list(range(max(0, 4 * qt - 4), 4 * qt + 4)),
                                            lambda kb, qt=qt: ((dmaskI[:, kb - 4 * qt, :], 'dmaskI') if kb >= 4 * qt
                                                               else (wmask[:, kb - (4 * qt - 4), :], 'wmask')), False, pump)
                                gate_mul(acc2, ob_sb[:], 'c_ob', 2)
                                P.op('pool', lambda e: e.tensor_tensor(out=acc[:], in0=acc[:], in1=acc2[:], op=ALU.add),
                                     reads=[acc.name, acc2.name], writes=[acc.name])
                                oo = o_out[oi[0] % 2]; oi[0] += 1
                                P.op('act', lambda e: e.activation(out=sqo[:], in_=acc[:], func=AF.Square),
                                     reads=[acc.name], writes=['c_sqo'])
                                P.op('pe', lambda e: e.matmul(psG[:], lhsT=ones_bf[0:64, 0:64], rhs=sqo[:], start=True, stop=True),
                                     reads=['c_sqo', 'ones_bf'], writes=['c_psG'])
                                P.op('act', lambda e: e.activation(out=rs[:], in_=psG[:], func=AF.Ln, bias=eps_t[0:64, :], scale=1.0 / 64),
                                     reads=['c_psG', 'eps_t'], writes=['c_rs'])
                                P.op('act', lambda e: e.activation(out=rs[:], in_=rs[:], func=AF.Exp, scale=-0.5),
                                     reads=['c_rs'], writes=['c_rs'])
                                P.op('dve', lambda e, oo=oo: e.scalar_tensor_tensor(
                                    out=oo[:], in0=acc[:], scalar=nw[0:64, :], in1=rs[:], op0=ALU.mult, op1=ALU.mult),
                                    reads=[acc.name, nw.name, 'c_rs'], writes=[oo.name])
                                P.dma(mixT[384 + hh * 64:384 + (hh + 1) * 64, qsl], oo[:], reads=[oo.name])

                    order = [(g, qt) for g in range(2) for qt in range(NT)]

                    def drain(gen):
                        for _ in gen:
                            pass

                    drain(pre_gen(order[0][0], order[0][1], oc_sets[0]))
                    for idx, (g, qt) in enumerate(order):
                        nxt = (pre_gen(order[idx + 1][0], order[idx + 1][1], oc_sets[(idx + 1) % 2])
                               if idx + 1 < len(order) else None)

                        def pump(nxt=nxt):
                            if nxt is None:
                                return
                            for _ in range(3):
                                try:
                                    next(nxt)
                                except StopIteration:
                                    return
                        main_part(g, qt, oc_sets[idx % 2], pump)
                        if nxt is not None:
                            drain(nxt)
                    P.barrier()
                    P.flush()

            if 'D' in stages:
              NG = 13 * 128
              with contextlib.ExitStack() as st:
                sbt = lambda name, shape, dt=F32, u=uid(): st.enter_context(nc.sbuf_tensor(name + u, shape, dt))
                with contextlib.ExitStack() as s1:
                    sb1 = lambda name, shape, dt=F32, u=uid(): s1.enter_context(nc.sbuf_tensor(name + u, shape, dt))
                    ps1 = lambda name, shape, dt=F32, u=uid(): s1.enter_context(nc.psum_tensor(name + u, shape, dt))
                    xc = [sb1("d_xc%d" % i, [128, T + 3], F32) for i in range(2)]
                    cv = [sb1("d_cv%d" % i, [128, T], F32) for i in range(2)]
                    cw = sb1("d_cw", [128, 9, 4], F32)
                    tst = [sb1("d_tst%d" % i, [128, 512], F32) for i in range(3)]
                    psT = [ps1("d_psT%d" % i, [128, 512]) for i in range(3)]
                    P.dma(cw[:], gdn_conv[l], writes=['d_cw'])
                    for i in range(2):
                        P.op('pool', lambda e, i=i: e.memset(xc[i][:, 0:3], 0.0), writes=[xc[i].name])
                    gview = gtok.rearrange("(kb p) f -> p kb f", p=128)
                    k = 0
                    for ci, ch in enumerate(list(range(12)) + [23]):
                        x_ = xc[ci % 2]
                        c_ = cv[ci % 2]
                        P.dma(x_[:, 3:T + 3], projF[ch * 128:(ch + 1) * 128, :], writes=[x_.name])
                        if ch < 9:
                            P.op('dve', lambda e, x_=x_, c_=c_, ch=ch: e.tensor_scalar(
                                out=c_[:], in0=x_[:, 0:T], scalar1=cw[:, ch, 0:1], scalar2=None, op0=ALU.mult),
                                reads=[x_.name, 'd_cw'], writes=[c_.name])
                            for tap in (1, 2, 3):
                                P.op('dve', lambda e, x_=x_, c_=c_, ch=ch, tap=tap: e.scalar_tensor_tensor(
                                    out=c_[:], in0=x_[:, tap:tap + T], scalar=cw[:, ch, tap:tap + 1], in1=c_[:],
                                    op0=ALU.mult, op1=ALU.add),
                                    reads=[x_.name, 'd_cw', c_.name], writes=[c_.name])
                            P.op('act', lambda e, c_=c_: e.activation(out=c_[:], in_=c_[:], func=AF.Silu),
                                 reads=[c_.name], writes=[c_.name])
                            src, skey = (lambda sl, c_=c_: c_[:, sl]), c_.name
                        else:
                            src, skey = (lambda sl, x_=x_: x_[:, 3 + sl.start:3 + sl.stop]), x_.name
                        for kb0 in range(0, NKB, 4):
                            pt = psT[k % 3]; ts_ = tst[k % 3]; k += 1
                            for q4 in range(4):
                                kb = kb0 + q4
                                P.op('pe', lambda e, pt=pt, q4=q4, kb=kb, src=src: e.matmul(
                                    pt[:, q4 * 128:(q4 + 1) * 128], lhsT=src(slice(kb * 128, (kb + 1) * 128)), rhs=ident_f[:],
                                    start=True, stop=True),
                                    reads=[skey, 'ident_f'], writes=[pt.name], inc=(q4 == 3))
                            cast(ts_[:], pt[:], [pt.name], [ts_.name], psum=True)
                            P.dma(gview[:, kb0:kb0 + 4, ci * 128:(ci + 1) * 128],
                                  ts_[:].rearrange("p (a f) -> p a f", f=128), reads=[ts_.name])
                    P.barrier()
                    P.flush()
                with contextlib.ExitStack() as s2:
                    sb2 = lambda name, shape, dt=F32, u=uid(): s2.enter_context(nc.sbuf_tensor(name + u, shape, dt))
                    ps2 = lambda name, shape, dt=F32, u=uid(): s2.enter_context(nc.psum_tensor(name + u, shape, dt))
                    dtb = sb2("d_dtb", [128, 6], F32)
                    nA = sb2("d_nA", [128, 6], F32)
                    nwb = sb2("d_nwb", [128, 64], F32)
                    P.dma(dtb[:], gdn_dtb[l].partition_broadcast(128), writes=['d_dtb'])
                    P.dma(nA[:], gdn_alog[l].partition_broadcast(128), writes=['d_nA'])
                    P.dma(nwb[:], gdn_normrow[l].partition_broadcast(128), writes=['d_nwb'])
                    P.op('act', lambda e: e.activation(out=nA[:], in_=nA[:], func=AF.Exp), reads=['d_nA'], writes=['d_nA'])
                    P.op('dve', lambda e: e.tensor_scalar(out=nA[:], in0=nA[:], scalar1=-1.0, scalar2=None, op0=ALU.mult),
                         reads=['d_nA'], writes=['d_nA'])
                    X = [sb2("d_X%d" % i, [128, NG], F32) for i in range(2)]
                    S = [sb2("d_S%d" % h, [64, 64], F32) for h in range(6)]
                    for h in range(6):
                        P.op('pool', lambda e, h=h: e.memset(S[h][:], 0.0), writes=[S[h].name])
                    gt = sb2("d_gt", [128, 48], F32)
                    gsb = sb2("d_gsb", [128, 16], F32)
                    sqq = sb2("d_sqq", [128, 384], F32)
                    rn = sb2("d_rn", [128, 12], F32)
                    H = lambda name, shape, dt=F32: [sb2("%s%d" % (name, h), shape, dt) for h in range(6)]
                    qn = H("d_qn", [128, 64]); kn = H("d_kn", [128, 64]); qg = H("d_qg", [128, 64])
                    kd = H("d_kd", [128, 64])
                    qnT = H("d_qnT", [64, 128]); knT = H("d_knT", [64, 128]); qgT = H("d_qgT", [64, 128])
                    yf = H("d_yf", [128, 256])
                    MT = H("d_MT", [128, 128]); tA = H("d_tA", [128, 128]); tB = H("d_tB", [128, 128])
                    dgn = H("d_dgn", [128, 128])
                    dec = H("d_dec", [128, 128])
                    tmpk = H("d_tmpk", [128, 128])
                    Rm = [H("d_R%d_" % i, [128, 128]) for i in range(2)]
                    Qm = [H("d_Q%d_" % i, [128, 128]) for i in range(2)]
                    attn = H("d_attn", [128, 128])
                    attnT = H("d_attnT", [128, 128])
                    wT = H("d_wT", [64, 128])
                    vnew = H("d_vnew", [128, 64])
                    o_sb = sb2("d_o", [128, 384], F32)
                    zs = sb2("d_zs", [128, 384], F32)
                    res = sb2("d_res", [128, 384], F32)
                    ot = [sb2("d_ot%d" % i, [128, 3, 128], BF16) for i in range(2)]
                    pA = [ps2("d_pA%d" % i, [128, 128]) for i in range(4)]
                    pB = [ps2("d_pB%d" % i, [128, 256]) for i in range(4)]
                    ia = [0]; ib = [0]

                    def nextA():
                        p_ = pA[ia[0] % 4]; ia[0] += 1
                        return p_

                    def nextB():
                        p_ = pB[ib[0] % 4]; ib[0] += 1
                        return p_

                    def evac(dst, dkey, src, skey):
                        cast(dst, src, [skey], [dkey], psum=True)

                    for n in range(NKB):
                        Xn = X[n % 2]
                        P.dma(Xn[:], gtok[n * 128:(n + 1) * 128, :], writes=[Xn.name])
                        xk = Xn.name
                        A0 = 1536
                        P.op('act', lambda e, Xn=Xn: e.activation(out=gt[:, 0:6], in_=Xn[:, A0 + 6:A0 + 12], func=AF.Sigmoid),
                             reads=[xk], writes=['d_gt'])
                        P.op('dve', lambda e, Xn=Xn: e.tensor_tensor(out=gt[:, 6:12], in0=Xn[:, A0:A0 + 6], in1=dtb[:], op=ALU.add),
                             reads=[xk, 'd_dtb'], writes=['d_gt'])
                        P.op('act', lambda e: e.activation(out=gt[:, 6:12], in_=gt[:, 6:12], func=AF.Exp), reads=['d_gt'], writes=['d_gt'])
                        P.op('act', lambda e: e.activation(out=gt[:, 6:12], in_=gt[:, 6:12], func=AF.Ln, bias=one_t[:]),
                             reads=['d_gt', 'one_t'], writes=['d_gt'])
                        P.op('dve', lambda e: e.tensor_tensor(out=gt[:, 6:12], in0=gt[:, 6:12], in1=nA[:], op=ALU.mult),
                             reads=['d_gt', 'd_nA'], writes=['d_gt'])
                        pg = nextA()
                        P.op('pe', lambda e, pg=pg: e.matmul(pg[:, 0:6], lhsT=Uincl[:], rhs=gt[:, 6:12], start=True, stop=True),
                             reads=['d_gt', 'Uincl'], writes=[pg.name], inc=False)
                        P.op('pe', lambda e, pg=pg: e.matmul(pg[:, 6:12], lhsT=ones_f[:], rhs=gt[:, 6:12], start=True, stop=True),
                             reads=['d_gt', 'ones_f'], writes=[pg.name])
                        P.op('dve', lambda e, pg=pg: e.tensor_copy(out=gsb[:, 0:12], in_=pg[:, 0:12]), reads=[pg.name], writes=['d_gsb'])
                        P.op('act', lambda e: e.activation(out=gt[:, 12:18], in_=gsb[:, 0:6], func=AF.Exp), reads=['d_gsb'], writes=['d_gt'])
                        P.op('dve', lambda e: e.tensor_tensor(out=gt[:, 18:24], in0=gsb[:, 6:12], in1=gsb[:, 0:6], op=ALU.subtract),
                             reads=['d_gsb'], writes=['d_gt'])
                        P.op('act', lambda e: e.activation(out=gt[:, 18:24], in_=gt[:, 18:24], func=AF.Exp), reads=['d_gt'], writes=['d_gt'])
                        P.op('act', lambda e: e.activation(out=gt[:, 24:30], in_=gsb[:, 6:12], func=AF.Exp), reads=['d_gsb'], writes=['d_gt'])
                        P.op('dve', lambda e: e.tensor_tensor(out=gt[:, 30:36], in0=gt[:, 0:6], in1=gt[:, 12:18], op=ALU.mult),
                             reads=['d_gt'], writes=['d_gt'])
                        P.op('dve', lambda e: e.tensor_scalar(out=gt[:, 36:42], in0=gt[:, 0:6], scalar1=-1.0, scalar2=None, op0=ALU.mult),
                             reads=['d_gt'], writes=['d_gt'])
                        for qi, c0 in enumerate((0, 384)):
                            P.op('dve', lambda e, Xn=Xn, c0=c0: e.tensor_tensor(out=sqq[:], in0=Xn[:, c0:c0 + 384], in1=Xn[:, c0:c0 + 384], op=ALU.mult),
                                 reads=[xk], writes=['d_sqq'])
                            P.op('dve', lambda e, qi=qi: e.reduce_sum(out=rn[:, qi * 6:(qi + 1) * 6],
                                                                     in_=sqq[:].rearrange("p (h d) -> p h d", d=64), axis=AX.X),
                                 reads=['d_sqq'], writes=['d_rn'])
                        P.op('act', lambda e: e.activation(out=rn[:], in_=rn[:], func=AF.Ln, bias=eps_t[:]), reads=['d_rn', 'eps_t'], writes=['d_rn'])
                        P.op('act', lambda e: e.activation(out=rn[:], in_=rn[:], func=AF.Exp, scale=-0.5), reads=['d_rn'], writes=['d_rn'])
                        for h in range(6):
                            hs = slice(h * 64, (h + 1) * 64)
                            P.op('dve', lambda e, h=h, hs=hs, Xn=Xn: e.tensor_scalar(
                                out=qn[h][:], in0=Xn[:, hs], scalar1=rn[:, h:h + 1], scalar2=0.125, op0=ALU.mult, op1=ALU.mult),
                                reads=[xk, 'd_rn'], writes=[qn[h].name])
                            P.op('dve', lambda e, h=h, Xn=Xn: e.tensor_scalar(
                                out=kn[h][:], in0=Xn[:, 384 + h * 64:384 + (h + 1) * 64], scalar1=rn[:, 6 + h:7 + h], scalar2=None, op0=ALU.mult),
                                reads=[xk, 'd_rn'], writes=[kn[h].name])
                            P.op('pool', lambda e, h=h: e.tensor_scalar(
                                out=qg[h][:], in0=qn[h][:], scalar1=gt[:, 12 + h:13 + h], scalar2=None, op0=ALU.mult),
                                reads=[qn[h].name, 'd_gt'], writes=[qg[h].name])
                            P.op('pool', lambda e, h=h: e.tensor_scalar(
                                out=kd[h][:], in0=kn[h][:], scalar1=gt[:, 18 + h:19 + h], scalar2=None, op0=ALU.mult),
                                reads=[kn[h].name, 'd_gt'], writes=[kd[h].name])
                            P.op('dve', lambda e, h=h, Xn=Xn: e.tensor_scalar(
                                out=yf[h][:, 0:64], in0=Xn[:, 768 + h * 64:768 + (h + 1) * 64], scalar1=gt[:, h:h + 1], scalar2=None, op0=ALU.mult),
                                reads=[xk, 'd_gt'], writes=[yf[h].name])
                            P.op('dve', lambda e, h=h: e.tensor_scalar(
                                out=yf[h][:, 64:128], in0=kn[h][:], scalar1=gt[:, 30 + h:31 + h], scalar2=None, op0=ALU.mult),
                                reads=[kn[h].name, 'd_gt'], writes=[yf[h].name])
                            P.op('pool', lambda e, h=h: e.tensor_scalar(
                                out=dgn[h][:], in0=ident_f[:], scalar1=gsb[:, h:h + 1], scalar2=-1.0, op0=ALU.mult, op1=ALU.mult),
                                reads=['ident_f', 'd_gsb'], writes=[dgn[h].name])
                        for h in range(6):
                            for (srcT, dstT) in ((qn, qnT), (kn, knT), (qg, qgT)):
                                p_ = nextA()
                                P.op('pe', lambda e, p_=p_, srcT=srcT, h=h: e.matmul(p_[0:64, :], lhsT=srcT[h][:], rhs=ident_f[:], start=True, stop=True),
                                     reads=[srcT[h].name, 'ident_f'], writes=[p_.name])
                                evac(dstT[h][:], dstT[h].name, p_[0:64, :], p_.name)
                        for h in range(6):
                            p_ = nextB()
                            P.op('pe', lambda e, p_=p_, h=h: e.matmul(p_[:, 0:128], lhsT=ones_f[:], rhs=dgn[h][:], start=True, stop=True),
                                 reads=[dgn[h].name, 'ones_f'], writes=[p_.name])
                            P.op('dve', lambda e, p_=p_, h=h: e.tensor_scalar(
                                out=dec[h][:], in0=p_[:, 0:128], scalar1=gsb[:, h:h + 1], scalar2=0.0, op0=ALU.add, op1=ALU.min),
                                reads=[p_.name, 'd_gsb'], writes=[dec[h].name])
                            P.op('act', lambda e, h=h: e.activation(out=dec[h][:], in_=dec[h][:], func=AF.Exp),
                                 reads=[dec[h].name], writes=[dec[h].name])
                            p_ = nextB()
                            P.op('pe', lambda e, p_=p_, h=h: e.matmul(p_[:, 0:128], lhsT=knT[h][:], rhs=knT[h][:], start=True, stop=True),
                                 reads=[knT[h].name], writes=[p_.name])
                            P.op('dve', lambda e, p_=p_, h=h: e.tensor_tensor(out=tmpk[h][:], in0=p_[:, 0:128], in1=dec[h][:], op=ALU.mult),
                                 reads=[p_.name, dec[h].name], writes=[tmpk[h].name])
                            P.op('dve', lambda e, h=h: e.scalar_tensor_tensor(
                                out=Rm[0][h][:], in0=tmpk[h][:], scalar=gt[:, 36 + h:37 + h], in1=LBD[:], op0=ALU.mult, op1=ALU.mult),
                                reads=[tmpk[h].name, 'd_gt', 'LBD'], writes=[Rm[0][h].name])
                            P.op('dve', lambda e, h=h: e.scalar_tensor_tensor(
                                out=yf[h][:, 128:256], in0=tmpk[h][:], scalar=gt[:, 36 + h:37 + h], in1=LOD[:], op0=ALU.mult, op1=ALU.mult),
                                reads=[tmpk[h].name, 'd_gt', 'LOD'], writes=[yf[h].name])
                            p_ = nextB()
                            P.op('pe', lambda e, p_=p_, h=h: e.matmul(p_[:, 0:128], lhsT=qnT[h][:], rhs=knT[h][:], start=True, stop=True),
                                 reads=[qnT[h].name, knT[h].name], writes=[p_.name])
                            P.op('dve', lambda e, p_=p_, h=h: e.tensor_tensor(out=tmpk[h][:], in0=p_[:, 0:128], in1=dec[h][:], op=ALU.mult),
                                 reads=[p_.name, dec[h].name], writes=[tmpk[h].name])
                            P.op('pool', lambda e, h=h: e.tensor_tensor(out=attn[h][:], in0=tmpk[h][:], in1=Lincl[:], op=ALU.mult),
                                 reads=[tmpk[h].name, 'Lincl'], writes=[attn[h].name])
                            for (srcM, dstM) in ((Rm[0], Qm[0]), (attn, attnT)):
                                p_ = nextA()
                                P.op('pe', lambda e, p_=p_, srcM=srcM, h=h: e.matmul(p_[:, 0:128], lhsT=srcM[h][:], rhs=ident_f[:], start=True, stop=True),
                                     reads=[srcM[h].name, 'ident_f'], writes=[p_.name])
                                evac(dstM[h][:], dstM[h].name, p_[:, 0:128], p_.name)
                        for j in range(5):
                            cur, nxt = j % 2, (j + 1) % 2
                            for h in range(6):
                                p_ = nextB()
                                P.op('pe', lambda e, p_=p_, h=h, cur=cur: e.matmul(p_[:], lhsT=Qm[cur][h][:], rhs=yf[h][:], start=True, stop=True),
                                     reads=[Qm[cur][h].name, yf[h].name], writes=[p_.name])
                                P.op('dve', lambda e, p_=p_, h=h: e.tensor_tensor(out=yf[h][:], in0=yf[h][:], in1=p_[:], op=ALU.add),
                                     reads=[p_.name, yf[h].name], writes=[yf[h].name])
                                if j < 3:
                                    p_ = nextA()
                                    P.op('pe', lambda e, p_=p_, h=h, cur=cur: e.matmul(p_[:], lhsT=Qm[cur][h][:], rhs=Rm[cur][h][:], start=True, stop=True),
                                         reads=[Qm[cur][h].name, Rm[cur][h].name], writes=[p_.name])
                                    evac(Rm[nxt][h][:], Rm[nxt][h].name, p_[:], p_.name)
                                if j < 4:
                                    p_ = nextA()
                                    P.op('pe', lambda e, p_=p_, h=h, cur=cur: e.matmul(p_[:], lhsT=Rm[cur][h][:], rhs=Qm[cur][h][:], start=True, stop=True),
                                         reads=[Qm[cur][h].name, Rm[cur][h].name], writes=[p_.name])
                                    evac(Qm[nxt][h][:], Qm[nxt][h].name, p_[:], p_.name)
                        for h in range(6):
                            p_ = nextA()
                            P.op('pe', lambda e, p_=p_, h=h: e.matmul(p_[:], lhsT=yf[h][:, 128:256], rhs=ident_f[:], start=True, stop=True),
                                 reads=[yf[h].name, 'ident_f'], writes=[p_.name])
                            evac(MT[h][:], MT[h].name, p_[:], p_.name)
                        for it3, (src3, dst3) in enumerate(((None, tA), (tA, tB), (tB, tA))):
                            for h in range(6):
                                p_ = nextA()
                                rhs_ap = yf[h][:, 0:128] if src3 is None else src3[h][:]
                                rkey = yf[h].name if src3 is None else src3[h].name
                                P.op('pe', lambda e, p_=p_, h=h, rhs_ap=rhs_ap: e.matmul(p_[:], lhsT=MT[h][:], rhs=rhs_ap, start=True, stop=True),
                                     reads=[MT[h].name, rkey], writes=[p_.name])
                                P.op('dve', lambda e, p_=p_, h=h, dst3=dst3: e.tensor_tensor(out=dst3[h][:], in0=yf[h][:, 0:128], in1=p_[:], op=ALU.add),
                                     reads=[p_.name, yf[h].name], writes=[dst3[h].name])
                        for h in range(6):
                            p_ = nextA()
                            P.op('pe', lambda e, p_=p_, h=h: e.matmul(p_[0:64, :], lhsT=tA[h][:, 64:128], rhs=ident_f[:], start=True, stop=True),
                                 reads=[tA[h].name, 'ident_f'], writes=[p_.name])
                            evac(wT[h][:], wT[h].name, p_[0:64, :], p_.name)
                        for h in range(6):
                            p1 = nextB()
                            P.op('pe', lambda e, p1=p1, h=h: e.matmul(p1[:, 0:64], lhsT=wT[h][:], rhs=S[h][:], start=True, stop=True),
                                 reads=[wT[h].name, S[h].name], writes=[p1.name])
                            P.op('dve', lambda e, p1=p1, h=h: e.tensor_tensor(out=vnew[h][:], in0=tA[h][:, 0:64], in1=p1[:, 0:64], op=ALU.subtract),
                                 reads=[p1.name, tA[h].name], writes=[vnew[h].name])
                            p2 = nextB()
                            P.op('pe', lambda e, p2=p2, h=h: e.matmul(p2[:, 0:64], lhsT=qgT[h][:], rhs=S[h][:], start=True, stop=False),
                                 reads=[qgT[h].name, S[h].name], writes=[p2.name], inc=False)
                            P.op('pe', lambda e, p2=p2, h=h: e.matmul(p2[:, 0:64], lhsT=attnT[h][:], rhs=vnew[h][:], start=False, stop=True),
                                 reads=[attnT[h].name, vnew[h].name], writes=[p2.name])
                            P.op('act', lambda e, p2=p2, h=h: e.copy(out=o_sb[:, h * 64:(h + 1) * 64], in_=p2[:, 0:64]),
                                 reads=[p2.name], writes=[('d_o', h)])
                            p3 = nextA()
                            P.op('pe', lambda e, p3=p3, h=h: e.matmul(p3[0:64, 0:64], lhsT=kd[h][:], rhs=vnew[h][:], start=True, stop=True),
                                 reads=[kd[h].name, vnew[h].name], writes=[p3.name])
                            P.op('dve', lambda e, p3=p3, h=h: e.scalar_tensor_tensor(
                                out=S[h][:], in0=S[h][:], scalar=gt[0:64, 24 + h:25 + h], in1=p3[0:64, 0:64], op0=ALU.mult, op1=ALU.add),
                                reads=[p3.name, S[h].name, 'd_gt'], writes=[S[h].name])
                        okeys = [('d_o', h) for h in range(6)]
                        P.op('dve', lambda e: e.tensor_tensor(out=sqq[:], in0=o_sb[:], in1=o_sb[:], op=ALU.mult), reads=okeys, writes=['d_sqq'])
                        P.op('dve', lambda e: e.reduce_sum(out=rn[:, 0:6], in_=sqq[:].rearrange("p (h d) -> p h d", d=64), axis=AX.X),
                             reads=['d_sqq'], writes=['d_rn'])
                        P.op('act', lambda e: e.activation(out=rn[:, 0:6], in_=rn[:, 0:6], func=AF.Ln, bias=eps_t[:], scale=1.0 / 64),
                             reads=['d_rn', 'eps_t'], writes=['d_rn'])
                        P.op('act', lambda e: e.activation(out=rn[:, 0:6], in_=rn[:, 0:6], func=AF.Exp, scale=-0.5), reads=['d_rn'], writes=['d_rn'])
                        P.op('act', lambda e, Xn=Xn: e.activation(out=zs[:], in_=Xn[:, 1152:1536], func=AF.Silu), reads=[xk], writes=['d_zs'])
                        for h in range(6):
                            hs = slice(h * 64, (h + 1) * 64)
                            P.op('dve', lambda e, h=h, hs=hs: e.scalar_tensor_tensor(
                                out=res[:, hs], in0=o_sb[:, hs], scalar=rn[:, h:h + 1], in1=nwb[:], op0=ALU.mult, op1=ALU.mult),
                                reads=okeys + ['d_rn', 'd_nwb'], writes=[('d_res', h)])
                            P.op('pool', lambda e, hs=hs: e.tensor_tensor(out=res[:, hs], in0=res[:, hs], in1=zs[:, hs], op=ALU.mult),
                                 reads=[('d_res', h), 'd_zs'], writes=[('d_res', h)])
                        rkeys = [('d_res', h) for h in range(6)]
                        oo = ot[n % 2]
                        for c3 in range(3):
                            p_ = nextA()
                            P.op('pe', lambda e, p_=p_, c3=c3: e.matmul(p_[:], lhsT=res[:, c3 * 128:(c3 + 1) * 128], rhs=ident_f[:], start=True, stop=True),
                                 reads=rkeys + ['ident_f'], writes=[p_.name])
                            evac(oo[:, c3, :], oo.name, p_[:], p_.name)
                        P.dma(mixT[0:384, n * 128:(n + 1) * 128].rearrange("(c p) t -> p c t", p=128), oo[:], reads=[oo.name])
                    P.barrier()
                    P.flush()


            if 'E' in stages:
              with contextlib.ExitStack() as st:
                sbt = lambda name, shape, dt=F32, u=uid(): st.enter_context(nc.sbuf_tensor(name + u, shape, dt))
                pst = lambda name, shape, dt=F32, u=uid(): st.enter_context(nc.psum_tensor(name + u, shape, dt))
                wob = sbt("e_w", [128, 8, D_MODEL], BF16)
                wst = [sbt("e_wst%d" % i, [128, D_MODEL], F32) for i in range(2)]
                for c in range(8):
                    s = wst[c % 2]
                    P.dma(s[:], w_out[l, c * 128:(c + 1) * 128, :], writes=[s.name])
                    cast(wob[:, c, :], s[:], [s.name], ['e_w'])
                h_sb = [sbt("e_h%d" % i, [128, 8, 512], F32) for i in range(2)]
                mx_sb = [sbt("e_mx%d" % i, [128, 8, 512], BF16) for i in range(2)]
                ps_o = [pst("e_ps%d" % i, [128, 512]) for i in range(4)]
                for tt in range(NT):
                    tsl = slice(tt * 512, (tt + 1) * 512)
                    h = h_sb[tt % 2]
                    mx = mx_sb[tt % 2]
                    P.dma(h[:], pc(hsrc)[:, :, tsl], writes=[h.name])
                    P.dma(mx[:], pc(mixT)[:, :, tsl], writes=[mx.name])
                    for m in range(8):
                        ps = ps_o[m % 4]
                        for c in range(8):
                            P.op('pe', lambda e, c=c, m=m, ps=ps, mx=mx: e.matmul(
                                ps[:], lhsT=wob[:, c, m * 128:(m + 1) * 128], rhs=mx[:, c, :],
                                start=(c == 0), stop=(c == 7)),
                                reads=[mx.name, 'e_w'], writes=[ps.name], inc=(c == 7))
                        P.op('dve', lambda e, m=m, ps=ps, h=h: e.tensor_tensor(
                            out=h[:, m, :], in0=h[:, m, :], in1=ps[:], op=ALU.add),
                            reads=[ps.name, h.name], writes=[h.name])
                    P.dma(pc(hT)[:, :, tsl], h[:], reads=[h.name], queue='act')
                P.barrier()
                P.flush()

            if 'F' in stages:
              for half in range(2):
                with contextlib.ExitStack() as st:
                    sbt = lambda name, shape, dt=F32, u=uid(): st.enter_context(nc.sbuf_tensor(name + u, shape, dt))
                    pst = lambda name, shape, dt=F32, u=uid(): st.enter_context(nc.psum_tensor(name + u, shape, dt))
                    HF = D_FF // 2
                    wup = sbt("f_wup", [128, 8, 2 * HF], BF16)
                    wdn = sbt("f_wdn", [128, 11, D_MODEL], BF16)
                    wst = [sbt("f_wst%d" % i, [128, HF], F32) for i in range(2)]
                    gain = sbt("f_gain", [128, 8], F32)
                    cw = sbt("f_cw", [128, 44, 3], F32)
                    halo = sbt("f_halo", [128, 22, 2], F32)
                    P.dma(gain[:], ln_ffn[l], writes=[gain.name])
                    P.dma(cw[:], ffn_conv[l], writes=['f_cw'])
                    P.op('pool', lambda e: e.memset(halo[:], 0.0), writes=['f_halo'])
                    k = 0
                    for c in range(8):
                        for which in range(2):
                            s = wst[k % 2]
                            k += 1
                            c0 = which * D_FF + half * HF
                            P.dma(s[:], w_up[l, c * 128:(c + 1) * 128, c0:c0 + HF], writes=[s.name])
                            cast(wup[:, c, which * HF:(which + 1) * HF], s[:], [s.name], ['f_wup'])
                    for j in range(11):
                        s = wst[k % 2]
                        k += 1
                        r0 = (half * 11 + j) * 128
                        P.dma(s[:, 0:D_MODEL], w_down[l, r0:r0 + 128, :], writes=[s.name])
                        cast(wdn[:, j, :], s[:, 0:D_MODEL], [s.name], ['f_wdn'])
                    h_sb = sbt("f_h", [128, 8, 512], F32)
                    sq = sbt("f_sq", [128, 8, 512], BF16)
                    xn = sbt("f_xn", [128, 8, 512], BF16)
                    rstd = sbt("f_rstd", [128, 512], F32)
                    actv = sbt("f_act", [128, 11, 512], BF16)
                    u_sb = [sbt("f_u%d" % i, [128, 514], F32) for i in range(4)]
                    cv_sb = [sbt("f_cv%d" % i, [128, 512], F32) for i in range(4)]
                    ps_n = pst("f_psn", [128, 512])
                    ps_o = [pst("f_ps%d" % i, [128, 512]) for i in range(4)]
                    ucnt = 0
                    for tt in range(NT):
                        tsl = slice(tt * 512, (tt + 1) * 512)
                        h = h_sb
                        P.dma(h[:], pc(hT)[:, :, tsl], writes=[h.name])
                        emit_norm(h, gain, sq, ps_n, rstd, xn)
                        hacc = h
                        if half == 1:
                            P.dma(h[:], pc(hT2)[:, :, tsl], writes=[h.name])
                        for j in range(11):
                            cvs = []
                            for which in range(2):
                                ps = ps_o[ucnt % 4]
                                u = u_sb[ucnt % 4]
                                cv = cv_sb[ucnt % 4]
                                ucnt += 1
                                hidx = which * 11 + j
                                fidx = which * 22 + half * 11 + j
                                for c in range(8):
                                    P.op('pe', lambda e, c=c, ps=ps, which=which, j=j: e.matmul(
                                        ps[:], lhsT=wup[:, c, which * HF + j * 128: which * HF + (j + 1) * 128],
                                        rhs=xn[:, c, :], start=(c == 0), stop=(c == 7)),
                                        reads=[xn.name, 'f_wup'], writes=[ps.name], inc=(c == 7))
                                P.op('act', lambda e, ps=ps, u=u: e.copy(out=u[:, 2:514], in_=ps[:]),
                                     reads=[ps.name], writes=[u.name])
                                P.op('pool', lambda e, u=u, hidx=hidx: e.tensor_copy(out=u[:, 0:2], in_=halo[:, hidx, :]),
                                     reads=[('f_halo', hidx)], writes=[u.name])
                                P.op('dve', lambda e, u=u, cv=cv, fidx=fidx: e.tensor_scalar(
                                    out=cv[:], in0=u[:, 0:512], scalar1=cw[:, fidx, 0:1], scalar2=None, op0=ALU.mult),
                                    reads=[u.name, 'f_cw'], writes=[cv.name])
                                for tap in (1, 2):
                                    P.op('dve', lambda e, u=u, cv=cv, fidx=fidx, tap=tap: e.scalar_tensor_tensor(
                                        out=cv[:], in0=u[:, tap:tap + 512], scalar=cw[:, fidx, tap:tap + 1], in1=cv[:],
                                        op0=ALU.mult, op1=ALU.add),
                                        reads=[u.name, 'f_cw', cv.name], writes=[cv.name])
                                P.op('pool', lambda e, u=u, hidx=hidx: e.tensor_copy(out=halo[:, hidx, :], in_=u[:, 512:514]),
                                     reads=[u.name], writes=[('f_halo', hidx)])
                                cvs.append(cv)
                            cg, cu = cvs
                            P.op('act', lambda e, cg=cg: e.activation(out=cg[:], in_=cg[:], func=AF.Silu),
                                 reads=[cg.name], writes=[cg.name])
                            P.op('dve', lambda e, cg=cg, cu=cu, j=j: e.tensor_tensor(
                                out=actv[:, j, :], in0=cg[:], in1=cu[:], op=ALU.mult),
                                reads=[cg.name, cu.name], writes=[('f_act', j)])
                        akeys = [('f_act', j) for j in range(11)]
                        for m in range(8):
                            ps = ps_o[m % 4]
                            for j in range(11):
                                P.op('pe', lambda e, j=j, m=m, ps=ps: e.matmul(
                                    ps[:], lhsT=wdn[:, j, m * 128:(m + 1) * 128], rhs=actv[:, j, :],
                                    start=(j == 0), stop=(j == 10)),
                                    reads=akeys + ['f_wdn'], writes=[ps.name], inc=(j == 10))
                            P.op('dve', lambda e, m=m, ps=ps, hacc=hacc: e.tensor_tensor(
                                out=hacc[:, m, :], in0=hacc[:, m, :], in1=ps[:], op=ALU.add),
                                reads=[ps.name, hacc.name], writes=[hacc.name])
                        dst = hT2 if half == 0 else hT
                        P.dma(pc(dst)[:, :, tsl], hacc[:], reads=[hacc.name], queue='act')
                    P.barrier()
                    P.flush()

            if 'G' in stages:
              with contextlib.ExitStack() as st:
                sbt = lambda name, shape, dt=F32, u=uid(): st.enter_context(nc.sbuf_tensor(name + u, shape, dt))
                pst = lambda name, shape, dt=F32, u=uid(): st.enter_context(nc.psum_tensor(name + u, shape, dt))
                last = (l == depth - 1)
                wg = sbt("g_wg", [128, 8, D_MODEL], BF16)
                wp = sbt("g_wp", [128, 2, D_MODEL], BF16)
                wst = [sbt("g_wst%d" % i, [128, D_MODEL], F32) for i in range(2)]
                gain = sbt("g_gain", [128, 8], F32)
                gpn = sbt("g_gpn", [128, 8], F32)
                gfin = sbt("g_gfin", [128, 8], F32)
                P.dma(gain[:], ln_ple[l], writes=[gain.name])
                P.dma(gpn[:], ple_norm[l], writes=[gpn.name])
                P.dma(gfin[:], ln_final, writes=[gfin.name])
                for c in range(8):
                    s = wst[c % 2]
                    P.dma(s[:], w_gate[l, c * 128:(c + 1) * 128, :], writes=[s.name])
                    cast(wg[:, c, :], s[:], [s.name], ['g_wg'])
                for c in range(2):
                    s = wst[c % 2]
                    P.dma(s[:], w_ple[l, c * 128:(c + 1) * 128, :], writes=[s.name])
                    cast(wp[:, c, :], s[:], [s.name], ['g_wp'])
                h_r = [sbt("g_h%d" % i, [128, 8, 512], F32) for i in range(2)]
                sq = sbt("g_sq", [128, 8, 512], BF16)
                xn_r = [sbt("g_xn%d" % i, [128, 8, 512], BF16) for i in range(2)]
                rstd = sbt("g_rstd", [128, 512], F32)
                p32_r = [sbt("g_p32", [128, 2, 512], F32)] * 2
                pbf_r = [sbt("g_pbf", [128, 2, 512], BF16)] * 2
                y_r = [sbt("g_y", [128, 8, 512], F32)] * 2
                gt_r = [sbt("g_gt%d" % i, [128, 8, 512], F32) for i in range(2)]
                ps_n = pst("g_psn", [128, 512])
                ps_o = [pst("g_ps%d" % i, [128, 512]) for i in range(4)]
                for tt in range(NT):
                    tsl = slice(tt * 512, (tt + 1) * 512)
                    h, xn, p32, pbf, y, gt = h_r[tt % 2], xn_r[tt % 2], p32_r[tt % 2], pbf_r[tt % 2], y_r[tt % 2], gt_r[tt % 2]
                    P.dma(h[:], pc(hT)[:, :, tsl], writes=[h.name])
                    P.dma(p32[:], pc(pT[l])[:, :, tsl], writes=[p32.name])
                    cast(pbf[:], p32[:], [p32.name], [pbf.name])
                    emit_norm(h, gain, sq, ps_n, rstd, xn)
                    for m in range(8):
                        ps = ps_o[m % 4]
                        for c in range(8):
                            P.op('pe', lambda e, c=c, m=m, ps=ps, h=h, xn=xn, y=y, gt=gt, pbf=pbf, p32=p32: e.matmul(
                                ps[:], lhsT=wg[:, c, m * 128:(m + 1) * 128], rhs=xn[:, c, :],
                                start=(c == 0), stop=(c == 7)),
                                reads=[xn.name, 'g_wg'], writes=[ps.name], inc=(c == 7))
                        P.op('act', lambda e, m=m, ps=ps, h=h, xn=xn, y=y, gt=gt, pbf=pbf, p32=p32: e.activation(out=gt[:, m, :], in_=ps[:], func=AF.Sigmoid),
                             reads=[ps.name], writes=[gt.name])
                    for m in range(8):
                        ps = ps_o[m % 4]
                        for c in range(2):
                            P.op('pe', lambda e, c=c, m=m, ps=ps, h=h, xn=xn, y=y, gt=gt, pbf=pbf, p32=p32: e.matmul(
                                ps[:], lhsT=wp[:, c, m * 128:(m + 1) * 128], rhs=pbf[:, c, :],
                                start=(c == 0), stop=(c == 1)),
                                reads=[pbf.name, 'g_wp'], writes=[ps.name], inc=(c == 1))
                        cast(y[:, m, :], ps[:], [ps.name], [y.name], psum=True)
                    emit_norm(y, gpn, sq, ps_n, rstd, y)
                    for m in range(8):
                        P.op('dve', lambda e, m=m, h=h, xn=xn, y=y, gt=gt, pbf=pbf, p32=p32: e.tensor_tensor(
                            out=gt[:, m, :], in0=gt[:, m, :], in1=y[:, m, :], op=ALU.mult),
                            reads=[gt.name, y.name], writes=[gt.name])
                        P.op('pool', lambda e, m=m, h=h, xn=xn, y=y, gt=gt, pbf=pbf, p32=p32: e.tensor_tensor(
                            out=h[:, m, :], in0=h[:, m, :], in1=gt[:, m, :], op=ALU.add),
                            reads=[gt.name, h.name], writes=[h.name])
                    if last:
                        emit_norm(h, gfin, sq, ps_n, rstd, y)
                        P.dma(pc(outT)[:, :, tsl], y[:], reads=[y.name], queue='act')
                    else:
                        P.dma(pc(hT)[:, :, tsl], h[:], reads=[h.name], queue='act')
                P.barrier()
                P.flush()

        P.barrier()
        P.flush()
    return nc


def _col_perm():
    r = lambda a, b: list(range(a, b))
    nq = [c for h in (0, 3, 1, 4, 2, 5) for c in r(1548 + h * 64, 1548 + (h + 1) * 64)]
    fm = r(0, 1536) + nq + r(1932, 2060) + r(2060, 2188) + r(2188, 2316) + r(2444, 2572) \
        + r(2718, 2974) + r(2974, 3230)
    small = r(1536, 1548) + r(2700, 2718)
    tm = r(2316, 2444) + r(2572, 2700) + r(3230, 3486)
    return fm, small, tm


def prep_w_in(w_in):
    fm, small, tm = _col_perm()
    d = w_in.shape[0]
    out = np.zeros((d, D_MODEL, N_IN_PAD), np.float32)
    out[:, :, 0:len(fm)] = w_in[:, :, fm]
    out[:, :, 2944:2944 + len(small)] = w_in[:, :, small]
    out[:, :, 3072:3584] = w_in[:, :, tm]
    return out


def vec_pc(v):
    sh = v.shape
    c = sh[-1] // 128
    return np.ascontiguousarray(np.swapaxes(v.reshape(sh[:-1] + (c, 128)), -1, -2))


def prep_conv(w):
    d, k, n = w.shape
    return np.ascontiguousarray(w.transpose(0, 2, 1).reshape(d, n // 128, 128, k).transpose(0, 2, 1, 3))


def dup128(v):
    return np.ascontiguousarray(np.concatenate([v, v], axis=-1)[..., None])


def prep_w1(w):
    d = w.shape[0]
    a = w.reshape(d, 32, 64, 128).transpose(0, 2, 1, 3)
    return np.ascontiguousarray(np.concatenate([a, a], axis=1))


def prep_pe(pe):
    a = pe.transpose(0, 2, 1)
    return np.ascontiguousarray(np.concatenate([a, a], axis=1))


STAGES = "ABCDEFG"


def kernel(**inputs):
    f32 = lambda a: np.ascontiguousarray(np.asarray(a, dtype=np.float32))
    x = f32(inputs['x'])
    B, T, _ = x.shape
    depth = int(np.asarray(inputs['w_in']).shape[0])
    p = f32(inputs['p'])
    shared = dict(
        ln_mix=vec_pc(f32(inputs['ln_mix'])), ln_ffn=vec_pc(f32(inputs['ln_ffn'])),
        ln_ple=vec_pc(f32(inputs['ln_ple'])), ple_norm=vec_pc(f32(inputs['ple_norm'])),
        ln_final=vec_pc(f32(inputs['ln_final'])),
        w_in=prep_w_in(f32(inputs['w_in'])), w_out=f32(inputs['w_out']), w_up=f32(inputs['w_up']),
        ffn_conv=prep_conv(f32(inputs['ffn_conv'])), w_down=f32(inputs['w_down']),
        w_gate=f32(inputs['w_ple_gate']), w_ple=f32(inputs['w_ple']),
        sb_norm=dup128(f32(inputs['sb_norm'])), nsa_norm=dup128(f32(inputs['nsa_norm'])),
        gdn_norm=dup128(f32(inputs['gdn_norm'])),
        gdn_conv=prep_conv(f32(inputs['gdn_conv'])), gdn_alog=f32(inputs['gdn_a_log'])[:, None, :],
        gdn_dtb=f32(inputs['gdn_dt_bias'])[:, None, :], gdn_normrow=f32(inputs['gdn_norm'])[:, None, :],
        cmp_k_w1=prep_w1(f32(inputs['nsa_cmp_k_w1'])), cmp_v_w1=prep_w1(f32(inputs['nsa_cmp_v_w1'])),
        cmp_k_w2=f32(inputs['nsa_cmp_k_w2']), cmp_v_w2=f32(inputs['nsa_cmp_v_w2']),
        pe_kT=prep_pe(f32(inputs['nsa_pe_k'])), pe_vT=prep_pe(f32(inputs['nsa_pe_v'])),
    )
    in_maps = []
    for core in range(8):
        b = core % B
        m = dict(shared)
        m['xT'] = np.ascontiguousarray(x[b].T)
        m['pT'] = np.ascontiguousarray(p[:, b].transpose(0, 2, 1))
        m['pos'] = np.ascontiguousarray(np.asarray(inputs['positions'])[b:b + 1].astype(np.int32))
        in_maps.append(m)
    nc = build(T, depth, stages=STAGES)
    res = run_bass_kernel_spmd(nc, in_maps, core_ids=list(range(8)))
    out = np.stack([np.ascontiguousarray(np.asarray(res.results[b]['outT']).T) for b in range(B)], axis=0)
    return out.astype(np.float32)
```
